# Optimizing a Trainium2 kernel written in Bass

```python
import math
import jax, jax.numpy as jnp
from jax import lax
import numpy as np

D_MODEL = 1024
BATCH = 2
SEQ = 16384
DEPTH = 4

CTX_LEN = 256
GRID_W = 64
HEAD_DIM = 64
ROPE_THETA = 10000.0
EPS = 1e-6
NEG_INF = -1e30

NA_HEADS = 8
NA_WIN_H = 8
NA_WIN_W = 16
DIFF_HEADS = 4
DIFF_QK_DIM = 64
DIFF_V_DIM = 128
DENSE_BLOCK = 128
SWA_Q_HEADS = 16
SWA_KV_HEADS = 4
SWA_WINDOW = 128
SWA_BLOCK = 128
PEER_HEADS = 8
PEER_NKEYS = 128
PEER_EXPERTS = PEER_NKEYS * PEER_NKEYS
PEER_QDIM = 256
PEER_TOPK = 16
PEER_CHUNK = 128

EVEN_IN = 3 * NA_HEADS * HEAD_DIM + DIFF_HEADS * (4 * DIFF_QK_DIM + DIFF_V_DIM)
EVEN_OUT = NA_HEADS * HEAD_DIM + DIFF_HEADS * DIFF_V_DIM
ODD_IN = (SWA_Q_HEADS + 2 * SWA_KV_HEADS) * HEAD_DIM
ODD_OUT = SWA_Q_HEADS * HEAD_DIM
N_EVEN = (DEPTH + 1) // 2
N_ODD = DEPTH // 2

kernel_name = "hybrid_natten_diff_swa_peer_dit"


def rmsnorm(x, g):
    xf = x.astype(jnp.float32)
    y = xf * lax.rsqrt(jnp.mean(xf * xf, axis=-1, keepdims=True) + EPS)
    return (y * g.astype(jnp.float32)).astype(x.dtype)


def split_mod(m, lead):
    m = m.reshape(lead + (6, D_MODEL))
    return tuple(m[..., k, :] for k in range(6))


def rope_tables(n_tokens):
    t = jnp.arange(n_tokens)
    row = (t // GRID_W).astype(jnp.float32)
    col = (t % GRID_W).astype(jnp.float32)
    half = HEAD_DIM // 2
    inv = ROPE_THETA ** (-jnp.arange(0, half, 2, dtype=jnp.float32) / half)
    ar = row[:, None] * inv
    ac = col[:, None] * inv
    ang = jnp.concatenate([ar, ar, ac, ac], axis=-1)
    return jnp.cos(ang), jnp.sin(ang)


def apply_rope2d(x, cos, sin):
    q = HEAD_DIM // 4
    rot = jnp.concatenate([-x[..., q:2 * q], x[..., :q], -x[..., 3 * q:], x[..., 2 * q:3 * q]], axis=-1)
    shp = (x.shape[1],) + (1,) * (x.ndim - 3) + (HEAD_DIM,)
    return (x * cos.reshape(shp) + rot * sin.reshape(shp)).astype(x.dtype)


def plain_ctx_attention(qc, kc, vc):
    B, C, H, d = qc.shape
    s = jnp.einsum('bqhd,bkhd->bhqk', qc, kc).astype(jnp.float32) * (d ** -0.5)
    p = jax.nn.softmax(s, axis=-1).astype(vc.dtype)
    return jnp.einsum('bhqk,bkhd->bqhd', p, vc).reshape(B, C, H * d)


def neighborhood_attention(q, k, v, qc, kc, vc, rpb, need_ctx):
    B, S, H, d = q.shape
    rows = S // GRID_W
    kh = min(NA_WIN_H, rows)
    kw = NA_WIN_W
    scale = d ** -0.5
    qg = q.reshape(B, rows, GRID_W, H, d)
    kg = k.reshape(B, rows, GRID_W, H, d)
    vg = v.reshape(B, rows, GRID_W, H, d)
    cols = jnp.arange(GRID_W)
    col_start = jnp.clip(cols - kw // 2, 0, GRID_W - kw)
    col_idx = col_start[:, None] + jnp.arange(kw)
    dc = col_idx - cols[:, None] + (NA_WIN_W - 1)
    rpb_c = rpb[:, :, dc]

    def one_row(args):
        r, q_row = args
        rs = jnp.clip(r - kh // 2, 0, rows - kh)
        k_rows = lax.dynamic_slice_in_dim(kg, rs, kh, axis=1)
        v_rows = lax.dynamic_slice_in_dim(vg, rs, kh, axis=1)
        k_nb = k_rows[:, :, col_idx]
        v_nb = v_rows[:, :, col_idx]
        dr = rs + jnp.arange(kh) - r + (NA_WIN_H - 1)
        bias = rpb_c[:, dr].transpose(0, 2, 1, 3)
        s_nb = jnp.einsum('bqhd,brqwhd->bhqrw', q_row, k_nb).astype(jnp.float32) * scale
        s_nb = (s_nb + bias[None].astype(jnp.float32)).reshape(B, H, GRID_W, kh * kw)
        s_ctx = jnp.einsum('bqhd,bchd->bhqc', q_row, kc).astype(jnp.float32) * scale
        p = jax.nn.softmax(jnp.concatenate([s_nb, s_ctx], axis=-1), axis=-1).astype(v.dtype)
        p_nb = p[..., :kh * kw].reshape(B, H, GRID_W, kh, kw)
        p_ctx = p[..., kh * kw:]
        return (jnp.einsum('bhqrw,brqwhd->bqhd', p_nb, v_nb)
                + jnp.einsum('bhqc,bchd->bqhd', p_ctx, vc))

    out = lax.map(one_row, (jnp.arange(rows), qg.transpose(1, 0, 2, 3, 4)))
    out = out.transpose(1, 0, 2, 3, 4).reshape(B, S, H * d)
    out_c = plain_ctx_attention(qc, kc, vc) if need_ctx else None
    return out, out_c


def diff_attention(q, k, v, qc, kc, vc, lam_p, subln_g, lam_init, need_ctx):
    B, S, H, _, dq = q.shape
    dv = v.shape[-1]
    scale = dq ** -0.5
    lp = lam_p.astype(jnp.float32)
    lam = jnp.exp(jnp.sum(lp[0] * lp[1])) - jnp.exp(jnp.sum(lp[2] * lp[3])) + lam_init

    def attend(qb, kk, vv):
        s = jnp.einsum('bqhmd,bkhmd->bmhqk', qb, kk).astype(jnp.float32) * scale
        p = jax.nn.softmax(s, axis=-1)
        pd = (p[:, 0] - lam * p[:, 1]).astype(vv.dtype)
        o = jnp.einsum('bhqk,bkhd->bqhd', pd, vv)
        return (rmsnorm(o, subln_g) * (1.0 - lam_init)).astype(vv.dtype)

    k_all = jnp.concatenate([kc, k], axis=1)
    v_all = jnp.concatenate([vc, v], axis=1)
    nb = S // DENSE_BLOCK
    qblocks = q.reshape(B, nb, DENSE_BLOCK, H, 2, dq).transpose(1, 0, 2, 3, 4, 5)
    out = lax.map(lambda qb: attend(qb, k_all, v_all), qblocks)
    out = out.transpose(1, 0, 2, 3, 4).reshape(B, S, H * dv)
    out_c = attend(qc, kc, vc).reshape(B, qc.shape[1], H * dv) if need_ctx else None
    return out, out_c


def window_gqa(q, k, v, qc, kc, vc, sink, need_ctx):
    B, S, Hq, d = q.shape
    Hkv = k.shape[2]
    G = Hq // Hkv
    scale = d ** -0.5
    blk = SWA_BLOCK
    nb = S // blk
    pad = ((0, 0), (blk, blk), (0, 0), (0, 0))
    kp = jnp.pad(k, pad)
    vp = jnp.pad(v, pad)
    qi = jnp.arange(blk)
    kj = jnp.arange(3 * blk)
    band = jnp.abs(kj[None, :] - blk - qi[:, None]) <= SWA_WINDOW
    sink_f = sink.astype(jnp.float32).reshape(Hkv, G)

    def sink_attend(qg, s_parts, v_parts):
        n = qg.shape[1]
        s_sink = jnp.broadcast_to(sink_f[None, :, :, None, None], (B, Hkv, G, n, 1))
        p = jax.nn.softmax(jnp.concatenate([s_sink] + s_parts, axis=-1), axis=-1)
        o = 0.0
        off = 1
        for s_i, v_i in zip(s_parts, v_parts):
            w = s_i.shape[-1]
            o = o + jnp.einsum('bngqj,bjnd->bqngd', p[..., off:off + w].astype(v_i.dtype), v_i)
            off += w
        return o.reshape(B, n, Hq * d).astype(v_parts[0].dtype)

    def one_block(args):
        bi, qb = args
        start = bi * blk
        kb = lax.dynamic_slice_in_dim(kp, start, 3 * blk, axis=1)
        vb = lax.dynamic_slice_in_dim(vp, start, 3 * blk, axis=1)
        kpos = start - blk + kj
        valid = band & ((kpos >= 0) & (kpos < S))[None, :]
        qg = qb.reshape(B, blk, Hkv, G, d)
        s_loc = jnp.einsum('bqngd,bjnd->bngqj', qg, kb).astype(jnp.float32) * scale
        s_loc = jnp.where(valid, s_loc, NEG_INF)
        s_ctx = jnp.einsum('bqngd,bjnd->bngqj', qg, kc).astype(jnp.float32) * scale
        return sink_attend(qg, [s_loc, s_ctx], [vb, vc])

    qblocks = q.reshape(B, nb, blk, Hq, d).transpose(1, 0, 2, 3, 4)
    out = lax.map(one_block, (jnp.arange(nb), qblocks))
    out = out.transpose(1, 0, 2, 3).reshape(B, S, Hq * d)
    out_c = None
    if need_ctx:
        qcg = qc.reshape(B, qc.shape[1], Hkv, G, d)
        s_c = jnp.einsum('bqngd,bjnd->bngqj', qcg, kc).astype(jnp.float32) * scale
        out_c = sink_attend(qcg, [s_c], [vc])
    return out, out_c


def even_mixer(hx, hc, w_in, w_out, rpb, lam_p, subln_g, cos, sin, lam_init, need_ctx):
    B, S, _ = hx.shape
    a = NA_HEADS * HEAD_DIM
    bqk = DIFF_HEADS * 2 * DIFF_QK_DIM

    def split(p, n):
        qa = p[..., :a].reshape(B, n, NA_HEADS, HEAD_DIM)
        ka = p[..., a:2 * a].reshape(B, n, NA_HEADS, HEAD_DIM)
        va = p[..., 2 * a:3 * a].reshape(B, n, NA_HEADS, HEAD_DIM)
        o = 3 * a
        qb = p[..., o:o + bqk].reshape(B, n, DIFF_HEADS, 2, DIFF_QK_DIM)
        kb = p[..., o + bqk:o + 2 * bqk].reshape(B, n, DIFF_HEADS, 2, DIFF_QK_DIM)
        vb = p[..., o + 2 * bqk:].reshape(B, n, DIFF_HEADS, DIFF_V_DIM)
        return qa, ka, va, qb, kb, vb

    qa, ka, va, qb, kb, vb = split(hx @ w_in, S)
    qac, kac, vac, qbc, kbc, vbc = split(hc @ w_in, hc.shape[1])
    qb = apply_rope2d(qb, cos, sin)
    kb = apply_rope2d(kb, cos, sin)
    oa, oac = neighborhood_attention(qa, ka, va, qac, kac, vac, rpb, need_ctx)
    ob, obc = diff_attention(qb, kb, vb, qbc, kbc, vbc, lam_p, subln_g, lam_init, need_ctx)
    y = jnp.concatenate([oa, ob], axis=-1) @ w_out
    yc = jnp.concatenate([oac, obc], axis=-1) @ w_out if need_ctx else None
    return y, yc


def odd_mixer(hx, hc, w_in, w_out, sink, cos, sin, need_ctx):
    B, S, _ = hx.shape
    nq = SWA_Q_HEADS * HEAD_DIM
    nkv = SWA_KV_HEADS * HEAD_DIM

    def split(p, n):
        return (p[..., :nq].reshape(B, n, SWA_Q_HEADS, HEAD_DIM),
                p[..., nq:nq + nkv].reshape(B, n, SWA_KV_HEADS, HEAD_DIM),
                p[..., nq + nkv:].reshape(B, n, SWA_KV_HEADS, HEAD_DIM))

    q, k, v = split(hx @ w_in, S)
    qc, kc, vc = split(hc @ w_in, hc.shape[1])
    q = apply_rope2d(q, cos, sin)
    k = apply_rope2d(k, cos, sin)
    o, oc = window_gqa(q, k, v, qc, kc, vc, sink, need_ctx)
    y = o @ w_out
    yc = oc @ w_out if need_ctx else None
    return y, yc


def peer(h, w_q, sub_keys, u, v):
    T, D = h.shape
    q = (h @ w_q).reshape(T, PEER_HEADS, 2, PEER_QDIM // 2)
    s = jnp.einsum('thpd,hpnd->thpn', q, sub_keys).astype(jnp.float32)
    sv, si = lax.top_k(s, PEER_TOPK)
    cand = (sv[:, :, 0, :, None] + sv[:, :, 1, None, :]).reshape(T, PEER_HEADS, PEER_TOPK * PEER_TOPK)
    fv, fi = lax.top_k(cand, PEER_TOPK)
    i1 = jnp.take_along_axis(si[:, :, 0], fi // PEER_TOPK, axis=-1)
    i2 = jnp.take_along_axis(si[:, :, 1], fi % PEER_TOPK, axis=-1)
    idx = (i1 * PEER_NKEYS + i2).reshape(T, PEER_HEADS * PEER_TOPK)
    g = jax.nn.softmax(fv, axis=-1).reshape(T, PEER_HEADS * PEER_TOPK).astype(h.dtype)
    nc = T // PEER_CHUNK

    def chunk(args):
        hc, ic, gc = args
        ue = u[ic]
        ve = v[ic]
        act = jax.nn.gelu(jnp.einsum('td,ted->te', hc, ue), approximate=False)
        return jnp.einsum('te,ted->td', gc * act, ve)

    out = lax.map(chunk, (h.reshape(nc, PEER_CHUNK, D),
                          idx.reshape(nc, PEER_CHUNK, -1),
                          g.reshape(nc, PEER_CHUNK, -1)))
    return out.reshape(T, D)


def setup_inputs(seed: int = 0) -> dict:
    key = jax.random.key(seed)
    ks = jax.random.split(key, 24)
    D = D_MODEL

    def nrm(k, shape, s):
        return jax.random.normal(k, shape, jnp.float32) * s

    return {
        "x": nrm(ks[0], (BATCH, SEQ, D), 1.0),
        "c": nrm(ks[1], (BATCH, D), 1.0),
        "ctx": nrm(ks[2], (BATCH, CTX_LEN, D), 1.0),
        "c_ctx": nrm(ks[3], (D,), 1.0),
        "w_mod": nrm(ks[4], (DEPTH, D, 6 * D), 0.5 * D ** -0.5),
        "b_mod": nrm(ks[5], (DEPTH, 6 * D), 0.02),
        "norm1_g": 1.0 + nrm(ks[6], (DEPTH, D), 0.05),
        "norm2_g": 1.0 + nrm(ks[7], (DEPTH, D), 0.05),
        "w_in_even": nrm(ks[8], (N_EVEN, D, EVEN_IN), D ** -0.5),
        "w_out_even": nrm(ks[9], (N_EVEN, EVEN_OUT, D), EVEN_OUT ** -0.5),
        "na_rpb": nrm(ks[10], (N_EVEN, NA_HEADS, 2 * NA_WIN_H - 1, 2 * NA_WIN_W - 1), 0.1),
        "diff_lambda": nrm(ks[11], (N_EVEN, 4, DIFF_QK_DIM), 0.1),
        "diff_subln_g": 1.0 + nrm(ks[12], (N_EVEN, DIFF_V_DIM), 0.05),
        "w_in_odd": nrm(ks[13], (N_ODD, D, ODD_IN), D ** -0.5),
        "w_out_odd": nrm(ks[14], (N_ODD, ODD_OUT, D), ODD_OUT ** -0.5),
        "swa_sink": nrm(ks[15], (N_ODD, SWA_Q_HEADS), 0.5),
        "peer_wq": nrm(ks[16], (DEPTH, D, PEER_HEADS * PEER_QDIM), D ** -0.5),
        "peer_keys": nrm(ks[17], (DEPTH, PEER_HEADS, 2, PEER_NKEYS, PEER_QDIM // 2), (PEER_QDIM // 2) ** -0.5),
        "peer_u": nrm(ks[18], (DEPTH, PEER_EXPERTS, D), D ** -0.5),
        "peer_v": nrm(ks[19], (DEPTH, PEER_EXPERTS, D), 0.25),
        "final_g": 1.0 + nrm(ks[20], (D,), 0.05),
    }


def reference(x, c, ctx, c_ctx, w_mod, b_mod, norm1_g, norm2_g, w_in_even, w_out_even,
              na_rpb, diff_lambda, diff_subln_g, w_in_odd, w_out_odd, swa_sink,
              peer_wq, peer_keys, peer_u, peer_v, final_g):
    B, S, D = x.shape
    C = ctx.shape[1]
    cos, sin = rope_tables(S)
    sc = jax.nn.silu(c)
    scc = jax.nn.silu(c_ctx)
    for i in range(DEPTH):
        need_ctx = i < DEPTH - 1
        sh1, sc1, g1, sh2, sc2, g2 = split_mod(sc @ w_mod[i] + b_mod[i], (B, 1))
        ch1, cc1, cg1, ch2, cc2, cg2 = split_mod(scc @ w_mod[i] + b_mod[i], (1, 1))
        hx = rmsnorm(x, norm1_g[i]) * (1.0 + sc1) + sh1
        hc = rmsnorm(ctx, norm1_g[i]) * (1.0 + cc1) + ch1
        if i % 2 == 0:
            j = i // 2
            lam_init = 0.8 - 0.6 * math.exp(-0.3 * i)
            y, yc = even_mixer(hx, hc, w_in_even[j], w_out_even[j], na_rpb[j], diff_lambda[j],
                               diff_subln_g[j], cos, sin, lam_init, need_ctx)
        else:
            j = i // 2
            y, yc = odd_mixer(hx, hc, w_in_odd[j], w_out_odd[j], swa_sink[j], cos, sin, need_ctx)
        x = x + g1 * y
        hx = rmsnorm(x, norm2_g[i]) * (1.0 + sc2) + sh2
        if need_ctx:
            ctx = ctx + cg1 * yc
            hc = rmsnorm(ctx, norm2_g[i]) * (1.0 + cc2) + ch2
            tokens = jnp.concatenate([hx.reshape(B * S, D), hc.reshape(B * C, D)], axis=0)
            f = peer(tokens, peer_wq[i], peer_keys[i], peer_u[i], peer_v[i])
            x = x + g2 * f[:B * S].reshape(B, S, D)
            ctx = ctx + cg2 * f[B * S:].reshape(B, C, D)
        else:
            f = peer(hx.reshape(B * S, D), peer_wq[i], peer_keys[i], peer_u[i], peer_v[i])
            x = x + g2 * f.reshape(B, S, D)
    return rmsnorm(x, final_g)
```

```python
from contextlib import ExitStack
import math
import numpy as np
import ml_dtypes
import concourse.bass as bass
import concourse.mybir as mybir
from concourse.bass_utils import run_bass_kernel_spmd

F32 = mybir.dt.float32
BF16 = mybir.dt.bfloat16
AF = mybir.ActivationFunctionType
ALU = mybir.AluOpType
AX = mybir.AxisListType
NPBF = ml_dtypes.bfloat16

ENGS = ("pe", "act", "dve", "pool", "sp")

D = 1024
NCORES = 8
SEQ = 16384
CTX = 256
TQ = SEQ // 4
NTL = TQ // 128
NTC = CTX // 128
NT = NTL + NTC
TALL = TQ + CTX
EPS = 1e-6
NEG = -1.0e30
PEER_MARGIN = 1e-4
import os
USE_CONV = os.environ.get('K_CONV', '1') == '1'


class Buf:
    __slots__ = ("name", "writers", "readers", "excl")

    def __init__(self, name="", excl=False):
        self.name = name
        self.writers = {}
        self.readers = {}
        self.excl = excl


class Op:
    __slots__ = ("id", "eng", "fn", "dma", "cc", "deps", "seq", "waits", "signal", "sigidx", "slot", "target")


class Prog:
    RING = {"sp": 14, "act": 4, "pool": 8}

    def __init__(self, nc):
        self.nc = nc
        self.ops = []
        self.seq = {e: 0 for e in ENGS}
        self.dmas = {q: [] for q in self.RING}
        self.ccs = []

    def add(self, eng, fn, reads=(), writes=(), dma=False, cc=False):
        dma = dma or cc
        op = Op()
        op.cc = cc
        op.id = len(self.ops)
        op.eng = eng
        op.fn = fn
        op.dma = dma
        op.signal = False
        op.sigidx = None
        op.slot = None
        op.target = None
        deps = set()
        if cc:
            op.slot = ("cc", len(self.ccs))
            op.target = 1
            self.ccs.append(op.id)
        elif dma:
            ring = self.RING[eng]
            lst = self.dmas[eng]
            n = len(lst)
            op.slot = (eng, n % ring)
            op.target = 16 * (n // ring + 1)
            if n >= ring:
                deps.add(lst[n - ring])
            lst.append(op.id)
        pkey = ("dma", op.slot) if dma else eng
        rd = [b for b in reads if not b.excl]
        wr = list(writes) + [b for b in reads if b.excl]
        for b in rd:
            for k, oid in b.writers.items():
                if k == eng and eng == "pe":
                    continue
                deps.add(oid)
        for b in wr:
            isread = b not in writes
            for k, oid in b.writers.items():
                if k == eng and not dma:
                    if isread and eng != "pe":
                        deps.add(oid)
                    continue
                if dma and isinstance(k, tuple):
                    continue
                deps.add(oid)
            for k, oid in b.readers.items():
                if k == eng and not dma:
                    continue
                deps.add(oid)
        for b in wr:
            if dma:
                b.writers = {k: v for k, v in b.writers.items() if isinstance(k, tuple)}
                b.writers[pkey] = op.id
            else:
                b.writers = {pkey: op.id}
            b.readers = {}
        for b in rd:
            if b in wr:
                continue
            b.readers[pkey] = op.id
        op.seq = self.seq[eng]
        self.seq[eng] += 1
        op.deps = deps
        self.ops.append(op)
        return op.id

    def barrier(self):
        start = getattr(self, "bar_start", 0)
        last = {}
        for op in self.ops[start:]:
            if op.dma:
                last[("dma", op.id)] = op.id
            else:
                last[op.eng] = op.id
        deps = set(last.values())
        for e in ENGS:
            oid = self.add(e, lambda eng: None)
            self.ops[oid].deps |= {d for d in deps if self.ops[d].dma or self.ops[d].eng != e}
        self.bar_start = len(self.ops)

    def emit(self):
        nc = self.nc
        ops = self.ops
        seen = {e: {} for e in ENGS}
        seen_dma = {e: {} for e in ENGS}
        for op in ops:
            best = {}
            waits = []
            for d in op.deps:
                p = ops[d]
                if p.dma:
                    if seen_dma[op.eng].get(p.slot, 0) >= p.target:
                        continue
                    seen_dma[op.eng][p.slot] = p.target
                    waits.append(d)
                else:
                    if p.eng not in best or ops[best[p.eng]].seq < p.seq:
                        best[p.eng] = d
            for e, d in best.items():
                p = ops[d]
                if seen[op.eng].get(e, -1) >= p.seq:
                    continue
                seen[op.eng][e] = p.seq
                p.signal = True
                waits.append(d)
            op.waits = waits
        cnt = {e: 0 for e in ENGS}
        for op in ops:
            if op.signal and not op.dma:
                cnt[op.eng] += 1
                op.sigidx = cnt[op.eng]
        with ExitStack() as es:
            sems = {e: es.enter_context(nc.semaphore("s_" + e)) for e in ENGS}
            rings = {}
            for q, n in self.RING.items():
                for i in range(n):
                    rings[(q, i)] = es.enter_context(nc.semaphore(f"r_{q}{i}"))
            for i in range(len(self.ccs)):
                rings[("cc", i)] = es.enter_context(nc.semaphore(f"cc{i}"))
            block = es.enter_context(nc.Block())
            per_eng = {e: [o for o in ops if o.eng == e] for e in ENGS}

            def run(engname):
                def body(eng):
                    for op in per_eng[engname]:
                        for d in op.waits:
                            p = ops[d]
                            if p.dma:
                                eng.wait_ge(rings[p.slot], p.target)
                            else:
                                eng.wait_ge(sems[p.eng], p.sigidx)
                        ins = op.fn(eng)
                        if ins is None:
                            continue
                        if op.cc:
                            ins.then_inc(rings[op.slot], 1)
                        elif op.dma:
                            ins.then_inc(rings[op.slot], 16)
                        elif op.signal:
                            ins.then_inc(sems[op.eng], 1)
                return body

            block.tensor(run("pe"))
            block.scalar(run("act"))
            block.vector(run("dve"))
            block.gpsimd(run("pool"))
            block.sync(run("sp"))


class KB:
    def __init__(self):
        self.nc = bass.Bass("TRN2", target_bir_lowering=False)
        self.P = Prog(self.nc)
        self.es = ExitStack()
        self.banks = []
        self.outs = []

    def din(self, name, shape, dt=F32):
        return self.nc.dram_tensor(name, list(shape), dt, kind="ExternalInput").ap()

    def dout(self, name, shape, dt=F32):
        b = Buf(name)
        self.outs.append(b)
        return self.nc.dram_tensor(name, list(shape), dt, kind="ExternalOutput").ap(), b

    def dscratch(self, name, shape, dt):
        return self.nc.dram_tensor(name, list(shape), dt, kind="Internal").ap(), Buf(name)

    pfx = ""

    def sb(self, name, shape, dt=F32):
        return self.es.enter_context(self.nc.sbuf_tensor(self.pfx + name, list(shape), dt)), Buf(name)

    def scope(self, pfx):
        kb = self

        class _S:
            def __enter__(s_):
                s_.saved = (kb.es, kb.pfx)
                kb.es = ExitStack()
                kb.pfx = pfx
                kb._ident = None

            def __exit__(s_, *a):
                kb.es.close()
                kb.es, kb.pfx = s_.saved
                kb.P.barrier()
                return False
        return _S()

    _ident = None
    _ident_d = None

    def ident(self):
        if self._ident is None:
            if self._ident_d is None:
                self._ident_d = self.din("c_ident", [128, 128], F32)
            f, IDF = self.sb("ident_f", [128, 128], F32)
            b, ID = self.sb("ident", [128, 128], BF16)
            self.dma("sp", f[:], self._ident_d, writes=[IDF])
            self.op("act", lambda e: e.copy(out=b[:], in_=f[:]), reads=[IDF], writes=[ID])
            self._ident = (f, IDF, b, ID)
        return self._ident

    def cc(self, kind, rg, src, dst, reads=(), writes=()):
        return self.P.add("pool", lambda e: e.collective_compute(kind, ALU.bypass, replica_groups=rg, ins=[src.opt()], outs=[dst.opt()]),
                          reads, writes, cc=True)

    def psum_banks(self):
        for i in range(8):
            t = self.es.enter_context(self.nc.psum_tensor(f"bank{i}", [128, 512], F32))
            self.banks.append((t, Buf(f"bank{i}", excl=True)))

    def op(self, eng, fn, reads=(), writes=()):
        return self.P.add(eng, fn, reads, writes)

    def dma(self, q, out, in_, reads=(), writes=()):
        return self.P.add(q, lambda e: e.dma_start(out=out, in_=in_), reads, writes, dma=True)

    def finish(self):
        self.P.add("sp", lambda e: None, reads=self.outs)
        self.P.emit()
        self.es.close()
        return self.nc


def rstd_ops(kb, sm, SM):
    kb.op("dve", lambda e: e.tensor_scalar(out=sm[:, 2:3], in0=sm[:, 0:1], scalar1=1.0 / D, scalar2=EPS,
                                           op0=ALU.mult, op1=ALU.add), reads=[SM], writes=[SM])
    kb.op("act", lambda e: e.activation(out=sm[:, 3:4], in_=sm[:, 2:3], func=AF.Sqrt), reads=[SM], writes=[SM])
    kb.op("dve", lambda e: e.reciprocal(out=sm[:, 1:2], in_=sm[:, 3:4]), reads=[SM], writes=[SM])


def emit_post(kb, ntiles, x_in, ao_dram, AO, modrow, w_out, norm2g, peer_wq, peer_keysT, peer_uT, peer_v,
              x_out, XOUT, final_g=None, xn_out=None, XN=None, tile_sets=None, conv=None):
    nc, P = kb.nc, kb.P
    B = kb.banks
    if tile_sets is None:
        tile_sets = [0] * ntiles
    ident_f, IDF, ident, ID = kb.ident()

    mod, MOD = kb.sb("modrows", [128, 4, D], F32)
    n2g, N2G = kb.sb("n2g_sb", [128, D], F32)
    kb.dma("sp", n2g[:], norm2g.unsqueeze(0).to_broadcast([128, D]), writes=[N2G])
    fg = None
    if final_g is not None:
        fg, FG = kb.sb("fg_sb", [128, D], F32)
        kb.dma("sp", fg[:], final_g.unsqueeze(0).to_broadcast([128, D]), writes=[FG])

    def load_mod(s):
        for j, src in enumerate((2, 4, 3, 5)):
            kb.dma("sp", mod[:, j, :], modrow[s, src:src + 1, :].to_broadcast([128, D]), writes=[MOD])
        kb.op("dve", lambda e: e.scalar_tensor_tensor(out=mod[:, 1, :], in0=mod[:, 1, :], scalar=1.0, in1=n2g[:],
                                                      op0=ALU.add, op1=ALU.mult), reads=[MOD, N2G], writes=[MOD])

    keys_f, KF = kb.sb("keys_f", [128, 16, 128], F32)
    keys_b, KBF = kb.sb("keys_b", [128, 16, 128], BF16)
    kb.dma("sp", keys_f[:], peer_keysT, writes=[KF])
    kb.op("act", lambda e: e.copy(out=keys_b[:], in_=keys_f[:]), reads=[KF], writes=[KBF])

    NWR = 3
    wr = [kb.sb(f"wr{i}", [128, 8, 512], BF16) for i in range(NWR)]
    NVR = 2
    vr = [kb.sb(f"vr{i}", [128, 4, D], BF16) for i in range(NVR)]
    wr_i = [0]
    vr_i = [0]

    def load_w(src_cols, pre=None):
        t, b = wr[wr_i[0] % NWR]
        wr_i[0] += 1
        if pre is not None:
            kb.dma("sp", t[:].rearrange("p k n -> p (k n)"), pre[0], reads=[pre[1]], writes=[b])
        else:
            kb.dma("pool", t[:], src_cols.rearrange("(k p) n -> p k n", p=128), writes=[b])
        return t, b

    def load_v(src_rows, pre=None):
        t, b = vr[vr_i[0] % NVR]
        vr_i[0] += 1
        if pre is not None:
            kb.dma("sp", t[:].rearrange("p c d -> p (c d)"), pre[0], reads=[pre[1]], writes=[b])
        else:
            kb.dma("pool", t[:], src_rows.rearrange("(c p) d -> p c d", p=128), writes=[b])
        return t, b

    def pre_of(name, idx):
        return None if conv is None else (conv[name][0][idx], conv[name][1])

    xt, XT = kb.sb("xt", [128, 2, D], F32)
    tmp, TMP = kb.sb("tmp", [128, D], F32)
    ao, AOB = kb.sb("ao_sb", [128, D], BF16)
    aoT, AOT = kb.sb("aoT", [128, 8, 128], BF16)
    h2, H2 = kb.sb("h2", [128, D], BF16)
    h2T, H2T = kb.sb("h2T", [128, 8, 256], BF16)
    qT, QT = kb.sb("qT", [128, 16, 256], BF16)
    s_sb, SSB = kb.sb("s_sb", [128, 16, 128], F32)
    work, WORK = kb.sb("work", [128, 2048], F32)
    cand, CAND = kb.sb("cand", [128, 8, 16, 16], F32)
    top, TOP = kb.sb("top", [128, 16, 16], F32)
    ctop, CTOP = kb.sb("ctop", [128, 8, 16], F32)
    sm, SM = kb.sb("sm", [128, 64], F32)
    e16, E16 = kb.sb("e16", [128, 8, 16], F32)
    av, AV = kb.sb("a_vec", [128, 2, 8, 128], F32)
    bv, BV = kb.sb("b_vec", [128, 2, 8, 128], F32)
    diag, DG = kb.sb("diag", [128, 2, 8, 128], BF16)
    pp, PP = kb.sb("pp", [128, 2, 8, 2, 128], F32)
    wps = [kb.sb(f"wp{i}", [128, 2, 8, 2, 128], BF16) for i in range(2)]
    gl = [kb.sb(f"gl{i}", [128, 4, 256], BF16) for i in range(2)]
    at = [kb.sb(f"at{i}", [128, 4, 256], BF16) for i in range(2)]
    xn, XNB = (None, None)
    if final_g is not None:
        xn, XNB = kb.sb("xn_sb", [128, D], F32)

    def bf(bank):
        return bank.bitcast(BF16)

    cur_set = [None]
    npairs = (ntiles + 1) // 2
    for pr in range(npairs):
        tiles = [t for t in (2 * pr, 2 * pr + 1) if t < ntiles]
        nj = len(tiles)
        NTOK = 128 * nj
        if tile_sets[tiles[0]] != cur_set[0]:
            cur_set[0] = tile_sets[tiles[0]]
            load_mod(cur_set[0])
        wo = [load_w(w_out[:, hf * 512:(hf + 1) * 512], pre_of('wout', hf)) for hf in range(2)]
        for j, tt in enumerate(tiles):
            rows = slice(tt * 128, (tt + 1) * 128)
            kb.dma("sp", xt[:, j, :], x_in[rows, :], writes=[XT])
            kb.dma("sp", ao[:], ao_dram[rows, :], reads=[AO], writes=[AOB])
            tb, TB = B[0]
            def tr1(e, tb=tb):
                for k in range(8):
                    ins = e.transpose(bf(tb)[:, k * 128:(k + 1) * 128], ao[:, k * 128:(k + 1) * 128], ident[:])
                return ins
            kb.op("pe", tr1, reads=[AOB, ID], writes=[TB])
            kb.op("act", lambda e, tb=tb: e.copy(out=aoT[:].rearrange("p k t -> p (k t)"), in_=bf(tb)[:, 0:1024]),
                  reads=[TB], writes=[AOT])
            for hf in range(2):
                yb, YB = B[1 + hf]
                wt, WB = wo[hf]
                def mmy(e, yb=yb, wt=wt):
                    for k in range(8):
                        ins = e.matmul(yb[:], lhsT=aoT[:, k, :], rhs=wt[:, k, :], start=(k == 0), stop=(k == 7))
                    return ins
                kb.op("pe", mmy, reads=[AOT, WB], writes=[YB])
                kb.op("dve", lambda e, yb=yb, hf=hf: e.tensor_tensor(out=tmp[:, hf * 512:(hf + 1) * 512], in0=yb[:],
                                                                   in1=mod[:, 0, hf * 512:(hf + 1) * 512], op=ALU.mult),
                      reads=[YB, MOD], writes=[TMP])
            kb.op("pool", lambda e, j=j: e.tensor_tensor(out=xt[:, j, :], in0=xt[:, j, :], in1=tmp[:], op=ALU.add),
                  reads=[XT, TMP], writes=[XT])
            kb.op("act", lambda e, j=j: e.activation(out=tmp[:], in_=xt[:, j, :], func=AF.Square, accum_out=sm[:, 0:1]),
                  reads=[XT], writes=[TMP, SM])
            rstd_ops(kb, sm, SM)
            kb.op("dve", lambda e, j=j: e.scalar_tensor_tensor(out=tmp[:], in0=xt[:, j, :], scalar=sm[:, 1:2], in1=mod[:, 1, :],
                                                               op0=ALU.mult, op1=ALU.mult), reads=[XT, SM, MOD], writes=[TMP])
            kb.op("pool", lambda e: e.tensor_tensor(out=h2[:], in0=tmp[:], in1=mod[:, 2, :], op=ALU.add),
                  reads=[TMP, MOD], writes=[H2])
            tb, TB = B[3]
            def tr2(e, tb=tb):
                for k in range(8):
                    ins = e.transpose(bf(tb)[:, k * 128:(k + 1) * 128], h2[:, k * 128:(k + 1) * 128], ident[:])
                return ins
            kb.op("pe", tr2, reads=[H2, ID], writes=[TB])
            kb.op("act", lambda e, tb=tb, j=j: e.copy(out=h2T[:, :, j * 128:(j + 1) * 128],
                                                      in_=bf(tb)[:, 0:1024].rearrange("p (k t) -> p k t", k=8)),
                  reads=[TB], writes=[H2T])
        for g in range(4):
            wt, WB = load_w(peer_wq[:, g * 512:(g + 1) * 512], pre_of('wq', g))
            for hh in range(2):
                qb, QB = B[4 + (2 * g + hh) % 2]
                def mmq(e, qb=qb, wt=wt, hh=hh, NTOK=NTOK):
                    for i in range(2):
                        for k in range(8):
                            c0 = (hh * 2 + i) * 128
                            ins = e.matmul(qb[:, i * 256:i * 256 + NTOK], lhsT=wt[:, k, c0:c0 + 128], rhs=h2T[:, k, 0:NTOK],
                                           start=(k == 0), stop=(k == 7))
                    return ins
                kb.op("pe", mmq, reads=[WB, H2T], writes=[QB])
                hp0 = g * 4 + hh * 2
                kb.op("act", lambda e, qb=qb, hp0=hp0, NTOK=NTOK: e.copy(out=qT[:, hp0:hp0 + 2, 0:NTOK],
                                                               in_=qb[:].rearrange("p (i t) -> p i t", i=2)[:, :, 0:NTOK]),
                      reads=[QB], writes=[QT])
        for j, tt in enumerate(tiles):
            for g in range(4):
                sbk, SB_ = B[6 + g % 2]
                def mms(e, sbk=sbk, g=g, j=j):
                    for i in range(4):
                        hp = g * 4 + i
                        ins = e.matmul(sbk[:, i * 128:(i + 1) * 128], lhsT=qT[:, hp, j * 128:(j + 1) * 128], rhs=keys_b[:, hp, :],
                                       start=True, stop=True)
                    return ins
                kb.op("pe", mms, reads=[QT, KBF], writes=[SB_])
                kb.op("act", lambda e, sbk=sbk, g=g: e.copy(out=s_sb[:, g * 4:(g + 1) * 4, :].rearrange("p a n -> p (a n)"), in_=sbk[:]),
                      reads=[SB_], writes=[SSB])
            for hp in range(16):
                kb.op("dve", lambda e, hp=hp: e.max(out=top[:, hp, 0:8], in_=s_sb[:, hp, :]), reads=[SSB], writes=[TOP])
                kb.op("dve", lambda e, hp=hp: e.match_replace(out=work[:, hp * 128:(hp + 1) * 128], in_to_replace=top[:, hp, 0:8],
                                                              in_values=s_sb[:, hp, :], imm_value=NEG), reads=[SSB, TOP], writes=[WORK])
                kb.op("dve", lambda e, hp=hp: e.max(out=top[:, hp, 8:16], in_=work[:, hp * 128:(hp + 1) * 128]), reads=[WORK], writes=[TOP])
            def fcand(e):
                t4 = top[:].rearrange("p (h q) k -> p h q k", q=2)
                i0 = t4[:, :, 0, :].unsqueeze(3).to_broadcast([128, 8, 16, 16])
                i1 = t4[:, :, 1, :].unsqueeze(2).to_broadcast([128, 8, 16, 16])
                return e.tensor_tensor(out=cand[:], in0=i0, in1=i1, op=ALU.add)
            kb.op("pool", fcand, reads=[TOP], writes=[CAND])
            for h in range(8):
                cv = cand[:, h, :, :].rearrange("p a b -> p (a b)")
                kb.op("dve", lambda e, h=h, cv=cv: e.max(out=ctop[:, h, 0:8], in_=cv), reads=[CAND], writes=[CTOP])
                kb.op("dve", lambda e, h=h, cv=cv: e.match_replace(out=work[:, h * 256:(h + 1) * 256], in_to_replace=ctop[:, h, 0:8],
                                                                   in_values=cv, imm_value=NEG), reads=[CAND, CTOP], writes=[WORK])
                kb.op("dve", lambda e, h=h: e.max(out=ctop[:, h, 8:16], in_=work[:, h * 256:(h + 1) * 256]), reads=[WORK], writes=[CTOP])
            kb.op("dve", lambda e: e.tensor_scalar(out=sm[:, 8:16], in0=ctop[:, :, 15], scalar1=-1.0, scalar2=PEER_MARGIN,
                                                   op0=ALU.mult, op1=ALU.add), reads=[CTOP], writes=[SM])
            kb.op("dve", lambda e: e.tensor_tensor(out=e16[:], in0=ctop[:], in1=sm[:, 8:16].unsqueeze(2).to_broadcast([128, 8, 16]),
                                                   op=ALU.add), reads=[CTOP, SM], writes=[E16])
            kb.op("act", lambda e: e.activation(out=e16[:], in_=e16[:], func=AF.Exp), reads=[E16], writes=[E16])
            kb.op("dve", lambda e: e.tensor_reduce(out=sm[:, 16:24], in_=e16[:], axis=AX.X, op=ALU.add), reads=[E16], writes=[SM])
            kb.op("dve", lambda e: e.reciprocal(out=sm[:, 24:32], in_=sm[:, 16:24]), reads=[SM], writes=[SM])
            s4 = s_sb[:].rearrange("p (h q) n -> p h q n", q=2)
            kb.op("dve", lambda e, j=j, s4=s4: e.tensor_tensor(out=av[:, j, :, :], in0=s4[:, :, 0, :],
                                                               in1=sm[:, 8:16].unsqueeze(2).to_broadcast([128, 8, 128]), op=ALU.add),
                  reads=[SSB, SM], writes=[AV])
            kb.op("act", lambda e, j=j: e.activation(out=av[:, j, :, :], in_=av[:, j, :, :], func=AF.Exp), reads=[AV], writes=[AV])
            kb.op("act", lambda e, j=j, s4=s4: e.activation(out=bv[:, j, :, :], in_=s4[:, :, 1, :], func=AF.Exp), reads=[SSB], writes=[BV])
            for h in range(8):
                kb.op("pool", lambda e, j=j, h=h: e.tensor_scalar(out=diag[:, j, h, :], in0=ident_f[:], scalar1=sm[:, 24 + h:25 + h],
                                                                  scalar2=None, op0=ALU.mult), reads=[IDF, SM], writes=[DG])
        for cg in range(32):
            ut, UB = load_w(peer_uT[:, cg * 512:(cg + 1) * 512], pre_of('uT', cg))
            vt, VB = load_v(peer_v[cg * 512:(cg + 1) * 512, :], pre_of('v', cg))
            gt, GB = gl[cg % 2]
            att, ATB = at[cg % 2]
            for half in range(2):
                c0 = cg * 4 + half * 2
                wp, WPB = wps[(cg * 2 + half) % 2]
                def fpp(e, c0=c0, nj=nj):
                    i0 = av[:, 0:nj, :, c0:c0 + 2].unsqueeze(4).to_broadcast([128, nj, 8, 2, 128])
                    i1 = bv[:, 0:nj, :, :].unsqueeze(3).to_broadcast([128, nj, 8, 2, 128])
                    return e.tensor_tensor(out=pp[:, 0:nj], in0=i0, in1=i1, op=ALU.mult)
                kb.op("pool", fpp, reads=[AV, BV], writes=[PP])
                kb.op("dve", lambda e, wp=wp, nj=nj: e.scalar_tensor_tensor(out=wp[:, 0:nj], in0=pp[:, 0:nj], scalar=1.0, in1=pp[:, 0:nj],
                                                                     op0=ALU.is_ge, op1=ALU.mult), reads=[PP], writes=[WPB])
                pb, PB = B[0 + half]
                wb, WTB = B[2 + half]
                def mmpre(e, pb=pb, half=half, ut=ut, NTOK=NTOK):
                    for i in range(2):
                        cl = half * 2 + i
                        for k in range(8):
                            ins = e.matmul(pb[:, i * 256:i * 256 + NTOK], lhsT=ut[:, k, cl * 128:(cl + 1) * 128], rhs=h2T[:, k, 0:NTOK],
                                           start=(k == 0), stop=(k == 7))
                    return ins
                kb.op("pe", mmpre, reads=[UB, H2T], writes=[PB])
                def mmwt(e, wb=wb, wp=wp, nj=nj):
                    for i in range(2):
                        for j in range(nj):
                            for h in range(8):
                                ins = e.matmul(wb[:, i * 256 + j * 128:i * 256 + (j + 1) * 128], lhsT=wp[:, j, h, i, :],
                                               rhs=diag[:, j, h, :], start=(h == 0), stop=(h == 7))
                    return ins
                kb.op("pe", mmwt, reads=[WPB, DG], writes=[WTB])
                kb.op("act", lambda e, pb=pb, gt=gt, half=half, NTOK=NTOK: e.activation(
                    out=gt[:, half * 2:half * 2 + 2, 0:NTOK], in_=pb[:].rearrange("p (i t) -> p i t", i=2)[:, :, 0:NTOK], func=AF.Gelu),
                    reads=[PB], writes=[GB])
                kb.op("dve", lambda e, wb=wb, gt=gt, att=att, half=half, NTOK=NTOK: e.tensor_tensor(
                    out=att[:, half * 2:half * 2 + 2, 0:NTOK], in0=wb[:].rearrange("p (i t) -> p i t", i=2)[:, :, 0:NTOK],
                    in1=gt[:, half * 2:half * 2 + 2, 0:NTOK], op=ALU.mult), reads=[WTB, GB], writes=[ATB])
            for j in range(nj):
                for hf in range(2):
                    ob, OB = B[4 + 2 * j + hf]
                    def mmo(e, ob=ob, att=att, vt=vt, j=j, hf=hf, cg=cg):
                        for cl in range(4):
                            ins = e.matmul(ob[:], lhsT=att[:, cl, j * 128:(j + 1) * 128], rhs=vt[:, cl, hf * 512:(hf + 1) * 512],
                                           start=(cg == 0 and cl == 0), stop=(cg == 31 and cl == 3))
                        return ins
                    kb.op("pe", mmo, reads=[ATB, VB], writes=[OB])
        for j, tt in enumerate(tiles):
            rows = slice(tt * 128, (tt + 1) * 128)
            for hf in range(2):
                ob, OB = B[4 + 2 * j + hf]
                kb.op("dve", lambda e, ob=ob, hf=hf: e.tensor_tensor(out=tmp[:, hf * 512:(hf + 1) * 512], in0=ob[:],
                                                                   in1=mod[:, 3, hf * 512:(hf + 1) * 512], op=ALU.mult),
                      reads=[OB, MOD], writes=[TMP])
            kb.op("pool", lambda e, j=j: e.tensor_tensor(out=xt[:, j, :], in0=xt[:, j, :], in1=tmp[:], op=ALU.add),
                  reads=[XT, TMP], writes=[XT])
            kb.dma("sp", x_out[rows, :], xt[:, j, :], reads=[XT], writes=[XOUT])
            if final_g is not None:
                kb.op("act", lambda e, j=j: e.activation(out=tmp[:], in_=xt[:, j, :], func=AF.Square, accum_out=sm[:, 0:1]),
                      reads=[XT], writes=[TMP, SM])
                rstd_ops(kb, sm, SM)
                kb.op("dve", lambda e, j=j: e.scalar_tensor_tensor(out=xn[:], in0=xt[:, j, :], scalar=sm[:, 1:2], in1=fg[:],
                                                                   op0=ALU.mult, op1=ALU.mult), reads=[XT, SM, FG], writes=[XNB])
                kb.dma("sp", xn_out[rows, :], xn[:], reads=[XNB], writes=[XN])


def build_pre(even):
    kb = KB()
    kb.psum_banks()
    B = kb.banks
    if even:
        NIN = 3072
        fm_blocks = [(c * 128, None) for c in range(8)] + [(1536 + c * 128, c) for c in range(8)]
        tm_groups = [(1024, 512), (2560, 512)]
    else:
        NIN = 1536
        fm_blocks = [(c * 128, c) for c in range(10)]
        tm_groups = [(1280, 256)]
    NROPE = 128 * sum(1 for _, r in fm_blocks if r is not None)
    NFM = len(fm_blocks)
    NTM = sum(n for _, n in tm_groups)
    x_in = kb.din("x", [TALL, D])
    csT = kb.din("csT", [128, 16])
    w_mod = kb.din("w_mod", [D, 6 * D])
    b_mod = kb.din("b_mod", [6 * D])
    n1g_in = kb.din("n1g", [D])
    w_in = kb.din("w_in", [D, NIN])
    w_perm = kb.din("w_perm", [D, NROPE])
    cosT = kb.din("cosT", [128, TALL])
    sinT = kb.din("sinT", [128, TALL])
    ident_d = kb.din("c_ident", [128, 128])
    fm_out, FMO = kb.dout("fmT", [NFM * 128, TALL], BF16)
    tm_out, TMO = kb.dout("tm", [TALL, NTM], BF16)
    modrow, MRO = kb.dout("modrow", [2, 6, D])

    ident_f, IDF = kb.sb("ident_f", [128, 128], F32)
    ident, ID = kb.sb("ident", [128, 128], BF16)
    kb.dma("sp", ident_f[:], ident_d, writes=[IDF])
    kb.op("act", lambda e: e.copy(out=ident[:], in_=ident_f[:]), reads=[IDF], writes=[ID])
    win, WIN = kb.sb("win", [128, 8, NIN], BF16)
    wpm, WPM = kb.sb("wpm", [128, 8, NROPE], BF16)
    for c0 in range(0, NIN, 512):
        kb.dma("pool", win[:, :, c0:c0 + 512], w_in[:, c0:c0 + 512].rearrange("(k p) n -> p k n", p=128), writes=[WIN])
    for c0 in range(0, NROPE, 512):
        n = min(512, NROPE - c0)
        kb.dma("pool", wpm[:, :, c0:c0 + n], w_perm[:, c0:c0 + n].rearrange("(k p) n -> p k n", p=128), writes=[WPM])
    cs_f, CSF = kb.sb("cs_f", [128, 16], F32)
    cs_b, CSB = kb.sb("cs_b", [128, 16], BF16)
    csbc, CSBC = kb.sb("csbc", [128, 16, 128], BF16)
    kb.dma("sp", cs_f[:], csT, writes=[CSF])
    kb.op("act", lambda e: e.activation(out=cs_b[:], in_=cs_f[:], func=AF.Silu), reads=[CSF], writes=[CSB])
    kb.op("dve", lambda e: e.tensor_copy(out=csbc[:], in_=cs_b[:].unsqueeze(2).to_broadcast([128, 16, 128])),
          reads=[CSB], writes=[CSBC])
    n1g, N1G = kb.sb("n1g_sb", [128, D], F32)
    kb.dma("sp", n1g[:], n1g_in.unsqueeze(0).to_broadcast([128, D]), writes=[N1G])
    g1s, G1S = kb.sb("g1s", [128, 2, 2, D], F32)
    wmr = [kb.sb(f"wmr{i}", [128, 8, 512], BF16) for i in range(2)]
    bmr = [kb.sb(f"bmr{i}", [128, 512], F32) for i in range(2)]
    mtmp = [kb.sb(f"mtmp{i}", [128, 512], F32) for i in range(2)]
    for cgp in range(12):
        wt, WB = wmr[cgp % 2]
        bt, BB = bmr[cgp % 2]
        cols = slice(cgp * 512, (cgp + 1) * 512)
        kb.dma("pool", wt[:], w_mod[:, cols].rearrange("(k p) n -> p k n", p=128), writes=[WB])
        kb.dma("sp", bt[:], b_mod[cols].unsqueeze(0).to_broadcast([128, 512]), writes=[BB])
        chunk = cgp // 2
        half = cgp % 2
        for s in range(2):
            mb, MB = B[6 + s]
            def mmm(e, mb=mb, wt=wt, s=s):
                for k in range(8):
                    ins = e.matmul(mb[:], lhsT=csbc[:, s * 8 + k, :], rhs=wt[:, k, :], start=(k == 0), stop=(k == 7))
                return ins
            kb.op("pe", mmm, reads=[CSBC, WB], writes=[MB])
            if chunk < 2:
                dst = g1s[:, s, chunk, half * 512:(half + 1) * 512]
                kb.op("dve", lambda e, mb=mb, bt=bt, dst=dst: e.tensor_tensor(out=dst, in0=mb[:], in1=bt[:], op=ALU.add),
                      reads=[MB, BB], writes=[G1S])
                kb.dma("sp", modrow[s, chunk:chunk + 1, half * 512:(half + 1) * 512], dst[0:1, :], reads=[G1S], writes=[MRO])
            else:
                mt, MT = mtmp[s]
                kb.op("dve", lambda e, mb=mb, bt=bt, mt=mt: e.tensor_tensor(out=mt[:], in0=mb[:], in1=bt[:], op=ALU.add),
                      reads=[MB, BB], writes=[MT])
                kb.dma("sp", modrow[s, chunk:chunk + 1, half * 512:(half + 1) * 512], mt[0:1, :], reads=[MT], writes=[MRO])
    for s in range(2):
        kb.op("dve", lambda e, s=s: e.scalar_tensor_tensor(out=g1s[:, s, 1, :], in0=g1s[:, s, 1, :], scalar=1.0, in1=n1g[:],
                                                           op0=ALU.add, op1=ALU.mult), reads=[G1S, N1G], writes=[G1S])
    xt = [kb.sb(f"xt{i}", [128, D], F32) for i in range(2)]
    tmp, TMP = kb.sb("tmp", [128, D], F32)
    hx, HX = kb.sb("hx", [128, D], BF16)
    hxT, HXT = kb.sb("hxT", [128, 8, 512], BF16)
    sm, SM = kb.sb("sm", [128, 8], F32)
    cst = [kb.sb(f"cst{i}", [128, 512], F32) for i in range(2)]
    snt = [kb.sb(f"snt{i}", [128, 512], F32) for i in range(2)]
    r1, R1 = kb.sb("r1", [128, 512], F32)
    r2, R2 = kb.sb("r2", [128, 512], F32)
    fmo = [kb.sb(f"fmo{i}", [128, 512], BF16) for i in range(2)]
    tmo = [kb.sb(f"tmo{i}", [128, 512], BF16) for i in range(2)]
    groups = [(g * 4, 4, 0) for g in range(NTL // 4)] + [(NTL, NTC, 1)]
    xi = 0
    fi = 0
    ti = 0
    for gi, (t0, ntl, s) in enumerate(groups):
        NTOK = ntl * 128
        tok0 = t0 * 128
        ct, CT = cst[gi % 2]
        st, ST = snt[gi % 2]
        kb.dma("sp", ct[:, 0:NTOK], cosT[:, tok0:tok0 + NTOK], writes=[CT])
        kb.dma("sp", st[:, 0:NTOK], sinT[:, tok0:tok0 + NTOK], writes=[ST])
        for j in range(ntl):
            x_t, XB = xt[xi % 2]
            xi += 1
            rows = slice(tok0 + j * 128, tok0 + (j + 1) * 128)
            kb.dma("sp", x_t[:], x_in[rows, :], writes=[XB])
            kb.op("act", lambda e, x_t=x_t: e.activation(out=tmp[:], in_=x_t[:], func=AF.Square, accum_out=sm[:, 0:1]),
                  reads=[XB], writes=[TMP, SM])
            rstd_ops(kb, sm, SM)
            kb.op("dve", lambda e, x_t=x_t, s=s: e.scalar_tensor_tensor(out=tmp[:], in0=x_t[:], scalar=sm[:, 1:2], in1=g1s[:, s, 1, :],
                                                                         op0=ALU.mult, op1=ALU.mult), reads=[XB, SM, G1S], writes=[TMP])
            kb.op("pool", lambda e, s=s: e.tensor_tensor(out=hx[:], in0=tmp[:], in1=g1s[:, s, 0, :], op=ALU.add),
                  reads=[TMP, G1S], writes=[HX])
            tb, TB = B[0]
            def tr(e, tb=tb):
                for k in range(8):
                    ins = e.transpose(tb.bitcast(BF16)[:, k * 128:(k + 1) * 128], hx[:, k * 128:(k + 1) * 128], ident[:])
                return ins
            kb.op("pe", tr, reads=[HX, ID], writes=[TB])
            kb.op("act", lambda e, tb=tb, j=j: e.copy(out=hxT[:, :, j * 128:(j + 1) * 128],
                                                      in_=tb.bitcast(BF16)[:, 0:1024].rearrange("p (k t) -> p k t", k=8)),
                  reads=[TB], writes=[HXT])
        for bi, (c0, ridx) in enumerate(fm_blocks):
            pa, PA = B[1 + bi % 2]
            def mma(e, pa=pa, c0=c0, NTOK=NTOK):
                for k in range(8):
                    ins = e.matmul(pa[:, 0:NTOK], lhsT=win[:, k, c0:c0 + 128], rhs=hxT[:, k, 0:NTOK], start=(k == 0), stop=(k == 7))
                return ins
            kb.op("pe", mma, reads=[WIN, HXT], writes=[PA])
            fo, FO = fmo[fi % 2]
            fi += 1
            if ridx is None:
                kb.op("act", lambda e, pa=pa, fo=fo, NTOK=NTOK: e.copy(out=fo[:, 0:NTOK], in_=pa[:, 0:NTOK]), reads=[PA], writes=[FO])
            else:
                pb, PB = B[3 + bi % 2]
                def mmb(e, pb=pb, ridx=ridx, NTOK=NTOK):
                    for k in range(8):
                        ins = e.matmul(pb[:, 0:NTOK], lhsT=wpm[:, k, ridx * 128:(ridx + 1) * 128], rhs=hxT[:, k, 0:NTOK],
                                       start=(k == 0), stop=(k == 7))
                    return ins
                kb.op("pe", mmb, reads=[WPM, HXT], writes=[PB])
                kb.op("dve", lambda e, pa=pa, ct=ct, NTOK=NTOK: e.tensor_tensor(out=r1[:, 0:NTOK], in0=pa[:, 0:NTOK], in1=ct[:, 0:NTOK], op=ALU.mult),
                      reads=[PA, CT], writes=[R1])
                kb.op("dve", lambda e, pb=pb, st=st, NTOK=NTOK: e.tensor_tensor(out=r2[:, 0:NTOK], in0=pb[:, 0:NTOK], in1=st[:, 0:NTOK], op=ALU.mult),
                      reads=[PB, ST], writes=[R2])
                kb.op("pool", lambda e, fo=fo, NTOK=NTOK: e.tensor_tensor(out=fo[:, 0:NTOK], in0=r1[:, 0:NTOK], in1=r2[:, 0:NTOK], op=ALU.add),
                      reads=[R1, R2], writes=[FO])
            for c0_ in range(0, NTOK, 256):
                kb.dma("sp", fm_out[bi * 128:(bi + 1) * 128, tok0 + c0_:tok0 + c0_ + 256], fo[:, c0_:c0_ + 256], reads=[FO], writes=[FMO])
        for j in range(ntl):
            oc = 0
            for (c0, ncol) in tm_groups:
                pt, PT = B[5 + ti % 2]
                to, TO = tmo[ti % 2]
                ti += 1
                def mmt(e, pt=pt, c0=c0, ncol=ncol, j=j):
                    for k in range(8):
                        ins = e.matmul(pt[:, 0:ncol], lhsT=hxT[:, k, j * 128:(j + 1) * 128], rhs=win[:, k, c0:c0 + ncol],
                                       start=(k == 0), stop=(k == 7))
                    return ins
                kb.op("pe", mmt, reads=[HXT, WIN], writes=[PT])
                kb.op("act", lambda e, pt=pt, to=to, ncol=ncol: e.copy(out=to[:, 0:ncol], in_=pt[:, 0:ncol]), reads=[PT], writes=[TO])
                rows = slice(tok0 + j * 128, tok0 + (j + 1) * 128)
                kb.dma("sp", tm_out[rows, oc:oc + ncol], to[:, 0:ncol], reads=[TO], writes=[TMO])
                oc += ncol
    return kb.finish()


def emit_attn_even(kb, ao, AO):
    B = kb.banks
    naqT = kb.din("naqT", [512, TALL], BF16)
    nakT = kb.din("nakT_h", [512, 74 * 64], BF16)
    nav = kb.din("nav_h", [74 * 64, 520], BF16)
    nakTc = kb.din("nakT_c", [512, CTX], BF16)
    navc = kb.din("nav_c", [CTX, 520], BF16)
    dqT = kb.din("dqT", [512, TALL], BF16)
    dkT = kb.din("dkT_all", [512, CTX + SEQ], BF16)
    dv = kb.din("dv_all", [CTX + SEQ, 516], BF16)
    nbias = kb.din("nbias", [5, 8, 128, 640])
    lam_in = kb.din("lam", [4, 64])
    subg_in = kb.din("subg", [128])
    lami_in = kb.din("lam_init", [128, 2])
    sc = 64 ** -0.5
    lm, LM = kb.sb("lm", [128, 4, 64], F32)
    lms, LMS = kb.sb("lms", [128, 16], F32)
    subg, SUBG = kb.sb("subg_sb", [128, 128], F32)
    kb.dma("sp", lm[:].rearrange("p a b -> p (a b)"), lam_in.rearrange("a b -> (a b)").unsqueeze(0).to_broadcast([128, 256]), writes=[LM])
    kb.dma("sp", subg[:], subg_in.unsqueeze(0).to_broadcast([128, 128]), writes=[SUBG])
    kb.dma("sp", lms[:, 8:10], lami_in, writes=[LMS])
    kb.op("dve", lambda e: e.tensor_tensor(out=lm[:, 0, :], in0=lm[:, 0, :], in1=lm[:, 1, :], op=ALU.mult), reads=[LM], writes=[LM])
    kb.op("dve", lambda e: e.tensor_tensor(out=lm[:, 2, :], in0=lm[:, 2, :], in1=lm[:, 3, :], op=ALU.mult), reads=[LM], writes=[LM])
    kb.op("dve", lambda e: e.tensor_reduce(out=lms[:, 0:1], in_=lm[:, 0, :], axis=AX.X, op=ALU.add), reads=[LM], writes=[LMS])
    kb.op("dve", lambda e: e.tensor_reduce(out=lms[:, 1:2], in_=lm[:, 2, :], axis=AX.X, op=ALU.add), reads=[LM], writes=[LMS])
    kb.op("act", lambda e: e.activation(out=lms[:, 2:4], in_=lms[:, 0:2], func=AF.Exp), reads=[LMS], writes=[LMS])
    kb.op("dve", lambda e: e.tensor_tensor(out=lms[:, 4:5], in0=lms[:, 2:3], in1=lms[:, 3:4], op=ALU.subtract), reads=[LMS], writes=[LMS])
    kb.op("dve", lambda e: e.tensor_tensor(out=lms[:, 5:6], in0=lms[:, 4:5], in1=lms[:, 8:9], op=ALU.add), reads=[LMS], writes=[LMS])
    kb.op("dve", lambda e: e.tensor_scalar(out=lms[:, 6:7], in0=lms[:, 5:6], scalar1=-1.0, scalar2=None, op0=ALU.mult), reads=[LMS], writes=[LMS])
    kb.op("dve", lambda e: e.tensor_scalar(out=subg[:], in0=subg[:], scalar1=lms[:, 9:10], scalar2=None, op0=ALU.mult), reads=[SUBG, LMS], writes=[SUBG])

    kcT, KCT = kb.sb("na_kcT", [128, 4, CTX], BF16)
    vca, VCA = kb.sb("na_vca", [128, 2, 8, 65], BF16)
    kb.dma("sp", kcT[:], nakTc.rearrange("(a p) n -> p a n", p=128), writes=[KCT])
    kb.dma("sp", vca[:].rearrange("p b h c -> p b (h c)"), navc.rearrange("(b p) c -> p b c", p=128), writes=[VCA])
    kts = [kb.sb(f"na_kt{i}", [128, 4, 640], BF16) for i in range(2)]
    vts = [kb.sb(f"na_vt{i}", [128, 5, 8, 65], BF16) for i in range(2)]
    qts = [kb.sb(f"na_qt{i}", [128, 4, 128], BF16) for i in range(2)]
    bts = [kb.sb(f"na_bt{i}", [128, 640], F32) for i in range(2)]
    sts = [kb.sb(f"na_st{i}", [128, 640], F32) for i in range(2)]
    pts = [kb.sb(f"na_pt{i}", [128, 896], BF16) for i in range(2)]
    aot = [kb.sb(f"na_ao{i}", [128, 512], BF16) for i in range(2)]
    rc, RC = kb.sb("na_rc", [128, 8], F32)
    hi = 0
    for qt in range(NT):
        isctx = qt >= NTL
        q_t, QB = qts[qt % 2]
        kb.dma("sp", q_t[:], naqT[:, qt * 128:(qt + 1) * 128].rearrange("(a p) n -> p a n", p=128), writes=[QB])
        if not isctx:
            k_t, KB_ = kts[qt % 2]
            v_t, VB = vts[qt % 2]
            kb.dma("sp", k_t[:], nakT[:, qt * 128:qt * 128 + 640].rearrange("(a p) n -> p a n", p=128), writes=[KB_])
            kb.dma("sp", v_t[:].rearrange("p b h c -> p b (h c)"), nav[qt * 128:qt * 128 + 640, :].rearrange("(b p) c -> p b c", p=128), writes=[VB])
            slot = 0 if qt == 0 else 1 if qt == 1 else 3 if qt == NTL - 2 else 4 if qt == NTL - 1 else 2
        a_t, AB = aot[qt % 2]
        for h in range(8):
            a, off = h // 2, (h % 2) * 64
            pa, PA = B[(2 * hi) % 4]
            pb, PB = B[(2 * hi + 1) % 4]
            st_, STB = sts[hi % 2]
            pt_, PTB = pts[hi % 2]
            acc, ACC = B[4 + (h // 4)]
            hi += 1
            if not isctx:
                b_t, BB = bts[hi % 2]
                kb.dma("sp", b_t[:], nbias[slot, h], writes=[BB])
                def mms(e, pa=pa, pb=pb, k_t=k_t, q_t=q_t, a=a, off=off):
                    for blk in range(4):
                        e.matmul(pa[:, blk * 128:(blk + 1) * 128], lhsT=k_t[off:off + 64, a, blk * 128:(blk + 1) * 128],
                                 rhs=q_t[off:off + 64, a, :], start=True, stop=True)
                    e.matmul(pb[:, 0:128], lhsT=k_t[off:off + 64, a, 512:640], rhs=q_t[off:off + 64, a, :], start=True, stop=True)
                    for cb in range(2):
                        ins = e.matmul(pb[:, 128 + cb * 128:256 + cb * 128], lhsT=kcT[off:off + 64, a, cb * 128:(cb + 1) * 128],
                                       rhs=q_t[off:off + 64, a, :], start=True, stop=True)
                    return ins
                kb.op("pe", mms, reads=[KB_, QB, KCT], writes=[PA, PB])
                kb.op("dve", lambda e, pa=pa, st_=st_, b_t=b_t: e.scalar_tensor_tensor(out=st_[:, 0:512], in0=pa[:], scalar=sc, in1=b_t[:, 0:512],
                                                                                    op0=ALU.mult, op1=ALU.add), reads=[PA, BB], writes=[STB])
                kb.op("dve", lambda e, pb=pb, st_=st_, b_t=b_t: e.scalar_tensor_tensor(out=st_[:, 512:640], in0=pb[:, 0:128], scalar=sc,
                                                                                    in1=b_t[:, 512:640], op0=ALU.mult, op1=ALU.add),
                      reads=[PB, BB], writes=[STB])
                kb.op("act", lambda e, st_=st_, pt_=pt_: e.activation(out=pt_[:, 0:640], in_=st_[:], func=AF.Exp), reads=[STB], writes=[PTB])
                kb.op("act", lambda e, pb=pb, pt_=pt_: e.activation(out=pt_[:, 640:896], in_=pb[:, 128:384], func=AF.Exp, scale=sc),
                      reads=[PB], writes=[PTB])
                def mmv(e, acc=acc, pt_=pt_, v_t=v_t, h=h):
                    o = acc[:, (h % 4) * 65:(h % 4) * 65 + 65]
                    for blk in range(5):
                        e.matmul(o, lhsT=pt_[:, blk * 128:(blk + 1) * 128], rhs=v_t[:, blk, h, :], start=(blk == 0), stop=False)
                    for cb in range(2):
                        ins = e.matmul(o, lhsT=pt_[:, 640 + cb * 128:768 + cb * 128], rhs=vca[:, cb, h, :], start=False, stop=(cb == 1))
                    return ins
                kb.op("pe", mmv, reads=[PTB, VB, VCA], writes=[ACC])
            else:
                def mms(e, pb=pb, q_t=q_t, a=a, off=off):
                    for cb in range(2):
                        ins = e.matmul(pb[:, 128 + cb * 128:256 + cb * 128], lhsT=kcT[off:off + 64, a, cb * 128:(cb + 1) * 128],
                                       rhs=q_t[off:off + 64, a, :], start=True, stop=True)
                    return ins
                kb.op("pe", mms, reads=[QB, KCT], writes=[PB])
                kb.op("act", lambda e, pb=pb, pt_=pt_: e.activation(out=pt_[:, 640:896], in_=pb[:, 128:384], func=AF.Exp, scale=sc),
                      reads=[PB], writes=[PTB])
                def mmv(e, acc=acc, pt_=pt_, h=h):
                    o = acc[:, (h % 4) * 65:(h % 4) * 65 + 65]
                    for cb in range(2):
                        ins = e.matmul(o, lhsT=pt_[:, 640 + cb * 128:768 + cb * 128], rhs=vca[:, cb, h, :], start=(cb == 0), stop=(cb == 1))
                    return ins
                kb.op("pe", mmv, reads=[PTB, VCA], writes=[ACC])
            if h % 4 == 3:
                g4 = h // 4
                av = acc[:, 0:260].rearrange("p (h c) -> p h c", c=65)
                kb.op("dve", lambda e, av=av, g4=g4: e.reciprocal(out=rc[:, g4 * 4:g4 * 4 + 4], in_=av[:, :, 64]), reads=[ACC], writes=[RC])
                kb.op("dve", lambda e, av=av, g4=g4, a_t=a_t: e.tensor_tensor(
                    out=a_t[:, g4 * 256:(g4 + 1) * 256].rearrange("p (h d) -> p h d", d=64), in0=av[:, :, 0:64],
                    in1=rc[:, g4 * 4:g4 * 4 + 4].unsqueeze(2).to_broadcast([128, 4, 64]), op=ALU.mult), reads=[ACC, RC], writes=[AB])
        kb.dma("sp", ao[qt * 128:(qt + 1) * 128, 0:512], a_t[:], reads=[AB], writes=[AO])

    NBLK = (CTX + SEQ) // 128
    dk, DK = kb.sb("d_k", [128, CTX + SEQ], BF16)
    dva, DVA = kb.sb("d_va", [128, NBLK, 129], BF16)
    dq, DQ = kb.sb("d_q", [128, TALL], BF16)
    dpt = [kb.sb(f"d_pt{i}", [128, 512], BF16) for i in range(3)]
    o0, O0 = kb.sb("d_o0", [128, 128], F32)
    o1, O1 = kb.sb("d_o1", [128, 128], F32)
    osq, OSQ = kb.sb("d_osq", [128, 128], F32)
    dsm, DSM = kb.sb("d_sm", [128, 8], F32)
    dob = [kb.sb(f"d_ob{i}", [128, 128], BF16) for i in range(2)]
    si = 0
    oi = 0
    for h in range(4):
        kb.dma("sp", dk[:], dkT[h * 128:(h + 1) * 128, :], writes=[DK])
        for c0 in range(0, NBLK, 26):
            kb.dma("sp", dva[:, c0:c0 + 26, :], dv[c0 * 128:(c0 + 26) * 128, h * 129:(h + 1) * 129].rearrange("(b p) d -> p b d", p=128),
                   writes=[DVA])
        kb.dma("sp", dq[:], dqT[h * 128:(h + 1) * 128, :], writes=[DQ])
        qgroups = [(g * 256, 256, NBLK) for g in range(TQ // 256)] + [(TQ, CTX, CTX // 128)]
        for (q0, nq, nblk) in qgroups:
            nqs = nq // 128
            for blk in range(nblk):
                for m in range(2):
                    ps, PS = B[si % 3]
                    pt_, PTB = dpt[si % 3]
                    si += 1
                    kb.op("pe", lambda e, ps=ps, m=m, blk=blk, q0=q0, nq=nq: e.matmul(
                        ps[:, 0:nq], lhsT=dk[m * 64:(m + 1) * 64, blk * 128:(blk + 1) * 128], rhs=dq[m * 64:(m + 1) * 64, q0:q0 + nq],
                        start=True, stop=True), reads=[DK, DQ], writes=[PS])
                    kb.op("act", lambda e, ps=ps, pt_=pt_, nq=nq: e.activation(out=pt_[:, 0:nq], in_=ps[:, 0:nq], func=AF.Exp, scale=sc),
                          reads=[PS], writes=[PTB])
                    def mmv(e, pt_=pt_, m=m, blk=blk, nqs=nqs, nblk=nblk):
                        for qs in range(nqs):
                            a = qs * 2 + m
                            acc = B[3 + a][0]
                            ins = e.matmul(acc[:, 0:129], lhsT=pt_[:, qs * 128:(qs + 1) * 128], rhs=dva[:, blk, :],
                                           start=(blk == 0), stop=(blk == nblk - 1))
                        return ins
                    kb.op("pe", mmv, reads=[PTB, DVA], writes=[B[3 + qs * 2 + m][1] for qs in range(nqs)])
            for qs in range(nqs):
                accs = []
                for m in range(2):
                    a = qs * 2 + m
                    accs.append((B[3 + a][0][:, 0:129], B[3 + a][1]))
                (a0, A0), (a1, A1) = accs
                kb.op("dve", lambda e, a0=a0: e.reciprocal(out=dsm[:, 0:1], in_=a0[:, 128:129]), reads=[A0], writes=[DSM])
                kb.op("dve", lambda e, a1=a1: e.reciprocal(out=dsm[:, 1:2], in_=a1[:, 128:129]), reads=[A1], writes=[DSM])
                kb.op("dve", lambda e: e.tensor_tensor(out=dsm[:, 1:2], in0=dsm[:, 1:2], in1=lms[:, 6:7], op=ALU.mult), reads=[DSM, LMS], writes=[DSM])
                kb.op("dve", lambda e, a0=a0: e.tensor_scalar(out=o0[:], in0=a0[:, 0:128], scalar1=dsm[:, 0:1], scalar2=None, op0=ALU.mult),
                      reads=[A0, DSM], writes=[O0])
                kb.op("dve", lambda e, a1=a1: e.scalar_tensor_tensor(out=o1[:], in0=a1[:, 0:128], scalar=dsm[:, 1:2], in1=o0[:],
                                                                   op0=ALU.mult, op1=ALU.add), reads=[A1, DSM, O0], writes=[O1])
                kb.op("act", lambda e: e.activation(out=osq[:], in_=o1[:], func=AF.Square, accum_out=dsm[:, 2:3]), reads=[O1], writes=[OSQ, DSM])
                kb.op("dve", lambda e: e.tensor_scalar(out=dsm[:, 3:4], in0=dsm[:, 2:3], scalar1=1.0 / 128, scalar2=EPS, op0=ALU.mult, op1=ALU.add),
                      reads=[DSM], writes=[DSM])
                kb.op("act", lambda e: e.activation(out=dsm[:, 4:5], in_=dsm[:, 3:4], func=AF.Sqrt), reads=[DSM], writes=[DSM])
                kb.op("dve", lambda e: e.reciprocal(out=dsm[:, 5:6], in_=dsm[:, 4:5]), reads=[DSM], writes=[DSM])
                ob, OB = dob[oi % 2]
                oi += 1
                kb.op("dve", lambda e, ob=ob: e.scalar_tensor_tensor(out=ob[:], in0=o1[:], scalar=dsm[:, 5:6], in1=subg[:], op0=ALU.mult, op1=ALU.mult),
                      reads=[O1, DSM, SUBG], writes=[OB])
                r0 = q0 + qs * 128
                kb.dma("sp", ao[r0:r0 + 128, 512 + h * 128:512 + (h + 1) * 128], ob[:], reads=[OB], writes=[AO])


def emit_attn_odd(kb, ao, AO):
    B = kb.banks
    HALO = TQ + 256
    qin = kb.din("swa_q", [64, 16, TALL], BF16)
    kin = kb.din("swa_kT_h", [64, 4, HALO], BF16)
    vin = kb.din("swa_v_h", [HALO, 260], BF16)
    kcin = kb.din("swa_kT_c", [64, 4, CTX], BF16)
    vcin = kb.din("swa_v_c", [CTX, 260], BF16)
    sbias = kb.din("sbias", [3, 128, 384])
    sink_in = kb.din("sink", [16])
    sc = 64 ** -0.5
    snk, SNK = kb.sb("snk", [128, 16], F32)
    kb.dma("sp", snk[:], sink_in.unsqueeze(0).to_broadcast([128, 16]), writes=[SNK])
    kb.op("act", lambda e: e.activation(out=snk[:], in_=snk[:], func=AF.Exp), reads=[SNK], writes=[SNK])
    kT, KT = kb.sb("s_kT", [64, 4, HALO], BF16)
    kb.dma("sp", kT[:], kin, writes=[KT])
    kcT, KCT = kb.sb("s_kcT", [64, 4, CTX], BF16)
    kb.dma("sp", kcT[:], kcin, writes=[KCT])
    va, VA = kb.sb("s_va", [128, HALO // 128, 4, 65], BF16)
    kb.dma("sp", va[:].rearrange("p b h c -> p b (h c)"), vin.rearrange("(b p) c -> p b c", p=128), writes=[VA])
    vca, VCA = kb.sb("s_vca", [128, 2, 4, 65], BF16)
    kb.dma("sp", vca[:].rearrange("p b h c -> p b (h c)"), vcin.rearrange("(b p) c -> p b c", p=128), writes=[VCA])
    sb_, SBB = kb.sb("s_bias", [128, 3, 384], F32)
    kb.dma("sp", sb_[:], sbias.rearrange("s p n -> p s n"), writes=[SBB])
    qts = [kb.sb(f"s_q{i}", [64, 16, 128], BF16) for i in range(2)]
    sts = [kb.sb(f"s_st{i}", [128, 512], F32) for i in range(2)]
    pts = [kb.sb(f"s_pt{i}", [128, 5, 512], BF16) for i in range(2)]
    aot = [kb.sb(f"s_ao{i}", [128, D], BF16) for i in range(2)]
    den, DEN = kb.sb("s_den", [128, 8], F32)
    si = 0
    gi = 0
    for qt in range(NT):
        isctx = qt >= NTL
        q_t, QB = qts[qt % 2]
        kb.dma("sp", q_t[:], qin[:, :, qt * 128:(qt + 1) * 128], writes=[QB])
        a_t, AB = aot[qt % 2]
        slot = 0 if qt == 0 else 2 if qt == NTL - 1 else 1
        for n in range(4):
            pt_, PTB = pts[gi % 2]
            acc, ACC = B[4 + gi % 2]
            gi += 1
            blks = ([] if isctx else [0, 1, 2]) + [3, 4]
            for blk in blks:
                ps, PS = B[si % 3]
                st_, STB = sts[si % 2]
                si += 1
                if blk < 3:
                    kb.op("pe", lambda e, ps=ps, blk=blk, n=n, q_t=q_t, qt=qt: e.matmul(
                        ps[:], lhsT=kT[:, n, (qt + blk) * 128:(qt + blk + 1) * 128], rhs=q_t[:, n * 4:(n + 1) * 4, :], start=True, stop=True),
                        reads=[KT, QB], writes=[PS])
                    kb.op("dve", lambda e, ps=ps, st_=st_, blk=blk, slot=slot: e.scalar_tensor_tensor(
                        out=st_[:].rearrange("p (g q) -> p g q", g=4), in0=ps[:].rearrange("p (g q) -> p g q", g=4), scalar=sc,
                        in1=sb_[:, slot, blk * 128:(blk + 1) * 128].unsqueeze(1).to_broadcast([128, 4, 128]), op0=ALU.mult, op1=ALU.add),
                        reads=[PS, SBB], writes=[STB])
                    kb.op("act", lambda e, st_=st_, pt_=pt_, blk=blk: e.activation(out=pt_[:, blk, :], in_=st_[:], func=AF.Exp), reads=[STB], writes=[PTB])
                else:
                    cb = blk - 3
                    kb.op("pe", lambda e, ps=ps, cb=cb, n=n, q_t=q_t: e.matmul(
                        ps[:], lhsT=kcT[:, n, cb * 128:(cb + 1) * 128], rhs=q_t[:, n * 4:(n + 1) * 4, :], start=True, stop=True),
                        reads=[KCT, QB], writes=[PS])
                    kb.op("act", lambda e, ps=ps, pt_=pt_, blk=blk: e.activation(out=pt_[:, blk, :], in_=ps[:], func=AF.Exp, scale=sc),
                          reads=[PS], writes=[PTB])
            def mmv(e, acc=acc, pt_=pt_, n=n, qt=qt, blks=blks):
                for g in range(4):
                    o = acc[:, g * 65:(g + 1) * 65]
                    for i, blk in enumerate(blks):
                        rhs = va[:, qt + blk, n, :] if blk < 3 else vca[:, blk - 3, n, :]
                        ins = e.matmul(o, lhsT=pt_[:, blk, g * 128:(g + 1) * 128], rhs=rhs, start=(i == 0), stop=(i == len(blks) - 1))
                return ins
            kb.op("pe", mmv, reads=[PTB, VA, VCA], writes=[ACC])
            av = acc[:, 0:260].rearrange("p (g c) -> p g c", c=65)
            kb.op("dve", lambda e, av=av, n=n: e.tensor_tensor(out=den[:, 0:4], in0=av[:, :, 64], in1=snk[:, n * 4:(n + 1) * 4], op=ALU.add),
                  reads=[ACC, SNK], writes=[DEN])
            kb.op("dve", lambda e: e.reciprocal(out=den[:, 4:8], in_=den[:, 0:4]), reads=[DEN], writes=[DEN])
            kb.op("dve", lambda e, av=av, n=n, a_t=a_t: e.tensor_tensor(
                out=a_t[:, n * 256:(n + 1) * 256].rearrange("p (g d) -> p g d", d=64), in0=av[:, :, 0:64],
                in1=den[:, 4:8].unsqueeze(2).to_broadcast([128, 4, 64]), op=ALU.mult), reads=[ACC, DEN], writes=[AB])
        kb.dma("sp", ao[qt * 128:(qt + 1) * 128, :], a_t[:], reads=[AB], writes=[AO])


def build_post(even):
    kb = KB()
    kb.psum_banks()
    ao, AO = kb.dscratch("ao_scr", [TALL, D], BF16)
    with ExitStack() as es:
        saved = kb.es
        kb.es = es
        if even:
            emit_attn_even(kb, ao, AO)
        else:
            emit_attn_odd(kb, ao, AO)
        kb.es = saved
        kb.P.barrier()
    x_in = kb.din("x", [TALL, D])
    modrow = kb.din("modrow", [2, 6, D])
    w_out = kb.din("w_out", [D, D])
    n2g = kb.din("n2g", [D])
    fg = kb.din("fg", [D])
    wq = kb.din("wq", [D, 2048])
    keysT = kb.din("keysT", [128, 16, 128])
    uT = kb.din("uT", [D, 16384])
    v = kb.din("v", [16384, D])
    x_out, XOUT = kb.dout("x_out", [TALL, D])
    xn_out, XN = kb.dout("xn_out", [TALL, D])
    emit_post(kb, NT, x_in, ao, AO, modrow, w_out, n2g, wq, keysT, uT, v, x_out, XOUT, final_g=fg, xn_out=xn_out, XN=XN,
              tile_sets=[0] * NTL + [1] * NTC)
    return kb.finish()


GRID_W = 64
_PERM64 = np.concatenate([np.arange(16, 32), np.arange(0, 16), np.arange(48, 64), np.arange(32, 48)])
_SGN64 = np.concatenate([-np.ones(16), np.ones(16), -np.ones(16), np.ones(16)]).astype(np.float32)
_PROGS = {}


def _prog(name):
    if name not in _PROGS:
        kind, par = name.split("_")
        _PROGS[name] = build_pre(par == "even") if kind == "pre" else build_post(par == "even")
    return _PROGS[name]


def _rope_tables_T(tok0):
    t = np.arange(tok0, tok0 + TQ)
    row = (t // GRID_W).astype(np.float32)
    col = (t % GRID_W).astype(np.float32)
    half = 32
    inv = (10000.0 ** (-np.arange(0, half, 2, dtype=np.float32) / half)).astype(np.float32)
    ar = row[:, None] * inv
    ac = col[:, None] * inv
    ang = np.concatenate([ar, ar, ac, ac], axis=-1)
    cos = np.cos(ang).astype(np.float32)
    sin = np.sin(ang).astype(np.float32) * _SGN64[None, :]
    cosT = np.ones((128, TALL), np.float32)
    sinT = np.zeros((128, TALL), np.float32)
    cosT[:, :TQ] = np.tile(cos.T, (2, 1))
    sinT[:, :TQ] = np.tile(sin.T, (2, 1))
    return cosT, sinT


def _na_bias(rpb, R0):
    H = rpb.shape[0]
    out = np.full((5, H, 128, 5, 128), NEG, np.float32)
    kk = np.arange(128)
    qq = np.arange(128)
    for slot, r0 in enumerate((R0, R0 + 2, R0 + 32, R0 + 60, R0 + 62)):
        if slot == 2:
            r0 = 100 if R0 not in (0,) else 100
        qr = r0 + qq // 64
        qc = qq % 64
        rs = np.clip(qr - 4, 0, 256 - 8)
        cs = np.clip(qc - 8, 0, 64 - 16)
        for blk in range(5):
            kr = r0 - 4 + 2 * blk + kk // 64
            kc = kk % 64
            valid = ((kr[:, None] >= rs[None, :]) & (kr[:, None] < rs[None, :] + 8) &
                     (kc[:, None] >= cs[None, :]) & (kc[:, None] < cs[None, :] + 16) &
                     (kr[:, None] >= 0) & (kr[:, None] < 256))
            dr = np.clip(kr[:, None] - qr[None, :] + 7, 0, 14)
            dc = np.clip(kc[:, None] - qc[None, :] + 15, 0, 30)
            vals = rpb[:, dr, dc]
            out[slot, :, :, blk, :] = np.where(valid[None], vals, NEG)
    return out.reshape(5, H, 128, 640)


def _swa_bias(qr):
    out = np.full((3, 128, 3, 128), NEG, np.float32)
    k = np.arange(128)[:, None]
    q = np.arange(128)[None, :]
    for slot, gb in enumerate((qr * 32, qr * 32 + 5, qr * 32 + 31)):
        for blk in range(3):
            kb_ = gb - 1 + blk
            if kb_ < 0 or kb_ >= SEQ // 128:
                continue
            diff = (blk - 1) * 128 + k - q
            out[slot, :, blk, :] = np.where(np.abs(diff) <= 128, 0.0, NEG)
    return out.reshape(3, 128, 384)


def _ones_col(v, nh, dv):
    T = v.shape[0]
    o = np.ones((T, nh, dv + 1), v.dtype)
    o[:, :, :dv] = v.reshape(T, nh, dv)
    return o.reshape(T, nh * (dv + 1))


def _halo(arr, axis, lo, hi):
    n = arr.shape[axis]
    shape = list(arr.shape)
    shape[axis] = hi - lo
    out = np.zeros(shape, arr.dtype)
    s0, s1 = max(lo, 0), min(hi, n)
    src = [slice(None)] * arr.ndim
    dst = [slice(None)] * arr.ndim
    src[axis] = slice(s0, s1)
    dst[axis] = slice(s0 - lo, s1 - lo)
    out[tuple(dst)] = arr[tuple(src)]
    return out


def _run(name, in_maps):
    res = run_bass_kernel_spmd(_prog(name), in_maps, core_ids=list(range(NCORES)))
    return res.results


def kernel(x, c, ctx, c_ctx, w_mod, b_mod, norm1_g, norm2_g, w_in_even, w_out_even, na_rpb, diff_lambda,
           diff_subln_g, w_in_odd, w_out_odd, swa_sink, peer_wq, peer_keys, peer_u, peer_v, final_g, _nlayers=4, _dbg=None):
    f32 = np.float32
    x = np.asarray(x, f32)
    ctx = np.asarray(ctx, f32)
    ident = np.eye(128, dtype=f32)
    xs = []
    for core in range(NCORES):
        b, qr = divmod(core, 4)
        xs.append(np.concatenate([x[b, qr * TQ:(qr + 1) * TQ], ctx[b]], axis=0))
    csT = []
    for core in range(NCORES):
        b = core // 4
        cs = np.stack([np.asarray(c, f32)[b], np.asarray(c_ctx, f32)], 0)
        csT.append(np.ascontiguousarray(cs.reshape(2, 8, 128).transpose(2, 0, 1).reshape(128, 16)))
    ropes = [_rope_tables_T((core % 4) * TQ) for core in range(NCORES)]
    xn = None
    for i in range(_nlayers):
        even = (i % 2 == 0)
        j = i // 2
        w_in = np.asarray(w_in_even[j] if even else w_in_odd[j], f32)
        if even:
            rc = w_in[:, 1536:2560]
        else:
            rc = w_in[:, 0:1280]
        w_perm = np.ascontiguousarray(rc.reshape(D, -1, 64)[:, :, _PERM64].reshape(D, -1))
        ims = []
        for core in range(NCORES):
            ims.append({"x": xs[core], "csT": csT[core], "w_mod": np.asarray(w_mod[i], f32), "b_mod": np.asarray(b_mod[i], f32),
                        "n1g": np.asarray(norm1_g[i], f32), "w_in": w_in, "w_perm": w_perm,
                        "cosT": ropes[core][0], "sinT": ropes[core][1], "c_ident": ident})
        pre = _run("pre_even" if even else "pre_odd", ims)
        fm = [np.asarray(r["fmT"]) for r in pre]
        tm = [np.asarray(r["tm"]) for r in pre]
        if _dbg is not None:
            _dbg[f"pre{i}"] = (fm, tm, [np.asarray(r["modrow"]) for r in pre])
        keysT = np.ascontiguousarray(np.asarray(peer_keys[i], f32).reshape(16, 128, 128).transpose(2, 0, 1))
        common = {"w_out": np.asarray(w_out_even[j] if even else w_out_odd[j], f32), "n2g": np.asarray(norm2_g[i], f32),
                  "fg": np.asarray(final_g, f32), "wq": np.asarray(peer_wq[i], f32), "keysT": keysT,
                  "uT": np.ascontiguousarray(np.asarray(peer_u[i], f32).T), "v": np.asarray(peer_v[i], f32), "c_ident": ident}
        ims = []
        for core in range(NCORES):
            b, qr = divmod(core, 4)
            grp = [b * 4 + k for k in range(4)]
            im = dict(common)
            im["x"] = xs[core]
            im["modrow"] = np.asarray(pre[core]["modrow"])
            if even:
                kT_lat = np.concatenate([fm[g][512:1024, :TQ] for g in grp], axis=1)
                v_lat = np.concatenate([tm[g][:TQ, 0:512] for g in grp], axis=0)
                lo = (qr * 64 - 4) * 64
                im["naqT"] = np.ascontiguousarray(fm[core][0:512])
                im["nakT_h"] = _halo(kT_lat, 1, lo, lo + 74 * 64)
                im["nav_h"] = _ones_col(_halo(v_lat, 0, lo, lo + 74 * 64), 8, 64)
                im["nakT_c"] = np.ascontiguousarray(fm[core][512:1024, TQ:])
                im["nav_c"] = _ones_col(tm[core][TQ:, 0:512], 8, 64)
                im["dqT"] = np.ascontiguousarray(fm[core][1024:1536])
                im["dkT_all"] = np.concatenate([fm[core][1536:2048, TQ:]] + [fm[g][1536:2048, :TQ] for g in grp], axis=1)
                im["dv_all"] = _ones_col(np.concatenate([tm[core][TQ:, 512:1024]] + [tm[g][:TQ, 512:1024] for g in grp], axis=0), 4, 128)
                im["nbias"] = _na_bias(np.asarray(na_rpb[j], f32), qr * 64)
                im["lam"] = np.asarray(diff_lambda[j], f32)
                im["subg"] = np.asarray(diff_subln_g[j], f32)
                li = 0.8 - 0.6 * math.exp(-0.3 * i)
                im["lam_init"] = np.tile(np.array([[li, 1.0 - li]], f32), (128, 1))
            else:
                kT_lat = np.concatenate([fm[g][1024:1280, :TQ] for g in grp], axis=1)
                v_lat = np.concatenate([tm[g][:TQ, :] for g in grp], axis=0)
                lo = qr * TQ - 128
                im["swa_q"] = np.ascontiguousarray(fm[core][0:1024].reshape(16, 64, TALL).transpose(1, 0, 2))
                im["swa_kT_h"] = np.ascontiguousarray(_halo(kT_lat, 1, lo, lo + TQ + 256).reshape(4, 64, TQ + 256).transpose(1, 0, 2))
                im["swa_v_h"] = _ones_col(_halo(v_lat, 0, lo, lo + TQ + 256), 4, 64)
                im["swa_kT_c"] = np.ascontiguousarray(fm[core][1024:1280, TQ:].reshape(4, 64, CTX).transpose(1, 0, 2))
                im["swa_v_c"] = _ones_col(tm[core][TQ:, :], 4, 64)
                im["sbias"] = _swa_bias(qr)
                im["sink"] = np.asarray(swa_sink[j], f32)
            ims.append(im)
        post = _run("post_even" if even else "post_odd", ims)
        xs = [np.asarray(r["x_out"]) for r in post]
        xn = [np.asarray(r["xn_out"]) for r in post]
        if _dbg is not None:
            _dbg[f"x{i}"] = xs
    out = np.zeros((2, SEQ, D), f32)
    for core in range(NCORES):
        b, qr = divmod(core, 4)
        out[b, qr * TQ:(qr + 1) * TQ] = xn[core][:TQ]
    return out


RG = [[0, 1, 2, 3], [4, 5, 6, 7]]


def emit_pre_f(kb, even, x_src, XS, csT, w_mod, b_mod, n1g_in, w_in, w_perm, cosT, sinT, fm_out, FMO, tm_out, TMO, modrow, MRO):
    B = kb.banks
    if even:
        NIN = 3072
        fm_blocks = [(c * 128, None) for c in range(8)] + [(1536 + c * 128, c) for c in range(8)]
        tm_groups = [(1024, 512, 8, 64, 0), (2560, 512, 4, 128, 520)]
    else:
        NIN = 1536
        fm_blocks = [(c * 128, c) for c in range(10)]
        tm_groups = [(1280, 256, 4, 64, 0)]
    NROPE = 128 * sum(1 for _, r in fm_blocks if r is not None)
    ident_f, IDF, ident, ID = kb.ident()
    win, WIN = kb.sb("win", [128, 8, NIN], BF16)
    wpm, WPM = kb.sb("wpm", [128, 8, NROPE], BF16)
    for c0 in range(0, NIN, 512):
        kb.dma("pool", win[:, :, c0:c0 + 512], w_in[:, c0:c0 + 512].rearrange("(k p) n -> p k n", p=128), writes=[WIN])
    for c0 in range(0, NROPE, 512):
        n = min(512, NROPE - c0)
        kb.dma("pool", wpm[:, :, c0:c0 + n], w_perm[:, c0:c0 + n].rearrange("(k p) n -> p k n", p=128), writes=[WPM])
    cs_f, CSF = kb.sb("cs_f", [128, 16], F32)
    cs_b, CSB = kb.sb("cs_b", [128, 16], BF16)
    csbc, CSBC = kb.sb("csbc", [128, 16, 128], BF16)
    kb.dma("sp", cs_f[:], csT, writes=[CSF])
    kb.op("act", lambda e: e.activation(out=cs_b[:], in_=cs_f[:], func=AF.Silu), reads=[CSF], writes=[CSB])
    kb.op("dve", lambda e: e.tensor_copy(out=csbc[:], in_=cs_b[:].unsqueeze(2).to_broadcast([128, 16, 128])),
          reads=[CSB], writes=[CSBC])
    n1g, N1G = kb.sb("n1g_sb", [128, D], F32)
    kb.dma("sp", n1g[:], n1g_in.unsqueeze(0).to_broadcast([128, D]), writes=[N1G])
    g1s, G1S = kb.sb("g1s", [128, 2, 2, D], F32)
    wmr = [kb.sb(f"wmr{i}", [128, 8, 512], BF16) for i in range(2)]
    bmr = [kb.sb(f"bmr{i}", [128, 512], F32) for i in range(2)]
    mtmp = [kb.sb(f"mtmp{i}", [128, 512], F32) for i in range(2)]
    for cgp in range(12):
        wt, WB = wmr[cgp % 2]
        bt, BB = bmr[cgp % 2]
        cols = slice(cgp * 512, (cgp + 1) * 512)
        kb.dma("pool", wt[:], w_mod[:, cols].rearrange("(k p) n -> p k n", p=128), writes=[WB])
        kb.dma("sp", bt[:], b_mod[cols].unsqueeze(0).to_broadcast([128, 512]), writes=[BB])
        chunk = cgp // 2
        half = cgp % 2
        for s in range(2):
            mb, MB = B[6 + s]
            def mmm(e, mb=mb, wt=wt, s=s):
                for k in range(8):
                    ins = e.matmul(mb[:], lhsT=csbc[:, s * 8 + k, :], rhs=wt[:, k, :], start=(k == 0), stop=(k == 7))
                return ins
            kb.op("pe", mmm, reads=[CSBC, WB], writes=[MB])
            if chunk < 2:
                dst = g1s[:, s, chunk, half * 512:(half + 1) * 512]
                kb.op("dve", lambda e, mb=mb, bt=bt, dst=dst: e.tensor_tensor(out=dst, in0=mb[:], in1=bt[:], op=ALU.add),
                      reads=[MB, BB], writes=[G1S])
                kb.dma("sp", modrow[s, chunk:chunk + 1, half * 512:(half + 1) * 512], dst[0:1, :], reads=[G1S], writes=[MRO])
            else:
                mt, MT = mtmp[s]
                kb.op("dve", lambda e, mb=mb, bt=bt, mt=mt: e.tensor_tensor(out=mt[:], in0=mb[:], in1=bt[:], op=ALU.add),
                      reads=[MB, BB], writes=[MT])
                kb.dma("sp", modrow[s, chunk:chunk + 1, half * 512:(half + 1) * 512], mt[0:1, :], reads=[MT], writes=[MRO])
    for s in range(2):
        kb.op("dve", lambda e, s=s: e.scalar_tensor_tensor(out=g1s[:, s, 1, :], in0=g1s[:, s, 1, :], scalar=1.0, in1=n1g[:],
                                                           op0=ALU.add, op1=ALU.mult), reads=[G1S, N1G], writes=[G1S])
    xt = [kb.sb(f"xt{i}", [128, D], F32) for i in range(2)]
    tmp, TMP = kb.sb("tmp", [128, D], F32)
    hx, HX = kb.sb("hx", [128, D], BF16)
    hxT, HXT = kb.sb("hxT", [128, 8, 512], BF16)
    sm, SM = kb.sb("sm", [128, 8], F32)
    cst = [kb.sb(f"cst{i}", [128, 512], F32) for i in range(2)]
    snt = [kb.sb(f"snt{i}", [128, 512], F32) for i in range(2)]
    r1, R1 = kb.sb("r1", [128, 512], F32)
    r2, R2 = kb.sb("r2", [128, 512], F32)
    fmo = [kb.sb(f"fmo{i}", [128, 512], BF16) for i in range(2)]
    tmo = [kb.sb(f"tmo{i}", [128, 520], BF16) for i in range(2)]
    for to, TO in tmo:
        kb.op("pool", lambda e, to=to: e.memset(to[:], 1.0), writes=[TO])
    groups = [(g * 4, 4, 0) for g in range(NTL // 4)] + [(NTL, NTC, 1)]
    xi = fi = ti = 0
    for gi, (t0, ntl, s) in enumerate(groups):
        NTOK = ntl * 128
        tok0 = t0 * 128
        ct, CT = cst[gi % 2]
        st, ST = snt[gi % 2]
        kb.dma("sp", ct[:, 0:NTOK], cosT[:, tok0:tok0 + NTOK], writes=[CT])
        kb.dma("sp", st[:, 0:NTOK], sinT[:, tok0:tok0 + NTOK], writes=[ST])
        for j in range(ntl):
            x_t, XB = xt[xi % 2]
            xi += 1
            rows = slice(tok0 + j * 128, tok0 + (j + 1) * 128)
            kb.dma("sp", x_t[:], x_src[rows, :], reads=[XS], writes=[XB])
            kb.op("act", lambda e, x_t=x_t: e.activation(out=tmp[:], in_=x_t[:], func=AF.Square, accum_out=sm[:, 0:1]),
                  reads=[XB], writes=[TMP, SM])
            rstd_ops(kb, sm, SM)
            kb.op("dve", lambda e, x_t=x_t, s=s: e.scalar_tensor_tensor(out=tmp[:], in0=x_t[:], scalar=sm[:, 1:2], in1=g1s[:, s, 1, :],
                                                                         op0=ALU.mult, op1=ALU.mult), reads=[XB, SM, G1S], writes=[TMP])
            kb.op("pool", lambda e, s=s: e.tensor_tensor(out=hx[:], in0=tmp[:], in1=g1s[:, s, 0, :], op=ALU.add),
                  reads=[TMP, G1S], writes=[HX])
            tb, TB = B[0]
            def tr(e, tb=tb):
                for k in range(8):
                    ins = e.transpose(tb.bitcast(BF16)[:, k * 128:(k + 1) * 128], hx[:, k * 128:(k + 1) * 128], ident[:])
                return ins
            kb.op("pe", tr, reads=[HX, ID], writes=[TB])
            kb.op("act", lambda e, tb=tb, j=j: e.copy(out=hxT[:, :, j * 128:(j + 1) * 128],
                                                      in_=tb.bitcast(BF16)[:, 0:1024].rearrange("p (k t) -> p k t", k=8)),
                  reads=[TB], writes=[HXT])
        for bi, (c0, ridx) in enumerate(fm_blocks):
            pa, PA = B[1 + bi % 2]
            def mma(e, pa=pa, c0=c0, NTOK=NTOK):
                for k in range(8):
                    ins = e.matmul(pa[:, 0:NTOK], lhsT=win[:, k, c0:c0 + 128], rhs=hxT[:, k, 0:NTOK], start=(k == 0), stop=(k == 7))
                return ins
            kb.op("pe", mma, reads=[WIN, HXT], writes=[PA])
            fo, FO = fmo[fi % 2]
            fi += 1
            if ridx is None:
                kb.op("act", lambda e, pa=pa, fo=fo, NTOK=NTOK: e.copy(out=fo[:, 0:NTOK], in_=pa[:, 0:NTOK]), reads=[PA], writes=[FO])
            else:
                pb, PB = B[3 + bi % 2]
                def mmb(e, pb=pb, ridx=ridx, NTOK=NTOK):
                    for k in range(8):
                        ins = e.matmul(pb[:, 0:NTOK], lhsT=wpm[:, k, ridx * 128:(ridx + 1) * 128], rhs=hxT[:, k, 0:NTOK],
                                       start=(k == 0), stop=(k == 7))
                    return ins
                kb.op("pe", mmb, reads=[WPM, HXT], writes=[PB])
                kb.op("dve", lambda e, pa=pa, ct=ct, NTOK=NTOK: e.tensor_tensor(out=r1[:, 0:NTOK], in0=pa[:, 0:NTOK], in1=ct[:, 0:NTOK], op=ALU.mult),
                      reads=[PA, CT], writes=[R1])
                kb.op("dve", lambda e, pb=pb, st=st, NTOK=NTOK: e.tensor_tensor(out=r2[:, 0:NTOK], in0=pb[:, 0:NTOK], in1=st[:, 0:NTOK], op=ALU.mult),
                      reads=[PB, ST], writes=[R2])
                kb.op("pool", lambda e, fo=fo, NTOK=NTOK: e.tensor_tensor(out=fo[:, 0:NTOK], in0=r1[:, 0:NTOK], in1=r2[:, 0:NTOK], op=ALU.add),
                      reads=[R1, R2], writes=[FO])
            kb.dma("sp", fm_out[bi * 128:(bi + 1) * 128, tok0:tok0 + NTOK], fo[:, 0:NTOK], reads=[FO], writes=[FMO])
        for j in range(ntl):
            for (c0, ncol, nh, dv, oc) in tm_groups:
                pt, PT = B[5 + ti % 2]
                to, TO = tmo[ti % 2]
                ti += 1
                def mmt(e, pt=pt, c0=c0, ncol=ncol, j=j):
                    for k in range(8):
                        ins = e.matmul(pt[:, 0:ncol], lhsT=hxT[:, k, j * 128:(j + 1) * 128], rhs=win[:, k, c0:c0 + ncol],
                                       start=(k == 0), stop=(k == 7))
                    return ins
                kb.op("pe", mmt, reads=[HXT, WIN], writes=[PT])
                wdt = nh * (dv + 1)
                kb.op("act", lambda e, pt=pt, to=to, ncol=ncol, nh=nh, dv=dv, wdt=wdt: e.copy(
                    out=to[:, 0:wdt].rearrange("p (h c) -> p h c", c=dv + 1)[:, :, 0:dv],
                    in_=pt[:, 0:ncol].rearrange("p (h c) -> p h c", c=dv)), reads=[PT], writes=[TO])
                rows = slice(tok0 + j * 128, tok0 + (j + 1) * 128)
                kb.dma("sp", tm_out[rows, oc:oc + wdt], to[:, 0:wdt], reads=[TO], writes=[TMO])


NA_NBMAX = 12


def _na_blocklist(qt):
    if qt == 0:
        return 0, 4, [("tail", 0), ("tail", 1)]
    if qt == 1:
        return 0, 4, [("tail", 1)]
    if qt == NTL - 2:
        return TQ - 512, 4, [("head", 0)]
    if qt == NTL - 1:
        return TQ - 512, 4, [("head", 0), ("head", 1)]
    return qt * 128 - 256, 5, []


def emit_attn_even_f(kb, fmT, FMT, tmv, TMV, dk_recv, DKR, dv_recv, DVR, nbk_recv, NBKR, nbv_recv, NBVR,
                     nbias, lam_in, subg_in, lami_in, ao, AO):
    B = kb.banks
    sc = 64 ** -0.5
    lm, LM = kb.sb("lm", [128, 4, 64], F32)
    lms, LMS = kb.sb("lms", [128, 16], F32)
    subg, SUBG = kb.sb("subg_sb", [128, 128], F32)
    kb.dma("sp", lm[:].rearrange("p a b -> p (a b)"), lam_in.rearrange("a b -> (a b)").unsqueeze(0).to_broadcast([128, 256]), writes=[LM])
    kb.dma("sp", subg[:], subg_in.unsqueeze(0).to_broadcast([128, 128]), writes=[SUBG])
    kb.dma("sp", lms[:, 8:10], lami_in, writes=[LMS])
    kb.op("dve", lambda e: e.tensor_tensor(out=lm[:, 0, :], in0=lm[:, 0, :], in1=lm[:, 1, :], op=ALU.mult), reads=[LM], writes=[LM])
    kb.op("dve", lambda e: e.tensor_tensor(out=lm[:, 2, :], in0=lm[:, 2, :], in1=lm[:, 3, :], op=ALU.mult), reads=[LM], writes=[LM])
    kb.op("dve", lambda e: e.tensor_reduce(out=lms[:, 0:1], in_=lm[:, 0, :], axis=AX.X, op=ALU.add), reads=[LM], writes=[LMS])
    kb.op("dve", lambda e: e.tensor_reduce(out=lms[:, 1:2], in_=lm[:, 2, :], axis=AX.X, op=ALU.add), reads=[LM], writes=[LMS])
    kb.op("act", lambda e: e.activation(out=lms[:, 2:4], in_=lms[:, 0:2], func=AF.Exp), reads=[LMS], writes=[LMS])
    kb.op("dve", lambda e: e.tensor_tensor(out=lms[:, 4:5], in0=lms[:, 2:3], in1=lms[:, 3:4], op=ALU.subtract), reads=[LMS], writes=[LMS])
    kb.op("dve", lambda e: e.tensor_tensor(out=lms[:, 5:6], in0=lms[:, 4:5], in1=lms[:, 8:9], op=ALU.add), reads=[LMS], writes=[LMS])
    kb.op("dve", lambda e: e.tensor_scalar(out=lms[:, 6:7], in0=lms[:, 5:6], scalar1=-1.0, scalar2=None, op0=ALU.mult), reads=[LMS], writes=[LMS])
    kb.op("dve", lambda e: e.tensor_scalar(out=subg[:], in0=subg[:], scalar1=lms[:, 9:10], scalar2=None, op0=ALU.mult), reads=[SUBG, LMS], writes=[SUBG])

    naqT = fmT[0:512, :]
    nakT = fmT[512:1024, :]
    kcT, KCT = kb.sb("na_kcT", [128, 4, CTX], BF16)
    vca, VCA = kb.sb("na_vca", [128, 2, 8, 65], BF16)
    kb.dma("sp", kcT[:], nakT[:, TQ:TALL].rearrange("(a p) n -> p a n", p=128), reads=[FMT], writes=[KCT])
    kb.dma("sp", vca[:].rearrange("p b h c -> p b (h c)"), tmv[TQ:TALL, 0:520].rearrange("(b p) c -> p b c", p=128), reads=[TMV], writes=[VCA])
    kts = [kb.sb(f"na_kt{i}", [128, 4, NA_NBMAX * 128], BF16) for i in range(2)]
    vts = [kb.sb(f"na_vt{i}", [128, NA_NBMAX, 8, 65], BF16) for i in range(2)]
    qts = [kb.sb(f"na_qt{i}", [128, 4, 128], BF16) for i in range(2)]
    bts = [kb.sb(f"na_bt{i}", [128, NA_NBMAX * 128], F32) for i in range(2)]
    sts = [kb.sb(f"na_st{i}", [128, NA_NBMAX * 128], F32) for i in range(2)]
    pts = [kb.sb(f"na_pt{i}", [128, (NA_NBMAX + 2) * 128], BF16) for i in range(2)]
    aot = [kb.sb(f"na_ao{i}", [128, 512], BF16) for i in range(2)]
    rc, RC = kb.sb("na_rc", [128, 8], F32)
    hi = 0
    bk = 0
    for qt in range(NT):
        isctx = qt >= NTL
        q_t, QB = qts[qt % 2]
        kb.dma("sp", q_t[:], naqT[:, qt * 128:(qt + 1) * 128].rearrange("(a p) n -> p a n", p=128), reads=[FMT], writes=[QB])
        nb = 0
        if not isctx:
            k_t, KB_ = kts[qt % 2]
            v_t, VB = vts[qt % 2]
            o0, onb, cands = _na_blocklist(qt)
            kb.dma("sp", k_t[:, :, 0:onb * 128], nakT[:, o0:o0 + onb * 128].rearrange("(a p) n -> p a n", p=128), reads=[FMT], writes=[KB_])
            kb.dma("sp", v_t[:, 0:onb].rearrange("p b h c -> p b (h c)"), tmv[o0:o0 + onb * 128, 0:520].rearrange("(b p) c -> p b c", p=128),
                   reads=[TMV], writes=[VB])
            nb = onb
            ncb = len(cands)
            if ncb:
                which = cands[0][0]
                cb0 = cands[0][1]
                col0 = (0 if which == "tail" else 256) + cb0 * 128
                for r in range(4):
                    kb.dma("sp", k_t[:, :, nb * 128:(nb + ncb) * 128],
                           nbk_recv[r * 512:(r + 1) * 512, col0:col0 + ncb * 128].rearrange("(a p) n -> p a n", p=128), reads=[NBKR], writes=[KB_])
                    kb.dma("sp", v_t[:, nb:nb + ncb].rearrange("p b h c -> p b (h c)"),
                           nbv_recv[r * 512 + col0:r * 512 + col0 + ncb * 128, :].rearrange("(b p) c -> p b c", p=128), reads=[NBVR], writes=[VB])
                    nb += ncb
            slot = 0 if qt == 0 else 1 if qt == 1 else 3 if qt == NTL - 2 else 4 if qt == NTL - 1 else 2
        a_t, AB = aot[qt % 2]
        for h in range(8):
            a, off = h // 2, (h % 2) * 64
            st_, STB = sts[hi % 2]
            pt_, PTB = pts[hi % 2]
            acc, ACC = B[4 + (h // 4)]
            hi += 1
            if not isctx:
                b_t, BB = bts[hi % 2]
                kb.dma("sp", b_t[:, 0:nb * 128], nbias[slot, h, :, 0:nb * 128], writes=[BB])
                for c0 in range(0, nb, 4):
                    cn = min(4, nb - c0)
                    ps, PS = B[bk % 4]
                    bk += 1
                    def mms(e, ps=ps, k_t=k_t, q_t=q_t, a=a, off=off, c0=c0, cn=cn):
                        for i in range(cn):
                            ins = e.matmul(ps[:, i * 128:(i + 1) * 128], lhsT=k_t[off:off + 64, a, (c0 + i) * 128:(c0 + i + 1) * 128],
                                           rhs=q_t[off:off + 64, a, :], start=True, stop=True)
                        return ins
                    kb.op("pe", mms, reads=[KB_, QB], writes=[PS])
                    kb.op("dve", lambda e, ps=ps, st_=st_, b_t=b_t, c0=c0, cn=cn: e.scalar_tensor_tensor(
                        out=st_[:, c0 * 128:(c0 + cn) * 128], in0=ps[:, 0:cn * 128], scalar=sc, in1=b_t[:, c0 * 128:(c0 + cn) * 128],
                        op0=ALU.mult, op1=ALU.add), reads=[PS, BB], writes=[STB])
                kb.op("act", lambda e, st_=st_, pt_=pt_, nb=nb: e.activation(out=pt_[:, 0:nb * 128], in_=st_[:, 0:nb * 128], func=AF.Exp),
                      reads=[STB], writes=[PTB])
            ps, PS = B[bk % 4]
            bk += 1
            def mmc(e, ps=ps, q_t=q_t, a=a, off=off):
                for cb in range(2):
                    ins = e.matmul(ps[:, cb * 128:(cb + 1) * 128], lhsT=kcT[off:off + 64, a, cb * 128:(cb + 1) * 128],
                                   rhs=q_t[off:off + 64, a, :], start=True, stop=True)
                return ins
            kb.op("pe", mmc, reads=[QB, KCT], writes=[PS])
            kb.op("act", lambda e, ps=ps, pt_=pt_, nb=nb: e.activation(out=pt_[:, nb * 128:(nb + 2) * 128], in_=ps[:, 0:256], func=AF.Exp, scale=sc),
                  reads=[PS], writes=[PTB])
            if not isctx:
                def mmv(e, acc=acc, pt_=pt_, v_t=v_t, h=h, nb=nb):
                    o = acc[:, (h % 4) * 65:(h % 4) * 65 + 65]
                    for blk in range(nb):
                        e.matmul(o, lhsT=pt_[:, blk * 128:(blk + 1) * 128], rhs=v_t[:, blk, h, :], start=(blk == 0), stop=False)
                    for cb in range(2):
                        ins = e.matmul(o, lhsT=pt_[:, (nb + cb) * 128:(nb + cb + 1) * 128], rhs=vca[:, cb, h, :], start=False, stop=(cb == 1))
                    return ins
                kb.op("pe", mmv, reads=[PTB, VB, VCA], writes=[ACC])
            else:
                def mmv(e, acc=acc, pt_=pt_, h=h):
                    o = acc[:, (h % 4) * 65:(h % 4) * 65 + 65]
                    for cb in range(2):
                        ins = e.matmul(o, lhsT=pt_[:, cb * 128:(cb + 1) * 128], rhs=vca[:, cb, h, :], start=(cb == 0), stop=(cb == 1))
                    return ins
                kb.op("pe", mmv, reads=[PTB, VCA], writes=[ACC])
            if h % 4 == 3:
                g4 = h // 4
                av = acc[:, 0:260].rearrange("p (h c) -> p h c", c=65)
                kb.op("dve", lambda e, av=av, g4=g4: e.reciprocal(out=rc[:, g4 * 4:g4 * 4 + 4], in_=av[:, :, 64]), reads=[ACC], writes=[RC])
                kb.op("dve", lambda e, av=av, g4=g4, a_t=a_t: e.tensor_tensor(
                    out=a_t[:, g4 * 256:(g4 + 1) * 256].rearrange("p (h d) -> p h d", d=64), in0=av[:, :, 0:64],
                    in1=rc[:, g4 * 4:g4 * 4 + 4].unsqueeze(2).to_broadcast([128, 4, 64]), op=ALU.mult), reads=[ACC, RC], writes=[AB])
        kb.dma("sp", ao[qt * 128:(qt + 1) * 128, 0:512], a_t[:], reads=[AB], writes=[AO])

    NBLK = (CTX + SEQ) // 128
    dk, DK = kb.sb("d_k", [128, CTX + SEQ], BF16)
    dva, DVA = kb.sb("d_va", [128, NBLK, 129], BF16)
    dq, DQ = kb.sb("d_q", [128, TALL], BF16)
    dpt = [kb.sb(f"d_pt{i}", [128, 256], BF16) for i in range(3)]
    o0_, O0 = kb.sb("d_o0", [128, 128], F32)
    o1, O1 = kb.sb("d_o1", [128, 128], F32)
    osq, OSQ = kb.sb("d_osq", [128, 128], F32)
    dsm, DSM = kb.sb("d_sm", [128, 8], F32)
    dob = [kb.sb(f"d_ob{i}", [128, 128], BF16) for i in range(2)]
    si = 0
    oi = 0
    for h in range(4):
        kb.dma("sp", dk[:, 0:CTX], fmT[1536 + h * 128:1536 + (h + 1) * 128, TQ:TALL], reads=[FMT], writes=[DK])
        for r in range(4):
            for k in range(4):
                kb.dma("sp", dk[:, CTX + r * TQ + k * 1024:CTX + r * TQ + (k + 1) * 1024],
                       dk_recv[k][0][r * 512 + h * 128:r * 512 + (h + 1) * 128, :], reads=[dk_recv[k][1]], writes=[DK])
        kb.dma("sp", dva[:, 0:2, :], tmv[TQ:TALL, 520 + h * 129:520 + (h + 1) * 129].rearrange("(b p) d -> p b d", p=128), reads=[TMV], writes=[DVA])
        for r in range(4):
            for k in range(8):
                b0 = 2 + (r * TQ + k * 512) // 128
                kb.dma("sp", dva[:, b0:b0 + 4, :], dv_recv[k][0][r * 512:(r + 1) * 512, h * 129:(h + 1) * 129].rearrange("(b p) d -> p b d", p=128),
                       reads=[dv_recv[k][1]], writes=[DVA])
        kb.dma("sp", dq[:], fmT[1024 + h * 128:1024 + (h + 1) * 128, :], reads=[FMT], writes=[DQ])
        qgroups = [(g * 256, 256, NBLK) for g in range(TQ // 256)] + [(TQ, CTX, CTX // 128)]
        for (q0, nq, nblk) in qgroups:
            nqs = nq // 128
            for blk in range(nblk):
                for m in range(2):
                    ps, PS = B[si % 3]
                    pt_, PTB = dpt[si % 3]
                    si += 1
                    kb.op("pe", lambda e, ps=ps, m=m, blk=blk, q0=q0, nq=nq: e.matmul(
                        ps[:, 0:nq], lhsT=dk[m * 64:(m + 1) * 64, blk * 128:(blk + 1) * 128], rhs=dq[m * 64:(m + 1) * 64, q0:q0 + nq],
                        start=True, stop=True), reads=[DK, DQ], writes=[PS])
                    kb.op("act", lambda e, ps=ps, pt_=pt_, nq=nq: e.activation(out=pt_[:, 0:nq], in_=ps[:, 0:nq], func=AF.Exp, scale=sc),
                          reads=[PS], writes=[PTB])
                    def mmv(e, pt_=pt_, m=m, blk=blk, nqs=nqs, nblk=nblk):
                        for qs in range(nqs):
                            acc = B[3 + qs * 2 + m][0]
                            ins = e.matmul(acc[:, 0:129], lhsT=pt_[:, qs * 128:(qs + 1) * 128], rhs=dva[:, blk, :],
                                           start=(blk == 0), stop=(blk == nblk - 1))
                        return ins
                    kb.op("pe", mmv, reads=[PTB, DVA], writes=[B[3 + qs * 2 + m][1] for qs in range(nqs)])
            for qs in range(nqs):
                (a0, A0), (a1, A1) = [(B[3 + qs * 2 + m][0][:, 0:129], B[3 + qs * 2 + m][1]) for m in range(2)]
                kb.op("dve", lambda e, a0=a0: e.reciprocal(out=dsm[:, 0:1], in_=a0[:, 128:129]), reads=[A0], writes=[DSM])
                kb.op("dve", lambda e, a1=a1: e.reciprocal(out=dsm[:, 1:2], in_=a1[:, 128:129]), reads=[A1], writes=[DSM])
                kb.op("dve", lambda e: e.tensor_tensor(out=dsm[:, 1:2], in0=dsm[:, 1:2], in1=lms[:, 6:7], op=ALU.mult), reads=[DSM, LMS], writes=[DSM])
                kb.op("dve", lambda e, a0=a0: e.tensor_scalar(out=o0_[:], in0=a0[:, 0:128], scalar1=dsm[:, 0:1], scalar2=None, op0=ALU.mult),
                      reads=[A0, DSM], writes=[O0])
                kb.op("dve", lambda e, a1=a1: e.scalar_tensor_tensor(out=o1[:], in0=a1[:, 0:128], scalar=dsm[:, 1:2], in1=o0_[:],
                                                                   op0=ALU.mult, op1=ALU.add), reads=[A1, DSM, O0], writes=[O1])
                kb.op("act", lambda e: e.activation(out=osq[:], in_=o1[:], func=AF.Square, accum_out=dsm[:, 2:3]), reads=[O1], writes=[OSQ, DSM])
                kb.op("dve", lambda e: e.tensor_scalar(out=dsm[:, 3:4], in0=dsm[:, 2:3], scalar1=1.0 / 128, scalar2=EPS, op0=ALU.mult, op1=ALU.add),
                      reads=[DSM], writes=[DSM])
                kb.op("act", lambda e: e.activation(out=dsm[:, 4:5], in_=dsm[:, 3:4], func=AF.Sqrt), reads=[DSM], writes=[DSM])
                kb.op("dve", lambda e: e.reciprocal(out=dsm[:, 5:6], in_=dsm[:, 4:5]), reads=[DSM], writes=[DSM])
                ob, OB = dob[oi % 2]
                oi += 1
                kb.op("dve", lambda e, ob=ob: e.scalar_tensor_tensor(out=ob[:], in0=o1[:], scalar=dsm[:, 5:6], in1=subg[:], op0=ALU.mult, op1=ALU.mult),
                      reads=[O1, DSM, SUBG], writes=[OB])
                r0 = q0 + qs * 128
                kb.dma("sp", ao[r0:r0 + 128, 512 + h * 128:512 + (h + 1) * 128], ob[:], reads=[OB], writes=[AO])


def emit_attn_odd_f(kb, fmT, FMT, tmv, TMV, sbk_recv, SBKR, sbv_recv, SBVR, sbias, sink_in, ao, AO):
    B = kb.banks
    sc = 64 ** -0.5
    snk, SNK = kb.sb("snk", [128, 16], F32)
    kb.dma("sp", snk[:], sink_in.unsqueeze(0).to_broadcast([128, 16]), writes=[SNK])
    kb.op("act", lambda e: e.activation(out=snk[:], in_=snk[:], func=AF.Exp), reads=[SNK], writes=[SNK])
    kT, KT = kb.sb("s_kT", [64, 4, TQ], BF16)
    kb.dma("sp", kT[:], fmT[1024:1280, 0:TQ].rearrange("(n d) t -> d n t", d=64), reads=[FMT], writes=[KT])
    kcT, KCT = kb.sb("s_kcT", [64, 4, CTX], BF16)
    kb.dma("sp", kcT[:], fmT[1024:1280, TQ:TALL].rearrange("(n d) t -> d n t", d=64), reads=[FMT], writes=[KCT])
    ck, CK = kb.sb("s_ck", [64, 4, 4, 256], BF16)
    for r in range(4):
        kb.dma("sp", ck[:, r], sbk_recv[r * 256:(r + 1) * 256, :].rearrange("(n d) t -> d n t", d=64), reads=[SBKR], writes=[CK])
    va, VA = kb.sb("s_va", [128, NTL, 4, 65], BF16)
    kb.dma("sp", va[:].rearrange("p b h c -> p b (h c)"), tmv[0:TQ, 0:260].rearrange("(b p) c -> p b c", p=128), reads=[TMV], writes=[VA])
    vca, VCA = kb.sb("s_vca", [128, 2, 4, 65], BF16)
    kb.dma("sp", vca[:].rearrange("p b h c -> p b (h c)"), tmv[TQ:TALL, 0:260].rearrange("(b p) c -> p b c", p=128), reads=[TMV], writes=[VCA])
    cv, CV = kb.sb("s_cv", [128, 8, 4, 65], BF16)
    kb.dma("sp", cv[:].rearrange("p b h c -> p b (h c)"), sbv_recv.rearrange("(b p) c -> p b c", p=128), reads=[SBVR], writes=[CV])
    sb_, SBB = kb.sb("s_bias", [128, 3, 768], F32)
    kb.dma("sp", sb_[:], sbias.rearrange("s p n -> p s n"), writes=[SBB])
    qts = [kb.sb(f"s_q{i}", [64, 16, 128], BF16) for i in range(2)]
    sts = [kb.sb(f"s_st{i}", [128, 512], F32) for i in range(2)]
    pts = [kb.sb(f"s_pt{i}", [128, 8, 512], BF16) for i in range(2)]
    aot = [kb.sb(f"s_ao{i}", [128, D], BF16) for i in range(2)]
    den, DEN = kb.sb("s_den", [128, 8], F32)
    si = 0
    gi = 0
    for qt in range(NT):
        isctx = qt >= NTL
        q_t, QB = qts[qt % 2]
        kb.dma("sp", q_t[:], fmT[0:1024, qt * 128:(qt + 1) * 128].rearrange("(h d) t -> d h t", d=64), reads=[FMT], writes=[QB])
        a_t, AB = aot[qt % 2]
        if isctx:
            nbl = []
            slot = 1
        elif qt == 0:
            nbl = [("own", 0), ("own", 1)] + [("cand", r, 0) for r in range(4)]
            slot = 0
        elif qt == NTL - 1:
            nbl = [("own", NTL - 2), ("own", NTL - 1)] + [("cand", r, 1) for r in range(4)]
            slot = 2
        else:
            nbl = [("own", qt - 1), ("own", qt), ("own", qt + 1)]
            slot = 1
        nnb = len(nbl)
        for n in range(4):
            pt_, PTB = pts[gi % 2]
            acc, ACC = B[4 + gi % 2]
            gi += 1
            for bi, bl in enumerate(nbl + [("ctx", 0), ("ctx", 1)]):
                ps, PS = B[si % 3]
                st_, STB = sts[si % 2]
                si += 1
                if bl[0] == "own":
                    lhsT = kT[:, n, bl[1] * 128:(bl[1] + 1) * 128]
                    rd = [KT, QB]
                elif bl[0] == "cand":
                    lhsT = ck[:, bl[1], n, bl[2] * 128:(bl[2] + 1) * 128]
                    rd = [CK, QB]
                else:
                    lhsT = kcT[:, n, bl[1] * 128:(bl[1] + 1) * 128]
                    rd = [KCT, QB]
                kb.op("pe", lambda e, ps=ps, lhsT=lhsT, n=n, q_t=q_t: e.matmul(ps[:], lhsT=lhsT, rhs=q_t[:, n * 4:(n + 1) * 4, :], start=True, stop=True),
                      reads=rd, writes=[PS])
                if bl[0] != "ctx":
                    kb.op("dve", lambda e, ps=ps, st_=st_, bi=bi, slot=slot: e.scalar_tensor_tensor(
                        out=st_[:].rearrange("p (g q) -> p g q", g=4), in0=ps[:].rearrange("p (g q) -> p g q", g=4), scalar=sc,
                        in1=sb_[:, slot, bi * 128:(bi + 1) * 128].unsqueeze(1).to_broadcast([128, 4, 128]), op0=ALU.mult, op1=ALU.add),
                        reads=[PS, SBB], writes=[STB])
                    kb.op("act", lambda e, st_=st_, pt_=pt_, bi=bi: e.activation(out=pt_[:, bi, :], in_=st_[:], func=AF.Exp), reads=[STB], writes=[PTB])
                else:
                    kb.op("act", lambda e, ps=ps, pt_=pt_, bi=bi: e.activation(out=pt_[:, bi, :], in_=ps[:], func=AF.Exp, scale=sc),
                          reads=[PS], writes=[PTB])
            allb = nbl + [("ctx", 0), ("ctx", 1)]
            def mmv(e, acc=acc, pt_=pt_, n=n, allb=allb):
                for g in range(4):
                    o = acc[:, g * 65:(g + 1) * 65]
                    for i, bl in enumerate(allb):
                        if bl[0] == "own":
                            rhs = va[:, bl[1], n, :]
                        elif bl[0] == "cand":
                            rhs = cv[:, bl[1] * 2 + bl[2], n, :]
                        else:
                            rhs = vca[:, bl[1], n, :]
                        ins = e.matmul(o, lhsT=pt_[:, i, g * 128:(g + 1) * 128], rhs=rhs, start=(i == 0), stop=(i == len(allb) - 1))
                return ins
            kb.op("pe", mmv, reads=[PTB, VA, VCA, CV], writes=[ACC])
            av = acc[:, 0:260].rearrange("p (g c) -> p g c", c=65)
            kb.op("dve", lambda e, av=av, n=n: e.tensor_tensor(out=den[:, 0:4], in0=av[:, :, 64], in1=snk[:, n * 4:(n + 1) * 4], op=ALU.add),
                  reads=[ACC, SNK], writes=[DEN])
            kb.op("dve", lambda e: e.reciprocal(out=den[:, 4:8], in_=den[:, 0:4]), reads=[DEN], writes=[DEN])
            kb.op("dve", lambda e, av=av, n=n, a_t=a_t: e.tensor_tensor(
                out=a_t[:, n * 256:(n + 1) * 256].rearrange("p (g d) -> p g d", d=64), in0=av[:, :, 0:64],
                in1=den[:, 4:8].unsqueeze(2).to_broadcast([128, 4, 64]), op=ALU.mult), reads=[ACC, DEN], writes=[AB])
        kb.dma("sp", ao[qt * 128:(qt + 1) * 128, :], a_t[:], reads=[AB], writes=[AO])


def build_fused(nlayers=4):
    kb = KB()
    kb.psum_banks()
    nc = kb.nc

    def scratch(name, shape, dt=BF16):
        return nc.dram_tensor(name, list(shape), dt).ap(), Buf(name)

    x_ext = kb.din("x", [TALL, D])
    csT = kb.din("csT", [128, 16])
    cosT = kb.din("cosT", [128, TALL])
    sinT = kb.din("sinT", [128, TALL])
    fg = kb.din("fg", [D])
    xn_out, XN = kb.dout("xn_out", [TALL, D])
    xbufs = [scratch(f"xs{i}", [TALL, D], F32) for i in range(2)]
    ao, AO = scratch("ao_scr", [TALL, D])
    fmT, FMT = scratch("fmT", [2048, TALL])
    tmv, TMV = scratch("tmv", [TALL, 1036])
    dk_send = [scratch(f"dk_send{k}", [512, 1024]) for k in range(4)]
    dk_recv = [scratch(f"dk_recv{k}", [2048, 1024]) for k in range(4)]
    dv_send = [scratch(f"dv_send{k}", [512, 516]) for k in range(8)]
    dv_recv = [scratch(f"dv_recv{k}", [2048, 516]) for k in range(8)]
    nbk_send, NBKS = scratch("nbk_send", [512, 512])
    nbk_recv, NBKR = scratch("nbk_recv", [2048, 512])
    nbv_send, NBVS = scratch("nbv_send", [512, 520])
    nbv_recv, NBVR = scratch("nbv_recv", [2048, 520])
    sbk_send, SBKS = scratch("sbk_send", [256, 256])
    sbk_recv, SBKR = scratch("sbk_recv", [1024, 256])
    sbv_send, SBVS = scratch("sbv_send", [256, 260])
    sbv_recv, SBVR = scratch("sbv_recv", [1024, 260])
    x_src, XS = x_ext, Buf("x_ext")
    lw = []
    for i in range(nlayers):
        d = {"w_out": kb.din(f"w_out{i}", [D, D]), "wq": kb.din(f"wq{i}", [D, 2048]),
             "uT": kb.din(f"uT{i}", [D, 16384]), "v": kb.din(f"v{i}", [16384, D])}
        d["conv"] = {"wout": scratch(f"woutb{i}", [2, 128, 4096]), "wq": scratch(f"wqb{i}", [4, 128, 4096]),
                     "uT": scratch(f"uTb{i}", [32, 128, 4096]), "v": scratch(f"vb{i}", [32, 128, 4096])}
        lw.append(d)

    def convert(i):
        d = lw[i]
        c = d["conv"]
        for hf in range(2):
            kb.dma("pool", c["wout"][0][hf].rearrange("p (k n) -> p k n", k=8),
                   d["w_out"][:, hf * 512:(hf + 1) * 512].rearrange("(k p) n -> p k n", p=128), writes=[c["wout"][1]])
        for g in range(4):
            kb.dma("pool", c["wq"][0][g].rearrange("p (k n) -> p k n", k=8),
                   d["wq"][:, g * 512:(g + 1) * 512].rearrange("(k p) n -> p k n", p=128), writes=[c["wq"][1]])
        for cg in range(32):
            kb.dma("pool", c["uT"][0][cg].rearrange("p (k n) -> p k n", k=8),
                   d["uT"][:, cg * 512:(cg + 1) * 512].rearrange("(k p) n -> p k n", p=128), writes=[c["uT"][1]])
            kb.dma("pool", c["v"][0][cg].rearrange("p (c d) -> p c d", c=4),
                   d["v"][cg * 512:(cg + 1) * 512, :].rearrange("(c p) d -> p c d", p=128), writes=[c["v"][1]])

    for i in range(nlayers):
        even = (i % 2 == 0)
        j = i // 2
        NIN = 3072 if even else 1536
        NROPE = 1024 if even else 1280
        w_mod = kb.din(f"w_mod{i}", [D, 6 * D])
        b_mod = kb.din(f"b_mod{i}", [6 * D])
        n1g = kb.din(f"n1g{i}", [D])
        n2g = kb.din(f"n2g{i}", [D])
        w_in = kb.din(f"w_in{i}", [D, NIN])
        w_perm = kb.din(f"w_perm{i}", [D, NROPE])
        w_out, wq, uT, v = lw[i]["w_out"], lw[i]["wq"], lw[i]["uT"], lw[i]["v"]
        keysT = kb.din(f"keysT{i}", [128, 16, 128])
        modrow, MRO = scratch(f"modrow{i}", [2, 6, D], F32)
        with kb.scope(f"L{i}a_"):
            if i == 0 and USE_CONV:
                convert(0)
            emit_pre_f(kb, even, x_src, XS, csT, w_mod, b_mod, n1g, w_in, w_perm, cosT, sinT, fmT, FMT, tmv, TMV, modrow, MRO)
        if even:
            nbias = kb.din(f"nbias{j}", [5, 8, 128, NA_NBMAX * 128])
            lam = kb.din(f"lam{j}", [4, 64])
            subg = kb.din(f"subg{j}", [128])
            lami = kb.din(f"lam_init{j}", [128, 2])
            for k in range(4):
                kb.dma("sp", dk_send[k][0], fmT[1536:2048, k * 1024:(k + 1) * 1024], reads=[FMT], writes=[dk_send[k][1]])
            for k in range(8):
                kb.dma("sp", dv_send[k][0], tmv[k * 512:(k + 1) * 512, 520:1036], reads=[TMV], writes=[dv_send[k][1]])
            kb.dma("sp", nbk_send[:, 0:256], fmT[512:1024, TQ - 256:TQ], reads=[FMT], writes=[NBKS])
            kb.dma("sp", nbk_send[:, 256:512], fmT[512:1024, 0:256], reads=[FMT], writes=[NBKS])
            kb.dma("sp", nbv_send[0:256, :], tmv[TQ - 256:TQ, 0:520], reads=[TMV], writes=[NBVS])
            kb.dma("sp", nbv_send[256:512, :], tmv[0:256, 0:520], reads=[TMV], writes=[NBVS])
            for k in range(4):
                kb.cc("AllGather", RG, dk_send[k][0], dk_recv[k][0], reads=[dk_send[k][1]], writes=[dk_recv[k][1]])
            for k in range(8):
                kb.cc("AllGather", RG, dv_send[k][0], dv_recv[k][0], reads=[dv_send[k][1]], writes=[dv_recv[k][1]])
            kb.cc("AllGather", RG, nbk_send, nbk_recv, reads=[NBKS], writes=[NBKR])
            kb.cc("AllGather", RG, nbv_send, nbv_recv, reads=[NBVS], writes=[NBVR])
            with kb.scope(f"L{i}b_"):
                emit_attn_even_f(kb, fmT, FMT, tmv, TMV, dk_recv, None, dv_recv, None, nbk_recv, NBKR, nbv_recv, NBVR,
                                 nbias, lam, subg, lami, ao, AO)
        else:
            sbias = kb.din(f"sbias{j}", [3, 128, 768])
            sink = kb.din(f"sink{j}", [16])
            kb.dma("sp", sbk_send[:, 0:128], fmT[1024:1280, TQ - 128:TQ], reads=[FMT], writes=[SBKS])
            kb.dma("sp", sbk_send[:, 128:256], fmT[1024:1280, 0:128], reads=[FMT], writes=[SBKS])
            kb.dma("sp", sbv_send[0:128, :], tmv[TQ - 128:TQ, 0:260], reads=[TMV], writes=[SBVS])
            kb.dma("sp", sbv_send[128:256, :], tmv[0:128, 0:260], reads=[TMV], writes=[SBVS])
            kb.cc("AllGather", RG, sbk_send, sbk_recv, reads=[SBKS], writes=[SBKR])
            kb.cc("AllGather", RG, sbv_send, sbv_recv, reads=[SBVS], writes=[SBVR])
            with kb.scope(f"L{i}b_"):
                emit_attn_odd_f(kb, fmT, FMT, tmv, TMV, sbk_recv, SBKR, sbv_recv, SBVR, sbias, sink, ao, AO)
        x_dst, XD = xbufs[i % 2]
        last = (i == nlayers - 1)
        with kb.scope(f"L{i}c_"):
            if not last and USE_CONV:
                convert(i + 1)
            emit_post(kb, NT, x_src, ao, AO, modrow, w_out, n2g, wq, keysT, uT, v, x_dst, XD,
                      final_g=fg if last else None, xn_out=xn_out if last else None, XN=XN if last else None,
                      tile_sets=[0] * NTL + [1] * NTC, conv=lw[i]["conv"] if USE_CONV else None)
        x_src, XS = x_dst, XD
    return kb.finish()


def _na_bias_f(rpb, qr):
    H = rpb.shape[0]
    R0 = qr * 64
    out = np.full((5, H, 128, NA_NBMAX, 128), NEG, np.float32)
    kk = np.arange(128)
    qq = np.arange(128)
    for slot, qt in enumerate((0, 1, 10, NTL - 2, NTL - 1)):
        r0 = R0 + 2 * qt
        if slot == 2:
            r0 = 100
        o0, onb, cands = _na_blocklist(qt)
        blocks = [((r0 - 4 + 2 * b) if slot == 2 else (R0 + o0 // 64 + 2 * b), None) for b in range(onb)]
        for r in range(4):
            for (which, cb) in cands:
                if which == "tail":
                    blocks.append((R0 - 4 + 2 * cb, r == qr - 1))
                else:
                    blocks.append((R0 + 64 + 2 * cb, r == qr + 1))
        qrow = r0 + qq // 64
        qc = qq % 64
        rs = np.clip(qrow - 4, 0, 256 - 8)
        cs = np.clip(qc - 8, 0, 64 - 16)
        for bi, (krow0, ok) in enumerate(blocks):
            if ok is False:
                continue
            kr = krow0 + kk // 64
            kc = kk % 64
            valid = ((kr[:, None] >= rs[None, :]) & (kr[:, None] < rs[None, :] + 8) &
                     (kc[:, None] >= cs[None, :]) & (kc[:, None] < cs[None, :] + 16) &
                     (kr[:, None] >= 0) & (kr[:, None] < 256))
            dr = np.clip(kr[:, None] - qrow[None, :] + 7, 0, 14)
            dc = np.clip(kc[:, None] - qc[None, :] + 15, 0, 30)
            vals = rpb[:, dr, dc]
            out[slot, :, :, bi, :] = np.where(valid[None], vals, NEG)
    return out.reshape(5, H, 128, NA_NBMAX * 128)


def _swa_bias_f(qr):
    out = np.full((3, 128, 6, 128), NEG, np.float32)
    k = np.arange(128)[:, None]
    q = np.arange(128)[None, :]
    band = lambda off: np.where(np.abs(off * 128 + k - q) <= 128, 0.0, NEG).astype(np.float32)
    out[0, :, 0] = band(0)
    out[0, :, 1] = band(1)
    for r in range(4):
        if r == qr - 1:
            out[0, :, 2 + r] = band(-1)
    out[1, :, 0] = band(-1)
    out[1, :, 1] = band(0)
    out[1, :, 2] = band(1)
    out[2, :, 0] = band(-1)
    out[2, :, 1] = band(0)
    for r in range(4):
        if r == qr + 1:
            out[2, :, 2 + r] = band(1)
    return out.reshape(3, 128, 768)


_FUSED = {}


def kernel(x, c, ctx, c_ctx, w_mod, b_mod, norm1_g, norm2_g, w_in_even, w_out_even, na_rpb, diff_lambda,
           diff_subln_g, w_in_odd, w_out_odd, swa_sink, peer_wq, peer_keys, peer_u, peer_v, final_g, _nlayers=4):
    f32 = np.float32
    x = np.asarray(x, f32)
    ctx = np.asarray(ctx, f32)
    if _nlayers not in _FUSED:
        _FUSED[_nlayers] = build_fused(_nlayers)
    nc = _FUSED[_nlayers]
    shared = {"c_ident": np.eye(128, dtype=f32), "fg": np.asarray(final_g, f32)}
    for i in range(_nlayers):
        even = (i % 2 == 0)
        j = i // 2
        w_in = np.asarray(w_in_even[j] if even else w_in_odd[j], f32)
        rc = w_in[:, 1536:2560] if even else w_in[:, 0:1280]
        shared[f"w_mod{i}"] = np.asarray(w_mod[i], f32)
        shared[f"b_mod{i}"] = np.asarray(b_mod[i], f32)
        shared[f"n1g{i}"] = np.asarray(norm1_g[i], f32)
        shared[f"n2g{i}"] = np.asarray(norm2_g[i], f32)
        shared[f"w_in{i}"] = w_in
        shared[f"w_perm{i}"] = np.ascontiguousarray(rc.reshape(D, -1, 64)[:, :, _PERM64].reshape(D, -1))
        shared[f"w_out{i}"] = np.asarray(w_out_even[j] if even else w_out_odd[j], f32)
        shared[f"wq{i}"] = np.asarray(peer_wq[i], f32)
        shared[f"keysT{i}"] = np.ascontiguousarray(np.asarray(peer_keys[i], f32).reshape(16, 128, 128).transpose(2, 0, 1))
        shared[f"uT{i}"] = np.ascontiguousarray(np.asarray(peer_u[i], f32).T)
        shared[f"v{i}"] = np.asarray(peer_v[i], f32)
        if even:
            shared[f"lam{j}"] = np.asarray(diff_lambda[j], f32)
            shared[f"subg{j}"] = np.asarray(diff_subln_g[j], f32)
            li = 0.8 - 0.6 * math.exp(-0.3 * i)
            shared[f"lam_init{j}"] = np.tile(np.array([[li, 1.0 - li]], f32), (128, 1))
        else:
            shared[f"sink{j}"] = np.asarray(swa_sink[j], f32)
    ims = []
    for core in range(NCORES):
        b, qr = divmod(core, 4)
        im = dict(shared)
        im["x"] = np.concatenate([x[b, qr * TQ:(qr + 1) * TQ], ctx[b]], axis=0)
        cs = np.stack([np.asarray(c, f32)[b], np.asarray(c_ctx, f32)], 0)
        im["csT"] = np.ascontiguousarray(cs.reshape(2, 8, 128).transpose(2, 0, 1).reshape(128, 16))
        im["cosT"], im["sinT"] = _rope_tables_T(qr * TQ)
        for i in range(_nlayers):
            j = i // 2
            if i % 2 == 0:
                im[f"nbias{j}"] = _na_bias_f(np.asarray(na_rpb[j], f32), qr)
            else:
                im[f"sbias{j}"] = _swa_bias_f(qr)
        ims.append(im)
    res = run_bass_kernel_spmd(nc, ims, core_ids=list(range(NCORES))).results
    out = np.zeros((2, SEQ, D), f32)
    for core in range(NCORES):
        b, qr = divmod(core, 4)
        out[b, qr * TQ:(qr + 1) * TQ] = np.asarray(res[core]["xn_out"])[:TQ]
    return out
```

```python
from contextlib import ExitStack
import math
import numpy as np
import ml_dtypes
import concourse.bass as bass
import concourse.mybir as mybir
from concourse.bass_utils import run_bass_kernel_spmd

F32 = mybir.dt.float32
BF16 = mybir.dt.bfloat16
AF = mybir.ActivationFunctionType
ALU = mybir.AluOpType
AX = mybir.AxisListType
NPBF = ml_dtypes.bfloat16

ENGS = ("pe", "act", "dve", "pool", "sp")

D = 1024
NCORES = 8
SEQ = 16384
CTX = 256
TQ = SEQ // 4
NTL = TQ // 128
NTC = CTX // 128
NT = NTL + NTC
TALL = TQ + CTX
EPS = 1e-6
NEG = -1.0e30
PEER_MARGIN = 1e-4
import os
USE_CONV = os.environ.get('K_CONV', '1') == '1'


class Buf:
    __slots__ = ("name", "writers", "readers", "excl")

    def __init__(self, name="", excl=False):
        self.name = name
        self.writers = {}
        self.readers = {}
        self.excl = excl


class Op:
    __slots__ = ("id", "eng", "fn", "dma", "cc", "deps", "seq", "waits", "signal", "sigidx", "slot", "target")


class Prog:
    RING = {"sp": 14, "act": 4, "pool": 8}

    def __init__(self, nc):
        self.nc = nc
        self.ops = []
        self.seq = {e: 0 for e in ENGS}
        self.dmas = {q: [] for q in self.RING}
        self.ccs = []

    def add(self, eng, fn, reads=(), writes=(), dma=False, cc=False):
        dma = dma or cc
        op = Op()
        op.cc = cc
        op.id = len(self.ops)
        op.eng = eng
        op.fn = fn
        op.dma = dma
        op.signal = False
        op.sigidx = None
        op.slot = None
        op.target = None
        deps = set()
        if cc:
            op.slot = ("cc", len(self.ccs))
            op.target = 1
            self.ccs.append(op.id)
        elif dma:
            ring = self.RING[eng]
            lst = self.dmas[eng]
            n = len(lst)
            op.slot = (eng, n % ring)
            op.target = 16 * (n // ring + 1)
            if n >= ring:
                deps.add(lst[n - ring])
            lst.append(op.id)
        pkey = ("dma", op.slot) if dma else eng
        rd = [b for b in reads if not b.excl]
        wr = list(writes) + [b for b in reads if b.excl]
        for b in rd:
            for k, oid in b.writers.items():
                if k == eng and eng == "pe":
                    continue
                deps.add(oid)
        for b in wr:
            isread = b not in writes
            for k, oid in b.writers.items():
                if k == eng and not dma:
                    if isread and eng != "pe":
                        deps.add(oid)
                    continue
                if dma and isinstance(k, tuple):
                    continue
                deps.add(oid)
            for k, oid in b.readers.items():
                if k == eng and not dma:
                    continue
                deps.add(oid)
        for b in wr:
            if dma:
                b.writers = {k: v for k, v in b.writers.items() if isinstance(k, tuple)}
                b.writers[pkey] = op.id
            else:
                b.writers = {pkey: op.id}
            b.readers = {}
        for b in rd:
            if b in wr:
                continue
            b.readers[pkey] = op.id
        op.seq = self.seq[eng]
        self.seq[eng] += 1
        op.deps = deps
        self.ops.append(op)
        return op.id

    def barrier(self):
        start = getattr(self, "bar_start", 0)
        last = {}
        for op in self.ops[start:]:
            if op.dma:
                last[("dma", op.id)] = op.id
            else:
                last[op.eng] = op.id
        deps = set(last.values())
        for e in ENGS:
            oid = self.add(e, lambda eng: None)
            self.ops[oid].deps |= {d for d in deps if self.ops[d].dma or self.ops[d].eng != e}
        self.bar_start = len(self.ops)

    def emit(self):
        nc = self.nc
        ops = self.ops
        seen = {e: {} for e in ENGS}
        seen_dma = {e: {} for e in ENGS}
        for op in ops:
            best = {}
            waits = []
            for d in op.deps:
                p = ops[d]
                if p.dma:
                    if seen_dma[op.eng].get(p.slot, 0) >= p.target:
                        continue
                    seen_dma[op.eng][p.slot] = p.target
                    waits.append(d)
                else:
                    if p.eng not in best or ops[best[p.eng]].seq < p.seq:
                        best[p.eng] = d
            for e, d in best.items():
                p = ops[d]
                if seen[op.eng].get(e, -1) >= p.seq:
                    continue
                seen[op.eng][e] = p.seq
                p.signal = True
                waits.append(d)
            op.waits = waits
        cnt = {e: 0 for e in ENGS}
        for op in ops:
            if op.signal and not op.dma:
                cnt[op.eng] += 1
                op.sigidx = cnt[op.eng]
        with ExitStack() as es:
            sems = {e: es.enter_context(nc.semaphore("s_" + e)) for e in ENGS}
            rings = {}
            for q, n in self.RING.items():
                for i in range(n):
                    rings[(q, i)] = es.enter_context(nc.semaphore(f"r_{q}{i}"))
            for i in range(len(self.ccs)):
                rings[("cc", i)] = es.enter_context(nc.semaphore(f"cc{i}"))
            block = es.enter_context(nc.Block())
            per_eng = {e: [o for o in ops if o.eng == e] for e in ENGS}

            def run(engname):
                def body(eng):
                    for op in per_eng[engname]:
                        for d in op.waits:
                            p = ops[d]
                            if p.dma:
                                eng.wait_ge(rings[p.slot], p.target)
                            else:
                                eng.wait_ge(sems[p.eng], p.sigidx)
                        ins = op.fn(eng)
                        if ins is None:
                            continue
                        if op.cc:
                            ins.then_inc(rings[op.slot], 1)
                        elif op.dma:
                            ins.then_inc(rings[op.slot], 16)
                        elif op.signal:
                            ins.then_inc(sems[op.eng], 1)
                return body

            block.tensor(run("pe"))
            block.scalar(run("act"))
            block.vector(run("dve"))
            block.gpsimd(run("pool"))
            block.sync(run("sp"))


class KB:
    def __init__(self):
        self.nc = bass.Bass("TRN2", target_bir_lowering=False)
        self.P = Prog(self.nc)
        self.es = ExitStack()
        self.banks = []
        self.outs = []

    def din(self, name, shape, dt=F32):
        return self.nc.dram_tensor(name, list(shape), dt, kind="ExternalInput").ap()

    def dout(self, name, shape, dt=F32):
        b = Buf(name)
        self.outs.append(b)
        return self.nc.dram_tensor(name, list(shape), dt, kind="ExternalOutput").ap(), b

    def dscratch(self, name, shape, dt):
        return self.nc.dram_tensor(name, list(shape), dt, kind="Internal").ap(), Buf(name)

    pfx = ""

    def sb(self, name, shape, dt=F32):
        return self.es.enter_context(self.nc.sbuf_tensor(self.pfx + name, list(shape), dt)), Buf(name)

    def scope(self, pfx):
        kb = self

        class _S:
            def __enter__(s_):
                s_.saved = (kb.es, kb.pfx)
                kb.es = ExitStack()
                kb.pfx = pfx
                kb._ident = None

            def __exit__(s_, *a):
                kb.es.close()
                kb.es, kb.pfx = s_.saved
                kb.P.barrier()
                return False
        return _S()

    _ident = None
    _ident_d = None

    def ident(self):
        if self._ident is None:
            if self._ident_d is None:
                self._ident_d = self.din("c_ident", [128, 128], F32)
            f, IDF = self.sb("ident_f", [128, 128], F32)
            b, ID = self.sb("ident", [128, 128], BF16)
            self.dma("sp", f[:], self._ident_d, writes=[IDF])
            self.op("act", lambda e: e.copy(out=b[:], in_=f[:]), reads=[IDF], writes=[ID])
            self._ident = (f, IDF, b, ID)
        return self._ident

    def cc(self, kind, rg, src, dst, reads=(), writes=()):
        return self.P.add("pool", lambda e: e.collective_compute(kind, ALU.bypass, replica_groups=rg, ins=[src.opt()], outs=[dst.opt()]),
                          reads, writes, cc=True)

    def psum_banks(self):
        for i in range(8):
            t = self.es.enter_context(self.nc.psum_tensor(f"bank{i}", [128, 512], F32))
            self.banks.append((t, Buf(f"bank{i}", excl=True)))

    def op(self, eng, fn, reads=(), writes=()):
        return self.P.add(eng, fn, reads, writes)

    def dma(self, q, out, in_, reads=(), writes=()):
        return self.P.add(q, lambda e: e.dma_start(out=out, in_=in_), reads, writes, dma=True)

    def finish(self):
        self.P.add("sp", lambda e: None, reads=self.outs)
        self.P.emit()
        self.es.close()
        return self.nc


def rstd_ops(kb, sm, SM):
    kb.op("dve", lambda e: e.tensor_scalar(out=sm[:, 2:3], in0=sm[:, 0:1], scalar1=1.0 / D, scalar2=EPS,
                                           op0=ALU.mult, op1=ALU.add), reads=[SM], writes=[SM])
    kb.op("act", lambda e: e.activation(out=sm[:, 3:4], in_=sm[:, 2:3], func=AF.Sqrt), reads=[SM], writes=[SM])
    kb.op("dve", lambda e: e.reciprocal(out=sm[:, 1:2], in_=sm[:, 3:4]), reads=[SM], writes=[SM])


def emit_post(kb, ntiles, x_in, ao_dram, AO, modrow, w_out, norm2g, peer_wq, peer_keysT, peer_uT, peer_v,
              x_out, XOUT, final_g=None, xn_out=None, XN=None, tile_sets=None, conv=None):
    nc, P = kb.nc, kb.P
    B = kb.banks
    if tile_sets is None:
        tile_sets = [0] * ntiles
    ident_f, IDF, ident, ID = kb.ident()

    mod, MOD = kb.sb("modrows", [128, 4, D], F32)
    n2g, N2G = kb.sb("n2g_sb", [128, D], F32)
    kb.dma("sp", n2g[:], norm2g.unsqueeze(0).to_broadcast([128, D]), writes=[N2G])
    fg = None
    if final_g is not None:
        fg, FG = kb.sb("fg_sb", [128, D], F32)
        kb.dma("sp", fg[:], final_g.unsqueeze(0).to_broadcast([128, D]), writes=[FG])

    def load_mod(s):
        for j, src in enumerate((2, 4, 3, 5)):
            kb.dma("sp", mod[:, j, :], modrow[s, src:src + 1, :].to_broadcast([128, D]), writes=[MOD])
        kb.op("dve", lambda e: e.scalar_tensor_tensor(out=mod[:, 1, :], in0=mod[:, 1, :], scalar=1.0, in1=n2g[:],
                                                      op0=ALU.add, op1=ALU.mult), reads=[MOD, N2G], writes=[MOD])

    keys_b, KBF = kb.sb("keys_b", [128, 16, 128], BF16)

    NWR = 3
    wr = [kb.sb(f"wr{i}", [128, 8, 512], BF16) for i in range(NWR)]
    NVR = 2
    vr = [kb.sb(f"vr{i}", [128, 4, D], BF16) for i in range(NVR)]
    wr_i = [0]
    vr_i = [0]

    def load_w(src_cols, pre=None):
        t, b = wr[wr_i[0] % NWR]
        wr_i[0] += 1
        if pre is not None:
            kb.dma("sp", t[:].rearrange("p k n -> p (k n)"), pre[0], reads=[pre[1]], writes=[b])
        else:
            kb.dma("pool", t[:], src_cols.rearrange("(k p) n -> p k n", p=128), writes=[b])
        return t, b

    def load_v(src_rows, pre=None):
        t, b = vr[vr_i[0] % NVR]
        vr_i[0] += 1
        if pre is not None:
            kb.dma("sp", t[:].rearrange("p c d -> p (c d)"), pre[0], reads=[pre[1]], writes=[b])
        else:
            kb.dma("pool", t[:], src_rows.rearrange("(c p) d -> p c d", p=128), writes=[b])
        return t, b

    def pre_of(name, idx):
        return None if conv is None else (conv[name][0][idx], conv[name][1])

    xt, XT = kb.sb("xt", [128, 2, D], F32)
    tmp, TMP = kb.sb("tmp", [128, D], F32)
    ao, AOB = kb.sb("ao_sb", [128, D], BF16)
    aoT, AOT = kb.sb("aoT", [128, 8, 128], BF16)
    h2, H2 = kb.sb("h2", [128, D], BF16)
    h2T, H2T = kb.sb("h2T", [128, 8, 256], BF16)
    qT, QT = kb.sb("qT", [128, 16, 256], BF16)
    s_sb, SSB = kb.sb("s_sb", [128, 16, 128], F32)
    work, WORK = kb.sb("work", [128, 2048], F32)
    kb.dma("sp", work[:], peer_keysT.rearrange("p a n -> p (a n)"), writes=[WORK])
    kb.op("act", lambda e: e.copy(out=keys_b[:].rearrange("p a n -> p (a n)"), in_=work[:]), reads=[WORK], writes=[KBF])
    cand, CAND = kb.sb("cand", [128, 8, 16, 16], F32)
    top, TOP = kb.sb("top", [128, 16, 16], F32)
    ctop, CTOP = kb.sb("ctop", [128, 8, 16], F32)
    sm, SM = kb.sb("sm", [128, 64], F32)
    e16, E16 = kb.sb("e16", [128, 8, 16], F32)
    av, AV = kb.sb("a_vec", [128, 2, 8, 128], F32)
    bv, BV = kb.sb("b_vec", [128, 2, 8, 128], F32)
    diag, DG = kb.sb("diag", [128, 2, 8, 128], BF16)
    pps = [kb.sb(f"pp{i}", [128, 2, 8, 2, 128], F32) for i in range(2)]
    wps = [kb.sb(f"wp{i}", [128, 2, 8, 2, 128], BF16) for i in range(2)]
    gl = [kb.sb(f"gl{i}", [128, 4, 256], BF16) for i in range(2)]
    at = [kb.sb(f"at{i}", [128, 4, 256], BF16) for i in range(2)]
    xn, XNB = work, WORK

    def bf(bank):
        return bank.bitcast(BF16)

    cur_set = [None]
    npairs = (ntiles + 1) // 2
    for pr in range(npairs):
        tiles = [t for t in (2 * pr, 2 * pr + 1) if t < ntiles]
        nj = len(tiles)
        NTOK = 128 * nj
        if tile_sets[tiles[0]] != cur_set[0]:
            cur_set[0] = tile_sets[tiles[0]]
            load_mod(cur_set[0])
        wo = [load_w(w_out[:, hf * 512:(hf + 1) * 512], pre_of('wout', hf)) for hf in range(2)]
        for j, tt in enumerate(tiles):
            rows = slice(tt * 128, (tt + 1) * 128)
            kb.dma("sp", xt[:, j, :], x_in[rows, :], writes=[XT])
            kb.dma("sp", ao[:], ao_dram[rows, :], reads=[AO], writes=[AOB])
            tb, TB = B[0]
            def tr1(e, tb=tb):
                for k in range(8):
                    ins = e.transpose(bf(tb)[:, k * 128:(k + 1) * 128], ao[:, k * 128:(k + 1) * 128], ident[:])
                return ins
            kb.op("pe", tr1, reads=[AOB, ID], writes=[TB])
            kb.op("act", lambda e, tb=tb: e.copy(out=aoT[:].rearrange("p k t -> p (k t)"), in_=bf(tb)[:, 0:1024]),
                  reads=[TB], writes=[AOT])
            for hf in range(2):
                yb, YB = B[1 + hf]
                wt, WB = wo[hf]
                def mmy(e, yb=yb, wt=wt):
                    for k in range(8):
                        ins = e.matmul(yb[:], lhsT=aoT[:, k, :], rhs=wt[:, k, :], start=(k == 0), stop=(k == 7))
                    return ins
                kb.op("pe", mmy, reads=[AOT, WB], writes=[YB])
                kb.op("dve", lambda e, yb=yb, hf=hf: e.tensor_tensor(out=tmp[:, hf * 512:(hf + 1) * 512], in0=yb[:],
                                                                   in1=mod[:, 0, hf * 512:(hf + 1) * 512], op=ALU.mult),
                      reads=[YB, MOD], writes=[TMP])
            kb.op("pool", lambda e, j=j: e.tensor_tensor(out=xt[:, j, :], in0=xt[:, j, :], in1=tmp[:], op=ALU.add),
                  reads=[XT, TMP], writes=[XT])
            kb.op("act", lambda e, j=j: e.activation(out=tmp[:], in_=xt[:, j, :], func=AF.Square, accum_out=sm[:, 0:1]),
                  reads=[XT], writes=[TMP, SM])
            rstd_ops(kb, sm, SM)
            kb.op("dve", lambda e, j=j: e.scalar_tensor_tensor(out=tmp[:], in0=xt[:, j, :], scalar=sm[:, 1:2], in1=mod[:, 1, :],
                                                               op0=ALU.mult, op1=ALU.mult), reads=[XT, SM, MOD], writes=[TMP])
            kb.op("pool", lambda e: e.tensor_tensor(out=h2[:], in0=tmp[:], in1=mod[:, 2, :], op=ALU.add),
                  reads=[TMP, MOD], writes=[H2])
            tb, TB = B[3]
            def tr2(e, tb=tb):
                for k in range(8):
                    ins = e.transpose(bf(tb)[:, k * 128:(k + 1) * 128], h2[:, k * 128:(k + 1) * 128], ident[:])
                return ins
            kb.op("pe", tr2, reads=[H2, ID], writes=[TB])
            kb.op("act", lambda e, tb=tb, j=j: e.copy(out=h2T[:, :, j * 128:(j + 1) * 128],
                                                      in_=bf(tb)[:, 0:1024].rearrange("p (k t) -> p k t", k=8)),
                  reads=[TB], writes=[H2T])
        for g in range(4):
            wt, WB = load_w(peer_wq[:, g * 512:(g + 1) * 512], pre_of('wq', g))
            for hh in range(2):
                qb, QB = B[4 + (2 * g + hh) % 2]
                def mmq(e, qb=qb, wt=wt, hh=hh, NTOK=NTOK):
                    for i in range(2):
                        for k in range(8):
                            c0 = (hh * 2 + i) * 128
                            ins = e.matmul(qb[:, i * 256:i * 256 + NTOK], lhsT=wt[:, k, c0:c0 + 128], rhs=h2T[:, k, 0:NTOK],
                                           start=(k == 0), stop=(k == 7))
                    return ins
                kb.op("pe", mmq, reads=[WB, H2T], writes=[QB])
                hp0 = g * 4 + hh * 2
                kb.op("act", lambda e, qb=qb, hp0=hp0, NTOK=NTOK: e.copy(out=qT[:, hp0:hp0 + 2, 0:NTOK],
                                                               in_=qb[:].rearrange("p (i t) -> p i t", i=2)[:, :, 0:NTOK]),
                      reads=[QB], writes=[QT])
        for j, tt in enumerate(tiles):
            for g in range(4):
                sbk, SB_ = B[6 + g % 2]
                def mms(e, sbk=sbk, g=g, j=j):
                    for i in range(4):
                        hp = g * 4 + i
                        ins = e.matmul(sbk[:, i * 128:(i + 1) * 128], lhsT=qT[:, hp, j * 128:(j + 1) * 128], rhs=keys_b[:, hp, :],
                                       start=True, stop=True)
                    return ins
                kb.op("pe", mms, reads=[QT, KBF], writes=[SB_])
                kb.op("act", lambda e, sbk=sbk, g=g: e.copy(out=s_sb[:, g * 4:(g + 1) * 4, :].rearrange("p a n -> p (a n)"), in_=sbk[:]),
                      reads=[SB_], writes=[SSB])
            for hp in range(16):
                kb.op("dve", lambda e, hp=hp: e.max(out=top[:, hp, 0:8], in_=s_sb[:, hp, :]), reads=[SSB], writes=[TOP])
                kb.op("dve", lambda e, hp=hp: e.match_replace(out=work[:, hp * 128:(hp + 1) * 128], in_to_replace=top[:, hp, 0:8],
                                                              in_values=s_sb[:, hp, :], imm_value=NEG), reads=[SSB, TOP], writes=[WORK])
                kb.op("dve", lambda e, hp=hp: e.max(out=top[:, hp, 8:16], in_=work[:, hp * 128:(hp + 1) * 128]), reads=[WORK], writes=[TOP])
            def fcand(e):
                t4 = top[:].rearrange("p (h q) k -> p h q k", q=2)
                i0 = t4[:, :, 0, :].unsqueeze(3).to_broadcast([128, 8, 16, 16])
                i1 = t4[:, :, 1, :].unsqueeze(2).to_broadcast([128, 8, 16, 16])
                return e.tensor_tensor(out=cand[:], in0=i0, in1=i1, op=ALU.add)
            kb.op("pool", fcand, reads=[TOP], writes=[CAND])
            for h in range(8):
                cv = cand[:, h, :, :].rearrange("p a b -> p (a b)")
                kb.op("dve", lambda e, h=h, cv=cv: e.max(out=ctop[:, h, 0:8], in_=cv), reads=[CAND], writes=[CTOP])
                kb.op("dve", lambda e, h=h, cv=cv: e.match_replace(out=work[:, h * 256:(h + 1) * 256], in_to_replace=ctop[:, h, 0:8],
                                                                   in_values=cv, imm_value=NEG), reads=[CAND, CTOP], writes=[WORK])
                kb.op("dve", lambda e, h=h: e.max(out=ctop[:, h, 8:16], in_=work[:, h * 256:(h + 1) * 256]), reads=[WORK], writes=[CTOP])
            kb.op("dve", lambda e: e.tensor_scalar(out=sm[:, 8:16], in0=ctop[:, :, 15], scalar1=-1.0, scalar2=PEER_MARGIN,
                                                   op0=ALU.mult, op1=ALU.add), reads=[CTOP], writes=[SM])
            kb.op("dve", lambda e: e.tensor_tensor(out=e16[:], in0=ctop[:], in1=sm[:, 8:16].unsqueeze(2).to_broadcast([128, 8, 16]),
                                                   op=ALU.add), reads=[CTOP, SM], writes=[E16])
            kb.op("act", lambda e: e.activation(out=e16[:], in_=e16[:], func=AF.Exp), reads=[E16], writes=[E16])
            kb.op("dve", lambda e: e.tensor_reduce(out=sm[:, 16:24], in_=e16[:], axis=AX.X, op=ALU.add), reads=[E16], writes=[SM])
            kb.op("dve", lambda e: e.reciprocal(out=sm[:, 24:32], in_=sm[:, 16:24]), reads=[SM], writes=[SM])
            s4 = s_sb[:].rearrange("p (h q) n -> p h q n", q=2)
            kb.op("dve", lambda e, j=j, s4=s4: e.tensor_tensor(out=av[:, j, :, :], in0=s4[:, :, 0, :],
                                                               in1=sm[:, 8:16].unsqueeze(2).to_broadcast([128, 8, 128]), op=ALU.add),
                  reads=[SSB, SM], writes=[AV])
            kb.op("act", lambda e, j=j: e.activation(out=av[:, j, :, :], in_=av[:, j, :, :], func=AF.Exp), reads=[AV], writes=[AV])
            kb.op("act", lambda e, j=j, s4=s4: e.activation(out=bv[:, j, :, :], in_=s4[:, :, 1, :], func=AF.Exp), reads=[SSB], writes=[BV])
            for h in range(8):
                kb.op("pool", lambda e, j=j, h=h: e.tensor_scalar(out=diag[:, j, h, :], in0=ident_f[:], scalar1=sm[:, 24 + h:25 + h],
                                                                  scalar2=None, op0=ALU.mult), reads=[IDF, SM], writes=[DG])
        for cg in range(32):
            ut, UB = load_w(peer_uT[:, cg * 512:(cg + 1) * 512], pre_of('uT', cg))
            vt, VB = load_v(peer_v[cg * 512:(cg + 1) * 512, :], pre_of('v', cg))
            gt, GB = gl[cg % 2]
            att, ATB = at[cg % 2]
            for half in range(2):
                c0 = cg * 4 + half * 2
                wp, WPB = wps[(cg * 2 + half) % 2]
                pp, PP = pps[(cg * 2 + half) % 2]
                def fpp(e, c0=c0, nj=nj, pp=pp):
                    i0 = av[:, 0:nj, :, c0:c0 + 2].unsqueeze(4).to_broadcast([128, nj, 8, 2, 128])
                    i1 = bv[:, 0:nj, :, :].unsqueeze(3).to_broadcast([128, nj, 8, 2, 128])
                    return e.tensor_tensor(out=pp[:, 0:nj], in0=i0, in1=i1, op=ALU.mult)
                kb.op("pool", fpp, reads=[AV, BV], writes=[PP])
                kb.op("dve", lambda e, wp=wp, nj=nj, pp=pp: e.scalar_tensor_tensor(out=wp[:, 0:nj], in0=pp[:, 0:nj], scalar=1.0, in1=pp[:, 0:nj],
                                                                     op0=ALU.is_ge, op1=ALU.mult), reads=[PP], writes=[WPB])
                pb, PB = B[0 + half]
                wb, WTB = B[2 + half]
                def mmpre(e, pb=pb, half=half, ut=ut, NTOK=NTOK):
                    for i in range(2):
                        cl = half * 2 + i
                        for k in range(8):
                            ins = e.matmul(pb[:, i * 256:i * 256 + NTOK], lhsT=ut[:, k, cl * 128:(cl + 1) * 128], rhs=h2T[:, k, 0:NTOK],
                                           start=(k == 0), stop=(k == 7))
                    return ins
                kb.op("pe", mmpre, reads=[UB, H2T], writes=[PB])
                def mmwt(e, wb=wb, wp=wp, nj=nj):
                    for i in range(2):
                        for j in range(nj):
                            for h in range(8):
                                ins = e.matmul(wb[:, i * 256 + j * 128:i * 256 + (j + 1) * 128], lhsT=wp[:, j, h, i, :],
                                               rhs=diag[:, j, h, :], start=(h == 0), stop=(h == 7))
                    return ins
                kb.op("pe", mmwt, reads=[WPB, DG], writes=[WTB])
                kb.op("act", lambda e, pb=pb, gt=gt, half=half, NTOK=NTOK: e.activation(
                    out=gt[:, half * 2:half * 2 + 2, 0:NTOK], in_=pb[:].rearrange("p (i t) -> p i t", i=2)[:, :, 0:NTOK], func=AF.Gelu),
                    reads=[PB], writes=[GB])
                kb.op("dve", lambda e, wb=wb, gt=gt, att=att, half=half, NTOK=NTOK: e.tensor_tensor(
                    out=att[:, half * 2:half * 2 + 2, 0:NTOK], in0=wb[:].rearrange("p (i t) -> p i t", i=2)[:, :, 0:NTOK],
                    in1=gt[:, half * 2:half * 2 + 2, 0:NTOK], op=ALU.mult), reads=[WTB, GB], writes=[ATB])
            for j in range(nj):
                for hf in range(2):
                    ob, OB = B[4 + 2 * j + hf]
                    def mmo(e, ob=ob, att=att, vt=vt, j=j, hf=hf, cg=cg):
                        for cl in range(4):
                            ins = e.matmul(ob[:], lhsT=att[:, cl, j * 128:(j + 1) * 128], rhs=vt[:, cl, hf * 512:(hf + 1) * 512],
                                           start=(cg == 0 and cl == 0), stop=(cg == 31 and cl == 3))
                        return ins
                    kb.op("pe", mmo, reads=[ATB, VB], writes=[OB])
        for j, tt in enumerate(tiles):
            rows = slice(tt * 128, (tt + 1) * 128)
            for hf in range(2):
                ob, OB = B[4 + 2 * j + hf]
                kb.op("dve", lambda e, ob=ob, hf=hf: e.tensor_tensor(out=tmp[:, hf * 512:(hf + 1) * 512], in0=ob[:],
                                                                   in1=mod[:, 3, hf * 512:(hf + 1) * 512], op=ALU.mult),
                      reads=[OB, MOD], writes=[TMP])
            kb.op("pool", lambda e, j=j: e.tensor_tensor(out=xt[:, j, :], in0=xt[:, j, :], in1=tmp[:], op=ALU.add),
                  reads=[XT, TMP], writes=[XT])
            kb.dma("sp", x_out[rows, :], xt[:, j, :], reads=[XT], writes=[XOUT])
            if final_g is not None:
                kb.op("act", lambda e, j=j: e.activation(out=tmp[:], in_=xt[:, j, :], func=AF.Square, accum_out=sm[:, 0:1]),
                      reads=[XT], writes=[TMP, SM])
                rstd_ops(kb, sm, SM)
                kb.op("dve", lambda e, j=j: e.scalar_tensor_tensor(out=xn[:, 0:D], in0=xt[:, j, :], scalar=sm[:, 1:2], in1=fg[:],
                                                                   op0=ALU.mult, op1=ALU.mult), reads=[XT, SM, FG], writes=[XNB])
                kb.dma("sp", xn_out[rows, :], xn[:, 0:D], reads=[XNB], writes=[XN])


def build_pre(even):
    kb = KB()
    kb.psum_banks()
    B = kb.banks
    if even:
        NIN = 3072
        fm_blocks = [(c * 128, None) for c in range(8)] + [(1536 + c * 128, c) for c in range(8)]
        tm_groups = [(1024, 512), (2560, 512)]
    else:
        NIN = 1536
        fm_blocks = [(c * 128, c) for c in range(10)]
        tm_groups = [(1280, 256)]
    NROPE = 128 * sum(1 for _, r in fm_blocks if r is not None)
    NFM = len(fm_blocks)
    NTM = sum(n for _, n in tm_groups)
    x_in = kb.din("x", [TALL, D])
    csT = kb.din("csT", [128, 16])
    w_mod = kb.din("w_mod", [D, 6 * D])
    b_mod = kb.din("b_mod", [6 * D])
    n1g_in = kb.din("n1g", [D])
    w_in = kb.din("w_in", [D, NIN])
    w_perm = kb.din("w_perm", [D, NROPE])
    cosT = kb.din("cosT", [128, TALL])
    sinT = kb.din("sinT", [128, TALL])
    ident_d = kb.din("c_ident", [128, 128])
    fm_out, FMO = kb.dout("fmT", [NFM * 128, TALL], BF16)
    tm_out, TMO = kb.dout("tm", [TALL, NTM], BF16)
    modrow, MRO = kb.dout("modrow", [2, 6, D])

    ident_f, IDF = kb.sb("ident_f", [128, 128], F32)
    ident, ID = kb.sb("ident", [128, 128], BF16)
    kb.dma("sp", ident_f[:], ident_d, writes=[IDF])
    kb.op("act", lambda e: e.copy(out=ident[:], in_=ident_f[:]), reads=[IDF], writes=[ID])
    win, WIN = kb.sb("win", [128, 8, NIN], BF16)
    wpm, WPM = kb.sb("wpm", [128, 8, NROPE], BF16)
    for c0 in range(0, NIN, 512):
        kb.dma("pool", win[:, :, c0:c0 + 512], w_in[:, c0:c0 + 512].rearrange("(k p) n -> p k n", p=128), writes=[WIN])
    for c0 in range(0, NROPE, 512):
        n = min(512, NROPE - c0)
        kb.dma("pool", wpm[:, :, c0:c0 + n], w_perm[:, c0:c0 + n].rearrange("(k p) n -> p k n", p=128), writes=[WPM])
    cs_f, CSF = kb.sb("cs_f", [128, 16], F32)
    cs_b, CSB = kb.sb("cs_b", [128, 16], BF16)
    csbc, CSBC = kb.sb("csbc", [128, 16, 128], BF16)
    kb.dma("sp", cs_f[:], csT, writes=[CSF])
    kb.op("act", lambda e: e.activation(out=cs_b[:], in_=cs_f[:], func=AF.Silu), reads=[CSF], writes=[CSB])
    kb.op("dve", lambda e: e.tensor_copy(out=csbc[:], in_=cs_b[:].unsqueeze(2).to_broadcast([128, 16, 128])),
          reads=[CSB], writes=[CSBC])
    n1g, N1G = kb.sb("n1g_sb", [128, D], F32)
    kb.dma("sp", n1g[:], n1g_in.unsqueeze(0).to_broadcast([128, D]), writes=[N1G])
    g1s, G1S = kb.sb("g1s", [128, 2, 2, D], F32)
    wmr = [kb.sb(f"wmr{i}", [128, 8, 512], BF16) for i in range(2)]
    bmr = [kb.sb(f"bmr{i}", [128, 512], F32) for i in range(2)]
    mtmp = [kb.sb(f"mtmp{i}", [128, 512], F32) for i in range(2)]
    for cgp in range(12):
        wt, WB = wmr[cgp % 2]
        bt, BB = bmr[cgp % 2]
        cols = slice(cgp * 512, (cgp + 1) * 512)
        kb.dma("pool", wt[:], w_mod[:, cols].rearrange("(k p) n -> p k n", p=128), writes=[WB])
        kb.dma("sp", bt[:], b_mod[cols].unsqueeze(0).to_broadcast([128, 512]), writes=[BB])
        chunk = cgp // 2
        half = cgp % 2
        for s in range(2):
            mb, MB = B[6 + s]
            def mmm(e, mb=mb, wt=wt, s=s):
                for k in range(8):
                    ins = e.matmul(mb[:], lhsT=csbc[:, s * 8 + k, :], rhs=wt[:, k, :], start=(k == 0), stop=(k == 7))
                return ins
            kb.op("pe", mmm, reads=[CSBC, WB], writes=[MB])
            if chunk < 2:
                dst = g1s[:, s, chunk, half * 512:(half + 1) * 512]
                kb.op("dve", lambda e, mb=mb, bt=bt, dst=dst: e.tensor_tensor(out=dst, in0=mb[:], in1=bt[:], op=ALU.add),
                      reads=[MB, BB], writes=[G1S])
                kb.dma("sp", modrow[s, chunk:chunk + 1, half * 512:(half + 1) * 512], dst[0:1, :], reads=[G1S], writes=[MRO])
            else:
                mt, MT = mtmp[s]
                kb.op("dve", lambda e, mb=mb, bt=bt, mt=mt: e.tensor_tensor(out=mt[:], in0=mb[:], in1=bt[:], op=ALU.add),
                      reads=[MB, BB], writes=[MT])
                kb.dma("sp", modrow[s, chunk:chunk + 1, half * 512:(half + 1) * 512], mt[0:1, :], reads=[MT], writes=[MRO])
    for s in range(2):
        kb.op("dve", lambda e, s=s: e.scalar_tensor_tensor(out=g1s[:, s, 1, :], in0=g1s[:, s, 1, :], scalar=1.0, in1=n1g[:],
                                                           op0=ALU.add, op1=ALU.mult), reads=[G1S, N1G], writes=[G1S])
    xt = [kb.sb(f"xt{i}", [128, D], F32) for i in range(2)]
    tmp, TMP = kb.sb("tmp", [128, D], F32)
    hx, HX = kb.sb("hx", [128, D], BF16)
    hxT, HXT = kb.sb("hxT", [128, 8, 512], BF16)
    sm, SM = kb.sb("sm", [128, 8], F32)
    cst = [kb.sb(f"cst{i}", [128, 512], F32) for i in range(2)]
    snt = [kb.sb(f"snt{i}", [128, 512], F32) for i in range(2)]
    r1, R1 = kb.sb("r1", [128, 512], F32)
    r2, R2 = kb.sb("r2", [128, 512], F32)
    fmo = [kb.sb(f"fmo{i}", [128, 512], BF16) for i in range(2)]
    tmo = [kb.sb(f"tmo{i}", [128, 512], BF16) for i in range(2)]
    groups = [(g * 4, 4, 0) for g in range(NTL // 4)] + [(NTL, NTC, 1)]
    xi = 0
    fi = 0
    ti = 0
    for gi, (t0, ntl, s) in enumerate(groups):
        NTOK = ntl * 128
        tok0 = t0 * 128
        ct, CT = cst[gi % 2]
        st, ST = snt[gi % 2]
        kb.dma("sp", ct[:, 0:NTOK], cosT[:, tok0:tok0 + NTOK], writes=[CT])
        kb.dma("sp", st[:, 0:NTOK], sinT[:, tok0:tok0 + NTOK], writes=[ST])
        for j in range(ntl):
            x_t, XB = xt[xi % 2]
            xi += 1
            rows = slice(tok0 + j * 128, tok0 + (j + 1) * 128)
            kb.dma("sp", x_t[:], x_in[rows, :], writes=[XB])
            kb.op("act", lambda e, x_t=x_t: e.activation(out=tmp[:], in_=x_t[:], func=AF.Square, accum_out=sm[:, 0:1]),
                  reads=[XB], writes=[TMP, SM])
            rstd_ops(kb, sm, SM)
            kb.op("dve", lambda e, x_t=x_t, s=s: e.scalar_tensor_tensor(out=tmp[:], in0=x_t[:], scalar=sm[:, 1:2], in1=g1s[:, s, 1, :],
                                                                         op0=ALU.mult, op1=ALU.mult), reads=[XB, SM, G1S], writes=[TMP])
            kb.op("pool", lambda e, s=s: e.tensor_tensor(out=hx[:], in0=tmp[:], in1=g1s[:, s, 0, :], op=ALU.add),
                  reads=[TMP, G1S], writes=[HX])
            tb, TB = B[0]
            def tr(e, tb=tb):
                for k in range(8):
                    ins = e.transpose(tb.bitcast(BF16)[:, k * 128:(k + 1) * 128], hx[:, k * 128:(k + 1) * 128], ident[:])
                return ins
            kb.op("pe", tr, reads=[HX, ID], writes=[TB])
            kb.op("act", lambda e, tb=tb, j=j: e.copy(out=hxT[:, :, j * 128:(j + 1) * 128],
                                                      in_=tb.bitcast(BF16)[:, 0:1024].rearrange("p (k t) -> p k t", k=8)),
                  reads=[TB], writes=[HXT])
        for bi, (c0, ridx) in enumerate(fm_blocks):
            pa, PA = B[1 + bi % 2]
            def mma(e, pa=pa, c0=c0, NTOK=NTOK):
                for k in range(8):
                    ins = e.matmul(pa[:, 0:NTOK], lhsT=win[:, k, c0:c0 + 128], rhs=hxT[:, k, 0:NTOK], start=(k == 0), stop=(k == 7))
                return ins
            kb.op("pe", mma, reads=[WIN, HXT], writes=[PA])
            fo, FO = fmo[fi % 2]
            fi += 1
            if ridx is None:
                kb.op("act", lambda e, pa=pa, fo=fo, NTOK=NTOK: e.copy(out=fo[:, 0:NTOK], in_=pa[:, 0:NTOK]), reads=[PA], writes=[FO])
            else:
                pb, PB = B[3 + bi % 2]
                def mmb(e, pb=pb, ridx=ridx, NTOK=NTOK):
                    for k in range(8):
                        ins = e.matmul(pb[:, 0:NTOK], lhsT=wpm[:, k, ridx * 128:(ridx + 1) * 128], rhs=hxT[:, k, 0:NTOK],
                                       start=(k == 0), stop=(k == 7))
                    return ins
                kb.op("pe", mmb, reads=[WPM, HXT], writes=[PB])
                kb.op("dve", lambda e, pa=pa, ct=ct, NTOK=NTOK: e.tensor_tensor(out=r1[:, 0:NTOK], in0=pa[:, 0:NTOK], in1=ct[:, 0:NTOK], op=ALU.mult),
                      reads=[PA, CT], writes=[R1])
                kb.op("dve", lambda e, pb=pb, st=st, NTOK=NTOK: e.tensor_tensor(out=r2[:, 0:NTOK], in0=pb[:, 0:NTOK], in1=st[:, 0:NTOK], op=ALU.mult),
                      reads=[PB, ST], writes=[R2])
                kb.op("pool", lambda e, fo=fo, NTOK=NTOK: e.tensor_tensor(out=fo[:, 0:NTOK], in0=r1[:, 0:NTOK], in1=r2[:, 0:NTOK], op=ALU.add),
                      reads=[R1, R2], writes=[FO])
            for c0_ in range(0, NTOK, 256):
                kb.dma("sp", fm_out[bi * 128:(bi + 1) * 128, tok0 + c0_:tok0 + c0_ + 256], fo[:, c0_:c0_ + 256], reads=[FO], writes=[FMO])
        for j in range(ntl):
            oc = 0
            for (c0, ncol) in tm_groups:
                pt, PT = B[5 + ti % 2]
                to, TO = tmo[ti % 2]
                ti += 1
                def mmt(e, pt=pt, c0=c0, ncol=ncol, j=j):
                    for k in range(8):
                        ins = e.matmul(pt[:, 0:ncol], lhsT=hxT[:, k, j * 128:(j + 1) * 128], rhs=win[:, k, c0:c0 + ncol],
                                       start=(k == 0), stop=(k == 7))
                    return ins
                kb.op("pe", mmt, reads=[HXT, WIN], writes=[PT])
                kb.op("act", lambda e, pt=pt, to=to, ncol=ncol: e.copy(out=to[:, 0:ncol], in_=pt[:, 0:ncol]), reads=[PT], writes=[TO])
                rows = slice(tok0 + j * 128, tok0 + (j + 1) * 128)
                kb.dma("sp", tm_out[rows, oc:oc + ncol], to[:, 0:ncol], reads=[TO], writes=[TMO])
                oc += ncol
    return kb.finish()


def emit_attn_even(kb, ao, AO):
    B = kb.banks
    naqT = kb.din("naqT", [512, TALL], BF16)
    nakT = kb.din("nakT_h", [512, 74 * 64], BF16)
    nav = kb.din("nav_h", [74 * 64, 520], BF16)
    nakTc = kb.din("nakT_c", [512, CTX], BF16)
    navc = kb.din("nav_c", [CTX, 520], BF16)
    dqT = kb.din("dqT", [512, TALL], BF16)
    dkT = kb.din("dkT_all", [512, CTX + SEQ], BF16)
    dv = kb.din("dv_all", [CTX + SEQ, 516], BF16)
    nbias = kb.din("nbias", [5, 8, 128, 640])
    lam_in = kb.din("lam", [4, 64])
    subg_in = kb.din("subg", [128])
    lami_in = kb.din("lam_init", [128, 2])
    sc = 64 ** -0.5
    lm, LM = kb.sb("lm", [128, 4, 64], F32)
    lms, LMS = kb.sb("lms", [128, 16], F32)
    subg, SUBG = kb.sb("subg_sb", [128, 128], F32)
    kb.dma("sp", lm[:].rearrange("p a b -> p (a b)"), lam_in.rearrange("a b -> (a b)").unsqueeze(0).to_broadcast([128, 256]), writes=[LM])
    kb.dma("sp", subg[:], subg_in.unsqueeze(0).to_broadcast([128, 128]), writes=[SUBG])
    kb.dma("sp", lms[:, 8:10], lami_in, writes=[LMS])
    kb.op("dve", lambda e: e.tensor_tensor(out=lm[:, 0, :], in0=lm[:, 0, :], in1=lm[:, 1, :], op=ALU.mult), reads=[LM], writes=[LM])
    kb.op("dve", lambda e: e.tensor_tensor(out=lm[:, 2, :], in0=lm[:, 2, :], in1=lm[:, 3, :], op=ALU.mult), reads=[LM], writes=[LM])
    kb.op("dve", lambda e: e.tensor_reduce(out=lms[:, 0:1], in_=lm[:, 0, :], axis=AX.X, op=ALU.add), reads=[LM], writes=[LMS])
    kb.op("dve", lambda e: e.tensor_reduce(out=lms[:, 1:2], in_=lm[:, 2, :], axis=AX.X, op=ALU.add), reads=[LM], writes=[LMS])
    kb.op("act", lambda e: e.activation(out=lms[:, 2:4], in_=lms[:, 0:2], func=AF.Exp), reads=[LMS], writes=[LMS])
    kb.op("dve", lambda e: e.tensor_tensor(out=lms[:, 4:5], in0=lms[:, 2:3], in1=lms[:, 3:4], op=ALU.subtract), reads=[LMS], writes=[LMS])
    kb.op("dve", lambda e: e.tensor_tensor(out=lms[:, 5:6], in0=lms[:, 4:5], in1=lms[:, 8:9], op=ALU.add), reads=[LMS], writes=[LMS])
    kb.op("dve", lambda e: e.tensor_scalar(out=lms[:, 6:7], in0=lms[:, 5:6], scalar1=-1.0, scalar2=None, op0=ALU.mult), reads=[LMS], writes=[LMS])
    kb.op("dve", lambda e: e.tensor_scalar(out=subg[:], in0=subg[:], scalar1=lms[:, 9:10], scalar2=None, op0=ALU.mult), reads=[SUBG, LMS], writes=[SUBG])

    kcT, KCT = kb.sb("na_kcT", [128, 4, CTX], BF16)
    vca, VCA = kb.sb("na_vca", [128, 2, 8, 65], BF16)
    kb.dma("sp", kcT[:], nakTc.rearrange("(a p) n -> p a n", p=128), writes=[KCT])
    kb.dma("sp", vca[:].rearrange("p b h c -> p b (h c)"), navc.rearrange("(b p) c -> p b c", p=128), writes=[VCA])
    kts = [kb.sb(f"na_kt{i}", [128, 4, 640], BF16) for i in range(2)]
    vts = [kb.sb(f"na_vt{i}", [128, 5, 8, 65], BF16) for i in range(2)]
    qts = [kb.sb(f"na_qt{i}", [128, 4, 128], BF16) for i in range(2)]
    bts = [kb.sb(f"na_bt{i}", [128, 640], F32) for i in range(2)]
    sts = [kb.sb(f"na_st{i}", [128, 640], F32) for i in range(2)]
    pts = [kb.sb(f"na_pt{i}", [128, 896], BF16) for i in range(2)]
    aot = [kb.sb(f"na_ao{i}", [128, 512], BF16) for i in range(2)]
    rc, RC = kb.sb("na_rc", [128, 8], F32)
    hi = 0
    for qt in range(NT):
        isctx = qt >= NTL
        q_t, QB = qts[qt % 2]
        kb.dma("sp", q_t[:], naqT[:, qt * 128:(qt + 1) * 128].rearrange("(a p) n -> p a n", p=128), writes=[QB])
        if not isctx:
            k_t, KB_ = kts[qt % 2]
            v_t, VB = vts[qt % 2]
            kb.dma("sp", k_t[:], nakT[:, qt * 128:qt * 128 + 640].rearrange("(a p) n -> p a n", p=128), writes=[KB_])
            kb.dma("sp", v_t[:].rearrange("p b h c -> p b (h c)"), nav[qt * 128:qt * 128 + 640, :].rearrange("(b p) c -> p b c", p=128), writes=[VB])
            slot = 0 if qt == 0 else 1 if qt == 1 else 3 if qt == NTL - 2 else 4 if qt == NTL - 1 else 2
        a_t, AB = aot[qt % 2]
        for h in range(8):
            a, off = h // 2, (h % 2) * 64
            pa, PA = B[(2 * hi) % 4]
            pb, PB = B[(2 * hi + 1) % 4]
            st_, STB = sts[hi % 2]
            pt_, PTB = pts[hi % 2]
            acc, ACC = B[4 + (h // 4)]
            hi += 1
            if not isctx:
                b_t, BB = bts[hi % 2]
                kb.dma("sp", b_t[:], nbias[slot, h], writes=[BB])
                def mms(e, pa=pa, pb=pb, k_t=k_t, q_t=q_t, a=a, off=off):
                    for blk in range(4):
                        e.matmul(pa[:, blk * 128:(blk + 1) * 128], lhsT=k_t[off:off + 64, a, blk * 128:(blk + 1) * 128],
                                 rhs=q_t[off:off + 64, a, :], start=True, stop=True)
                    e.matmul(pb[:, 0:128], lhsT=k_t[off:off + 64, a, 512:640], rhs=q_t[off:off + 64, a, :], start=True, stop=True)
                    for cb in range(2):
                        ins = e.matmul(pb[:, 128 + cb * 128:256 + cb * 128], lhsT=kcT[off:off + 64, a, cb * 128:(cb + 1) * 128],
                                       rhs=q_t[off:off + 64, a, :], start=True, stop=True)
                    return ins
                kb.op("pe", mms, reads=[KB_, QB, KCT], writes=[PA, PB])
                kb.op("dve", lambda e, pa=pa, st_=st_, b_t=b_t: e.scalar_tensor_tensor(out=st_[:, 0:512], in0=pa[:], scalar=sc, in1=b_t[:, 0:512],
                                                                                    op0=ALU.mult, op1=ALU.add), reads=[PA, BB], writes=[STB])
                kb.op("dve", lambda e, pb=pb, st_=st_, b_t=b_t: e.scalar_tensor_tensor(out=st_[:, 512:640], in0=pb[:, 0:128], scalar=sc,
                                                                                    in1=b_t[:, 512:640], op0=ALU.mult, op1=ALU.add),
                      reads=[PB, BB], writes=[STB])
                kb.op("act", lambda e, st_=st_, pt_=pt_: e.activation(out=pt_[:, 0:640], in_=st_[:], func=AF.Exp), reads=[STB], writes=[PTB])
                kb.op("act", lambda e, pb=pb, pt_=pt_: e.activation(out=pt_[:, 640:896], in_=pb[:, 128:384], func=AF.Exp, scale=sc),
                      reads=[PB], writes=[PTB])
                def mmv(e, acc=acc, pt_=pt_, v_t=v_t, h=h):
                    o = acc[:, (h % 4) * 65:(h % 4) * 65 + 65]
                    for blk in range(5):
                        e.matmul(o, lhsT=pt_[:, blk * 128:(blk + 1) * 128], rhs=v_t[:, blk, h, :], start=(blk == 0), stop=False)
                    for cb in range(2):
                        ins = e.matmul(o, lhsT=pt_[:, 640 + cb * 128:768 + cb * 128], rhs=vca[:, cb, h, :], start=False, stop=(cb == 1))
                    return ins
                kb.op("pe", mmv, reads=[PTB, VB, VCA], writes=[ACC])
            else:
                def mms(e, pb=pb, q_t=q_t, a=a, off=off):
                    for cb in range(2):
                        ins = e.matmul(pb[:, 128 + cb * 128:256 + cb * 128], lhsT=kcT[off:off + 64, a, cb * 128:(cb + 1) * 128],
                                       rhs=q_t[off:off + 64, a, :], start=True, stop=True)
                    return ins
                kb.op("pe", mms, reads=[QB, KCT], writes=[PB])
                kb.op("act", lambda e, pb=pb, pt_=pt_: e.activation(out=pt_[:, 640:896], in_=pb[:, 128:384], func=AF.Exp, scale=sc),
                      reads=[PB], writes=[PTB])
                def mmv(e, acc=acc, pt_=pt_, h=h):
                    o = acc[:, (h % 4) * 65:(h % 4) * 65 + 65]
                    for cb in range(2):
                        ins = e.matmul(o, lhsT=pt_[:, 640 + cb * 128:768 + cb * 128], rhs=vca[:, cb, h, :], start=(cb == 0), stop=(cb == 1))
                    return ins
                kb.op("pe", mmv, reads=[PTB, VCA], writes=[ACC])
            if h % 4 == 3:
                g4 = h // 4
                av = acc[:, 0:260].rearrange("p (h c) -> p h c", c=65)
                kb.op("dve", lambda e, av=av, g4=g4: e.reciprocal(out=rc[:, g4 * 4:g4 * 4 + 4], in_=av[:, :, 64]), reads=[ACC], writes=[RC])
                kb.op("dve", lambda e, av=av, g4=g4, a_t=a_t: e.tensor_tensor(
                    out=a_t[:, g4 * 256:(g4 + 1) * 256].rearrange("p (h d) -> p h d", d=64), in0=av[:, :, 0:64],
                    in1=rc[:, g4 * 4:g4 * 4 + 4].unsqueeze(2).to_broadcast([128, 4, 64]), op=ALU.mult), reads=[ACC, RC], writes=[AB])
        kb.dma("sp", ao[qt * 128:(qt + 1) * 128, 0:512], a_t[:], reads=[AB], writes=[AO])

    NBLK = (CTX + SEQ) // 128
    dk, DK = kb.sb("d_k", [128, CTX + SEQ], BF16)
    dva, DVA = kb.sb("d_va", [128, NBLK, 129], BF16)
    dq, DQ = kb.sb("d_q", [128, TALL], BF16)
    dpt = [kb.sb(f"d_pt{i}", [128, 512], BF16) for i in range(3)]
    o0, O0 = kb.sb("d_o0", [128, 128], F32)
    o1, O1 = kb.sb("d_o1", [128, 128], F32)
    osq, OSQ = kb.sb("d_osq", [128, 128], F32)
    dsm, DSM = kb.sb("d_sm", [128, 8], F32)
    dob = [kb.sb(f"d_ob{i}", [128, 128], BF16) for i in range(2)]
    si = 0
    oi = 0
    for h in range(4):
        kb.dma("sp", dk[:], dkT[h * 128:(h + 1) * 128, :], writes=[DK])
        for c0 in range(0, NBLK, 26):
            kb.dma("sp", dva[:, c0:c0 + 26, :], dv[c0 * 128:(c0 + 26) * 128, h * 129:(h + 1) * 129].rearrange("(b p) d -> p b d", p=128),
                   writes=[DVA])
        kb.dma("sp", dq[:], dqT[h * 128:(h + 1) * 128, :], writes=[DQ])
        qgroups = [(g * 256, 256, NBLK) for g in range(TQ // 256)] + [(TQ, CTX, CTX // 128)]
        for (q0, nq, nblk) in qgroups:
            nqs = nq // 128
            for blk in range(nblk):
                for m in range(2):
                    ps, PS = B[si % 3]
                    pt_, PTB = dpt[si % 3]
                    si += 1
                    kb.op("pe", lambda e, ps=ps, m=m, blk=blk, q0=q0, nq=nq: e.matmul(
                        ps[:, 0:nq], lhsT=dk[m * 64:(m + 1) * 64, blk * 128:(blk + 1) * 128], rhs=dq[m * 64:(m + 1) * 64, q0:q0 + nq],
                        start=True, stop=True), reads=[DK, DQ], writes=[PS])
                    kb.op("act", lambda e, ps=ps, pt_=pt_, nq=nq: e.activation(out=pt_[:, 0:nq], in_=ps[:, 0:nq], func=AF.Exp, scale=sc),
                          reads=[PS], writes=[PTB])
                    def mmv(e, pt_=pt_, m=m, blk=blk, nqs=nqs, nblk=nblk):
                        for qs in range(nqs):
                            a = qs * 2 + m
                            acc = B[3 + a][0]
                            ins = e.matmul(acc[:, 0:129], lhsT=pt_[:, qs * 128:(qs + 1) * 128], rhs=dva[:, blk, :],
                                           start=(blk == 0), stop=(blk == nblk - 1))
                        return ins
                    kb.op("pe", mmv, reads=[PTB, DVA], writes=[B[3 + qs * 2 + m][1] for qs in range(nqs)])
            for qs in range(nqs):
                accs = []
                for m in range(2):
                    a = qs * 2 + m
                    accs.append((B[3 + a][0][:, 0:129], B[3 + a][1]))
                (a0, A0), (a1, A1) = accs
                kb.op("dve", lambda e, a0=a0: e.reciprocal(out=dsm[:, 0:1], in_=a0[:, 128:129]), reads=[A0], writes=[DSM])
                kb.op("dve", lambda e, a1=a1: e.reciprocal(out=dsm[:, 1:2], in_=a1[:, 128:129]), reads=[A1], writes=[DSM])
                kb.op("dve", lambda e: e.tensor_tensor(out=dsm[:, 1:2], in0=dsm[:, 1:2], in1=lms[:, 6:7], op=ALU.mult), reads=[DSM, LMS], writes=[DSM])
                kb.op("dve", lambda e, a0=a0: e.tensor_scalar(out=o0[:], in0=a0[:, 0:128], scalar1=dsm[:, 0:1], scalar2=None, op0=ALU.mult),
                      reads=[A0, DSM], writes=[O0])
                kb.op("dve", lambda e, a1=a1: e.scalar_tensor_tensor(out=o1[:], in0=a1[:, 0:128], scalar=dsm[:, 1:2], in1=o0[:],
                                                                   op0=ALU.mult, op1=ALU.add), reads=[A1, DSM, O0], writes=[O1])
                kb.op("act", lambda e: e.activation(out=osq[:], in_=o1[:], func=AF.Square, accum_out=dsm[:, 2:3]), reads=[O1], writes=[OSQ, DSM])
                kb.op("dve", lambda e: e.tensor_scalar(out=dsm[:, 3:4], in0=dsm[:, 2:3], scalar1=1.0 / 128, scalar2=EPS, op0=ALU.mult, op1=ALU.add),
                      reads=[DSM], writes=[DSM])
                kb.op("act", lambda e: e.activation(out=dsm[:, 4:5], in_=dsm[:, 3:4], func=AF.Sqrt), reads=[DSM], writes=[DSM])
                kb.op("dve", lambda e: e.reciprocal(out=dsm[:, 5:6], in_=dsm[:, 4:5]), reads=[DSM], writes=[DSM])
                ob, OB = dob[oi % 2]
                oi += 1
                kb.op("dve", lambda e, ob=ob: e.scalar_tensor_tensor(out=ob[:], in0=o1[:], scalar=dsm[:, 5:6], in1=subg[:], op0=ALU.mult, op1=ALU.mult),
                      reads=[O1, DSM, SUBG], writes=[OB])
                r0 = q0 + qs * 128
                kb.dma("sp", ao[r0:r0 + 128, 512 + h * 128:512 + (h + 1) * 128], ob[:], reads=[OB], writes=[AO])


def emit_attn_odd(kb, ao, AO):
    B = kb.banks
    HALO = TQ + 256
    qin = kb.din("swa_q", [64, 16, TALL], BF16)
    kin = kb.din("swa_kT_h", [64, 4, HALO], BF16)
    vin = kb.din("swa_v_h", [HALO, 260], BF16)
    kcin = kb.din("swa_kT_c", [64, 4, CTX], BF16)
    vcin = kb.din("swa_v_c", [CTX, 260], BF16)
    sbias = kb.din("sbias", [3, 128, 384])
    sink_in = kb.din("sink", [16])
    sc = 64 ** -0.5
    snk, SNK = kb.sb("snk", [128, 16], F32)
    kb.dma("sp", snk[:], sink_in.unsqueeze(0).to_broadcast([128, 16]), writes=[SNK])
    kb.op("act", lambda e: e.activation(out=snk[:], in_=snk[:], func=AF.Exp), reads=[SNK], writes=[SNK])
    kT, KT = kb.sb("s_kT", [64, 4, HALO], BF16)
    kb.dma("sp", kT[:], kin, writes=[KT])
    kcT, KCT = kb.sb("s_kcT", [64, 4, CTX], BF16)
    kb.dma("sp", kcT[:], kcin, writes=[KCT])
    va, VA = kb.sb("s_va", [128, HALO // 128, 4, 65], BF16)
    kb.dma("sp", va[:].rearrange("p b h c -> p b (h c)"), vin.rearrange("(b p) c -> p b c", p=128), writes=[VA])
    vca, VCA = kb.sb("s_vca", [128, 2, 4, 65], BF16)
    kb.dma("sp", vca[:].rearrange("p b h c -> p b (h c)"), vcin.rearrange("(b p) c -> p b c", p=128), writes=[VCA])
    sb_, SBB = kb.sb("s_bias", [128, 3, 384], F32)
    kb.dma("sp", sb_[:], sbias.rearrange("s p n -> p s n"), writes=[SBB])
    qts = [kb.sb(f"s_q{i}", [64, 16, 128], BF16) for i in range(2)]
    sts = [kb.sb(f"s_st{i}", [128, 512], F32) for i in range(2)]
    pts = [kb.sb(f"s_pt{i}", [128, 5, 512], BF16) for i in range(2)]
    aot = [kb.sb(f"s_ao{i}", [128, D], BF16) for i in range(2)]
    den, DEN = kb.sb("s_den", [128, 8], F32)
    si = 0
    gi = 0
    for qt in range(NT):
        isctx = qt >= NTL
        q_t, QB = qts[qt % 2]
        kb.dma("sp", q_t[:], qin[:, :, qt * 128:(qt + 1) * 128], writes=[QB])
        a_t, AB = aot[qt % 2]
        slot = 0 if qt == 0 else 2 if qt == NTL - 1 else 1
        for n in range(4):
            pt_, PTB = pts[gi % 2]
            acc, ACC = B[4 + gi % 2]
            gi += 1
            blks = ([] if isctx else [0, 1, 2]) + [3, 4]
            for blk in blks:
                ps, PS = B[si % 3]
                st_, STB = sts[si % 2]
                si += 1
                if blk < 3:
                    kb.op("pe", lambda e, ps=ps, blk=blk, n=n, q_t=q_t, qt=qt: e.matmul(
                        ps[:], lhsT=kT[:, n, (qt + blk) * 128:(qt + blk + 1) * 128], rhs=q_t[:, n * 4:(n + 1) * 4, :], start=True, stop=True),
                        reads=[KT, QB], writes=[PS])
                    kb.op("dve", lambda e, ps=ps, st_=st_, blk=blk, slot=slot: e.scalar_tensor_tensor(
                        out=st_[:].rearrange("p (g q) -> p g q", g=4), in0=ps[:].rearrange("p (g q) -> p g q", g=4), scalar=sc,
                        in1=sb_[:, slot, blk * 128:(blk + 1) * 128].unsqueeze(1).to_broadcast([128, 4, 128]), op0=ALU.mult, op1=ALU.add),
                        reads=[PS, SBB], writes=[STB])
                    kb.op("act", lambda e, st_=st_, pt_=pt_, blk=blk: e.activation(out=pt_[:, blk, :], in_=st_[:], func=AF.Exp), reads=[STB], writes=[PTB])
                else:
                    cb = blk - 3
                    kb.op("pe", lambda e, ps=ps, cb=cb, n=n, q_t=q_t: e.matmul(
                        ps[:], lhsT=kcT[:, n, cb * 128:(cb + 1) * 128], rhs=q_t[:, n * 4:(n + 1) * 4, :], start=True, stop=True),
                        reads=[KCT, QB], writes=[PS])
                    kb.op("act", lambda e, ps=ps, pt_=pt_, blk=blk: e.activation(out=pt_[:, blk, :], in_=ps[:], func=AF.Exp, scale=sc),
                          reads=[PS], writes=[PTB])
            def mmv(e, acc=acc, pt_=pt_, n=n, qt=qt, blks=blks):
                for g in range(4):
                    o = acc[:, g * 65:(g + 1) * 65]
                    for i, blk in enumerate(blks):
                        rhs = va[:, qt + blk, n, :] if blk < 3 else vca[:, blk - 3, n, :]
                        ins = e.matmul(o, lhsT=pt_[:, blk, g * 128:(g + 1) * 128], rhs=rhs, start=(i == 0), stop=(i == len(blks) - 1))
                return ins
            kb.op("pe", mmv, reads=[PTB, VA, VCA], writes=[ACC])
            av = acc[:, 0:260].rearrange("p (g c) -> p g c", c=65)
            kb.op("dve", lambda e, av=av, n=n: e.tensor_tensor(out=den[:, 0:4], in0=av[:, :, 64], in1=snk[:, n * 4:(n + 1) * 4], op=ALU.add),
                  reads=[ACC, SNK], writes=[DEN])
            kb.op("dve", lambda e: e.reciprocal(out=den[:, 4:8], in_=den[:, 0:4]), reads=[DEN], writes=[DEN])
            kb.op("dve", lambda e, av=av, n=n, a_t=a_t: e.tensor_tensor(
                out=a_t[:, n * 256:(n + 1) * 256].rearrange("p (g d) -> p g d", d=64), in0=av[:, :, 0:64],
                in1=den[:, 4:8].unsqueeze(2).to_broadcast([128, 4, 64]), op=ALU.mult), reads=[ACC, DEN], writes=[AB])
        kb.dma("sp", ao[qt * 128:(qt + 1) * 128, :], a_t[:], reads=[AB], writes=[AO])


def build_post(even):
    kb = KB()
    kb.psum_banks()
    ao, AO = kb.dscratch("ao_scr", [TALL, D], BF16)
    with ExitStack() as es:
        saved = kb.es
        kb.es = es
        if even:
            emit_attn_even(kb, ao, AO)
        else:
            emit_attn_odd(kb, ao, AO)
        kb.es = saved
        kb.P.barrier()
    x_in = kb.din("x", [TALL, D])
    modrow = kb.din("modrow", [2, 6, D])
    w_out = kb.din("w_out", [D, D])
    n2g = kb.din("n2g", [D])
    fg = kb.din("fg", [D])
    wq = kb.din("wq", [D, 2048])
    keysT = kb.din("keysT", [128, 16, 128])
    uT = kb.din("uT", [D, 16384])
    v = kb.din("v", [16384, D])
    x_out, XOUT = kb.dout("x_out", [TALL, D])
    xn_out, XN = kb.dout("xn_out", [TALL, D])
    emit_post(kb, NT, x_in, ao, AO, modrow, w_out, n2g, wq, keysT, uT, v, x_out, XOUT, final_g=fg, xn_out=xn_out, XN=XN,
              tile_sets=[0] * NTL + [1] * NTC)
    return kb.finish()


GRID_W = 64
_PERM64 = np.concatenate([np.arange(16, 32), np.arange(0, 16), np.arange(48, 64), np.arange(32, 48)])
_SGN64 = np.concatenate([-np.ones(16), np.ones(16), -np.ones(16), np.ones(16)]).astype(np.float32)
_PROGS = {}


def _prog(name):
    if name not in _PROGS:
        kind, par = name.split("_")
        _PROGS[name] = build_pre(par == "even") if kind == "pre" else build_post(par == "even")
    return _PROGS[name]


def _rope_tables_T(tok0):
    t = np.arange(tok0, tok0 + TQ)
    row = (t // GRID_W).astype(np.float32)
    col = (t % GRID_W).astype(np.float32)
    half = 32
    inv = (10000.0 ** (-np.arange(0, half, 2, dtype=np.float32) / half)).astype(np.float32)
    ar = row[:, None] * inv
    ac = col[:, None] * inv
    ang = np.concatenate([ar, ar, ac, ac], axis=-1)
    cos = np.cos(ang).astype(np.float32)
    sin = np.sin(ang).astype(np.float32) * _SGN64[None, :]
    cosT = np.ones((128, TALL), np.float32)
    sinT = np.zeros((128, TALL), np.float32)
    cosT[:, :TQ] = np.tile(cos.T, (2, 1))
    sinT[:, :TQ] = np.tile(sin.T, (2, 1))
    return cosT, sinT


def _na_bias(rpb, R0):
    H = rpb.shape[0]
    out = np.full((5, H, 128, 5, 128), NEG, np.float32)
    kk = np.arange(128)
    qq = np.arange(128)
    for slot, r0 in enumerate((R0, R0 + 2, R0 + 32, R0 + 60, R0 + 62)):
        if slot == 2:
            r0 = 100 if R0 not in (0,) else 100
        qr = r0 + qq // 64
        qc = qq % 64
        rs = np.clip(qr - 4, 0, 256 - 8)
        cs = np.clip(qc - 8, 0, 64 - 16)
        for blk in range(5):
            kr = r0 - 4 + 2 * blk + kk // 64
            kc = kk % 64
            valid = ((kr[:, None] >= rs[None, :]) & (kr[:, None] < rs[None, :] + 8) &
                     (kc[:, None] >= cs[None, :]) & (kc[:, None] < cs[None, :] + 16) &
                     (kr[:, None] >= 0) & (kr[:, None] < 256))
            dr = np.clip(kr[:, None] - qr[None, :] + 7, 0, 14)
            dc = np.clip(kc[:, None] - qc[None, :] + 15, 0, 30)
            vals = rpb[:, dr, dc]
            out[slot, :, :, blk, :] = np.where(valid[None], vals, NEG)
    return out.reshape(5, H, 128, 640)


def _swa_bias(qr):
    out = np.full((3, 128, 3, 128), NEG, np.float32)
    k = np.arange(128)[:, None]
    q = np.arange(128)[None, :]
    for slot, gb in enumerate((qr * 32, qr * 32 + 5, qr * 32 + 31)):
        for blk in range(3):
            kb_ = gb - 1 + blk
            if kb_ < 0 or kb_ >= SEQ // 128:
                continue
            diff = (blk - 1) * 128 + k - q
            out[slot, :, blk, :] = np.where(np.abs(diff) <= 128, 0.0, NEG)
    return out.reshape(3, 128, 384)


def _ones_col(v, nh, dv):
    T = v.shape[0]
    o = np.ones((T, nh, dv + 1), v.dtype)
    o[:, :, :dv] = v.reshape(T, nh, dv)
    return o.reshape(T, nh * (dv + 1))


def _halo(arr, axis, lo, hi):
    n = arr.shape[axis]
    shape = list(arr.shape)
    shape[axis] = hi - lo
    out = np.zeros(shape, arr.dtype)
    s0, s1 = max(lo, 0), min(hi, n)
    src = [slice(None)] * arr.ndim
    dst = [slice(None)] * arr.ndim
    src[axis] = slice(s0, s1)
    dst[axis] = slice(s0 - lo, s1 - lo)
    out[tuple(dst)] = arr[tuple(src)]
    return out


def _run(name, in_maps):
    res = run_bass_kernel_spmd(_prog(name), in_maps, core_ids=list(range(NCORES)))
    return res.results


def kernel(x, c, ctx, c_ctx, w_mod, b_mod, norm1_g, norm2_g, w_in_even, w_out_even, na_rpb, diff_lambda,
           diff_subln_g, w_in_odd, w_out_odd, swa_sink, peer_wq, peer_keys, peer_u, peer_v, final_g, _nlayers=4, _dbg=None):
    f32 = np.float32
    x = np.asarray(x, f32)
    ctx = np.asarray(ctx, f32)
    ident = np.eye(128, dtype=f32)
    xs = []
    for core in range(NCORES):
        b, qr = divmod(core, 4)
        xs.append(np.concatenate([x[b, qr * TQ:(qr + 1) * TQ], ctx[b]], axis=0))
    csT = []
    for core in range(NCORES):
        b = core // 4
        cs = np.stack([np.asarray(c, f32)[b], np.asarray(c_ctx, f32)], 0)
        csT.append(np.ascontiguousarray(cs.reshape(2, 8, 128).transpose(2, 0, 1).reshape(128, 16)))
    ropes = [_rope_tables_T((core % 4) * TQ) for core in range(NCORES)]
    xn = None
    for i in range(_nlayers):
        even = (i % 2 == 0)
        j = i // 2
        w_in = np.asarray(w_in_even[j] if even else w_in_odd[j], f32)
        if even:
            rc = w_in[:, 1536:2560]
        else:
            rc = w_in[:, 0:1280]
        w_perm = np.ascontiguousarray(rc.reshape(D, -1, 64)[:, :, _PERM64].reshape(D, -1))
        ims = []
        for core in range(NCORES):
            ims.append({"x": xs[core], "csT": csT[core], "w_mod": np.asarray(w_mod[i], f32), "b_mod": np.asarray(b_mod[i], f32),
                        "n1g": np.asarray(norm1_g[i], f32), "w_in": w_in, "w_perm": w_perm,
                        "cosT": ropes[core][0], "sinT": ropes[core][1], "c_ident": ident})
        pre = _run("pre_even" if even else "pre_odd", ims)
        fm = [np.asarray(r["fmT"]) for r in pre]
        tm = [np.asarray(r["tm"]) for r in pre]
        if _dbg is not None:
            _dbg[f"pre{i}"] = (fm, tm, [np.asarray(r["modrow"]) for r in pre])
        keysT = np.ascontiguousarray(np.asarray(peer_keys[i], f32).reshape(16, 128, 128).transpose(2, 0, 1))
        common = {"w_out": np.asarray(w_out_even[j] if even else w_out_odd[j], f32), "n2g": np.asarray(norm2_g[i], f32),
                  "fg": np.asarray(final_g, f32), "wq": np.asarray(peer_wq[i], f32), "keysT": keysT,
                  "uT": np.ascontiguousarray(np.asarray(peer_u[i], f32).T), "v": np.asarray(peer_v[i], f32), "c_ident": ident}
        ims = []
        for core in range(NCORES):
            b, qr = divmod(core, 4)
            grp = [b * 4 + k for k in range(4)]
            im = dict(common)
            im["x"] = xs[core]
            im["modrow"] = np.asarray(pre[core]["modrow"])
            if even:
                kT_lat = np.concatenate([fm[g][512:1024, :TQ] for g in grp], axis=1)
                v_lat = np.concatenate([tm[g][:TQ, 0:512] for g in grp], axis=0)
                lo = (qr * 64 - 4) * 64
                im["naqT"] = np.ascontiguousarray(fm[core][0:512])
                im["nakT_h"] = _halo(kT_lat, 1, lo, lo + 74 * 64)
                im["nav_h"] = _ones_col(_halo(v_lat, 0, lo, lo + 74 * 64), 8, 64)
                im["nakT_c"] = np.ascontiguousarray(fm[core][512:1024, TQ:])
                im["nav_c"] = _ones_col(tm[core][TQ:, 0:512], 8, 64)
                im["dqT"] = np.ascontiguousarray(fm[core][1024:1536])
                im["dkT_all"] = np.concatenate([fm[core][1536:2048, TQ:]] + [fm[g][1536:2048, :TQ] for g in grp], axis=1)
                im["dv_all"] = _ones_col(np.concatenate([tm[core][TQ:, 512:1024]] + [tm[g][:TQ, 512:1024] for g in grp], axis=0), 4, 128)
                im["nbias"] = _na_bias(np.asarray(na_rpb[j], f32), qr * 64)
                im["lam"] = np.asarray(diff_lambda[j], f32)
                im["subg"] = np.asarray(diff_subln_g[j], f32)
                li = 0.8 - 0.6 * math.exp(-0.3 * i)
                im["lam_init"] = np.tile(np.array([[li, 1.0 - li]], f32), (128, 1))
            else:
                kT_lat = np.concatenate([fm[g][1024:1280, :TQ] for g in grp], axis=1)
                v_lat = np.concatenate([tm[g][:TQ, :] for g in grp], axis=0)
                lo = qr * TQ - 128
                im["swa_q"] = np.ascontiguousarray(fm[core][0:1024].reshape(16, 64, TALL).transpose(1, 0, 2))
                im["swa_kT_h"] = np.ascontiguousarray(_halo(kT_lat, 1, lo, lo + TQ + 256).reshape(4, 64, TQ + 256).transpose(1, 0, 2))
                im["swa_v_h"] = _ones_col(_halo(v_lat, 0, lo, lo + TQ + 256), 4, 64)
                im["swa_kT_c"] = np.ascontiguousarray(fm[core][1024:1280, TQ:].reshape(4, 64, CTX).transpose(1, 0, 2))
                im["swa_v_c"] = _ones_col(tm[core][TQ:, :], 4, 64)
                im["sbias"] = _swa_bias(qr)
                im["sink"] = np.asarray(swa_sink[j], f32)
            ims.append(im)
        post = _run("post_even" if even else "post_odd", ims)
        xs = [np.asarray(r["x_out"]) for r in post]
        xn = [np.asarray(r["xn_out"]) for r in post]
        if _dbg is not None:
            _dbg[f"x{i}"] = xs
    out = np.zeros((2, SEQ, D), f32)
    for core in range(NCORES):
        b, qr = divmod(core, 4)
        out[b, qr * TQ:(qr + 1) * TQ] = xn[core][:TQ]
    return out


RG = [[0, 1, 2, 3], [4, 5, 6, 7]]


def emit_pre_f(kb, even, x_src, XS, csT, w_mod, b_mod, n1g_in, w_in, w_perm, cosT, sinT, fm_out, FMO, tm_out, TMO, modrow, MRO):
    B = kb.banks
    if even:
        NIN = 3072
        fm_blocks = [(c * 128, None) for c in range(8)] + [(1536 + c * 128, c) for c in range(8)]
        tm_groups = [(1024, 512, 8, 64, 0), (2560, 512, 4, 128, 520)]
    else:
        NIN = 1536
        fm_blocks = [(c * 128, c) for c in range(10)]
        tm_groups = [(1280, 256, 4, 64, 0)]
    NROPE = 128 * sum(1 for _, r in fm_blocks if r is not None)
    ident_f, IDF, ident, ID = kb.ident()
    win, WIN = kb.sb("win", [128, 8, NIN], BF16)
    wpm, WPM = kb.sb("wpm", [128, 8, NROPE], BF16)
    for c0 in range(0, NIN, 512):
        kb.dma("pool", win[:, :, c0:c0 + 512], w_in[:, c0:c0 + 512].rearrange("(k p) n -> p k n", p=128), writes=[WIN])
    for c0 in range(0, NROPE, 512):
        n = min(512, NROPE - c0)
        kb.dma("pool", wpm[:, :, c0:c0 + n], w_perm[:, c0:c0 + n].rearrange("(k p) n -> p k n", p=128), writes=[WPM])
    cs_f, CSF = kb.sb("cs_f", [128, 16], F32)
    cs_b, CSB = kb.sb("cs_b", [128, 16], BF16)
    csbc, CSBC = kb.sb("csbc", [128, 16, 128], BF16)
    kb.dma("sp", cs_f[:], csT, writes=[CSF])
    kb.op("act", lambda e: e.activation(out=cs_b[:], in_=cs_f[:], func=AF.Silu), reads=[CSF], writes=[CSB])
    kb.op("dve", lambda e: e.tensor_copy(out=csbc[:], in_=cs_b[:].unsqueeze(2).to_broadcast([128, 16, 128])),
          reads=[CSB], writes=[CSBC])
    n1g, N1G = kb.sb("n1g_sb", [128, D], F32)
    kb.dma("sp", n1g[:], n1g_in.unsqueeze(0).to_broadcast([128, D]), writes=[N1G])
    g1s, G1S = kb.sb("g1s", [128, 2, 2, D], F32)
    wmr = [kb.sb(f"wmr{i}", [128, 8, 512], BF16) for i in range(2)]
    bmr = [kb.sb(f"bmr{i}", [128, 512], F32) for i in range(2)]
    mtmp = [kb.sb(f"mtmp{i}", [128, 512], F32) for i in range(2)]
    for cgp in range(12):
        wt, WB = wmr[cgp % 2]
        bt, BB = bmr[cgp % 2]
        cols = slice(cgp * 512, (cgp + 1) * 512)
        kb.dma("pool", wt[:], w_mod[:, cols].rearrange("(k p) n -> p k n", p=128), writes=[WB])
        kb.dma("sp", bt[:], b_mod[cols].unsqueeze(0).to_broadcast([128, 512]), writes=[BB])
        chunk = cgp // 2
        half = cgp % 2
        for s in range(2):
            mb, MB = B[6 + s]
            def mmm(e, mb=mb, wt=wt, s=s):
                for k in range(8):
                    ins = e.matmul(mb[:], lhsT=csbc[:, s * 8 + k, :], rhs=wt[:, k, :], start=(k == 0), stop=(k == 7))
                return ins
            kb.op("pe", mmm, reads=[CSBC, WB], writes=[MB])
            if chunk < 2:
                dst = g1s[:, s, chunk, half * 512:(half + 1) * 512]
                kb.op("dve", lambda e, mb=mb, bt=bt, dst=dst: e.tensor_tensor(out=dst, in0=mb[:], in1=bt[:], op=ALU.add),
                      reads=[MB, BB], writes=[G1S])
                kb.dma("sp", modrow[s, chunk:chunk + 1, half * 512:(half + 1) * 512], dst[0:1, :], reads=[G1S], writes=[MRO])
            else:
                mt, MT = mtmp[s]
                kb.op("dve", lambda e, mb=mb, bt=bt, mt=mt: e.tensor_tensor(out=mt[:], in0=mb[:], in1=bt[:], op=ALU.add),
                      reads=[MB, BB], writes=[MT])
                kb.dma("sp", modrow[s, chunk:chunk + 1, half * 512:(half + 1) * 512], mt[0:1, :], reads=[MT], writes=[MRO])
    for s in range(2):
        kb.op("dve", lambda e, s=s: e.scalar_tensor_tensor(out=g1s[:, s, 1, :], in0=g1s[:, s, 1, :], scalar=1.0, in1=n1g[:],
                                                           op0=ALU.add, op1=ALU.mult), reads=[G1S, N1G], writes=[G1S])
    xt = [kb.sb(f"xt{i}", [128, D], F32) for i in range(2)]
    tmp, TMP = kb.sb("tmp", [128, D], F32)
    hx, HX = kb.sb("hx", [128, D], BF16)
    hxT, HXT = kb.sb("hxT", [128, 8, 512], BF16)
    sm, SM = kb.sb("sm", [128, 8], F32)
    cst = [kb.sb(f"cst{i}", [128, 512], F32) for i in range(2)]
    snt = [kb.sb(f"snt{i}", [128, 512], F32) for i in range(2)]
    r1, R1 = kb.sb("r1", [128, 512], F32)
    r2, R2 = kb.sb("r2", [128, 512], F32)
    fmo = [kb.sb(f"fmo{i}", [128, 512], BF16) for i in range(2)]
    tmo = [kb.sb(f"tmo{i}", [128, 520], BF16) for i in range(2)]
    for to, TO in tmo:
        kb.op("pool", lambda e, to=to: e.memset(to[:], 1.0), writes=[TO])
    groups = [(g * 4, 4, 0) for g in range(NTL // 4)] + [(NTL, NTC, 1)]
    xi = fi = ti = 0
    for gi, (t0, ntl, s) in enumerate(groups):
        NTOK = ntl * 128
        tok0 = t0 * 128
        ct, CT = cst[gi % 2]
        st, ST = snt[gi % 2]
        kb.dma("sp", ct[:, 0:NTOK], cosT[:, tok0:tok0 + NTOK], writes=[CT])
        kb.dma("sp", st[:, 0:NTOK], sinT[:, tok0:tok0 + NTOK], writes=[ST])
        for j in range(ntl):
            x_t, XB = xt[xi % 2]
            xi += 1
            rows = slice(tok0 + j * 128, tok0 + (j + 1) * 128)
            kb.dma("sp", x_t[:], x_src[rows, :], reads=[XS], writes=[XB])
            kb.op("act", lambda e, x_t=x_t: e.activation(out=tmp[:], in_=x_t[:], func=AF.Square, accum_out=sm[:, 0:1]),
                  reads=[XB], writes=[TMP, SM])
            rstd_ops(kb, sm, SM)
            kb.op("dve", lambda e, x_t=x_t, s=s: e.scalar_tensor_tensor(out=tmp[:], in0=x_t[:], scalar=sm[:, 1:2], in1=g1s[:, s, 1, :],
                                                                         op0=ALU.mult, op1=ALU.mult), reads=[XB, SM, G1S], writes=[TMP])
            kb.op("pool", lambda e, s=s: e.tensor_tensor(out=hx[:], in0=tmp[:], in1=g1s[:, s, 0, :], op=ALU.add),
                  reads=[TMP, G1S], writes=[HX])
            tb, TB = B[0]
            def tr(e, tb=tb):
                for k in range(8):
                    ins = e.transpose(tb.bitcast(BF16)[:, k * 128:(k + 1) * 128], hx[:, k * 128:(k + 1) * 128], ident[:])
                return ins
            kb.op("pe", tr, reads=[HX, ID], writes=[TB])
            kb.op("act", lambda e, tb=tb, j=j: e.copy(out=hxT[:, :, j * 128:(j + 1) * 128],
                                                      in_=tb.bitcast(BF16)[:, 0:1024].rearrange("p (k t) -> p k t", k=8)),
                  reads=[TB], writes=[HXT])
        for bi, (c0, ridx) in enumerate(fm_blocks):
            pa, PA = B[1 + bi % 2]
            def mma(e, pa=pa, c0=c0, NTOK=NTOK):
                for k in range(8):
                    ins = e.matmul(pa[:, 0:NTOK], lhsT=win[:, k, c0:c0 + 128], rhs=hxT[:, k, 0:NTOK], start=(k == 0), stop=(k == 7))
                return ins
            kb.op("pe", mma, reads=[WIN, HXT], writes=[PA])
            fo, FO = fmo[fi % 2]
            fi += 1
            if ridx is None:
                kb.op("act", lambda e, pa=pa, fo=fo, NTOK=NTOK: e.copy(out=fo[:, 0:NTOK], in_=pa[:, 0:NTOK]), reads=[PA], writes=[FO])
            else:
                pb, PB = B[3 + bi % 2]
                def mmb(e, pb=pb, ridx=ridx, NTOK=NTOK):
                    for k in range(8):
                        ins = e.matmul(pb[:, 0:NTOK], lhsT=wpm[:, k, ridx * 128:(ridx + 1) * 128], rhs=hxT[:, k, 0:NTOK],
                                       start=(k == 0), stop=(k == 7))
                    return ins
                kb.op("pe", mmb, reads=[WPM, HXT], writes=[PB])
                kb.op("dve", lambda e, pa=pa, ct=ct, NTOK=NTOK: e.tensor_tensor(out=r1[:, 0:NTOK], in0=pa[:, 0:NTOK], in1=ct[:, 0:NTOK], op=ALU.mult),
                      reads=[PA, CT], writes=[R1])
                kb.op("dve", lambda e, pb=pb, st=st, NTOK=NTOK: e.tensor_tensor(out=r2[:, 0:NTOK], in0=pb[:, 0:NTOK], in1=st[:, 0:NTOK], op=ALU.mult),
                      reads=[PB, ST], writes=[R2])
                kb.op("pool", lambda e, fo=fo, NTOK=NTOK: e.tensor_tensor(out=fo[:, 0:NTOK], in0=r1[:, 0:NTOK], in1=r2[:, 0:NTOK], op=ALU.add),
                      reads=[R1, R2], writes=[FO])
            kb.dma("sp", fm_out[bi * 128:(bi + 1) * 128, tok0:tok0 + NTOK], fo[:, 0:NTOK], reads=[FO], writes=[FMO])
        for j in range(ntl):
            for (c0, ncol, nh, dv, oc) in tm_groups:
                pt, PT = B[5 + ti % 2]
                to, TO = tmo[ti % 2]
                ti += 1
                def mmt(e, pt=pt, c0=c0, ncol=ncol, j=j):
                    for k in range(8):
                        ins = e.matmul(pt[:, 0:ncol], lhsT=hxT[:, k, j * 128:(j + 1) * 128], rhs=win[:, k, c0:c0 + ncol],
                                       start=(k == 0), stop=(k == 7))
                    return ins
                kb.op("pe", mmt, reads=[HXT, WIN], writes=[PT])
                wdt = nh * (dv + 1)
                kb.op("act", lambda e, pt=pt, to=to, ncol=ncol, nh=nh, dv=dv, wdt=wdt: e.copy(
                    out=to[:, 0:wdt].rearrange("p (h c) -> p h c", c=dv + 1)[:, :, 0:dv],
                    in_=pt[:, 0:ncol].rearrange("p (h c) -> p h c", c=dv)), reads=[PT], writes=[TO])
                rows = slice(tok0 + j * 128, tok0 + (j + 1) * 128)
                kb.dma("sp", tm_out[rows, oc:oc + wdt], to[:, 0:wdt], reads=[TO], writes=[TMO])


NA_NBMAX = 12


def _na_blocklist(qt):
    if qt == 0:
        return 0, 4, [("tail", 0), ("tail", 1)]
    if qt == 1:
        return 0, 4, [("tail", 1)]
    if qt == NTL - 2:
        return TQ - 512, 4, [("head", 0)]
    if qt == NTL - 1:
        return TQ - 512, 4, [("head", 0), ("head", 1)]
    return qt * 128 - 256, 5, []


def emit_attn_even_f(kb, fmT, FMT, tmv, TMV, dk_recv, DKR, dv_recv, DVR, nbk_recv, NBKR, nbv_recv, NBVR,
                     nbias, lam_in, subg_in, lami_in, ao, AO):
    B = kb.banks
    sc = 64 ** -0.5
    lm, LM = kb.sb("lm", [128, 4, 64], F32)
    lms, LMS = kb.sb("lms", [128, 16], F32)
    subg, SUBG = kb.sb("subg_sb", [128, 128], F32)
    kb.dma("sp", lm[:].rearrange("p a b -> p (a b)"), lam_in.rearrange("a b -> (a b)").unsqueeze(0).to_broadcast([128, 256]), writes=[LM])
    kb.dma("sp", subg[:], subg_in.unsqueeze(0).to_broadcast([128, 128]), writes=[SUBG])
    kb.dma("sp", lms[:, 8:10], lami_in, writes=[LMS])
    kb.op("dve", lambda e: e.tensor_tensor(out=lm[:, 0, :], in0=lm[:, 0, :], in1=lm[:, 1, :], op=ALU.mult), reads=[LM], writes=[LM])
    kb.op("dve", lambda e: e.tensor_tensor(out=lm[:, 2, :], in0=lm[:, 2, :], in1=lm[:, 3, :], op=ALU.mult), reads=[LM], writes=[LM])
    kb.op("dve", lambda e: e.tensor_reduce(out=lms[:, 0:1], in_=lm[:, 0, :], axis=AX.X, op=ALU.add), reads=[LM], writes=[LMS])
    kb.op("dve", lambda e: e.tensor_reduce(out=lms[:, 1:2], in_=lm[:, 2, :], axis=AX.X, op=ALU.add), reads=[LM], writes=[LMS])
    kb.op("act", lambda e: e.activation(out=lms[:, 2:4], in_=lms[:, 0:2], func=AF.Exp), reads=[LMS], writes=[LMS])
    kb.op("dve", lambda e: e.tensor_tensor(out=lms[:, 4:5], in0=lms[:, 2:3], in1=lms[:, 3:4], op=ALU.subtract), reads=[LMS], writes=[LMS])
    kb.op("dve", lambda e: e.tensor_tensor(out=lms[:, 5:6], in0=lms[:, 4:5], in1=lms[:, 8:9], op=ALU.add), reads=[LMS], writes=[LMS])
    kb.op("dve", lambda e: e.tensor_scalar(out=lms[:, 6:7], in0=lms[:, 5:6], scalar1=-1.0, scalar2=None, op0=ALU.mult), reads=[LMS], writes=[LMS])
    kb.op("dve", lambda e: e.tensor_scalar(out=subg[:], in0=subg[:], scalar1=lms[:, 9:10], scalar2=None, op0=ALU.mult), reads=[SUBG, LMS], writes=[SUBG])

    naqT = fmT[0:512, :]
    nakT = fmT[512:1024, :]
    kcT, KCT = kb.sb("na_kcT", [128, 4, CTX], BF16)
    vca, VCA = kb.sb("na_vca", [128, 2, 8, 65], BF16)
    kb.dma("sp", kcT[:], nakT[:, TQ:TALL].rearrange("(a p) n -> p a n", p=128), reads=[FMT], writes=[KCT])
    kb.dma("sp", vca[:].rearrange("p b h c -> p b (h c)"), tmv[TQ:TALL, 0:520].rearrange("(b p) c -> p b c", p=128), reads=[TMV], writes=[VCA])
    kts = [kb.sb(f"na_kt{i}", [128, 4, NA_NBMAX * 128], BF16) for i in range(2)]
    vts = [kb.sb(f"na_vt{i}", [128, NA_NBMAX, 8, 65], BF16) for i in range(2)]
    qts = [kb.sb(f"na_qt{i}", [128, 4, 128], BF16) for i in range(2)]
    bts = [kb.sb(f"na_bt{i}", [128, NA_NBMAX * 128], F32) for i in range(2)]
    sts = [kb.sb(f"na_st{i}", [128, NA_NBMAX * 128], F32) for i in range(2)]
    pts = [kb.sb(f"na_pt{i}", [128, (NA_NBMAX + 2) * 128], BF16) for i in range(2)]
    aot = [kb.sb(f"na_ao{i}", [128, 512], BF16) for i in range(2)]
    rc, RC = kb.sb("na_rc", [128, 8], F32)
    hi = 0
    bk = 0
    for qt in range(NT):
        isctx = qt >= NTL
        q_t, QB = qts[qt % 2]
        kb.dma("sp", q_t[:], naqT[:, qt * 128:(qt + 1) * 128].rearrange("(a p) n -> p a n", p=128), reads=[FMT], writes=[QB])
        nb = 0
        if not isctx:
            k_t, KB_ = kts[qt % 2]
            v_t, VB = vts[qt % 2]
            o0, onb, cands = _na_blocklist(qt)
            kb.dma("sp", k_t[:, :, 0:onb * 128], nakT[:, o0:o0 + onb * 128].rearrange("(a p) n -> p a n", p=128), reads=[FMT], writes=[KB_])
            kb.dma("sp", v_t[:, 0:onb].rearrange("p b h c -> p b (h c)"), tmv[o0:o0 + onb * 128, 0:520].rearrange("(b p) c -> p b c", p=128),
                   reads=[TMV], writes=[VB])
            nb = onb
            ncb = len(cands)
            if ncb:
                which = cands[0][0]
                cb0 = cands[0][1]
                col0 = (0 if which == "tail" else 256) + cb0 * 128
                for r in range(4):
                    kb.dma("sp", k_t[:, :, nb * 128:(nb + ncb) * 128],
                           nbk_recv[r * 512:(r + 1) * 512, col0:col0 + ncb * 128].rearrange("(a p) n -> p a n", p=128), reads=[NBKR], writes=[KB_])
                    kb.dma("sp", v_t[:, nb:nb + ncb].rearrange("p b h c -> p b (h c)"),
                           nbv_recv[r * 512 + col0:r * 512 + col0 + ncb * 128, :].rearrange("(b p) c -> p b c", p=128), reads=[NBVR], writes=[VB])
                    nb += ncb
            slot = 0 if qt == 0 else 1 if qt == 1 else 3 if qt == NTL - 2 else 4 if qt == NTL - 1 else 2
        a_t, AB = aot[qt % 2]
        for h in range(8):
            a, off = h // 2, (h % 2) * 64
            st_, STB = sts[hi % 2]
            pt_, PTB = pts[hi % 2]
            acc, ACC = B[4 + (h // 4)]
            hi += 1
            if not isctx:
                b_t, BB = bts[hi % 2]
                kb.dma("sp", b_t[:, 0:nb * 128], nbias[slot, h, :, 0:nb * 128], writes=[BB])
                for c0 in range(0, nb, 4):
                    cn = min(4, nb - c0)
                    ps, PS = B[bk % 4]
                    bk += 1
                    def mms(e, ps=ps, k_t=k_t, q_t=q_t, a=a, off=off, c0=c0, cn=cn):
                        for i in range(cn):
                            ins = e.matmul(ps[:, i * 128:(i + 1) * 128], lhsT=k_t[off:off + 64, a, (c0 + i) * 128:(c0 + i + 1) * 128],
                                           rhs=q_t[off:off + 64, a, :], start=True, stop=True)
                        return ins
                    kb.op("pe", mms, reads=[KB_, QB], writes=[PS])
                    kb.op("dve", lambda e, ps=ps, st_=st_, b_t=b_t, c0=c0, cn=cn: e.scalar_tensor_tensor(
                        out=st_[:, c0 * 128:(c0 + cn) * 128], in0=ps[:, 0:cn * 128], scalar=sc, in1=b_t[:, c0 * 128:(c0 + cn) * 128],
                        op0=ALU.mult, op1=ALU.add), reads=[PS, BB], writes=[STB])
                kb.op("act", lambda e, st_=st_, pt_=pt_, nb=nb: e.activation(out=pt_[:, 0:nb * 128], in_=st_[:, 0:nb * 128], func=AF.Exp),
                      reads=[STB], writes=[PTB])
            ps, PS = B[bk % 4]
            bk += 1
            def mmc(e, ps=ps, q_t=q_t, a=a, off=off):
                for cb in range(2):
                    ins = e.matmul(ps[:, cb * 128:(cb + 1) * 128], lhsT=kcT[off:off + 64, a, cb * 128:(cb + 1) * 128],
                                   rhs=q_t[off:off + 64, a, :], start=True, stop=True)
                return ins
            kb.op("pe", mmc, reads=[QB, KCT], writes=[PS])
            kb.op("act", lambda e, ps=ps, pt_=pt_, nb=nb: e.activation(out=pt_[:, nb * 128:(nb + 2) * 128], in_=ps[:, 0:256], func=AF.Exp, scale=sc),
                  reads=[PS], writes=[PTB])
            if not isctx:
                def mmv(e, acc=acc, pt_=pt_, v_t=v_t, h=h, nb=nb):
                    o = acc[:, (h % 4) * 65:(h % 4) * 65 + 65]
                    for blk in range(nb):
                        e.matmul(o, lhsT=pt_[:, blk * 128:(blk + 1) * 128], rhs=v_t[:, blk, h, :], start=(blk == 0), stop=False)
                    for cb in range(2):
                        ins = e.matmul(o, lhsT=pt_[:, (nb + cb) * 128:(nb + cb + 1) * 128], rhs=vca[:, cb, h, :], start=False, stop=(cb == 1))
                    return ins
                kb.op("pe", mmv, reads=[PTB, VB, VCA], writes=[ACC])
            else:
                def mmv(e, acc=acc, pt_=pt_, h=h):
                    o = acc[:, (h % 4) * 65:(h % 4) * 65 + 65]
                    for cb in range(2):
                        ins = e.matmul(o, lhsT=pt_[:, cb * 128:(cb + 1) * 128], rhs=vca[:, cb, h, :], start=(cb == 0), stop=(cb == 1))
                    return ins
                kb.op("pe", mmv, reads=[PTB, VCA], writes=[ACC])
            if h % 4 == 3:
                g4 = h // 4
                av = acc[:, 0:260].rearrange("p (h c) -> p h c", c=65)
                kb.op("dve", lambda e, av=av, g4=g4: e.reciprocal(out=rc[:, g4 * 4:g4 * 4 + 4], in_=av[:, :, 64]), reads=[ACC], writes=[RC])
                kb.op("dve", lambda e, av=av, g4=g4, a_t=a_t: e.tensor_tensor(
                    out=a_t[:, g4 * 256:(g4 + 1) * 256].rearrange("p (h d) -> p h d", d=64), in0=av[:, :, 0:64],
                    in1=rc[:, g4 * 4:g4 * 4 + 4].unsqueeze(2).to_broadcast([128, 4, 64]), op=ALU.mult), reads=[ACC, RC], writes=[AB])
        kb.dma("sp", ao[qt * 128:(qt + 1) * 128, 0:512], a_t[:], reads=[AB], writes=[AO])

    NBLK = (CTX + SEQ) // 128
    dk, DK = kb.sb("d_k", [128, CTX + SEQ], BF16)
    dva, DVA = kb.sb("d_va", [128, NBLK, 129], BF16)
    dq, DQ = kb.sb("d_q", [128, TALL], BF16)
    dpt = [kb.sb(f"d_pt{i}", [128, 256], BF16) for i in range(3)]
    o0_, O0 = kb.sb("d_o0", [128, 128], F32)
    o1, O1 = kb.sb("d_o1", [128, 128], F32)
    osq, OSQ = kb.sb("d_osq", [128, 128], F32)
    dsm, DSM = kb.sb("d_sm", [128, 8], F32)
    dob = [kb.sb(f"d_ob{i}", [128, 128], BF16) for i in range(2)]
    si = 0
    oi = 0
    for h in range(4):
        kb.dma("sp", dk[:, 0:CTX], fmT[1536 + h * 128:1536 + (h + 1) * 128, TQ:TALL], reads=[FMT], writes=[DK])
        for r in range(4):
            for k in range(4):
                kb.dma("sp", dk[:, CTX + r * TQ + k * 1024:CTX + r * TQ + (k + 1) * 1024],
                       dk_recv[k][0][r * 512 + h * 128:r * 512 + (h + 1) * 128, :], reads=[dk_recv[k][1]], writes=[DK])
        kb.dma("sp", dva[:, 0:2, :], tmv[TQ:TALL, 520 + h * 129:520 + (h + 1) * 129].rearrange("(b p) d -> p b d", p=128), reads=[TMV], writes=[DVA])
        for r in range(4):
            for k in range(8):
                b0 = 2 + (r * TQ + k * 512) // 128
                kb.dma("sp", dva[:, b0:b0 + 4, :], dv_recv[k][0][r * 512:(r + 1) * 512, h * 129:(h + 1) * 129].rearrange("(b p) d -> p b d", p=128),
                       reads=[dv_recv[k][1]], writes=[DVA])
        kb.dma("sp", dq[:], fmT[1024 + h * 128:1024 + (h + 1) * 128, :], reads=[FMT], writes=[DQ])
        qgroups = [(g * 256, 256, NBLK) for g in range(TQ // 256)] + [(TQ, CTX, CTX // 128)]
        for (q0, nq, nblk) in qgroups:
            nqs = nq // 128
            steps = [(blk, m) for blk in range(nblk) for m in range(2)]
            LOOK = 2
            bufs = {}
            for idx in range(len(steps) + LOOK):
                if idx < len(steps):
                    blk, m = steps[idx]
                    ps, PS = B[si % 3]
                    pt_, PTB = dpt[si % 3]
                    si += 1
                    bufs[idx] = (pt_, PTB)
                    kb.op("pe", lambda e, ps=ps, m=m, blk=blk, q0=q0, nq=nq: e.matmul(
                        ps[:, 0:nq], lhsT=dk[m * 64:(m + 1) * 64, blk * 128:(blk + 1) * 128], rhs=dq[m * 64:(m + 1) * 64, q0:q0 + nq],
                        start=True, stop=True), reads=[DK, DQ], writes=[PS])
                    kb.op("act", lambda e, ps=ps, pt_=pt_, nq=nq: e.activation(out=pt_[:, 0:nq], in_=ps[:, 0:nq], func=AF.Exp, scale=sc),
                          reads=[PS], writes=[PTB])
                if idx - LOOK >= 0:
                    blk, m = steps[idx - LOOK]
                    pt_, PTB = bufs.pop(idx - LOOK)
                    def mmv(e, pt_=pt_, m=m, blk=blk, nqs=nqs, nblk=nblk):
                        for qs in range(nqs):
                            acc = B[3 + qs * 2 + m][0]
                            ins = e.matmul(acc[:, 0:129], lhsT=pt_[:, qs * 128:(qs + 1) * 128], rhs=dva[:, blk, :],
                                           start=(blk == 0), stop=(blk == nblk - 1))
                        return ins
                    kb.op("pe", mmv, reads=[PTB, DVA], writes=[B[3 + qs * 2 + m][1] for qs in range(nqs)])
            for qs in range(nqs):
                (a0, A0), (a1, A1) = [(B[3 + qs * 2 + m][0][:, 0:129], B[3 + qs * 2 + m][1]) for m in range(2)]
                kb.op("dve", lambda e, a0=a0: e.reciprocal(out=dsm[:, 0:1], in_=a0[:, 128:129]), reads=[A0], writes=[DSM])
                kb.op("dve", lambda e, a1=a1: e.reciprocal(out=dsm[:, 1:2], in_=a1[:, 128:129]), reads=[A1], writes=[DSM])
                kb.op("dve", lambda e: e.tensor_tensor(out=dsm[:, 1:2], in0=dsm[:, 1:2], in1=lms[:, 6:7], op=ALU.mult), reads=[DSM, LMS], writes=[DSM])
                kb.op("dve", lambda e, a0=a0: e.tensor_scalar(out=o0_[:], in0=a0[:, 0:128], scalar1=dsm[:, 0:1], scalar2=None, op0=ALU.mult),
                      reads=[A0, DSM], writes=[O0])
                kb.op("dve", lambda e, a1=a1: e.scalar_tensor_tensor(out=o1[:], in0=a1[:, 0:128], scalar=dsm[:, 1:2], in1=o0_[:],
                                                                   op0=ALU.mult, op1=ALU.add), reads=[A1, DSM, O0], writes=[O1])
                kb.op("act", lambda e: e.activation(out=osq[:], in_=o1[:], func=AF.Square, accum_out=dsm[:, 2:3]), reads=[O1], writes=[OSQ, DSM])
                kb.op("dve", lambda e: e.tensor_scalar(out=dsm[:, 3:4], in0=dsm[:, 2:3], scalar1=1.0 / 128, scalar2=EPS, op0=ALU.mult, op1=ALU.add),
                      reads=[DSM], writes=[DSM])
                kb.op("act", lambda e: e.activation(out=dsm[:, 4:5], in_=dsm[:, 3:4], func=AF.Sqrt), reads=[DSM], writes=[DSM])
                kb.op("dve", lambda e: e.reciprocal(out=dsm[:, 5:6], in_=dsm[:, 4:5]), reads=[DSM], writes=[DSM])
                ob, OB = dob[oi % 2]
                oi += 1
                kb.op("dve", lambda e, ob=ob: e.scalar_tensor_tensor(out=ob[:], in0=o1[:], scalar=dsm[:, 5:6], in1=subg[:], op0=ALU.mult, op1=ALU.mult),
                      reads=[O1, DSM, SUBG], writes=[OB])
                r0 = q0 + qs * 128
                kb.dma("sp", ao[r0:r0 + 128, 512 + h * 128:512 + (h + 1) * 128], ob[:], reads=[OB], writes=[AO])


def emit_attn_odd_f(kb, fmT, FMT, tmv, TMV, sbk_recv, SBKR, sbv_recv, SBVR, sbias, sink_in, ao, AO):
    B = kb.banks
    sc = 64 ** -0.5
    snk, SNK = kb.sb("snk", [128, 16], F32)
    kb.dma("sp", snk[:], sink_in.unsqueeze(0).to_broadcast([128, 16]), writes=[SNK])
    kb.op("act", lambda e: e.activation(out=snk[:], in_=snk[:], func=AF.Exp), reads=[SNK], writes=[SNK])
    kT, KT = kb.sb("s_kT", [64, 4, TQ], BF16)
    kb.dma("sp", kT[:], fmT[1024:1280, 0:TQ].rearrange("(n d) t -> d n t", d=64), reads=[FMT], writes=[KT])
    kcT, KCT = kb.sb("s_kcT", [64, 4, CTX], BF16)
    kb.dma("sp", kcT[:], fmT[1024:1280, TQ:TALL].rearrange("(n d) t -> d n t", d=64), reads=[FMT], writes=[KCT])
    ck, CK = kb.sb("s_ck", [64, 4, 4, 256], BF16)
    for r in range(4):
        kb.dma("sp", ck[:, r], sbk_recv[r * 256:(r + 1) * 256, :].rearrange("(n d) t -> d n t", d=64), reads=[SBKR], writes=[CK])
    va, VA = kb.sb("s_va", [128, NTL, 4, 65], BF16)
    kb.dma("sp", va[:].rearrange("p b h c -> p b (h c)"), tmv[0:TQ, 0:260].rearrange("(b p) c -> p b c", p=128), reads=[TMV], writes=[VA])
    vca, VCA = kb.sb("s_vca", [128, 2, 4, 65], BF16)
    kb.dma("sp", vca[:].rearrange("p b h c -> p b (h c)"), tmv[TQ:TALL, 0:260].rearrange("(b p) c -> p b c", p=128), reads=[TMV], writes=[VCA])
    cv, CV = kb.sb("s_cv", [128, 8, 4, 65], BF16)
    kb.dma("sp", cv[:].rearrange("p b h c -> p b (h c)"), sbv_recv.rearrange("(b p) c -> p b c", p=128), reads=[SBVR], writes=[CV])
    sb_, SBB = kb.sb("s_bias", [128, 3, 768], F32)
    kb.dma("sp", sb_[:], sbias.rearrange("s p n -> p s n"), writes=[SBB])
    qts = [kb.sb(f"s_q{i}", [64, 16, 128], BF16) for i in range(2)]
    sts = [kb.sb(f"s_st{i}", [128, 512], F32) for i in range(2)]
    pts = [kb.sb(f"s_pt{i}", [128, 8, 512], BF16) for i in range(2)]
    aot = [kb.sb(f"s_ao{i}", [128, D], BF16) for i in range(2)]
    den, DEN = kb.sb("s_den", [128, 8], F32)
    si = 0
    gi = 0
    for qt in range(NT):
        isctx = qt >= NTL
        q_t, QB = qts[qt % 2]
        kb.dma("sp", q_t[:], fmT[0:1024, qt * 128:(qt + 1) * 128].rearrange("(h d) t -> d h t", d=64), reads=[FMT], writes=[QB])
        a_t, AB = aot[qt % 2]
        if isctx:
            nbl = []
            slot = 1
        elif qt == 0:
            nbl = [("own", 0), ("own", 1)] + [("cand", r, 0) for r in range(4)]
            slot = 0
        elif qt == NTL - 1:
            nbl = [("own", NTL - 2), ("own", NTL - 1)] + [("cand", r, 1) for r in range(4)]
            slot = 2
        else:
            nbl = [("own", qt - 1), ("own", qt), ("own", qt + 1)]
            slot = 1
        nnb = len(nbl)
        for n in range(4):
            pt_, PTB = pts[gi % 2]
            acc, ACC = B[4 + gi % 2]
            gi += 1
            for bi, bl in enumerate(nbl + [("ctx", 0), ("ctx", 1)]):
                ps, PS = B[si % 3]
                st_, STB = sts[si % 2]
                si += 1
                if bl[0] == "own":
                    lhsT = kT[:, n, bl[1] * 128:(bl[1] + 1) * 128]
                    rd = [KT, QB]
                elif bl[0] == "cand":
                    lhsT = ck[:, bl[1], n, bl[2] * 128:(bl[2] + 1) * 128]
                    rd = [CK, QB]
                else:
                    lhsT = kcT[:, n, bl[1] * 128:(bl[1] + 1) * 128]
                    rd = [KCT, QB]
                kb.op("pe", lambda e, ps=ps, lhsT=lhsT, n=n, q_t=q_t: e.matmul(ps[:], lhsT=lhsT, rhs=q_t[:, n * 4:(n + 1) * 4, :], start=True, stop=True),
                      reads=rd, writes=[PS])
                if bl[0] != "ctx":
                    kb.op("dve", lambda e, ps=ps, st_=st_, bi=bi, slot=slot: e.scalar_tensor_tensor(
                        out=st_[:].rearrange("p (g q) -> p g q", g=4), in0=ps[:].rearrange("p (g q) -> p g q", g=4), scalar=sc,
                        in1=sb_[:, slot, bi * 128:(bi + 1) * 128].unsqueeze(1).to_broadcast([128, 4, 128]), op0=ALU.mult, op1=ALU.add),
                        reads=[PS, SBB], writes=[STB])
                    kb.op("act", lambda e, st_=st_, pt_=pt_, bi=bi: e.activation(out=pt_[:, bi, :], in_=st_[:], func=AF.Exp), reads=[STB], writes=[PTB])
                else:
                    kb.op("act", lambda e, ps=ps, pt_=pt_, bi=bi: e.activation(out=pt_[:, bi, :], in_=ps[:], func=AF.Exp, scale=sc),
                          reads=[PS], writes=[PTB])
            allb = nbl + [("ctx", 0), ("ctx", 1)]
            def mmv(e, acc=acc, pt_=pt_, n=n, allb=allb):
                for g in range(4):
                    o = acc[:, g * 65:(g + 1) * 65]
                    for i, bl in enumerate(allb):
                        if bl[0] == "own":
                            rhs = va[:, bl[1], n, :]
                        elif bl[0] == "cand":
                            rhs = cv[:, bl[1] * 2 + bl[2], n, :]
                        else:
                            rhs = vca[:, bl[1], n, :]
                        ins = e.matmul(o, lhsT=pt_[:, i, g * 128:(g + 1) * 128], rhs=rhs, start=(i == 0), stop=(i == len(allb) - 1))
                return ins
            kb.op("pe", mmv, reads=[PTB, VA, VCA, CV], writes=[ACC])
            av = acc[:, 0:260].rearrange("p (g c) -> p g c", c=65)
            kb.op("dve", lambda e, av=av, n=n: e.tensor_tensor(out=den[:, 0:4], in0=av[:, :, 64], in1=snk[:, n * 4:(n + 1) * 4], op=ALU.add),
                  reads=[ACC, SNK], writes=[DEN])
            kb.op("dve", lambda e: e.reciprocal(out=den[:, 4:8], in_=den[:, 0:4]), reads=[DEN], writes=[DEN])
            kb.op("dve", lambda e, av=av, n=n, a_t=a_t: e.tensor_tensor(
                out=a_t[:, n * 256:(n + 1) * 256].rearrange("p (g d) -> p g d", d=64), in0=av[:, :, 0:64],
                in1=den[:, 4:8].unsqueeze(2).to_broadcast([128, 4, 64]), op=ALU.mult), reads=[ACC, DEN], writes=[AB])
        kb.dma("sp", ao[qt * 128:(qt + 1) * 128, :], a_t[:], reads=[AB], writes=[AO])


def build_fused(nlayers=4):
    kb = KB()
    kb.psum_banks()
    nc = kb.nc

    def scratch(name, shape, dt=BF16):
        return nc.dram_tensor(name, list(shape), dt).ap(), Buf(name)

    x_ext = kb.din("x", [TALL, D])
    csT = kb.din("csT", [128, 16])
    cosT = kb.din("cosT", [128, TALL])
    sinT = kb.din("sinT", [128, TALL])
    fg = kb.din("fg", [D])
    xn_out, XN = kb.dout("xn_out", [TALL, D])
    xbufs = [scratch(f"xs{i}", [TALL, D], F32) for i in range(2)]
    ao, AO = scratch("ao_scr", [TALL, D])
    fmT, FMT = scratch("fmT", [2048, TALL])
    tmv, TMV = scratch("tmv", [TALL, 1036])
    dk_send = [scratch(f"dk_send{k}", [512, 1024]) for k in range(4)]
    dk_recv = [scratch(f"dk_recv{k}", [2048, 1024]) for k in range(4)]
    dv_send = [scratch(f"dv_send{k}", [512, 516]) for k in range(8)]
    dv_recv = [scratch(f"dv_recv{k}", [2048, 516]) for k in range(8)]
    nbk_send, NBKS = scratch("nbk_send", [512, 512])
    nbk_recv, NBKR = scratch("nbk_recv", [2048, 512])
    nbv_send, NBVS = scratch("nbv_send", [512, 520])
    nbv_recv, NBVR = scratch("nbv_recv", [2048, 520])
    sbk_send, SBKS = scratch("sbk_send", [256, 256])
    sbk_recv, SBKR = scratch("sbk_recv", [1024, 256])
    sbv_send, SBVS = scratch("sbv_send", [256, 260])
    sbv_recv, SBVR = scratch("sbv_recv", [1024, 260])
    x_src, XS = x_ext, Buf("x_ext")
    lw = []
    for i in range(nlayers):
        d = {"w_out": kb.din(f"w_out{i}", [D, D]), "wq": kb.din(f"wq{i}", [D, 2048]),
             "uT": kb.din(f"uT{i}", [D, 16384]), "v": kb.din(f"v{i}", [16384, D])}
        d["conv"] = {"wout": scratch(f"woutb{i}", [2, 128, 4096]), "wq": scratch(f"wqb{i}", [4, 128, 4096]),
                     "uT": scratch(f"uTb{i}", [32, 128, 4096]), "v": scratch(f"vb{i}", [32, 128, 4096])}
        lw.append(d)

    def convert(i):
        d = lw[i]
        c = d["conv"]
        for hf in range(2):
            kb.dma("pool", c["wout"][0][hf].rearrange("p (k n) -> p k n", k=8),
                   d["w_out"][:, hf * 512:(hf + 1) * 512].rearrange("(k p) n -> p k n", p=128), writes=[c["wout"][1]])
        for g in range(4):
            kb.dma("pool", c["wq"][0][g].rearrange("p (k n) -> p k n", k=8),
                   d["wq"][:, g * 512:(g + 1) * 512].rearrange("(k p) n -> p k n", p=128), writes=[c["wq"][1]])
        for cg in range(32):
            kb.dma("pool", c["uT"][0][cg].rearrange("p (k n) -> p k n", k=8),
                   d["uT"][:, cg * 512:(cg + 1) * 512].rearrange("(k p) n -> p k n", p=128), writes=[c["uT"][1]])
            kb.dma("pool", c["v"][0][cg].rearrange("p (c d) -> p c d", c=4),
                   d["v"][cg * 512:(cg + 1) * 512, :].rearrange("(c p) d -> p c d", p=128), writes=[c["v"][1]])

    for i in range(nlayers):
        even = (i % 2 == 0)
        j = i // 2
        NIN = 3072 if even else 1536
        NROPE = 1024 if even else 1280
        w_mod = kb.din(f"w_mod{i}", [D, 6 * D])
        b_mod = kb.din(f"b_mod{i}", [6 * D])
        n1g = kb.din(f"n1g{i}", [D])
        n2g = kb.din(f"n2g{i}", [D])
        w_in = kb.din(f"w_in{i}", [D, NIN])
        w_perm = kb.din(f"w_perm{i}", [D, NROPE])
        w_out, wq, uT, v = lw[i]["w_out"], lw[i]["wq"], lw[i]["uT"], lw[i]["v"]
        keysT = kb.din(f"keysT{i}", [128, 16, 128])
        modrow, MRO = scratch(f"modrow{i}", [2, 6, D], F32)
        with kb.scope(f"L{i}a_"):
            if i == 0 and USE_CONV:
                convert(0)
            emit_pre_f(kb, even, x_src, XS, csT, w_mod, b_mod, n1g, w_in, w_perm, cosT, sinT, fmT, FMT, tmv, TMV, modrow, MRO)
        if even:
            nbias = kb.din(f"nbias{j}", [5, 8, 128, NA_NBMAX * 128])
            lam = kb.din(f"lam{j}", [4, 64])
            subg = kb.din(f"subg{j}", [128])
            lami = kb.din(f"lam_init{j}", [128, 2])
            for k in range(4):
                kb.dma("sp", dk_send[k][0], fmT[1536:2048, k * 1024:(k + 1) * 1024], reads=[FMT], writes=[dk_send[k][1]])
            for k in range(8):
                kb.dma("sp", dv_send[k][0], tmv[k * 512:(k + 1) * 512, 520:1036], reads=[TMV], writes=[dv_send[k][1]])
            kb.dma("sp", nbk_send[:, 0:256], fmT[512:1024, TQ - 256:TQ], reads=[FMT], writes=[NBKS])
            kb.dma("sp", nbk_send[:, 256:512], fmT[512:1024, 0:256], reads=[FMT], writes=[NBKS])
            kb.dma("sp", nbv_send[0:256, :], tmv[TQ - 256:TQ, 0:520], reads=[TMV], writes=[NBVS])
            kb.dma("sp", nbv_send[256:512, :], tmv[0:256, 0:520], reads=[TMV], writes=[NBVS])
            for k in range(4):
                kb.cc("AllGather", RG, dk_send[k][0], dk_recv[k][0], reads=[dk_send[k][1]], writes=[dk_recv[k][1]])
            for k in range(8):
                kb.cc("AllGather", RG, dv_send[k][0], dv_recv[k][0], reads=[dv_send[k][1]], writes=[dv_recv[k][1]])
            kb.cc("AllGather", RG, nbk_send, nbk_recv, reads=[NBKS], writes=[NBKR])
            kb.cc("AllGather", RG, nbv_send, nbv_recv, reads=[NBVS], writes=[NBVR])
            with kb.scope(f"L{i}b_"):
                emit_attn_even_f(kb, fmT, FMT, tmv, TMV, dk_recv, None, dv_recv, None, nbk_recv, NBKR, nbv_recv, NBVR,
                                 nbias, lam, subg, lami, ao, AO)
        else:
            sbias = kb.din(f"sbias{j}", [3, 128, 768])
            sink = kb.din(f"sink{j}", [16])
            kb.dma("sp", sbk_send[:, 0:128], fmT[1024:1280, TQ - 128:TQ], reads=[FMT], writes=[SBKS])
            kb.dma("sp", sbk_send[:, 128:256], fmT[1024:1280, 0:128], reads=[FMT], writes=[SBKS])
            kb.dma("sp", sbv_send[0:128, :], tmv[TQ - 128:TQ, 0:260], reads=[TMV], writes=[SBVS])
            kb.dma("sp", sbv_send[128:256, :], tmv[0:128, 0:260], reads=[TMV], writes=[SBVS])
            kb.cc("AllGather", RG, sbk_send, sbk_recv, reads=[SBKS], writes=[SBKR])
            kb.cc("AllGather", RG, sbv_send, sbv_recv, reads=[SBVS], writes=[SBVR])
            with kb.scope(f"L{i}b_"):
                emit_attn_odd_f(kb, fmT, FMT, tmv, TMV, sbk_recv, SBKR, sbv_recv, SBVR, sbias, sink, ao, AO)
        x_dst, XD = xbufs[i % 2]
        last = (i == nlayers - 1)
        with kb.scope(f"L{i}c_"):
            if not last and USE_CONV:
                convert(i + 1)
            emit_post(kb, NT, x_src, ao, AO, modrow, w_out, n2g, wq, keysT, uT, v, x_dst, XD,
                      final_g=fg if last else None, xn_out=xn_out if last else None, XN=XN if last else None,
                      tile_sets=[0] * NTL + [1] * NTC, conv=lw[i]["conv"] if USE_CONV else None)
        x_src, XS = x_dst, XD
    return kb.finish()


def _na_bias_f(rpb, qr):
    H = rpb.shape[0]
    R0 = qr * 64
    out = np.full((5, H, 128, NA_NBMAX, 128), NEG, np.float32)
    kk = np.arange(128)
    qq = np.arange(128)
    for slot, qt in enumerate((0, 1, 10, NTL - 2, NTL - 1)):
        r0 = R0 + 2 * qt
        if slot == 2:
            r0 = 100
        o0, onb, cands = _na_blocklist(qt)
        blocks = [((r0 - 4 + 2 * b) if slot == 2 else (R0 + o0 // 64 + 2 * b), None) for b in range(onb)]
        for r in range(4):
            for (which, cb) in cands:
                if which == "tail":
                    blocks.append((R0 - 4 + 2 * cb, r == qr - 1))
                else:
                    blocks.append((R0 + 64 + 2 * cb, r == qr + 1))
        qrow = r0 + qq // 64
        qc = qq % 64
        rs = np.clip(qrow - 4, 0, 256 - 8)
        cs = np.clip(qc - 8, 0, 64 - 16)
        for bi, (krow0, ok) in enumerate(blocks):
            if ok is False:
                continue
            kr = krow0 + kk // 64
            kc = kk % 64
            valid = ((kr[:, None] >= rs[None, :]) & (kr[:, None] < rs[None, :] + 8) &
                     (kc[:, None] >= cs[None, :]) & (kc[:, None] < cs[None, :] + 16) &
                     (kr[:, None] >= 0) & (kr[:, None] < 256))
            dr = np.clip(kr[:, None] - qrow[None, :] + 7, 0, 14)
            dc = np.clip(kc[:, None] - qc[None, :] + 15, 0, 30)
            vals = rpb[:, dr, dc]
            out[slot, :, :, bi, :] = np.where(valid[None], vals, NEG)
    return out.reshape(5, H, 128, NA_NBMAX * 128)


def _swa_bias_f(qr):
    out = np.full((3, 128, 6, 128), NEG, np.float32)
    k = np.arange(128)[:, None]
    q = np.arange(128)[None, :]
    band = lambda off: np.where(np.abs(off * 128 + k - q) <= 128, 0.0, NEG).astype(np.float32)
    out[0, :, 0] = band(0)
    out[0, :, 1] = band(1)
    for r in range(4):
        if r == qr - 1:
            out[0, :, 2 + r] = band(-1)
    out[1, :, 0] = band(-1)
    out[1, :, 1] = band(0)
    out[1, :, 2] = band(1)
    out[2, :, 0] = band(-1)
    out[2, :, 1] = band(0)
    for r in range(4):
        if r == qr + 1:
            out[2, :, 2 + r] = band(1)
    return out.reshape(3, 128, 768)


_FUSED = {}


def kernel(x, c, ctx, c_ctx, w_mod, b_mod, norm1_g, norm2_g, w_in_even, w_out_even, na_rpb, diff_lambda,
           diff_subln_g, w_in_odd, w_out_odd, swa_sink, peer_wq, peer_keys, peer_u, peer_v, final_g, _nlayers=4):
    f32 = np.float32
    x = np.asarray(x, f32)
    ctx = np.asarray(ctx, f32)
    if _nlayers not in _FUSED:
        _FUSED[_nlayers] = build_fused(_nlayers)
    nc = _FUSED[_nlayers]
    shared = {"c_ident": np.eye(128, dtype=f32), "fg": np.asarray(final_g, f32)}
    for i in range(_nlayers):
        even = (i % 2 == 0)
        j = i // 2
        w_in = np.asarray(w_in_even[j] if even else w_in_odd[j], f32)
        rc = w_in[:, 1536:2560] if even else w_in[:, 0:1280]
        shared[f"w_mod{i}"] = np.asarray(w_mod[i], f32)
        shared[f"b_mod{i}"] = np.asarray(b_mod[i], f32)
        shared[f"n1g{i}"] = np.asarray(norm1_g[i], f32)
        shared[f"n2g{i}"] = np.asarray(norm2_g[i], f32)
        shared[f"w_in{i}"] = w_in
        shared[f"w_perm{i}"] = np.ascontiguousarray(rc.reshape(D, -1, 64)[:, :, _PERM64].reshape(D, -1))
        shared[f"w_out{i}"] = np.asarray(w_out_even[j] if even else w_out_odd[j], f32)
        shared[f"wq{i}"] = np.asarray(peer_wq[i], f32)
        shared[f"keysT{i}"] = np.ascontiguousarray(np.asarray(peer_keys[i], f32).reshape(16, 128, 128).transpose(2, 0, 1))
        shared[f"uT{i}"] = np.ascontiguousarray(np.asarray(peer_u[i], f32).T)
        shared[f"v{i}"] = np.asarray(peer_v[i], f32)
        if even:
            shared[f"lam{j}"] = np.asarray(diff_lambda[j], f32)
            shared[f"subg{j}"] = np.asarray(diff_subln_g[j], f32)
            li = 0.8 - 0.6 * math.exp(-0.3 * i)
            shared[f"lam_init{j}"] = np.tile(np.array([[li, 1.0 - li]], f32), (128, 1))
        else:
            shared[f"sink{j}"] = np.asarray(swa_sink[j], f32)
    ims = []
    for core in range(NCORES):
        b, qr = divmod(core, 4)
        im = dict(shared)
        im["x"] = np.concatenate([x[b, qr * TQ:(qr + 1) * TQ], ctx[b]], axis=0)
        cs = np.stack([np.asarray(c, f32)[b], np.asarray(c_ctx, f32)], 0)
        im["csT"] = np.ascontiguousarray(cs.reshape(2, 8, 128).transpose(2, 0, 1).reshape(128, 16))
        im["cosT"], im["sinT"] = _rope_tables_T(qr * TQ)
        for i in range(_nlayers):
            j = i // 2
            if i % 2 == 0:
                im[f"nbias{j}"] = _na_bias_f(np.asarray(na_rpb[j], f32), qr)
            else:
                im[f"sbias{j}"] = _swa_bias_f(qr)
        ims.append(im)
    res = run_bass_kernel_spmd(nc, ims, core_ids=list(range(NCORES))).results
    out = np.zeros((2, SEQ, D), f32)
    for core in range(NCORES):
        b, qr = divmod(core, 4)
        out[b, qr * TQ:(qr + 1) * TQ] = np.asarray(res[core]["xn_out"])[:TQ]
    return out
```

```python
from contextlib import ExitStack
import math
import numpy as np
import ml_dtypes
import concourse.bass as bass
import concourse.mybir as mybir
from concourse.bass_utils import run_bass_kernel_spmd

F32 = mybir.dt.float32
BF16 = mybir.dt.bfloat16
AF = mybir.ActivationFunctionType
ALU = mybir.AluOpType
AX = mybir.AxisListType
NPBF = ml_dtypes.bfloat16

ENGS = ("pe", "act", "dve", "pool", "sp")

D = 1024
NCORES = 8
SEQ = 16384
CTX = 256
TQ = SEQ // 4
NTL = TQ // 128
NTC = CTX // 128
NT = NTL + NTC
TALL = TQ + CTX
EPS = 1e-6
NEG = -1.0e30
PEER_MARGIN = 1e-4
import os
USE_CONV = os.environ.get('K_CONV', '1') == '1'


class Buf:
    __slots__ = ("name", "writers", "readers", "excl")

    def __init__(self, name="", excl=False):
        self.name = name
        self.writers = {}
        self.readers = {}
        self.excl = excl


class Op:
    __slots__ = ("id", "eng", "fn", "dma", "cc", "deps", "seq", "waits", "signal", "sigidx", "slot", "target")


class Prog:
    RING = {"sp": 14, "act": 4, "pool": 8}

    def __init__(self, nc):
        self.nc = nc
        self.ops = []
        self.seq = {e: 0 for e in ENGS}
        self.dmas = {q: [] for q in self.RING}
        self.ccs = []

    def add(self, eng, fn, reads=(), writes=(), dma=False, cc=False):
        dma = dma or cc
        op = Op()
        op.cc = cc
        op.id = len(self.ops)
        op.eng = eng
        op.fn = fn
        op.dma = dma
        op.signal = False
        op.sigidx = None
        op.slot = None
        op.target = None
        deps = set()
        if cc:
            op.slot = ("cc", len(self.ccs))
            op.target = 1
            self.ccs.append(op.id)
        elif dma:
            ring = self.RING[eng]
            lst = self.dmas[eng]
            n = len(lst)
            op.slot = (eng, n % ring)
            op.target = 16 * (n // ring + 1)
            if n >= ring:
                deps.add(lst[n - ring])
            lst.append(op.id)
        pkey = ("dma", op.slot) if dma else eng
        rd = [b for b in reads if not b.excl]
        wr = list(writes) + [b for b in reads if b.excl]
        for b in rd:
            for k, oid in b.writers.items():
                if k == eng and eng == "pe":
                    continue
                deps.add(oid)
        for b in wr:
            isread = b not in writes
            for k, oid in b.writers.items():
                if k == eng and not dma:
                    if isread and eng != "pe":
                        deps.add(oid)
                    continue
                if dma and isinstance(k, tuple):
                    continue
                deps.add(oid)
            for k, oid in b.readers.items():
                if k == eng and not dma:
                    continue
                deps.add(oid)
        for b in wr:
            if dma:
                b.writers = {k: v for k, v in b.writers.items() if isinstance(k, tuple)}
                b.writers[pkey] = op.id
            else:
                b.writers = {pkey: op.id}
            b.readers = {}
        for b in rd:
            if b in wr:
                continue
            b.readers[pkey] = op.id
        op.seq = self.seq[eng]
        self.seq[eng] += 1
        op.deps = deps
        self.ops.append(op)
        return op.id

    def barrier(self):
        start = getattr(self, "bar_start", 0)
        last = {}
        for op in self.ops[start:]:
            if op.dma:
                last[("dma", op.id)] = op.id
            else:
                last[op.eng] = op.id
        deps = set(last.values())
        for e in ENGS:
            oid = self.add(e, lambda eng: None)
            self.ops[oid].deps |= {d for d in deps if self.ops[d].dma or self.ops[d].eng != e}
        self.bar_start = len(self.ops)

    def emit(self):
        nc = self.nc
        ops = self.ops
        seen = {e: {} for e in ENGS}
        seen_dma = {e: {} for e in ENGS}
        for op in ops:
            best = {}
            waits = []
            for d in op.deps:
                p = ops[d]
                if p.dma:
                    if seen_dma[op.eng].get(p.slot, 0) >= p.target:
                        continue
                    seen_dma[op.eng][p.slot] = p.target
                    waits.append(d)
                else:
                    if p.eng not in best or ops[best[p.eng]].seq < p.seq:
                        best[p.eng] = d
            for e, d in best.items():
                p = ops[d]
                if seen[op.eng].get(e, -1) >= p.seq:
                    continue
                seen[op.eng][e] = p.seq
                p.signal = True
                waits.append(d)
            op.waits = waits
        cnt = {e: 0 for e in ENGS}
        for op in ops:
            if op.signal and not op.dma:
                cnt[op.eng] += 1
                op.sigidx = cnt[op.eng]
        with ExitStack() as es:
            sems = {e: es.enter_context(nc.semaphore("s_" + e)) for e in ENGS}
            rings = {}
            for q, n in self.RING.items():
                for i in range(n):
                    rings[(q, i)] = es.enter_context(nc.semaphore(f"r_{q}{i}"))
            for i in range(len(self.ccs)):
                rings[("cc", i)] = es.enter_context(nc.semaphore(f"cc{i}"))
            block = es.enter_context(nc.Block())
            per_eng = {e: [o for o in ops if o.eng == e] for e in ENGS}

            def run(engname):
                def body(eng):
                    for op in per_eng[engname]:
                        for d in op.waits:
                            p = ops[d]
                            if p.dma:
                                eng.wait_ge(rings[p.slot], p.target)
                            else:
                                eng.wait_ge(sems[p.eng], p.sigidx)
                        ins = op.fn(eng)
                        if ins is None:
                            continue
                        if op.cc:
                            ins.then_inc(rings[op.slot], 1)
                        elif op.dma:
                            ins.then_inc(rings[op.slot], 16)
                        elif op.signal:
                            ins.then_inc(sems[op.eng], 1)
                return body

            block.tensor(run("pe"))
            block.scalar(run("act"))
            block.vector(run("dve"))
            block.gpsimd(run("pool"))
            block.sync(run("sp"))


class KB:
    def __init__(self):
        self.nc = bass.Bass("TRN2", target_bir_lowering=False)
        self.P = Prog(self.nc)
        self.es = ExitStack()
        self.banks = []
        self.outs = []

    def din(self, name, shape, dt=F32):
        return self.nc.dram_tensor(name, list(shape), dt, kind="ExternalInput").ap()

    def dout(self, name, shape, dt=F32):
        b = Buf(name)
        self.outs.append(b)
        return self.nc.dram_tensor(name, list(shape), dt, kind="ExternalOutput").ap(), b

    def dscratch(self, name, shape, dt):
        return self.nc.dram_tensor(name, list(shape), dt, kind="Internal").ap(), Buf(name)

    pfx = ""

    def sb(self, name, shape, dt=F32):
        return self.es.enter_context(self.nc.sbuf_tensor(self.pfx + name, list(shape), dt)), Buf(name)

    def scope(self, pfx):
        kb = self

        class _S:
            def __enter__(s_):
                s_.saved = (kb.es, kb.pfx)
                kb.es = ExitStack()
                kb.pfx = pfx
                kb._ident = None

            def __exit__(s_, *a):
                kb.es.close()
                kb.es, kb.pfx = s_.saved
                kb.P.barrier()
                return False
        return _S()

    _ident = None
    _ident_d = None

    def ident(self):
        if self._ident is None:
            if self._ident_d is None:
                self._ident_d = self.din("c_ident", [128, 128], F32)
            f, IDF = self.sb("ident_f", [128, 128], F32)
            b, ID = self.sb("ident", [128, 128], BF16)
            self.dma("sp", f[:], self._ident_d, writes=[IDF])
            self.op("act", lambda e: e.copy(out=b[:], in_=f[:]), reads=[IDF], writes=[ID])
            self._ident = (f, IDF, b, ID)
        return self._ident

    def cc(self, kind, rg, src, dst, reads=(), writes=()):
        return self.P.add("pool", lambda e: e.collective_compute(kind, ALU.bypass, replica_groups=rg, ins=[src.opt()], outs=[dst.opt()]),
                          reads, writes, cc=True)

    def psum_banks(self):
        for i in range(8):
            t = self.es.enter_context(self.nc.psum_tensor(f"bank{i}", [128, 512], F32))
            self.banks.append((t, Buf(f"bank{i}", excl=True)))

    def op(self, eng, fn, reads=(), writes=()):
        return self.P.add(eng, fn, reads, writes)

    def dma(self, q, out, in_, reads=(), writes=()):
        return self.P.add(q, lambda e: e.dma_start(out=out, in_=in_), reads, writes, dma=True)

    def finish(self):
        self.P.add("sp", lambda e: None, reads=self.outs)
        self.P.emit()
        self.es.close()
        return self.nc


def rstd_ops(kb, sm, SM):
    kb.op("dve", lambda e: e.tensor_scalar(out=sm[:, 2:3], in0=sm[:, 0:1], scalar1=1.0 / D, scalar2=EPS,
                                           op0=ALU.mult, op1=ALU.add), reads=[SM], writes=[SM])
    kb.op("act", lambda e: e.activation(out=sm[:, 3:4], in_=sm[:, 2:3], func=AF.Sqrt), reads=[SM], writes=[SM])
    kb.op("dve", lambda e: e.reciprocal(out=sm[:, 1:2], in_=sm[:, 3:4]), reads=[SM], writes=[SM])


def emit_post(kb, ntiles, x_in, ao_dram, AO, modrow, w_out, norm2g, peer_wq, peer_keysT, peer_uT, peer_v,
              x_out, XOUT, final_g=None, xn_out=None, XN=None, tile_sets=None, conv=None):
    nc, P = kb.nc, kb.P
    B = kb.banks
    if tile_sets is None:
        tile_sets = [0] * ntiles
    ident_f, IDF, ident, ID = kb.ident()

    mod, MOD = kb.sb("modrows", [128, 4, D], F32)
    n2g, N2G = kb.sb("n2g_sb", [128, D], F32)
    kb.dma("sp", n2g[:], norm2g.unsqueeze(0).to_broadcast([128, D]), writes=[N2G])
    fg = None
    if final_g is not None:
        fg, FG = kb.sb("fg_sb", [128, D], F32)
        kb.dma("sp", fg[:], final_g.unsqueeze(0).to_broadcast([128, D]), writes=[FG])

    def load_mod(s):
        for j, src in enumerate((2, 4, 3, 5)):
            kb.dma("sp", mod[:, j, :], modrow[s, src:src + 1, :].to_broadcast([128, D]), writes=[MOD])
        kb.op("dve", lambda e: e.scalar_tensor_tensor(out=mod[:, 1, :], in0=mod[:, 1, :], scalar=1.0, in1=n2g[:],
                                                      op0=ALU.add, op1=ALU.mult), reads=[MOD, N2G], writes=[MOD])

    keys_b, KBF = kb.sb("keys_b", [128, 16, 128], BF16)

    NWR = 3
    wr = [kb.sb(f"wr{i}", [128, 8, 512], BF16) for i in range(NWR)]
    NVR = 2
    vr = [kb.sb(f"vr{i}", [128, 4, D], BF16) for i in range(NVR)]
    wr_i = [0]
    vr_i = [0]

    def load_w(src_cols, pre=None):
        t, b = wr[wr_i[0] % NWR]
        wr_i[0] += 1
        if pre is not None:
            kb.dma("sp", t[:].rearrange("p k n -> p (k n)"), pre[0], reads=[pre[1]], writes=[b])
        else:
            kb.dma("pool", t[:], src_cols.rearrange("(k p) n -> p k n", p=128), writes=[b])
        return t, b

    def load_v(src_rows, pre=None):
        t, b = vr[vr_i[0] % NVR]
        vr_i[0] += 1
        if pre is not None:
            kb.dma("sp", t[:].rearrange("p c d -> p (c d)"), pre[0], reads=[pre[1]], writes=[b])
        else:
            kb.dma("pool", t[:], src_rows.rearrange("(c p) d -> p c d", p=128), writes=[b])
        return t, b

    def pre_of(name, idx):
        return None if conv is None else (conv[name][0][idx], conv[name][1])

    xt, XT = kb.sb("xt", [128, 2, D], F32)
    tmp, TMP = kb.sb("tmp", [128, D], F32)
    ao, AOB = kb.sb("ao_sb", [128, D], BF16)
    aoT, AOT = kb.sb("aoT", [128, 8, 128], BF16)
    h2, H2 = kb.sb("h2", [128, D], BF16)
    h2T, H2T = kb.sb("h2T", [128, 8, 256], BF16)
    qT, QT = kb.sb("qT", [128, 16, 256], BF16)
    s_sb, SSB = kb.sb("s_sb", [128, 16, 128], F32)
    work, WORK = kb.sb("work", [128, 2048], F32)
    kb.dma("sp", work[:], peer_keysT.rearrange("p a n -> p (a n)"), writes=[WORK])
    kb.op("act", lambda e: e.copy(out=keys_b[:].rearrange("p a n -> p (a n)"), in_=work[:]), reads=[WORK], writes=[KBF])
    cand, CAND = kb.sb("cand", [128, 8, 16, 16], F32)
    top, TOP = kb.sb("top", [128, 16, 16], F32)
    ctop, CTOP = kb.sb("ctop", [128, 8, 16], F32)
    sm, SM = kb.sb("sm", [128, 64], F32)
    e16, E16 = kb.sb("e16", [128, 8, 16], F32)
    av, AV = kb.sb("a_vec", [128, 2, 8, 128], F32)
    bv, BV = kb.sb("b_vec", [128, 2, 8, 128], F32)
    diag, DG = kb.sb("diag", [128, 2, 8, 128], BF16)
    pps = [kb.sb(f"pp{i}", [128, 2, 8, 2, 128], F32) for i in range(2)]
    wps = [kb.sb(f"wp{i}", [128, 2, 8, 2, 128], BF16) for i in range(2)]
    gl = [kb.sb(f"gl{i}", [128, 4, 256], BF16) for i in range(2)]
    at = [kb.sb(f"at{i}", [128, 4, 256], BF16) for i in range(2)]
    xn, XNB = work, WORK

    def bf(bank):
        return bank.bitcast(BF16)

    cur_set = [None]
    npairs = (ntiles + 1) // 2
    for pr in range(npairs):
        tiles = [t for t in (2 * pr, 2 * pr + 1) if t < ntiles]
        nj = len(tiles)
        NTOK = 128 * nj
        if tile_sets[tiles[0]] != cur_set[0]:
            cur_set[0] = tile_sets[tiles[0]]
            load_mod(cur_set[0])
        wo = [load_w(w_out[:, hf * 512:(hf + 1) * 512], pre_of('wout', hf)) for hf in range(2)]
        for j, tt in enumerate(tiles):
            rows = slice(tt * 128, (tt + 1) * 128)
            kb.dma("sp", xt[:, j, :], x_in[rows, :], writes=[XT])
            kb.dma("sp", ao[:], ao_dram[rows, :], reads=[AO], writes=[AOB])
            tb, TB = B[0]
            def tr1(e, tb=tb):
                for k in range(8):
                    ins = e.transpose(bf(tb)[:, k * 128:(k + 1) * 128], ao[:, k * 128:(k + 1) * 128], ident[:])
                return ins
            kb.op("pe", tr1, reads=[AOB, ID], writes=[TB])
            kb.op("act", lambda e, tb=tb: e.copy(out=aoT[:].rearrange("p k t -> p (k t)"), in_=bf(tb)[:, 0:1024]),
                  reads=[TB], writes=[AOT])
            for hf in range(2):
                yb, YB = B[1 + hf]
                wt, WB = wo[hf]
                def mmy(e, yb=yb, wt=wt):
                    for k in range(8):
                        ins = e.matmul(yb[:], lhsT=aoT[:, k, :], rhs=wt[:, k, :], start=(k == 0), stop=(k == 7))
                    return ins
                kb.op("pe", mmy, reads=[AOT, WB], writes=[YB])
                kb.op("dve", lambda e, yb=yb, hf=hf: e.tensor_tensor(out=tmp[:, hf * 512:(hf + 1) * 512], in0=yb[:],
                                                                   in1=mod[:, 0, hf * 512:(hf + 1) * 512], op=ALU.mult),
                      reads=[YB, MOD], writes=[TMP])
            kb.op("pool", lambda e, j=j: e.tensor_tensor(out=xt[:, j, :], in0=xt[:, j, :], in1=tmp[:], op=ALU.add),
                  reads=[XT, TMP], writes=[XT])
            kb.op("act", lambda e, j=j: e.activation(out=tmp[:], in_=xt[:, j, :], func=AF.Square, accum_out=sm[:, 0:1]),
                  reads=[XT], writes=[TMP, SM])
            rstd_ops(kb, sm, SM)
            kb.op("dve", lambda e, j=j: e.scalar_tensor_tensor(out=tmp[:], in0=xt[:, j, :], scalar=sm[:, 1:2], in1=mod[:, 1, :],
                                                               op0=ALU.mult, op1=ALU.mult), reads=[XT, SM, MOD], writes=[TMP])
            kb.op("pool", lambda e: e.tensor_tensor(out=h2[:], in0=tmp[:], in1=mod[:, 2, :], op=ALU.add),
                  reads=[TMP, MOD], writes=[H2])
            tb, TB = B[3]
            def tr2(e, tb=tb):
                for k in range(8):
                    ins = e.transpose(bf(tb)[:, k * 128:(k + 1) * 128], h2[:, k * 128:(k + 1) * 128], ident[:])
                return ins
            kb.op("pe", tr2, reads=[H2, ID], writes=[TB])
            kb.op("act", lambda e, tb=tb, j=j: e.copy(out=h2T[:, :, j * 128:(j + 1) * 128],
                                                      in_=bf(tb)[:, 0:1024].rearrange("p (k t) -> p k t", k=8)),
                  reads=[TB], writes=[H2T])
        for g in range(4):
            wt, WB = load_w(peer_wq[:, g * 512:(g + 1) * 512], pre_of('wq', g))
            for hh in range(2):
                qb, QB = B[4 + (2 * g + hh) % 2]
                def mmq(e, qb=qb, wt=wt, hh=hh, NTOK=NTOK):
                    for i in range(2):
                        for k in range(8):
                            c0 = (hh * 2 + i) * 128
                            ins = e.matmul(qb[:, i * 256:i * 256 + NTOK], lhsT=wt[:, k, c0:c0 + 128], rhs=h2T[:, k, 0:NTOK],
                                           start=(k == 0), stop=(k == 7))
                    return ins
                kb.op("pe", mmq, reads=[WB, H2T], writes=[QB])
                hp0 = g * 4 + hh * 2
                kb.op("act", lambda e, qb=qb, hp0=hp0, NTOK=NTOK: e.copy(out=qT[:, hp0:hp0 + 2, 0:NTOK],
                                                               in_=qb[:].rearrange("p (i t) -> p i t", i=2)[:, :, 0:NTOK]),
                      reads=[QB], writes=[QT])
        for j, tt in enumerate(tiles):
            for g in range(4):
                sbk, SB_ = B[6 + g % 2]
                def mms(e, sbk=sbk, g=g, j=j):
                    for i in range(4):
                        hp = g * 4 + i
                        ins = e.matmul(sbk[:, i * 128:(i + 1) * 128], lhsT=qT[:, hp, j * 128:(j + 1) * 128], rhs=keys_b[:, hp, :],
                                       start=True, stop=True)
                    return ins
                kb.op("pe", mms, reads=[QT, KBF], writes=[SB_])
                kb.op("act", lambda e, sbk=sbk, g=g: e.copy(out=s_sb[:, g * 4:(g + 1) * 4, :].rearrange("p a n -> p (a n)"), in_=sbk[:]),
                      reads=[SB_], writes=[SSB])
            for hp in range(16):
                kb.op("dve", lambda e, hp=hp: e.max(out=top[:, hp, 0:8], in_=s_sb[:, hp, :]), reads=[SSB], writes=[TOP])
                kb.op("dve", lambda e, hp=hp: e.match_replace(out=work[:, hp * 128:(hp + 1) * 128], in_to_replace=top[:, hp, 0:8],
                                                              in_values=s_sb[:, hp, :], imm_value=NEG), reads=[SSB, TOP], writes=[WORK])
                kb.op("dve", lambda e, hp=hp: e.max(out=top[:, hp, 8:16], in_=work[:, hp * 128:(hp + 1) * 128]), reads=[WORK], writes=[TOP])
            def fcand(e):
                t4 = top[:].rearrange("p (h q) k -> p h q k", q=2)
                i0 = t4[:, :, 0, :].unsqueeze(3).to_broadcast([128, 8, 16, 16])
                i1 = t4[:, :, 1, :].unsqueeze(2).to_broadcast([128, 8, 16, 16])
                return e.tensor_tensor(out=cand[:], in0=i0, in1=i1, op=ALU.add)
            kb.op("pool", fcand, reads=[TOP], writes=[CAND])
            for h in range(8):
                cv = cand[:, h, :, :].rearrange("p a b -> p (a b)")
                kb.op("dve", lambda e, h=h, cv=cv: e.max(out=ctop[:, h, 0:8], in_=cv), reads=[CAND], writes=[CTOP])
                kb.op("dve", lambda e, h=h, cv=cv: e.match_replace(out=work[:, h * 256:(h + 1) * 256], in_to_replace=ctop[:, h, 0:8],
                                                                   in_values=cv, imm_value=NEG), reads=[CAND, CTOP], writes=[WORK])
                kb.op("dve", lambda e, h=h: e.max(out=ctop[:, h, 8:16], in_=work[:, h * 256:(h + 1) * 256]), reads=[WORK], writes=[CTOP])
            kb.op("dve", lambda e: e.tensor_scalar(out=sm[:, 8:16], in0=ctop[:, :, 15], scalar1=-1.0, scalar2=PEER_MARGIN,
                                                   op0=ALU.mult, op1=ALU.add), reads=[CTOP], writes=[SM])
            kb.op("dve", lambda e: e.tensor_tensor(out=e16[:], in0=ctop[:], in1=sm[:, 8:16].unsqueeze(2).to_broadcast([128, 8, 16]),
                                                   op=ALU.add), reads=[CTOP, SM], writes=[E16])
            kb.op("act", lambda e: e.activation(out=e16[:], in_=e16[:], func=AF.Exp), reads=[E16], writes=[E16])
            kb.op("dve", lambda e: e.tensor_reduce(out=sm[:, 16:24], in_=e16[:], axis=AX.X, op=ALU.add), reads=[E16], writes=[SM])
            kb.op("dve", lambda e: e.reciprocal(out=sm[:, 24:32], in_=sm[:, 16:24]), reads=[SM], writes=[SM])
            s4 = s_sb[:].rearrange("p (h q) n -> p h q n", q=2)
            kb.op("dve", lambda e, j=j, s4=s4: e.tensor_tensor(out=av[:, j, :, :], in0=s4[:, :, 0, :],
                                                               in1=sm[:, 8:16].unsqueeze(2).to_broadcast([128, 8, 128]), op=ALU.add),
                  reads=[SSB, SM], writes=[AV])
            kb.op("act", lambda e, j=j: e.activation(out=av[:, j, :, :], in_=av[:, j, :, :], func=AF.Exp), reads=[AV], writes=[AV])
            kb.op("act", lambda e, j=j, s4=s4: e.activation(out=bv[:, j, :, :], in_=s4[:, :, 1, :], func=AF.Exp), reads=[SSB], writes=[BV])
            for h in range(8):
                kb.op("pool", lambda e, j=j, h=h: e.tensor_scalar(out=diag[:, j, h, :], in0=ident_f[:], scalar1=sm[:, 24 + h:25 + h],
                                                                  scalar2=None, op0=ALU.mult), reads=[IDF, SM], writes=[DG])
        for cg in range(32):
            ut, UB = load_w(peer_uT[:, cg * 512:(cg + 1) * 512], pre_of('uT', cg))
            vt, VB = load_v(peer_v[cg * 512:(cg + 1) * 512, :], pre_of('v', cg))
            gt, GB = gl[cg % 2]
            att, ATB = at[cg % 2]
            for half in range(2):
                c0 = cg * 4 + half * 2
                wp, WPB = wps[(cg * 2 + half) % 2]
                pp, PP = pps[(cg * 2 + half) % 2]
                def fpp(e, c0=c0, nj=nj, pp=pp):
                    i0 = av[:, 0:nj, :, c0:c0 + 2].unsqueeze(4).to_broadcast([128, nj, 8, 2, 128])
                    i1 = bv[:, 0:nj, :, :].unsqueeze(3).to_broadcast([128, nj, 8, 2, 128])
                    return e.tensor_tensor(out=pp[:, 0:nj], in0=i0, in1=i1, op=ALU.mult)
                kb.op("pool", fpp, reads=[AV, BV], writes=[PP])
                kb.op("dve", lambda e, wp=wp, nj=nj, pp=pp: e.scalar_tensor_tensor(out=wp[:, 0:nj], in0=pp[:, 0:nj], scalar=1.0, in1=pp[:, 0:nj],
                                                                     op0=ALU.is_ge, op1=ALU.mult), reads=[PP], writes=[WPB])
                pb, PB = B[0 + half]
                wb, WTB = B[2 + half]
                def mmpre(e, pb=pb, half=half, ut=ut, NTOK=NTOK):
                    for i in range(2):
                        cl = half * 2 + i
                        for k in range(8):
                            ins = e.matmul(pb[:, i * 256:i * 256 + NTOK], lhsT=ut[:, k, cl * 128:(cl + 1) * 128], rhs=h2T[:, k, 0:NTOK],
                                           start=(k == 0), stop=(k == 7))
                    return ins
                kb.op("pe", mmpre, reads=[UB, H2T], writes=[PB])
                def mmwt(e, wb=wb, wp=wp, nj=nj):
                    for i in range(2):
                        for j in range(nj):
                            for h in range(8):
                                ins = e.matmul(wb[:, i * 256 + j * 128:i * 256 + (j + 1) * 128], lhsT=wp[:, j, h, i, :],
                                               rhs=diag[:, j, h, :], start=(h == 0), stop=(h == 7))
                    return ins
                kb.op("pe", mmwt, reads=[WPB, DG], writes=[WTB])
                kb.op("act", lambda e, pb=pb, gt=gt, half=half, NTOK=NTOK: e.activation(
                    out=gt[:, half * 2:half * 2 + 2, 0:NTOK], in_=pb[:].rearrange("p (i t) -> p i t", i=2)[:, :, 0:NTOK], func=AF.Gelu),
                    reads=[PB], writes=[GB])
                kb.op("dve", lambda e, wb=wb, gt=gt, att=att, half=half, NTOK=NTOK: e.tensor_tensor(
                    out=att[:, half * 2:half * 2 + 2, 0:NTOK], in0=wb[:].rearrange("p (i t) -> p i t", i=2)[:, :, 0:NTOK],
                    in1=gt[:, half * 2:half * 2 + 2, 0:NTOK], op=ALU.mult), reads=[WTB, GB], writes=[ATB])
            for j in range(nj):
                for hf in range(2):
                    ob, OB = B[4 + 2 * j + hf]
                    def mmo(e, ob=ob, att=att, vt=vt, j=j, hf=hf, cg=cg):
                        for cl in range(4):
                            ins = e.matmul(ob[:], lhsT=att[:, cl, j * 128:(j + 1) * 128], rhs=vt[:, cl, hf * 512:(hf + 1) * 512],
                                           start=(cg == 0 and cl == 0), stop=(cg == 31 and cl == 3))
                        return ins
                    kb.op("pe", mmo, reads=[ATB, VB], writes=[OB])
        for j, tt in enumerate(tiles):
            rows = slice(tt * 128, (tt + 1) * 128)
            for hf in range(2):
                ob, OB = B[4 + 2 * j + hf]
                kb.op("dve", lambda e, ob=ob, hf=hf: e.tensor_tensor(out=tmp[:, hf * 512:(hf + 1) * 512], in0=ob[:],
                                                                   in1=mod[:, 3, hf * 512:(hf + 1) * 512], op=ALU.mult),
                      reads=[OB, MOD], writes=[TMP])
            kb.op("pool", lambda e, j=j: e.tensor_tensor(out=xt[:, j, :], in0=xt[:, j, :], in1=tmp[:], op=ALU.add),
                  reads=[XT, TMP], writes=[XT])
            kb.dma("sp", x_out[rows, :], xt[:, j, :], reads=[XT], writes=[XOUT])
            if final_g is not None:
                kb.op("act", lambda e, j=j: e.activation(out=tmp[:], in_=xt[:, j, :], func=AF.Square, accum_out=sm[:, 0:1]),
                      reads=[XT], writes=[TMP, SM])
                rstd_ops(kb, sm, SM)
                kb.op("dve", lambda e, j=j: e.scalar_tensor_tensor(out=xn[:, 0:D], in0=xt[:, j, :], scalar=sm[:, 1:2], in1=fg[:],
                                                                   op0=ALU.mult, op1=ALU.mult), reads=[XT, SM, FG], writes=[XNB])
                kb.dma("sp", xn_out[rows, :], xn[:, 0:D], reads=[XNB], writes=[XN])


def build_pre(even):
    kb = KB()
    kb.psum_banks()
    B = kb.banks
    if even:
        NIN = 3072
        fm_blocks = [(c * 128, None) for c in range(8)] + [(1536 + c * 128, c) for c in range(8)]
        tm_groups = [(1024, 512), (2560, 512)]
    else:
        NIN = 1536
        fm_blocks = [(c * 128, c) for c in range(10)]
        tm_groups = [(1280, 256)]
    NROPE = 128 * sum(1 for _, r in fm_blocks if r is not None)
    NFM = len(fm_blocks)
    NTM = sum(n for _, n in tm_groups)
    x_in = kb.din("x", [TALL, D])
    csT = kb.din("csT", [128, 16])
    w_mod = kb.din("w_mod", [D, 6 * D])
    b_mod = kb.din("b_mod", [6 * D])
    n1g_in = kb.din("n1g", [D])
    w_in = kb.din("w_in", [D, NIN])
    w_perm = kb.din("w_perm", [D, NROPE])
    cosT = kb.din("cosT", [128, TALL])
    sinT = kb.din("sinT", [128, TALL])
    ident_d = kb.din("c_ident", [128, 128])
    fm_out, FMO = kb.dout("fmT", [NFM * 128, TALL], BF16)
    tm_out, TMO = kb.dout("tm", [TALL, NTM], BF16)
    modrow, MRO = kb.dout("modrow", [2, 6, D])

    ident_f, IDF = kb.sb("ident_f", [128, 128], F32)
    ident, ID = kb.sb("ident", [128, 128], BF16)
    kb.dma("sp", ident_f[:], ident_d, writes=[IDF])
    kb.op("act", lambda e: e.copy(out=ident[:], in_=ident_f[:]), reads=[IDF], writes=[ID])
    win, WIN = kb.sb("win", [128, 8, NIN], BF16)
    wpm, WPM = kb.sb("wpm", [128, 8, NROPE], BF16)
    for c0 in range(0, NIN, 512):
        kb.dma("pool", win[:, :, c0:c0 + 512], w_in[:, c0:c0 + 512].rearrange("(k p) n -> p k n", p=128), writes=[WIN])
    for c0 in range(0, NROPE, 512):
        n = min(512, NROPE - c0)
        kb.dma("pool", wpm[:, :, c0:c0 + n], w_perm[:, c0:c0 + n].rearrange("(k p) n -> p k n", p=128), writes=[WPM])
    cs_f, CSF = kb.sb("cs_f", [128, 16], F32)
    cs_b, CSB = kb.sb("cs_b", [128, 16], BF16)
    csbc, CSBC = kb.sb("csbc", [128, 16, 128], BF16)
    kb.dma("sp", cs_f[:], csT, writes=[CSF])
    kb.op("act", lambda e: e.activation(out=cs_b[:], in_=cs_f[:], func=AF.Silu), reads=[CSF], writes=[CSB])
    kb.op("dve", lambda e: e.tensor_copy(out=csbc[:], in_=cs_b[:].unsqueeze(2).to_broadcast([128, 16, 128])),
          reads=[CSB], writes=[CSBC])
    n1g, N1G = kb.sb("n1g_sb", [128, D], F32)
    kb.dma("sp", n1g[:], n1g_in.unsqueeze(0).to_broadcast([128, D]), writes=[N1G])
    g1s, G1S = kb.sb("g1s", [128, 2, 2, D], F32)
    wmr = [kb.sb(f"wmr{i}", [128, 8, 512], BF16) for i in range(2)]
    bmr = [kb.sb(f"bmr{i}", [128, 512], F32) for i in range(2)]
    mtmp = [kb.sb(f"mtmp{i}", [128, 512], F32) for i in range(2)]
    for cgp in range(12):
        wt, WB = wmr[cgp % 2]
        bt, BB = bmr[cgp % 2]
        cols = slice(cgp * 512, (cgp + 1) * 512)
        kb.dma("pool", wt[:], w_mod[:, cols].rearrange("(k p) n -> p k n", p=128), writes=[WB])
        kb.dma("sp", bt[:], b_mod[cols].unsqueeze(0).to_broadcast([128, 512]), writes=[BB])
        chunk = cgp // 2
        half = cgp % 2
        for s in range(2):
            mb, MB = B[6 + s]
            def mmm(e, mb=mb, wt=wt, s=s):
                for k in range(8):
                    ins = e.matmul(mb[:], lhsT=csbc[:, s * 8 + k, :], rhs=wt[:, k, :], start=(k == 0), stop=(k == 7))
                return ins
            kb.op("pe", mmm, reads=[CSBC, WB], writes=[MB])
            if chunk < 2:
                dst = g1s[:, s, chunk, half * 512:(half + 1) * 512]
                kb.op("dve", lambda e, mb=mb, bt=bt, dst=dst: e.tensor_tensor(out=dst, in0=mb[:], in1=bt[:], op=ALU.add),
                      reads=[MB, BB], writes=[G1S])
                kb.dma("sp", modrow[s, chunk:chunk + 1, half * 512:(half + 1) * 512], dst[0:1, :], reads=[G1S], writes=[MRO])
            else:
                mt, MT = mtmp[s]
                kb.op("dve", lambda e, mb=mb, bt=bt, mt=mt: e.tensor_tensor(out=mt[:], in0=mb[:], in1=bt[:], op=ALU.add),
                      reads=[MB, BB], writes=[MT])
                kb.dma("sp", modrow[s, chunk:chunk + 1, half * 512:(half + 1) * 512], mt[0:1, :], reads=[MT], writes=[MRO])
    for s in range(2):
        kb.op("dve", lambda e, s=s: e.scalar_tensor_tensor(out=g1s[:, s, 1, :], in0=g1s[:, s, 1, :], scalar=1.0, in1=n1g[:],
                                                           op0=ALU.add, op1=ALU.mult), reads=[G1S, N1G], writes=[G1S])
    xt = [kb.sb(f"xt{i}", [128, D], F32) for i in range(2)]
    tmp, TMP = kb.sb("tmp", [128, D], F32)
    hx, HX = kb.sb("hx", [128, D], BF16)
    hxT, HXT = kb.sb("hxT", [128, 8, 512], BF16)
    sm, SM = kb.sb("sm", [128, 8], F32)
    cst = [kb.sb(f"cst{i}", [128, 512], F32) for i in range(2)]
    snt = [kb.sb(f"snt{i}", [128, 512], F32) for i in range(2)]
    r1, R1 = kb.sb("r1", [128, 512], F32)
    r2, R2 = kb.sb("r2", [128, 512], F32)
    fmo = [kb.sb(f"fmo{i}", [128, 512], BF16) for i in range(2)]
    tmo = [kb.sb(f"tmo{i}", [128, 512], BF16) for i in range(2)]
    groups = [(g * 4, 4, 0) for g in range(NTL // 4)] + [(NTL, NTC, 1)]
    xi = 0
    fi = 0
    ti = 0
    for gi, (t0, ntl, s) in enumerate(groups):
        NTOK = ntl * 128
        tok0 = t0 * 128
        ct, CT = cst[gi % 2]
        st, ST = snt[gi % 2]
        kb.dma("sp", ct[:, 0:NTOK], cosT[:, tok0:tok0 + NTOK], writes=[CT])
        kb.dma("sp", st[:, 0:NTOK], sinT[:, tok0:tok0 + NTOK], writes=[ST])
        for j in range(ntl):
            x_t, XB = xt[xi % 2]
            xi += 1
            rows = slice(tok0 + j * 128, tok0 + (j + 1) * 128)
            kb.dma("sp", x_t[:], x_in[rows, :], writes=[XB])
            kb.op("act", lambda e, x_t=x_t: e.activation(out=tmp[:], in_=x_t[:], func=AF.Square, accum_out=sm[:, 0:1]),
                  reads=[XB], writes=[TMP, SM])
            rstd_ops(kb, sm, SM)
            kb.op("dve", lambda e, x_t=x_t, s=s: e.scalar_tensor_tensor(out=tmp[:], in0=x_t[:], scalar=sm[:, 1:2], in1=g1s[:, s, 1, :],
                                                                         op0=ALU.mult, op1=ALU.mult), reads=[XB, SM, G1S], writes=[TMP])
            kb.op("pool", lambda e, s=s: e.tensor_tensor(out=hx[:], in0=tmp[:], in1=g1s[:, s, 0, :], op=ALU.add),
                  reads=[TMP, G1S], writes=[HX])
            tb, TB = B[0]
            def tr(e, tb=tb):
                for k in range(8):
                    ins = e.transpose(tb.bitcast(BF16)[:, k * 128:(k + 1) * 128], hx[:, k * 128:(k + 1) * 128], ident[:])
                return ins
            kb.op("pe", tr, reads=[HX, ID], writes=[TB])
            kb.op("act", lambda e, tb=tb, j=j: e.copy(out=hxT[:, :, j * 128:(j + 1) * 128],
                                                      in_=tb.bitcast(BF16)[:, 0:1024].rearrange("p (k t) -> p k t", k=8)),
                  reads=[TB], writes=[HXT])
        for bi, (c0, ridx) in enumerate(fm_blocks):
            pa, PA = B[1 + bi % 2]
            def mma(e, pa=pa, c0=c0, NTOK=NTOK):
                for k in range(8):
                    ins = e.matmul(pa[:, 0:NTOK], lhsT=win[:, k, c0:c0 + 128], rhs=hxT[:, k, 0:NTOK], start=(k == 0), stop=(k == 7))
                return ins
            kb.op("pe", mma, reads=[WIN, HXT], writes=[PA])
            fo, FO = fmo[fi % 2]
            fi += 1
            if ridx is None:
                kb.op("act", lambda e, pa=pa, fo=fo, NTOK=NTOK: e.copy(out=fo[:, 0:NTOK], in_=pa[:, 0:NTOK]), reads=[PA], writes=[FO])
            else:
                pb, PB = B[3 + bi % 2]
                def mmb(e, pb=pb, ridx=ridx, NTOK=NTOK):
                    for k in range(8):
                        ins = e.matmul(pb[:, 0:NTOK], lhsT=wpm[:, k, ridx * 128:(ridx + 1) * 128], rhs=hxT[:, k, 0:NTOK],
                                       start=(k == 0), stop=(k == 7))
                    return ins
                kb.op("pe", mmb, reads=[WPM, HXT], writes=[PB])
                kb.op("dve", lambda e, pa=pa, ct=ct, NTOK=NTOK: e.tensor_tensor(out=r1[:, 0:NTOK], in0=pa[:, 0:NTOK], in1=ct[:, 0:NTOK], op=ALU.mult),
                      reads=[PA, CT], writes=[R1])
                kb.op("dve", lambda e, pb=pb, st=st, NTOK=NTOK: e.tensor_tensor(out=r2[:, 0:NTOK], in0=pb[:, 0:NTOK], in1=st[:, 0:NTOK], op=ALU.mult),
                      reads=[PB, ST], writes=[R2])
                kb.op("pool", lambda e, fo=fo, NTOK=NTOK: e.tensor_tensor(out=fo[:, 0:NTOK], in0=r1[:, 0:NTOK], in1=r2[:, 0:NTOK], op=ALU.add),
                      reads=[R1, R2], writes=[FO])
            for c0_ in range(0, NTOK, 256):
                kb.dma("sp", fm_out[bi * 128:(bi + 1) * 128, tok0 + c0_:tok0 + c0_ + 256], fo[:, c0_:c0_ + 256], reads=[FO], writes=[FMO])
        for j in range(ntl):
            oc = 0
            for (c0, ncol) in tm_groups:
                pt, PT = B[5 + ti % 2]
                to, TO = tmo[ti % 2]
                ti += 1
                def mmt(e, pt=pt, c0=c0, ncol=ncol, j=j):
                    for k in range(8):
                        ins = e.matmul(pt[:, 0:ncol], lhsT=hxT[:, k, j * 128:(j + 1) * 128], rhs=win[:, k, c0:c0 + ncol],
                                       start=(k == 0), stop=(k == 7))
                    return ins
                kb.op("pe", mmt, reads=[HXT, WIN], writes=[PT])
                kb.op("act", lambda e, pt=pt, to=to, ncol=ncol: e.copy(out=to[:, 0:ncol], in_=pt[:, 0:ncol]), reads=[PT], writes=[TO])
                rows = slice(tok0 + j * 128, tok0 + (j + 1) * 128)
                kb.dma("sp", tm_out[rows, oc:oc + ncol], to[:, 0:ncol], reads=[TO], writes=[TMO])
                oc += ncol
    return kb.finish()


def emit_attn_even(kb, ao, AO):
    B = kb.banks
    naqT = kb.din("naqT", [512, TALL], BF16)
    nakT = kb.din("nakT_h", [512, 74 * 64], BF16)
    nav = kb.din("nav_h", [74 * 64, 520], BF16)
    nakTc = kb.din("nakT_c", [512, CTX], BF16)
    navc = kb.din("nav_c", [CTX, 520], BF16)
    dqT = kb.din("dqT", [512, TALL], BF16)
    dkT = kb.din("dkT_all", [512, CTX + SEQ], BF16)
    dv = kb.din("dv_all", [CTX + SEQ, 516], BF16)
    nbias = kb.din("nbias", [5, 8, 128, 640])
    lam_in = kb.din("lam", [4, 64])
    subg_in = kb.din("subg", [128])
    lami_in = kb.din("lam_init", [128, 2])
    sc = 64 ** -0.5
    lm, LM = kb.sb("lm", [128, 4, 64], F32)
    lms, LMS = kb.sb("lms", [128, 16], F32)
    subg, SUBG = kb.sb("subg_sb", [128, 128], F32)
    kb.dma("sp", lm[:].rearrange("p a b -> p (a b)"), lam_in.rearrange("a b -> (a b)").unsqueeze(0).to_broadcast([128, 256]), writes=[LM])
    kb.dma("sp", subg[:], subg_in.unsqueeze(0).to_broadcast([128, 128]), writes=[SUBG])
    kb.dma("sp", lms[:, 8:10], lami_in, writes=[LMS])
    kb.op("dve", lambda e: e.tensor_tensor(out=lm[:, 0, :], in0=lm[:, 0, :], in1=lm[:, 1, :], op=ALU.mult), reads=[LM], writes=[LM])
    kb.op("dve", lambda e: e.tensor_tensor(out=lm[:, 2, :], in0=lm[:, 2, :], in1=lm[:, 3, :], op=ALU.mult), reads=[LM], writes=[LM])
    kb.op("dve", lambda e: e.tensor_reduce(out=lms[:, 0:1], in_=lm[:, 0, :], axis=AX.X, op=ALU.add), reads=[LM], writes=[LMS])
    kb.op("dve", lambda e: e.tensor_reduce(out=lms[:, 1:2], in_=lm[:, 2, :], axis=AX.X, op=ALU.add), reads=[LM], writes=[LMS])
    kb.op("act", lambda e: e.activation(out=lms[:, 2:4], in_=lms[:, 0:2], func=AF.Exp), reads=[LMS], writes=[LMS])
    kb.op("dve", lambda e: e.tensor_tensor(out=lms[:, 4:5], in0=lms[:, 2:3], in1=lms[:, 3:4], op=ALU.subtract), reads=[LMS], writes=[LMS])
    kb.op("dve", lambda e: e.tensor_tensor(out=lms[:, 5:6], in0=lms[:, 4:5], in1=lms[:, 8:9], op=ALU.add), reads=[LMS], writes=[LMS])
    kb.op("dve", lambda e: e.tensor_scalar(out=lms[:, 6:7], in0=lms[:, 5:6], scalar1=-1.0, scalar2=None, op0=ALU.mult), reads=[LMS], writes=[LMS])
    kb.op("dve", lambda e: e.tensor_scalar(out=subg[:], in0=subg[:], scalar1=lms[:, 9:10], scalar2=None, op0=ALU.mult), reads=[SUBG, LMS], writes=[SUBG])

    kcT, KCT = kb.sb("na_kcT", [128, 4, CTX], BF16)
    vca, VCA = kb.sb("na_vca", [128, 2, 8, 65], BF16)
    kb.dma("sp", kcT[:], nakTc.rearrange("(a p) n -> p a n", p=128), writes=[KCT])
    kb.dma("sp", vca[:].rearrange("p b h c -> p b (h c)"), navc.rearrange("(b p) c -> p b c", p=128), writes=[VCA])
    kts = [kb.sb(f"na_kt{i}", [128, 4, 640], BF16) for i in range(2)]
    vts = [kb.sb(f"na_vt{i}", [128, 5, 8, 65], BF16) for i in range(2)]
    qts = [kb.sb(f"na_qt{i}", [128, 4, 128], BF16) for i in range(2)]
    bts = [kb.sb(f"na_bt{i}", [128, 640], F32) for i in range(2)]
    sts = [kb.sb(f"na_st{i}", [128, 640], F32) for i in range(2)]
    pts = [kb.sb(f"na_pt{i}", [128, 896], BF16) for i in range(2)]
    aot = [kb.sb(f"na_ao{i}", [128, 512], BF16) for i in range(2)]
    rc, RC = kb.sb("na_rc", [128, 8], F32)
    hi = 0
    for qt in range(NT):
        isctx = qt >= NTL
        q_t, QB = qts[qt % 2]
        kb.dma("sp", q_t[:], naqT[:, qt * 128:(qt + 1) * 128].rearrange("(a p) n -> p a n", p=128), writes=[QB])
        if not isctx:
            k_t, KB_ = kts[qt % 2]
            v_t, VB = vts[qt % 2]
            kb.dma("sp", k_t[:], nakT[:, qt * 128:qt * 128 + 640].rearrange("(a p) n -> p a n", p=128), writes=[KB_])
            kb.dma("sp", v_t[:].rearrange("p b h c -> p b (h c)"), nav[qt * 128:qt * 128 + 640, :].rearrange("(b p) c -> p b c", p=128), writes=[VB])
            slot = 0 if qt == 0 else 1 if qt == 1 else 3 if qt == NTL - 2 else 4 if qt == NTL - 1 else 2
        a_t, AB = aot[qt % 2]
        for h in range(8):
            a, off = h // 2, (h % 2) * 64
            pa, PA = B[(2 * hi) % 4]
            pb, PB = B[(2 * hi + 1) % 4]
            st_, STB = sts[hi % 2]
            pt_, PTB = pts[hi % 2]
            acc, ACC = B[4 + (h // 4)]
            hi += 1
            if not isctx:
                b_t, BB = bts[hi % 2]
                kb.dma("sp", b_t[:], nbias[slot, h], writes=[BB])
                def mms(e, pa=pa, pb=pb, k_t=k_t, q_t=q_t, a=a, off=off):
                    for blk in range(4):
                        e.matmul(pa[:, blk * 128:(blk + 1) * 128], lhsT=k_t[off:off + 64, a, blk * 128:(blk + 1) * 128],
                                 rhs=q_t[off:off + 64, a, :], start=True, stop=True)
                    e.matmul(pb[:, 0:128], lhsT=k_t[off:off + 64, a, 512:640], rhs=q_t[off:off + 64, a, :], start=True, stop=True)
                    for cb in range(2):
                        ins = e.matmul(pb[:, 128 + cb * 128:256 + cb * 128], lhsT=kcT[off:off + 64, a, cb * 128:(cb + 1) * 128],
                                       rhs=q_t[off:off + 64, a, :], start=True, stop=True)
                    return ins
                kb.op("pe", mms, reads=[KB_, QB, KCT], writes=[PA, PB])
                kb.op("dve", lambda e, pa=pa, st_=st_, b_t=b_t: e.scalar_tensor_tensor(out=st_[:, 0:512], in0=pa[:], scalar=sc, in1=b_t[:, 0:512],
                                                                                    op0=ALU.mult, op1=ALU.add), reads=[PA, BB], writes=[STB])
                kb.op("dve", lambda e, pb=pb, st_=st_, b_t=b_t: e.scalar_tensor_tensor(out=st_[:, 512:640], in0=pb[:, 0:128], scalar=sc,
                                                                                    in1=b_t[:, 512:640], op0=ALU.mult, op1=ALU.add),
                      reads=[PB, BB], writes=[STB])
                kb.op("act", lambda e, st_=st_, pt_=pt_: e.activation(out=pt_[:, 0:640], in_=st_[:], func=AF.Exp), reads=[STB], writes=[PTB])
                kb.op("act", lambda e, pb=pb, pt_=pt_: e.activation(out=pt_[:, 640:896], in_=pb[:, 128:384], func=AF.Exp, scale=sc),
                      reads=[PB], writes=[PTB])
                def mmv(e, acc=acc, pt_=pt_, v_t=v_t, h=h):
                    o = acc[:, (h % 4) * 65:(h % 4) * 65 + 65]
                    for blk in range(5):
                        e.matmul(o, lhsT=pt_[:, blk * 128:(blk + 1) * 128], rhs=v_t[:, blk, h, :], start=(blk == 0), stop=False)
                    for cb in range(2):
                        ins = e.matmul(o, lhsT=pt_[:, 640 + cb * 128:768 + cb * 128], rhs=vca[:, cb, h, :], start=False, stop=(cb == 1))
                    return ins
                kb.op("pe", mmv, reads=[PTB, VB, VCA], writes=[ACC])
            else:
                def mms(e, pb=pb, q_t=q_t, a=a, off=off):
                    for cb in range(2):
                        ins = e.matmul(pb[:, 128 + cb * 128:256 + cb * 128], lhsT=kcT[off:off + 64, a, cb * 128:(cb + 1) * 128],
                                       rhs=q_t[off:off + 64, a, :], start=True, stop=True)
                    return ins
                kb.op("pe", mms, reads=[QB, KCT], writes=[PB])
                kb.op("act", lambda e, pb=pb, pt_=pt_: e.activation(out=pt_[:, 640:896], in_=pb[:, 128:384], func=AF.Exp, scale=sc),
                      reads=[PB], writes=[PTB])
                def mmv(e, acc=acc, pt_=pt_, h=h):
                    o = acc[:, (h % 4) * 65:(h % 4) * 65 + 65]
                    for cb in range(2):
                        ins = e.matmul(o, lhsT=pt_[:, 640 + cb * 128:768 + cb * 128], rhs=vca[:, cb, h, :], start=(cb == 0), stop=(cb == 1))
                    return ins
                kb.op("pe", mmv, reads=[PTB, VCA], writes=[ACC])
            if h % 4 == 3:
                g4 = h // 4
                av = acc[:, 0:260].rearrange("p (h c) -> p h c", c=65)
                kb.op("dve", lambda e, av=av, g4=g4: e.reciprocal(out=rc[:, g4 * 4:g4 * 4 + 4], in_=av[:, :, 64]), reads=[ACC], writes=[RC])
                kb.op("dve", lambda e, av=av, g4=g4, a_t=a_t: e.tensor_tensor(
                    out=a_t[:, g4 * 256:(g4 + 1) * 256].rearrange("p (h d) -> p h d", d=64), in0=av[:, :, 0:64],
                    in1=rc[:, g4 * 4:g4 * 4 + 4].unsqueeze(2).to_broadcast([128, 4, 64]), op=ALU.mult), reads=[ACC, RC], writes=[AB])
        kb.dma("sp", ao[qt * 128:(qt + 1) * 128, 0:512], a_t[:], reads=[AB], writes=[AO])

    NBLK = (CTX + SEQ) // 128
    dk, DK = kb.sb("d_k", [128, CTX + SEQ], BF16)
    dva, DVA = kb.sb("d_va", [128, NBLK, 129], BF16)
    dq, DQ = kb.sb("d_q", [128, TALL], BF16)
    dpt = [kb.sb(f"d_pt{i}", [128, 512], BF16) for i in range(3)]
    o0, O0 = kb.sb("d_o0", [128, 128], F32)
    o1, O1 = kb.sb("d_o1", [128, 128], F32)
    osq, OSQ = kb.sb("d_osq", [128, 128], F32)
    dsm, DSM = kb.sb("d_sm", [128, 8], F32)
    dob = [kb.sb(f"d_ob{i}", [128, 128], BF16) for i in range(2)]
    si = 0
    oi = 0
    for h in range(4):
        kb.dma("sp", dk[:], dkT[h * 128:(h + 1) * 128, :], writes=[DK])
        for c0 in range(0, NBLK, 26):
            kb.dma("sp", dva[:, c0:c0 + 26, :], dv[c0 * 128:(c0 + 26) * 128, h * 129:(h + 1) * 129].rearrange("(b p) d -> p b d", p=128),
                   writes=[DVA])
        kb.dma("sp", dq[:], dqT[h * 128:(h + 1) * 128, :], writes=[DQ])
        qgroups = [(g * 256, 256, NBLK) for g in range(TQ // 256)] + [(TQ, CTX, CTX // 128)]
        for (q0, nq, nblk) in qgroups:
            nqs = nq // 128
            for blk in range(nblk):
                for m in range(2):
                    ps, PS = B[si % 3]
                    pt_, PTB = dpt[si % 3]
                    si += 1
                    kb.op("pe", lambda e, ps=ps, m=m, blk=blk, q0=q0, nq=nq: e.matmul(
                        ps[:, 0:nq], lhsT=dk[m * 64:(m + 1) * 64, blk * 128:(blk + 1) * 128], rhs=dq[m * 64:(m + 1) * 64, q0:q0 + nq],
                        start=True, stop=True), reads=[DK, DQ], writes=[PS])
                    kb.op("act", lambda e, ps=ps, pt_=pt_, nq=nq: e.activation(out=pt_[:, 0:nq], in_=ps[:, 0:nq], func=AF.Exp, scale=sc),
                          reads=[PS], writes=[PTB])
                    def mmv(e, pt_=pt_, m=m, blk=blk, nqs=nqs, nblk=nblk):
                        for qs in range(nqs):
                            a = qs * 2 + m
                            acc = B[3 + a][0]
                            ins = e.matmul(acc[:, 0:129], lhsT=pt_[:, qs * 128:(qs + 1) * 128], rhs=dva[:, blk, :],
                                           start=(blk == 0), stop=(blk == nblk - 1))
                        return ins
                    kb.op("pe", mmv, reads=[PTB, DVA], writes=[B[3 + qs * 2 + m][1] for qs in range(nqs)])
            for qs in range(nqs):
                accs = []
                for m in range(2):
                    a = qs * 2 + m
                    accs.append((B[3 + a][0][:, 0:129], B[3 + a][1]))
                (a0, A0), (a1, A1) = accs
                kb.op("dve", lambda e, a0=a0: e.reciprocal(out=dsm[:, 0:1], in_=a0[:, 128:129]), reads=[A0], writes=[DSM])
                kb.op("dve", lambda e, a1=a1: e.reciprocal(out=dsm[:, 1:2], in_=a1[:, 128:129]), reads=[A1], writes=[DSM])
                kb.op("dve", lambda e: e.tensor_tensor(out=dsm[:, 1:2], in0=dsm[:, 1:2], in1=lms[:, 6:7], op=ALU.mult), reads=[DSM, LMS], writes=[DSM])
                kb.op("dve", lambda e, a0=a0: e.tensor_scalar(out=o0[:], in0=a0[:, 0:128], scalar1=dsm[:, 0:1], scalar2=None, op0=ALU.mult),
                      reads=[A0, DSM], writes=[O0])
                kb.op("dve", lambda e, a1=a1: e.scalar_tensor_tensor(out=o1[:], in0=a1[:, 0:128], scalar=dsm[:, 1:2], in1=o0[:],
                                                                   op0=ALU.mult, op1=ALU.add), reads=[A1, DSM, O0], writes=[O1])
                kb.op("act", lambda e: e.activation(out=osq[:], in_=o1[:], func=AF.Square, accum_out=dsm[:, 2:3]), reads=[O1], writes=[OSQ, DSM])
                kb.op("dve", lambda e: e.tensor_scalar(out=dsm[:, 3:4], in0=dsm[:, 2:3], scalar1=1.0 / 128, scalar2=EPS, op0=ALU.mult, op1=ALU.add),
                      reads=[DSM], writes=[DSM])
                kb.op("act", lambda e: e.activation(out=dsm[:, 4:5], in_=dsm[:, 3:4], func=AF.Sqrt), reads=[DSM], writes=[DSM])
                kb.op("dve", lambda e: e.reciprocal(out=dsm[:, 5:6], in_=dsm[:, 4:5]), reads=[DSM], writes=[DSM])
                ob, OB = dob[oi % 2]
                oi += 1
                kb.op("dve", lambda e, ob=ob: e.scalar_tensor_tensor(out=ob[:], in0=o1[:], scalar=dsm[:, 5:6], in1=subg[:], op0=ALU.mult, op1=ALU.mult),
                      reads=[O1, DSM, SUBG], writes=[OB])
                r0 = q0 + qs * 128
                kb.dma("sp", ao[r0:r0 + 128, 512 + h * 128:512 + (h + 1) * 128], ob[:], reads=[OB], writes=[AO])


def emit_attn_odd(kb, ao, AO):
    B = kb.banks
    HALO = TQ + 256
    qin = kb.din("swa_q", [64, 16, TALL], BF16)
    kin = kb.din("swa_kT_h", [64, 4, HALO], BF16)
    vin = kb.din("swa_v_h", [HALO, 260], BF16)
    kcin = kb.din("swa_kT_c", [64, 4, CTX], BF16)
    vcin = kb.din("swa_v_c", [CTX, 260], BF16)
    sbias = kb.din("sbias", [3, 128, 384])
    sink_in = kb.din("sink", [16])
    sc = 64 ** -0.5
    snk, SNK = kb.sb("snk", [128, 16], F32)
    kb.dma("sp", snk[:], sink_in.unsqueeze(0).to_broadcast([128, 16]), writes=[SNK])
    kb.op("act", lambda e: e.activation(out=snk[:], in_=snk[:], func=AF.Exp), reads=[SNK], writes=[SNK])
    kT, KT = kb.sb("s_kT", [64, 4, HALO], BF16)
    kb.dma("sp", kT[:], kin, writes=[KT])
    kcT, KCT = kb.sb("s_kcT", [64, 4, CTX], BF16)
    kb.dma("sp", kcT[:], kcin, writes=[KCT])
    va, VA = kb.sb("s_va", [128, HALO // 128, 4, 65], BF16)
    kb.dma("sp", va[:].rearrange("p b h c -> p b (h c)"), vin.rearrange("(b p) c -> p b c", p=128), writes=[VA])
    vca, VCA = kb.sb("s_vca", [128, 2, 4, 65], BF16)
    kb.dma("sp", vca[:].rearrange("p b h c -> p b (h c)"), vcin.rearrange("(b p) c -> p b c", p=128), writes=[VCA])
    sb_, SBB = kb.sb("s_bias", [128, 3, 384], F32)
    kb.dma("sp", sb_[:], sbias.rearrange("s p n -> p s n"), writes=[SBB])
    qts = [kb.sb(f"s_q{i}", [64, 16, 128], BF16) for i in range(2)]
    sts = [kb.sb(f"s_st{i}", [128, 512], F32) for i in range(2)]
    pts = [kb.sb(f"s_pt{i}", [128, 5, 512], BF16) for i in range(2)]
    aot = [kb.sb(f"s_ao{i}", [128, D], BF16) for i in range(2)]
    den, DEN = kb.sb("s_den", [128, 8], F32)
    si = 0
    gi = 0
    for qt in range(NT):
        isctx = qt >= NTL
        q_t, QB = qts[qt % 2]
        kb.dma("sp", q_t[:], qin[:, :, qt * 128:(qt + 1) * 128], writes=[QB])
        a_t, AB = aot[qt % 2]
        slot = 0 if qt == 0 else 2 if qt == NTL - 1 else 1
        for n in range(4):
            pt_, PTB = pts[gi % 2]
            acc, ACC = B[4 + gi % 2]
            gi += 1
            blks = ([] if isctx else [0, 1, 2]) + [3, 4]
            for blk in blks:
                ps, PS = B[si % 3]
                st_, STB = sts[si % 2]
                si += 1
                if blk < 3:
                    kb.op("pe", lambda e, ps=ps, blk=blk, n=n, q_t=q_t, qt=qt: e.matmul(
                        ps[:], lhsT=kT[:, n, (qt + blk) * 128:(qt + blk + 1) * 128], rhs=q_t[:, n * 4:(n + 1) * 4, :], start=True, stop=True),
                        reads=[KT, QB], writes=[PS])
                    kb.op("dve", lambda e, ps=ps, st_=st_, blk=blk, slot=slot: e.scalar_tensor_tensor(
                        out=st_[:].rearrange("p (g q) -> p g q", g=4), in0=ps[:].rearrange("p (g q) -> p g q", g=4), scalar=sc,
                        in1=sb_[:, slot, blk * 128:(blk + 1) * 128].unsqueeze(1).to_broadcast([128, 4, 128]), op0=ALU.mult, op1=ALU.add),
                        reads=[PS, SBB], writes=[STB])
                    kb.op("act", lambda e, st_=st_, pt_=pt_, blk=blk: e.activation(out=pt_[:, blk, :], in_=st_[:], func=AF.Exp), reads=[STB], writes=[PTB])
                else:
                    cb = blk - 3
                    kb.op("pe", lambda e, ps=ps, cb=cb, n=n, q_t=q_t: e.matmul(
                        ps[:], lhsT=kcT[:, n, cb * 128:(cb + 1) * 128], rhs=q_t[:, n * 4:(n + 1) * 4, :], start=True, stop=True),
                        reads=[KCT, QB], writes=[PS])
                    kb.op("act", lambda e, ps=ps, pt_=pt_, blk=blk: e.activation(out=pt_[:, blk, :], in_=ps[:], func=AF.Exp, scale=sc),
                          reads=[PS], writes=[PTB])
            def mmv(e, acc=acc, pt_=pt_, n=n, qt=qt, blks=blks):
                for g in range(4):
                    o = acc[:, g * 65:(g + 1) * 65]
                    for i, blk in enumerate(blks):
                        rhs = va[:, qt + blk, n, :] if blk < 3 else vca[:, blk - 3, n, :]
                        ins = e.matmul(o, lhsT=pt_[:, blk, g * 128:(g + 1) * 128], rhs=rhs, start=(i == 0), stop=(i == len(blks) - 1))
                return ins
            kb.op("pe", mmv, reads=[PTB, VA, VCA], writes=[ACC])
            av = acc[:, 0:260].rearrange("p (g c) -> p g c", c=65)
            kb.op("dve", lambda e, av=av, n=n: e.tensor_tensor(out=den[:, 0:4], in0=av[:, :, 64], in1=snk[:, n * 4:(n + 1) * 4], op=ALU.add),
                  reads=[ACC, SNK], writes=[DEN])
            kb.op("dve", lambda e: e.reciprocal(out=den[:, 4:8], in_=den[:, 0:4]), reads=[DEN], writes=[DEN])
            kb.op("dve", lambda e, av=av, n=n, a_t=a_t: e.tensor_tensor(
                out=a_t[:, n * 256:(n + 1) * 256].rearrange("p (g d) -> p g d", d=64), in0=av[:, :, 0:64],
                in1=den[:, 4:8].unsqueeze(2).to_broadcast([128, 4, 64]), op=ALU.mult), reads=[ACC, DEN], writes=[AB])
        kb.dma("sp", ao[qt * 128:(qt + 1) * 128, :], a_t[:], reads=[AB], writes=[AO])


def build_post(even):
    kb = KB()
    kb.psum_banks()
    ao, AO = kb.dscratch("ao_scr", [TALL, D], BF16)
    with ExitStack() as es:
        saved = kb.es
        kb.es = es
        if even:
            emit_attn_even(kb, ao, AO)
        else:
            emit_attn_odd(kb, ao, AO)
        kb.es = saved
        kb.P.barrier()
    x_in = kb.din("x", [TALL, D])
    modrow = kb.din("modrow", [2, 6, D])
    w_out = kb.din("w_out", [D, D])
    n2g = kb.din("n2g", [D])
    fg = kb.din("fg", [D])
    wq = kb.din("wq", [D, 2048])
    keysT = kb.din("keysT", [128, 16, 128])
    uT = kb.din("uT", [D, 16384])
    v = kb.din("v", [16384, D])
    x_out, XOUT = kb.dout("x_out", [TALL, D])
    xn_out, XN = kb.dout("xn_out", [TALL, D])
    emit_post(kb, NT, x_in, ao, AO, modrow, w_out, n2g, wq, keysT, uT, v, x_out, XOUT, final_g=fg, xn_out=xn_out, XN=XN,
              tile_sets=[0] * NTL + [1] * NTC)
    return kb.finish()


GRID_W = 64
_PERM64 = np.concatenate([np.arange(16, 32), np.arange(0, 16), np.arange(48, 64), np.arange(32, 48)])
_SGN64 = np.concatenate([-np.ones(16), np.ones(16), -np.ones(16), np.ones(16)]).astype(np.float32)
_PROGS = {}


def _prog(name):
    if name not in _PROGS:
        kind, par = name.split("_")
        _PROGS[name] = build_pre(par == "even") if kind == "pre" else build_post(par == "even")
    return _PROGS[name]


def _rope_tables_T(tok0):
    t = np.arange(tok0, tok0 + TQ)
    row = (t // GRID_W).astype(np.float32)
    col = (t % GRID_W).astype(np.float32)
    half = 32
    inv = (10000.0 ** (-np.arange(0, half, 2, dtype=np.float32) / half)).astype(np.float32)
    ar = row[:, None] * inv
    ac = col[:, None] * inv
    ang = np.concatenate([ar, ar, ac, ac], axis=-1)
    cos = np.cos(ang).astype(np.float32)
    sin = np.sin(ang).astype(np.float32) * _SGN64[None, :]
    cosT = np.ones((128, TALL), np.float32)
    sinT = np.zeros((128, TALL), np.float32)
    cosT[:, :TQ] = np.tile(cos.T, (2, 1))
    sinT[:, :TQ] = np.tile(sin.T, (2, 1))
    return cosT, sinT


def _na_bias(rpb, R0):
    H = rpb.shape[0]
    out = np.full((5, H, 128, 5, 128), NEG, np.float32)
    kk = np.arange(128)
    qq = np.arange(128)
    for slot, r0 in enumerate((R0, R0 + 2, R0 + 32, R0 + 60, R0 + 62)):
        if slot == 2:
            r0 = 100 if R0 not in (0,) else 100
        qr = r0 + qq // 64
        qc = qq % 64
        rs = np.clip(qr - 4, 0, 256 - 8)
        cs = np.clip(qc - 8, 0, 64 - 16)
        for blk in range(5):
            kr = r0 - 4 + 2 * blk + kk // 64
            kc = kk % 64
            valid = ((kr[:, None] >= rs[None, :]) & (kr[:, None] < rs[None, :] + 8) &
                     (kc[:, None] >= cs[None, :]) & (kc[:, None] < cs[None, :] + 16) &
                     (kr[:, None] >= 0) & (kr[:, None] < 256))
            dr = np.clip(kr[:, None] - qr[None, :] + 7, 0, 14)
            dc = np.clip(kc[:, None] - qc[None, :] + 15, 0, 30)
            vals = rpb[:, dr, dc]
            out[slot, :, :, blk, :] = np.where(valid[None], vals, NEG)
    return out.reshape(5, H, 128, 640)


def _swa_bias(qr):
    out = np.full((3, 128, 3, 128), NEG, np.float32)
    k = np.arange(128)[:, None]
    q = np.arange(128)[None, :]
    for slot, gb in enumerate((qr * 32, qr * 32 + 5, qr * 32 + 31)):
        for blk in range(3):
            kb_ = gb - 1 + blk
            if kb_ < 0 or kb_ >= SEQ // 128:
                continue
            diff = (blk - 1) * 128 + k - q
            out[slot, :, blk, :] = np.where(np.abs(diff) <= 128, 0.0, NEG)
    return out.reshape(3, 128, 384)


def _ones_col(v, nh, dv):
    T = v.shape[0]
    o = np.ones((T, nh, dv + 1), v.dtype)
    o[:, :, :dv] = v.reshape(T, nh, dv)
    return o.reshape(T, nh * (dv + 1))


def _halo(arr, axis, lo, hi):
    n = arr.shape[axis]
    shape = list(arr.shape)
    shape[axis] = hi - lo
    out = np.zeros(shape, arr.dtype)
    s0, s1 = max(lo, 0), min(hi, n)
    src = [slice(None)] * arr.ndim
    dst = [slice(None)] * arr.ndim
    src[axis] = slice(s0, s1)
    dst[axis] = slice(s0 - lo, s1 - lo)
    out[tuple(dst)] = arr[tuple(src)]
    return out


def _run(name, in_maps):
    res = run_bass_kernel_spmd(_prog(name), in_maps, core_ids=list(range(NCORES)))
    return res.results


def kernel(x, c, ctx, c_ctx, w_mod, b_mod, norm1_g, norm2_g, w_in_even, w_out_even, na_rpb, diff_lambda,
           diff_subln_g, w_in_odd, w_out_odd, swa_sink, peer_wq, peer_keys, peer_u, peer_v, final_g, _nlayers=4, _dbg=None):
    f32 = np.float32
    x = np.asarray(x, f32)
    ctx = np.asarray(ctx, f32)
    ident = np.eye(128, dtype=f32)
    xs = []
    for core in range(NCORES):
        b, qr = divmod(core, 4)
        xs.append(np.concatenate([x[b, qr * TQ:(qr + 1) * TQ], ctx[b]], axis=0))
    csT = []
    for core in range(NCORES):
        b = core // 4
        cs = np.stack([np.asarray(c, f32)[b], np.asarray(c_ctx, f32)], 0)
        csT.append(np.ascontiguousarray(cs.reshape(2, 8, 128).transpose(2, 0, 1).reshape(128, 16)))
    ropes = [_rope_tables_T((core % 4) * TQ) for core in range(NCORES)]
    xn = None
    for i in range(_nlayers):
        even = (i % 2 == 0)
        j = i // 2
        w_in = np.asarray(w_in_even[j] if even else w_in_odd[j], f32)
        if even:
            rc = w_in[:, 1536:2560]
        else:
            rc = w_in[:, 0:1280]
        w_perm = np.ascontiguousarray(rc.reshape(D, -1, 64)[:, :, _PERM64].reshape(D, -1))
        ims = []
        for core in range(NCORES):
            ims.append({"x": xs[core], "csT": csT[core], "w_mod": np.asarray(w_mod[i], f32), "b_mod": np.asarray(b_mod[i], f32),
                        "n1g": np.asarray(norm1_g[i], f32), "w_in": w_in, "w_perm": w_perm,
                        "cosT": ropes[core][0], "sinT": ropes[core][1], "c_ident": ident})
        pre = _run("pre_even" if even else "pre_odd", ims)
        fm = [np.asarray(r["fmT"]) for r in pre]
        tm = [np.asarray(r["tm"]) for r in pre]
        if _dbg is not None:
            _dbg[f"pre{i}"] = (fm, tm, [np.asarray(r["modrow"]) for r in pre])
        keysT = np.ascontiguousarray(np.asarray(peer_keys[i], f32).reshape(16, 128, 128).transpose(2, 0, 1))
        common = {"w_out": np.asarray(w_out_even[j] if even else w_out_odd[j], f32), "n2g": np.asarray(norm2_g[i], f32),
                  "fg": np.asarray(final_g, f32), "wq": np.asarray(peer_wq[i], f32), "keysT": keysT,
                  "uT": np.ascontiguousarray(np.asarray(peer_u[i], f32).T), "v": np.asarray(peer_v[i], f32), "c_ident": ident}
        ims = []
        for core in range(NCORES):
            b, qr = divmod(core, 4)
            grp = [b * 4 + k for k in range(4)]
            im = dict(common)
            im["x"] = xs[core]
            im["modrow"] = np.asarray(pre[core]["modrow"])
            if even:
                kT_lat = np.concatenate([fm[g][512:1024, :TQ] for g in grp], axis=1)
                v_lat = np.concatenate([tm[g][:TQ, 0:512] for g in grp], axis=0)
                lo = (qr * 64 - 4) * 64
                im["naqT"] = np.ascontiguousarray(fm[core][0:512])
                im["nakT_h"] = _halo(kT_lat, 1, lo, lo + 74 * 64)
                im["nav_h"] = _ones_col(_halo(v_lat, 0, lo, lo + 74 * 64), 8, 64)
                im["nakT_c"] = np.ascontiguousarray(fm[core][512:1024, TQ:])
                im["nav_c"] = _ones_col(tm[core][TQ:, 0:512], 8, 64)
                im["dqT"] = np.ascontiguousarray(fm[core][1024:1536])
                im["dkT_all"] = np.concatenate([fm[core][1536:2048, TQ:]] + [fm[g][1536:2048, :TQ] for g in grp], axis=1)
                im["dv_all"] = _ones_col(np.concatenate([tm[core][TQ:, 512:1024]] + [tm[g][:TQ, 512:1024] for g in grp], axis=0), 4, 128)
                im["nbias"] = _na_bias(np.asarray(na_rpb[j], f32), qr * 64)
                im["lam"] = np.asarray(diff_lambda[j], f32)
                im["subg"] = np.asarray(diff_subln_g[j], f32)
                li = 0.8 - 0.6 * math.exp(-0.3 * i)
                im["lam_init"] = np.tile(np.array([[li, 1.0 - li]], f32), (128, 1))
            else:
                kT_lat = np.concatenate([fm[g][1024:1280, :TQ] for g in grp], axis=1)
                v_lat = np.concatenate([tm[g][:TQ, :] for g in grp], axis=0)
                lo = qr * TQ - 128
                im["swa_q"] = np.ascontiguousarray(fm[core][0:1024].reshape(16, 64, TALL).transpose(1, 0, 2))
                im["swa_kT_h"] = np.ascontiguousarray(_halo(kT_lat, 1, lo, lo + TQ + 256).reshape(4, 64, TQ + 256).transpose(1, 0, 2))
                im["swa_v_h"] = _ones_col(_halo(v_lat, 0, lo, lo + TQ + 256), 4, 64)
                im["swa_kT_c"] = np.ascontiguousarray(fm[core][1024:1280, TQ:].reshape(4, 64, CTX).transpose(1, 0, 2))
                im["swa_v_c"] = _ones_col(tm[core][TQ:, :], 4, 64)
                im["sbias"] = _swa_bias(qr)
                im["sink"] = np.asarray(swa_sink[j], f32)
            ims.append(im)
        post = _run("post_even" if even else "post_odd", ims)
        xs = [np.asarray(r["x_out"]) for r in post]
        xn = [np.asarray(r["xn_out"]) for r in post]
        if _dbg is not None:
            _dbg[f"x{i}"] = xs
    out = np.zeros((2, SEQ, D), f32)
    for core in range(NCORES):
        b, qr = divmod(core, 4)
        out[b, qr * TQ:(qr + 1) * TQ] = xn[core][:TQ]
    return out


RG = [[0, 1, 2, 3], [4, 5, 6, 7]]


def emit_pre_f(kb, even, x_src, XS, csT, w_mod, b_mod, n1g_in, w_in, w_perm, cosT, sinT, fm_out, FMO, tm_out, TMO, modrow, MRO):
    B = kb.banks
    if even:
        NIN = 3072
        fm_blocks = [(c * 128, None) for c in range(8)] + [(1536 + c * 128, c) for c in range(8)]
        tm_groups = [(1024, 512, 8, 64, 0), (2560, 512, 4, 128, 520)]
    else:
        NIN = 1536
        fm_blocks = [(c * 128, c) for c in range(10)]
        tm_groups = [(1280, 256, 4, 64, 0)]
    NROPE = 128 * sum(1 for _, r in fm_blocks if r is not None)
    ident_f, IDF, ident, ID = kb.ident()
    win, WIN = kb.sb("win", [128, 8, NIN], BF16)
    wpm, WPM = kb.sb("wpm", [128, 8, NROPE], BF16)
    for c0 in range(0, NIN, 512):
        kb.dma("pool", win[:, :, c0:c0 + 512], w_in[:, c0:c0 + 512].rearrange("(k p) n -> p k n", p=128), writes=[WIN])
    for c0 in range(0, NROPE, 512):
        n = min(512, NROPE - c0)
        kb.dma("pool", wpm[:, :, c0:c0 + n], w_perm[:, c0:c0 + n].rearrange("(k p) n -> p k n", p=128), writes=[WPM])
    cs_f, CSF = kb.sb("cs_f", [128, 16], F32)
    cs_b, CSB = kb.sb("cs_b", [128, 16], BF16)
    csbc, CSBC = kb.sb("csbc", [128, 16, 128], BF16)
    kb.dma("sp", cs_f[:], csT, writes=[CSF])
    kb.op("act", lambda e: e.activation(out=cs_b[:], in_=cs_f[:], func=AF.Silu), reads=[CSF], writes=[CSB])
    kb.op("dve", lambda e: e.tensor_copy(out=csbc[:], in_=cs_b[:].unsqueeze(2).to_broadcast([128, 16, 128])),
          reads=[CSB], writes=[CSBC])
    n1g, N1G = kb.sb("n1g_sb", [128, D], F32)
    kb.dma("sp", n1g[:], n1g_in.unsqueeze(0).to_broadcast([128, D]), writes=[N1G])
    g1s, G1S = kb.sb("g1s", [128, 2, 2, D], F32)
    wmr = [kb.sb(f"wmr{i}", [128, 8, 512], BF16) for i in range(2)]
    bmr = [kb.sb(f"bmr{i}", [128, 512], F32) for i in range(2)]
    mtmp = [kb.sb(f"mtmp{i}", [128, 512], F32) for i in range(2)]
    for cgp in range(12):
        wt, WB = wmr[cgp % 2]
        bt, BB = bmr[cgp % 2]
        cols = slice(cgp * 512, (cgp + 1) * 512)
        kb.dma("pool", wt[:], w_mod[:, cols].rearrange("(k p) n -> p k n", p=128), writes=[WB])
        kb.dma("sp", bt[:], b_mod[cols].unsqueeze(0).to_broadcast([128, 512]), writes=[BB])
        chunk = cgp // 2
        half = cgp % 2
        for s in range(2):
            mb, MB = B[6 + s]
            def mmm(e, mb=mb, wt=wt, s=s):
                for k in range(8):
                    ins = e.matmul(mb[:], lhsT=csbc[:, s * 8 + k, :], rhs=wt[:, k, :], start=(k == 0), stop=(k == 7))
                return ins
            kb.op("pe", mmm, reads=[CSBC, WB], writes=[MB])
            if chunk < 2:
                dst = g1s[:, s, chunk, half * 512:(half + 1) * 512]
                kb.op("dve", lambda e, mb=mb, bt=bt, dst=dst: e.tensor_tensor(out=dst, in0=mb[:], in1=bt[:], op=ALU.add),
                      reads=[MB, BB], writes=[G1S])
                kb.dma("sp", modrow[s, chunk:chunk + 1, half * 512:(half + 1) * 512], dst[0:1, :], reads=[G1S], writes=[MRO])
            else:
                mt, MT = mtmp[s]
                kb.op("dve", lambda e, mb=mb, bt=bt, mt=mt: e.tensor_tensor(out=mt[:], in0=mb[:], in1=bt[:], op=ALU.add),
                      reads=[MB, BB], writes=[MT])
                kb.dma("sp", modrow[s, chunk:chunk + 1, half * 512:(half + 1) * 512], mt[0:1, :], reads=[MT], writes=[MRO])
    for s in range(2):
        kb.op("dve", lambda e, s=s: e.scalar_tensor_tensor(out=g1s[:, s, 1, :], in0=g1s[:, s, 1, :], scalar=1.0, in1=n1g[:],
                                                           op0=ALU.add, op1=ALU.mult), reads=[G1S, N1G], writes=[G1S])
    xt = [kb.sb(f"xt{i}", [128, D], F32) for i in range(2)]
    tmp, TMP = kb.sb("tmp", [128, D], F32)
    hx, HX = kb.sb("hx", [128, D], BF16)
    hxT, HXT = kb.sb("hxT", [128, 8, 512], BF16)
    sm, SM = kb.sb("sm", [128, 8], F32)
    cst = [kb.sb(f"cst{i}", [128, 512], F32) for i in range(2)]
    snt = [kb.sb(f"snt{i}", [128, 512], F32) for i in range(2)]
    r1, R1 = kb.sb("r1", [128, 512], F32)
    r2, R2 = kb.sb("r2", [128, 512], F32)
    fmo = [kb.sb(f"fmo{i}", [128, 512], BF16) for i in range(2)]
    tmo = [kb.sb(f"tmo{i}", [128, 520], BF16) for i in range(2)]
    for to, TO in tmo:
        kb.op("pool", lambda e, to=to: e.memset(to[:], 1.0), writes=[TO])
    groups = [(g * 4, 4, 0) for g in range(NTL // 4)] + [(NTL, NTC, 1)]
    xi = fi = ti = 0
    for gi, (t0, ntl, s) in enumerate(groups):
        NTOK = ntl * 128
        tok0 = t0 * 128
        ct, CT = cst[gi % 2]
        st, ST = snt[gi % 2]
        kb.dma("sp", ct[:, 0:NTOK], cosT[:, tok0:tok0 + NTOK], writes=[CT])
        kb.dma("sp", st[:, 0:NTOK], sinT[:, tok0:tok0 + NTOK], writes=[ST])
        for j in range(ntl):
            x_t, XB = xt[xi % 2]
            xi += 1
            rows = slice(tok0 + j * 128, tok0 + (j + 1) * 128)
            kb.dma("sp", x_t[:], x_src[rows, :], reads=[XS], writes=[XB])
            kb.op("act", lambda e, x_t=x_t: e.activation(out=tmp[:], in_=x_t[:], func=AF.Square, accum_out=sm[:, 0:1]),
                  reads=[XB], writes=[TMP, SM])
            rstd_ops(kb, sm, SM)
            kb.op("dve", lambda e, x_t=x_t, s=s: e.scalar_tensor_tensor(out=tmp[:], in0=x_t[:], scalar=sm[:, 1:2], in1=g1s[:, s, 1, :],
                                                                         op0=ALU.mult, op1=ALU.mult), reads=[XB, SM, G1S], writes=[TMP])
            kb.op("pool", lambda e, s=s: e.tensor_tensor(out=hx[:], in0=tmp[:], in1=g1s[:, s, 0, :], op=ALU.add),
                  reads=[TMP, G1S], writes=[HX])
            tb, TB = B[0]
            def tr(e, tb=tb):
                for k in range(8):
                    ins = e.transpose(tb.bitcast(BF16)[:, k * 128:(k + 1) * 128], hx[:, k * 128:(k + 1) * 128], ident[:])
                return ins
            kb.op("pe", tr, reads=[HX, ID], writes=[TB])
            kb.op("act", lambda e, tb=tb, j=j: e.copy(out=hxT[:, :, j * 128:(j + 1) * 128],
                                                      in_=tb.bitcast(BF16)[:, 0:1024].rearrange("p (k t) -> p k t", k=8)),
                  reads=[TB], writes=[HXT])
        for bi, (c0, ridx) in enumerate(fm_blocks):
            pa, PA = B[1 + bi % 2]
            def mma(e, pa=pa, c0=c0, NTOK=NTOK):
                for k in range(8):
                    ins = e.matmul(pa[:, 0:NTOK], lhsT=win[:, k, c0:c0 + 128], rhs=hxT[:, k, 0:NTOK], start=(k == 0), stop=(k == 7))
                return ins
            kb.op("pe", mma, reads=[WIN, HXT], writes=[PA])
            fo, FO = fmo[fi % 2]
            fi += 1
            if ridx is None:
                kb.op("act", lambda e, pa=pa, fo=fo, NTOK=NTOK: e.copy(out=fo[:, 0:NTOK], in_=pa[:, 0:NTOK]), reads=[PA], writes=[FO])
            else:
                pb, PB = B[3 + bi % 2]
                def mmb(e, pb=pb, ridx=ridx, NTOK=NTOK):
                    for k in range(8):
                        ins = e.matmul(pb[:, 0:NTOK], lhsT=wpm[:, k, ridx * 128:(ridx + 1) * 128], rhs=hxT[:, k, 0:NTOK],
                                       start=(k == 0), stop=(k == 7))
                    return ins
                kb.op("pe", mmb, reads=[WPM, HXT], writes=[PB])
                kb.op("dve", lambda e, pa=pa, ct=ct, NTOK=NTOK: e.tensor_tensor(out=r1[:, 0:NTOK], in0=pa[:, 0:NTOK], in1=ct[:, 0:NTOK], op=ALU.mult),
                      reads=[PA, CT], writes=[R1])
                kb.op("dve", lambda e, pb=pb, st=st, NTOK=NTOK: e.tensor_tensor(out=r2[:, 0:NTOK], in0=pb[:, 0:NTOK], in1=st[:, 0:NTOK], op=ALU.mult),
                      reads=[PB, ST], writes=[R2])
                kb.op("pool", lambda e, fo=fo, NTOK=NTOK: e.tensor_tensor(out=fo[:, 0:NTOK], in0=r1[:, 0:NTOK], in1=r2[:, 0:NTOK], op=ALU.add),
                      reads=[R1, R2], writes=[FO])
            kb.dma("sp", fm_out[bi * 128:(bi + 1) * 128, tok0:tok0 + NTOK], fo[:, 0:NTOK], reads=[FO], writes=[FMO])
        for j in range(ntl):
            for (c0, ncol, nh, dv, oc) in tm_groups:
                pt, PT = B[5 + ti % 2]
                to, TO = tmo[ti % 2]
                ti += 1
                def mmt(e, pt=pt, c0=c0, ncol=ncol, j=j):
                    for k in range(8):
                        ins = e.matmul(pt[:, 0:ncol], lhsT=hxT[:, k, j * 128:(j + 1) * 128], rhs=win[:, k, c0:c0 + ncol],
                                       start=(k == 0), stop=(k == 7))
                    return ins
                kb.op("pe", mmt, reads=[HXT, WIN], writes=[PT])
                wdt = nh * (dv + 1)
                kb.op("act", lambda e, pt=pt, to=to, ncol=ncol, nh=nh, dv=dv, wdt=wdt: e.copy(
                    out=to[:, 0:wdt].rearrange("p (h c) -> p h c", c=dv + 1)[:, :, 0:dv],
                    in_=pt[:, 0:ncol].rearrange("p (h c) -> p h c", c=dv)), reads=[PT], writes=[TO])
                rows = slice(tok0 + j * 128, tok0 + (j + 1) * 128)
                kb.dma("sp", tm_out[rows, oc:oc + wdt], to[:, 0:wdt], reads=[TO], writes=[TMO])


NA_NBMAX = 12


def _na_blocklist(qt):
    if qt == 0:
        return 0, 4, [("tail", 0), ("tail", 1)]
    if qt == 1:
        return 0, 4, [("tail", 1)]
    if qt == NTL - 2:
        return TQ - 512, 4, [("head", 0)]
    if qt == NTL - 1:
        return TQ - 512, 4, [("head", 0), ("head", 1)]
    return qt * 128 - 256, 5, []


def emit_attn_even_f(kb, fmT, FMT, tmv, TMV, dk_recv, DKR, dv_recv, DVR, nbk_recv, NBKR, nbv_recv, NBVR,
                     nbias, lam_in, subg_in, lami_in, ao, AO):
    B = kb.banks
    sc = 64 ** -0.5
    lm, LM = kb.sb("lm", [128, 4, 64], F32)
    lms, LMS = kb.sb("lms", [128, 16], F32)
    subg, SUBG = kb.sb("subg_sb", [128, 128], F32)
    kb.dma("sp", lm[:].rearrange("p a b -> p (a b)"), lam_in.rearrange("a b -> (a b)").unsqueeze(0).to_broadcast([128, 256]), writes=[LM])
    kb.dma("sp", subg[:], subg_in.unsqueeze(0).to_broadcast([128, 128]), writes=[SUBG])
    kb.dma("sp", lms[:, 8:10], lami_in, writes=[LMS])
    kb.op("dve", lambda e: e.tensor_tensor(out=lm[:, 0, :], in0=lm[:, 0, :], in1=lm[:, 1, :], op=ALU.mult), reads=[LM], writes=[LM])
    kb.op("dve", lambda e: e.tensor_tensor(out=lm[:, 2, :], in0=lm[:, 2, :], in1=lm[:, 3, :], op=ALU.mult), reads=[LM], writes=[LM])
    kb.op("dve", lambda e: e.tensor_reduce(out=lms[:, 0:1], in_=lm[:, 0, :], axis=AX.X, op=ALU.add), reads=[LM], writes=[LMS])
    kb.op("dve", lambda e: e.tensor_reduce(out=lms[:, 1:2], in_=lm[:, 2, :], axis=AX.X, op=ALU.add), reads=[LM], writes=[LMS])
    kb.op("act", lambda e: e.activation(out=lms[:, 2:4], in_=lms[:, 0:2], func=AF.Exp), reads=[LMS], writes=[LMS])
    kb.op("dve", lambda e: e.tensor_tensor(out=lms[:, 4:5], in0=lms[:, 2:3], in1=lms[:, 3:4], op=ALU.subtract), reads=[LMS], writes=[LMS])
    kb.op("dve", lambda e: e.tensor_tensor(out=lms[:, 5:6], in0=lms[:, 4:5], in1=lms[:, 8:9], op=ALU.add), reads=[LMS], writes=[LMS])
    kb.op("dve", lambda e: e.tensor_scalar(out=lms[:, 6:7], in0=lms[:, 5:6], scalar1=-1.0, scalar2=None, op0=ALU.mult), reads=[LMS], writes=[LMS])
    kb.op("dve", lambda e: e.tensor_scalar(out=subg[:], in0=subg[:], scalar1=lms[:, 9:10], scalar2=None, op0=ALU.mult), reads=[SUBG, LMS], writes=[SUBG])

    naqT = fmT[0:512, :]
    nakT = fmT[512:1024, :]
    kcT, KCT = kb.sb("na_kcT", [128, 4, CTX], BF16)
    vca, VCA = kb.sb("na_vca", [128, 2, 8, 65], BF16)
    kb.dma("sp", kcT[:], nakT[:, TQ:TALL].rearrange("(a p) n -> p a n", p=128), reads=[FMT], writes=[KCT])
    kb.dma("sp", vca[:].rearrange("p b h c -> p b (h c)"), tmv[TQ:TALL, 0:520].rearrange("(b p) c -> p b c", p=128), reads=[TMV], writes=[VCA])
    kts = [kb.sb(f"na_kt{i}", [128, 4, NA_NBMAX * 128], BF16) for i in range(2)]
    vts = [kb.sb(f"na_vt{i}", [128, NA_NBMAX, 8, 65], BF16) for i in range(2)]
    qts = [kb.sb(f"na_qt{i}", [128, 4, 128], BF16) for i in range(2)]
    bts = [kb.sb(f"na_bt{i}", [128, NA_NBMAX * 128], F32) for i in range(2)]
    sts = [kb.sb(f"na_st{i}", [128, NA_NBMAX * 128], F32) for i in range(2)]
    pts = [kb.sb(f"na_pt{i}", [128, (NA_NBMAX + 2) * 128], BF16) for i in range(2)]
    aot = [kb.sb(f"na_ao{i}", [128, 512], BF16) for i in range(2)]
    rc, RC = kb.sb("na_rc", [128, 8], F32)
    hi = 0
    bk = 0
    for qt in range(NT):
        isctx = qt >= NTL
        q_t, QB = qts[qt % 2]
        kb.dma("sp", q_t[:], naqT[:, qt * 128:(qt + 1) * 128].rearrange("(a p) n -> p a n", p=128), reads=[FMT], writes=[QB])
        nb = 0
        if not isctx:
            k_t, KB_ = kts[qt % 2]
            v_t, VB = vts[qt % 2]
            o0, onb, cands = _na_blocklist(qt)
            kb.dma("sp", k_t[:, :, 0:onb * 128], nakT[:, o0:o0 + onb * 128].rearrange("(a p) n -> p a n", p=128), reads=[FMT], writes=[KB_])
            kb.dma("sp", v_t[:, 0:onb].rearrange("p b h c -> p b (h c)"), tmv[o0:o0 + onb * 128, 0:520].rearrange("(b p) c -> p b c", p=128),
                   reads=[TMV], writes=[VB])
            nb = onb
            ncb = len(cands)
            if ncb:
                which = cands[0][0]
                cb0 = cands[0][1]
                col0 = (0 if which == "tail" else 256) + cb0 * 128
                for r in range(4):
                    kb.dma("sp", k_t[:, :, nb * 128:(nb + ncb) * 128],
                           nbk_recv[r * 512:(r + 1) * 512, col0:col0 + ncb * 128].rearrange("(a p) n -> p a n", p=128), reads=[NBKR], writes=[KB_])
                    kb.dma("sp", v_t[:, nb:nb + ncb].rearrange("p b h c -> p b (h c)"),
                           nbv_recv[r * 512 + col0:r * 512 + col0 + ncb * 128, :].rearrange("(b p) c -> p b c", p=128), reads=[NBVR], writes=[VB])
                    nb += ncb
            slot = 0 if qt == 0 else 1 if qt == 1 else 3 if qt == NTL - 2 else 4 if qt == NTL - 1 else 2
        a_t, AB = aot[qt % 2]
        for h in range(8):
            a, off = h // 2, (h % 2) * 64
            st_, STB = sts[hi % 2]
            pt_, PTB = pts[hi % 2]
            acc, ACC = B[4 + (h // 4)]
            hi += 1
            if not isctx:
                b_t, BB = bts[hi % 2]
                kb.dma("sp", b_t[:, 0:nb * 128], nbias[slot, h, :, 0:nb * 128], writes=[BB])
                for c0 in range(0, nb, 4):
                    cn = min(4, nb - c0)
                    ps, PS = B[bk % 4]
                    bk += 1
                    def mms(e, ps=ps, k_t=k_t, q_t=q_t, a=a, off=off, c0=c0, cn=cn):
                        for i in range(cn):
                            ins = e.matmul(ps[:, i * 128:(i + 1) * 128], lhsT=k_t[off:off + 64, a, (c0 + i) * 128:(c0 + i + 1) * 128],
                                           rhs=q_t[off:off + 64, a, :], start=True, stop=True)
                        return ins
                    kb.op("pe", mms, reads=[KB_, QB], writes=[PS])
                    kb.op("dve", lambda e, ps=ps, st_=st_, b_t=b_t, c0=c0, cn=cn: e.scalar_tensor_tensor(
                        out=st_[:, c0 * 128:(c0 + cn) * 128], in0=ps[:, 0:cn * 128], scalar=sc, in1=b_t[:, c0 * 128:(c0 + cn) * 128],
                        op0=ALU.mult, op1=ALU.add), reads=[PS, BB], writes=[STB])
                kb.op("act", lambda e, st_=st_, pt_=pt_, nb=nb: e.activation(out=pt_[:, 0:nb * 128], in_=st_[:, 0:nb * 128], func=AF.Exp),
                      reads=[STB], writes=[PTB])
            ps, PS = B[bk % 4]
            bk += 1
            def mmc(e, ps=ps, q_t=q_t, a=a, off=off):
                for cb in range(2):
                    ins = e.matmul(ps[:, cb * 128:(cb + 1) * 128], lhsT=kcT[off:off + 64, a, cb * 128:(cb + 1) * 128],
                                   rhs=q_t[off:off + 64, a, :], start=True, stop=True)
                return ins
            kb.op("pe", mmc, reads=[QB, KCT], writes=[PS])
            kb.op("act", lambda e, ps=ps, pt_=pt_, nb=nb: e.activation(out=pt_[:, nb * 128:(nb + 2) * 128], in_=ps[:, 0:256], func=AF.Exp, scale=sc),
                  reads=[PS], writes=[PTB])
            if not isctx:
                def mmv(e, acc=acc, pt_=pt_, v_t=v_t, h=h, nb=nb):
                    o = acc[:, (h % 4) * 65:(h % 4) * 65 + 65]
                    for blk in range(nb):
                        e.matmul(o, lhsT=pt_[:, blk * 128:(blk + 1) * 128], rhs=v_t[:, blk, h, :], start=(blk == 0), stop=False)
                    for cb in range(2):
                        ins = e.matmul(o, lhsT=pt_[:, (nb + cb) * 128:(nb + cb + 1) * 128], rhs=vca[:, cb, h, :], start=False, stop=(cb == 1))
                    return ins
                kb.op("pe", mmv, reads=[PTB, VB, VCA], writes=[ACC])
            else:
                def mmv(e, acc=acc, pt_=pt_, h=h):
                    o = acc[:, (h % 4) * 65:(h % 4) * 65 + 65]
                    for cb in range(2):
                        ins = e.matmul(o, lhsT=pt_[:, cb * 128:(cb + 1) * 128], rhs=vca[:, cb, h, :], start=(cb == 0), stop=(cb == 1))
                    return ins
                kb.op("pe", mmv, reads=[PTB, VCA], writes=[ACC])
            if h % 4 == 3:
                g4 = h // 4
                av = acc[:, 0:260].rearrange("p (h c) -> p h c", c=65)
                kb.op("dve", lambda e, av=av, g4=g4: e.reciprocal(out=rc[:, g4 * 4:g4 * 4 + 4], in_=av[:, :, 64]), reads=[ACC], writes=[RC])
                kb.op("dve", lambda e, av=av, g4=g4, a_t=a_t: e.tensor_tensor(
                    out=a_t[:, g4 * 256:(g4 + 1) * 256].rearrange("p (h d) -> p h d", d=64), in0=av[:, :, 0:64],
                    in1=rc[:, g4 * 4:g4 * 4 + 4].unsqueeze(2).to_broadcast([128, 4, 64]), op=ALU.mult), reads=[ACC, RC], writes=[AB])
        kb.dma("sp", ao[qt * 128:(qt + 1) * 128, 0:512], a_t[:], reads=[AB], writes=[AO])

    NBLK = (CTX + SEQ) // 128
    dk, DK = kb.sb("d_k", [128, CTX + SEQ], BF16)
    dva, DVA = kb.sb("d_va", [128, NBLK, 129], BF16)
    dq, DQ = kb.sb("d_q", [128, TALL], BF16)
    dpt = [kb.sb(f"d_pt{i}", [128, 256], BF16) for i in range(4)]
    o0_, O0 = kb.sb("d_o0", [128, 128], F32)
    o1, O1 = kb.sb("d_o1", [128, 128], F32)
    osq, OSQ = kb.sb("d_osq", [128, 128], F32)
    dsm, DSM = kb.sb("d_sm", [128, 8], F32)
    dob = [kb.sb(f"d_ob{i}", [128, 128], BF16) for i in range(2)]
    si = 0
    oi = 0
    for h in range(4):
        kb.dma("sp", dk[:, 0:CTX], fmT[1536 + h * 128:1536 + (h + 1) * 128, TQ:TALL], reads=[FMT], writes=[DK])
        for r in range(4):
            for k in range(4):
                kb.dma("sp", dk[:, CTX + r * TQ + k * 1024:CTX + r * TQ + (k + 1) * 1024],
                       dk_recv[k][0][r * 512 + h * 128:r * 512 + (h + 1) * 128, :], reads=[dk_recv[k][1]], writes=[DK])
        kb.dma("sp", dva[:, 0:2, :], tmv[TQ:TALL, 520 + h * 129:520 + (h + 1) * 129].rearrange("(b p) d -> p b d", p=128), reads=[TMV], writes=[DVA])
        for r in range(4):
            for k in range(8):
                b0 = 2 + (r * TQ + k * 512) // 128
                kb.dma("sp", dva[:, b0:b0 + 4, :], dv_recv[k][0][r * 512:(r + 1) * 512, h * 129:(h + 1) * 129].rearrange("(b p) d -> p b d", p=128),
                       reads=[dv_recv[k][1]], writes=[DVA])
        kb.dma("sp", dq[:], fmT[1024 + h * 128:1024 + (h + 1) * 128, :], reads=[FMT], writes=[DQ])
        qgroups = [(g * 256, 256, NBLK) for g in range(TQ // 256)] + [(TQ, CTX, CTX // 128)]
        for (q0, nq, nblk) in qgroups:
            nqs = nq // 128
            steps = [(blk, m) for blk in range(nblk) for m in range(2)]
            LOOK = 3
            bufs = {}
            for idx in range(len(steps) + LOOK):
                if idx < len(steps):
                    blk, m = steps[idx]
                    ps, PS = B[(0, 1, 2, 7)[si % 4]]
                    pt_, PTB = dpt[si % 4]
                    si += 1
                    bufs[idx] = (pt_, PTB)
                    kb.op("pe", lambda e, ps=ps, m=m, blk=blk, q0=q0, nq=nq: e.matmul(
                        ps[:, 0:nq], lhsT=dk[m * 64:(m + 1) * 64, blk * 128:(blk + 1) * 128], rhs=dq[m * 64:(m + 1) * 64, q0:q0 + nq],
                        start=True, stop=True), reads=[DK, DQ], writes=[PS])
                    kb.op("act", lambda e, ps=ps, pt_=pt_, nq=nq: e.activation(out=pt_[:, 0:nq], in_=ps[:, 0:nq], func=AF.Exp, scale=sc),
                          reads=[PS], writes=[PTB])
                if idx - LOOK >= 0:
                    blk, m = steps[idx - LOOK]
                    pt_, PTB = bufs.pop(idx - LOOK)
                    def mmv(e, pt_=pt_, m=m, blk=blk, nqs=nqs, nblk=nblk):
                        for qs in range(nqs):
                            acc = B[3 + qs * 2 + m][0]
                            ins = e.matmul(acc[:, 0:129], lhsT=pt_[:, qs * 128:(qs + 1) * 128], rhs=dva[:, blk, :],
                                           start=(blk == 0), stop=(blk == nblk - 1))
                        return ins
                    kb.op("pe", mmv, reads=[PTB, DVA], writes=[B[3 + qs * 2 + m][1] for qs in range(nqs)])
            for qs in range(nqs):
                (a0, A0), (a1, A1) = [(B[3 + qs * 2 + m][0][:, 0:129], B[3 + qs * 2 + m][1]) for m in range(2)]
                kb.op("dve", lambda e, a0=a0: e.reciprocal(out=dsm[:, 0:1], in_=a0[:, 128:129]), reads=[A0], writes=[DSM])
                kb.op("dve", lambda e, a1=a1: e.reciprocal(out=dsm[:, 1:2], in_=a1[:, 128:129]), reads=[A1], writes=[DSM])
                kb.op("dve", lambda e: e.tensor_tensor(out=dsm[:, 1:2], in0=dsm[:, 1:2], in1=lms[:, 6:7], op=ALU.mult), reads=[DSM, LMS], writes=[DSM])
                kb.op("dve", lambda e, a0=a0: e.tensor_scalar(out=o0_[:], in0=a0[:, 0:128], scalar1=dsm[:, 0:1], scalar2=None, op0=ALU.mult),
                      reads=[A0, DSM], writes=[O0])
                kb.op("dve", lambda e, a1=a1: e.scalar_tensor_tensor(out=o1[:], in0=a1[:, 0:128], scalar=dsm[:, 1:2], in1=o0_[:],
                                                                   op0=ALU.mult, op1=ALU.add), reads=[A1, DSM, O0], writes=[O1])
                kb.op("act", lambda e: e.activation(out=osq[:], in_=o1[:], func=AF.Square, accum_out=dsm[:, 2:3]), reads=[O1], writes=[OSQ, DSM])
                kb.op("dve", lambda e: e.tensor_scalar(out=dsm[:, 3:4], in0=dsm[:, 2:3], scalar1=1.0 / 128, scalar2=EPS, op0=ALU.mult, op1=ALU.add),
                      reads=[DSM], writes=[DSM])
                kb.op("act", lambda e: e.activation(out=dsm[:, 4:5], in_=dsm[:, 3:4], func=AF.Sqrt), reads=[DSM], writes=[DSM])
                kb.op("dve", lambda e: e.reciprocal(out=dsm[:, 5:6], in_=dsm[:, 4:5]), reads=[DSM], writes=[DSM])
                ob, OB = dob[oi % 2]
                oi += 1
                kb.op("dve", lambda e, ob=ob: e.scalar_tensor_tensor(out=ob[:], in0=o1[:], scalar=dsm[:, 5:6], in1=subg[:], op0=ALU.mult, op1=ALU.mult),
                      reads=[O1, DSM, SUBG], writes=[OB])
                r0 = q0 + qs * 128
                kb.dma("sp", ao[r0:r0 + 128, 512 + h * 128:512 + (h + 1) * 128], ob[:], reads=[OB], writes=[AO])


def emit_attn_odd_f(kb, fmT, FMT, tmv, TMV, sbk_recv, SBKR, sbv_recv, SBVR, sbias, sink_in, ao, AO):
    B = kb.banks
    sc = 64 ** -0.5
    snk, SNK = kb.sb("snk", [128, 16], F32)
    kb.dma("sp", snk[:], sink_in.unsqueeze(0).to_broadcast([128, 16]), writes=[SNK])
    kb.op("act", lambda e: e.activation(out=snk[:], in_=snk[:], func=AF.Exp), reads=[SNK], writes=[SNK])
    kT, KT = kb.sb("s_kT", [64, 4, TQ], BF16)
    kb.dma("sp", kT[:], fmT[1024:1280, 0:TQ].rearrange("(n d) t -> d n t", d=64), reads=[FMT], writes=[KT])
    kcT, KCT = kb.sb("s_kcT", [64, 4, CTX], BF16)
    kb.dma("sp", kcT[:], fmT[1024:1280, TQ:TALL].rearrange("(n d) t -> d n t", d=64), reads=[FMT], writes=[KCT])
    ck, CK = kb.sb("s_ck", [64, 4, 4, 256], BF16)
    for r in range(4):
        kb.dma("sp", ck[:, r], sbk_recv[r * 256:(r + 1) * 256, :].rearrange("(n d) t -> d n t", d=64), reads=[SBKR], writes=[CK])
    va, VA = kb.sb("s_va", [128, NTL, 4, 65], BF16)
    kb.dma("sp", va[:].rearrange("p b h c -> p b (h c)"), tmv[0:TQ, 0:260].rearrange("(b p) c -> p b c", p=128), reads=[TMV], writes=[VA])
    vca, VCA = kb.sb("s_vca", [128, 2, 4, 65], BF16)
    kb.dma("sp", vca[:].rearrange("p b h c -> p b (h c)"), tmv[TQ:TALL, 0:260].rearrange("(b p) c -> p b c", p=128), reads=[TMV], writes=[VCA])
    cv, CV = kb.sb("s_cv", [128, 8, 4, 65], BF16)
    kb.dma("sp", cv[:].rearrange("p b h c -> p b (h c)"), sbv_recv.rearrange("(b p) c -> p b c", p=128), reads=[SBVR], writes=[CV])
    sb_, SBB = kb.sb("s_bias", [128, 3, 768], F32)
    kb.dma("sp", sb_[:], sbias.rearrange("s p n -> p s n"), writes=[SBB])
    qts = [kb.sb(f"s_q{i}", [64, 16, 128], BF16) for i in range(2)]
    sts = [kb.sb(f"s_st{i}", [128, 512], F32) for i in range(2)]
    pts = [kb.sb(f"s_pt{i}", [128, 8, 512], BF16) for i in range(2)]
    aot = [kb.sb(f"s_ao{i}", [128, D], BF16) for i in range(2)]
    den, DEN = kb.sb("s_den", [128, 8], F32)
    si = 0
    gi = 0
    for qt in range(NT):
        isctx = qt >= NTL
        q_t, QB = qts[qt % 2]
        kb.dma("sp", q_t[:], fmT[0:1024, qt * 128:(qt + 1) * 128].rearrange("(h d) t -> d h t", d=64), reads=[FMT], writes=[QB])
        a_t, AB = aot[qt % 2]
        if isctx:
            nbl = []
            slot = 1
        elif qt == 0:
            nbl = [("own", 0), ("own", 1)] + [("cand", r, 0) for r in range(4)]
            slot = 0
        elif qt == NTL - 1:
            nbl = [("own", NTL - 2), ("own", NTL - 1)] + [("cand", r, 1) for r in range(4)]
            slot = 2
        else:
            nbl = [("own", qt - 1), ("own", qt), ("own", qt + 1)]
            slot = 1
        nnb = len(nbl)
        for n in range(4):
            pt_, PTB = pts[gi % 2]
            acc, ACC = B[4 + gi % 2]
            gi += 1
            for bi, bl in enumerate(nbl + [("ctx", 0), ("ctx", 1)]):
                ps, PS = B[si % 3]
                st_, STB = sts[si % 2]
                si += 1
                if bl[0] == "own":
                    lhsT = kT[:, n, bl[1] * 128:(bl[1] + 1) * 128]
                    rd = [KT, QB]
                elif bl[0] == "cand":
                    lhsT = ck[:, bl[1], n, bl[2] * 128:(bl[2] + 1) * 128]
                    rd = [CK, QB]
                else:
                    lhsT = kcT[:, n, bl[1] * 128:(bl[1] + 1) * 128]
                    rd = [KCT, QB]
                kb.op("pe", lambda e, ps=ps, lhsT=lhsT, n=n, q_t=q_t: e.matmul(ps[:], lhsT=lhsT, rhs=q_t[:, n * 4:(n + 1) * 4, :], start=True, stop=True),
                      reads=rd, writes=[PS])
                if bl[0] != "ctx":
                    kb.op("dve", lambda e, ps=ps, st_=st_, bi=bi, slot=slot: e.scalar_tensor_tensor(
                        out=st_[:].rearrange("p (g q) -> p g q", g=4), in0=ps[:].rearrange("p (g q) -> p g q", g=4), scalar=sc,
                        in1=sb_[:, slot, bi * 128:(bi + 1) * 128].unsqueeze(1).to_broadcast([128, 4, 128]), op0=ALU.mult, op1=ALU.add),
                        reads=[PS, SBB], writes=[STB])
                    kb.op("act", lambda e, st_=st_, pt_=pt_, bi=bi: e.activation(out=pt_[:, bi, :], in_=st_[:], func=AF.Exp), reads=[STB], writes=[PTB])
                else:
                    kb.op("act", lambda e, ps=ps, pt_=pt_, bi=bi: e.activation(out=pt_[:, bi, :], in_=ps[:], func=AF.Exp, scale=sc),
                          reads=[PS], writes=[PTB])
            allb = nbl + [("ctx", 0), ("ctx", 1)]
            def mmv(e, acc=acc, pt_=pt_, n=n, allb=allb):
                for g in range(4):
                    o = acc[:, g * 65:(g + 1) * 65]
                    for i, bl in enumerate(allb):
                        if bl[0] == "own":
                            rhs = va[:, bl[1], n, :]
                        elif bl[0] == "cand":
                            rhs = cv[:, bl[1] * 2 + bl[2], n, :]
                        else:
                            rhs = vca[:, bl[1], n, :]
                        ins = e.matmul(o, lhsT=pt_[:, i, g * 128:(g + 1) * 128], rhs=rhs, start=(i == 0), stop=(i == len(allb) - 1))
                return ins
            kb.op("pe", mmv, reads=[PTB, VA, VCA, CV], writes=[ACC])
            av = acc[:, 0:260].rearrange("p (g c) -> p g c", c=65)
            kb.op("dve", lambda e, av=av, n=n: e.tensor_tensor(out=den[:, 0:4], in0=av[:, :, 64], in1=snk[:, n * 4:(n + 1) * 4], op=ALU.add),
                  reads=[ACC, SNK], writes=[DEN])
            kb.op("dve", lambda e: e.reciprocal(out=den[:, 4:8], in_=den[:, 0:4]), reads=[DEN], writes=[DEN])
            kb.op("dve", lambda e, av=av, n=n, a_t=a_t: e.tensor_tensor(
                out=a_t[:, n * 256:(n + 1) * 256].rearrange("p (g d) -> p g d", d=64), in0=av[:, :, 0:64],
                in1=den[:, 4:8].unsqueeze(2).to_broadcast([128, 4, 64]), op=ALU.mult), reads=[ACC, DEN], writes=[AB])
        kb.dma("sp", ao[qt * 128:(qt + 1) * 128, :], a_t[:], reads=[AB], writes=[AO])


def build_fused(nlayers=4):
    kb = KB()
    kb.psum_banks()
    nc = kb.nc

    def scratch(name, shape, dt=BF16):
        return nc.dram_tensor(name, list(shape), dt).ap(), Buf(name)

    x_ext = kb.din("x", [TALL, D])
    csT = kb.din("csT", [128, 16])
    cosT = kb.din("cosT", [128, TALL])
    sinT = kb.din("sinT", [128, TALL])
    fg = kb.din("fg", [D])
    xn_out, XN = kb.dout("xn_out", [TALL, D])
    xbufs = [scratch(f"xs{i}", [TALL, D], F32) for i in range(2)]
    ao, AO = scratch("ao_scr", [TALL, D])
    fmT, FMT = scratch("fmT", [2048, TALL])
    tmv, TMV = scratch("tmv", [TALL, 1036])
    dk_send = [scratch(f"dk_send{k}", [512, 1024]) for k in range(4)]
    dk_recv = [scratch(f"dk_recv{k}", [2048, 1024]) for k in range(4)]
    dv_send = [scratch(f"dv_send{k}", [512, 516]) for k in range(8)]
    dv_recv = [scratch(f"dv_recv{k}", [2048, 516]) for k in range(8)]
    nbk_send, NBKS = scratch("nbk_send", [512, 512])
    nbk_recv, NBKR = scratch("nbk_recv", [2048, 512])
    nbv_send, NBVS = scratch("nbv_send", [512, 520])
    nbv_recv, NBVR = scratch("nbv_recv", [2048, 520])
    sbk_send, SBKS = scratch("sbk_send", [256, 256])
    sbk_recv, SBKR = scratch("sbk_recv", [1024, 256])
    sbv_send, SBVS = scratch("sbv_send", [256, 260])
    sbv_recv, SBVR = scratch("sbv_recv", [1024, 260])
    x_src, XS = x_ext, Buf("x_ext")
    lw = []
    for i in range(nlayers):
        d = {"w_out": kb.din(f"w_out{i}", [D, D]), "wq": kb.din(f"wq{i}", [D, 2048]),
             "uT": kb.din(f"uT{i}", [D, 16384]), "v": kb.din(f"v{i}", [16384, D])}
        d["conv"] = {"wout": scratch(f"woutb{i}", [2, 128, 4096]), "wq": scratch(f"wqb{i}", [4, 128, 4096]),
                     "uT": scratch(f"uTb{i}", [32, 128, 4096]), "v": scratch(f"vb{i}", [32, 128, 4096])}
        lw.append(d)

    def convert(i):
        d = lw[i]
        c = d["conv"]
        for hf in range(2):
            kb.dma("pool", c["wout"][0][hf].rearrange("p (k n) -> p k n", k=8),
                   d["w_out"][:, hf * 512:(hf + 1) * 512].rearrange("(k p) n -> p k n", p=128), writes=[c["wout"][1]])
        for g in range(4):
            kb.dma("pool", c["wq"][0][g].rearrange("p (k n) -> p k n", k=8),
                   d["wq"][:, g * 512:(g + 1) * 512].rearrange("(k p) n -> p k n", p=128), writes=[c["wq"][1]])
        for cg in range(32):
            kb.dma("pool", c["uT"][0][cg].rearrange("p (k n) -> p k n", k=8),
                   d["uT"][:, cg * 512:(cg + 1) * 512].rearrange("(k p) n -> p k n", p=128), writes=[c["uT"][1]])
            kb.dma("pool", c["v"][0][cg].rearrange("p (c d) -> p c d", c=4),
                   d["v"][cg * 512:(cg + 1) * 512, :].rearrange("(c p) d -> p c d", p=128), writes=[c["v"][1]])

    for i in range(nlayers):
        even = (i % 2 == 0)
        j = i // 2
        NIN = 3072 if even else 1536
        NROPE = 1024 if even else 1280
        w_mod = kb.din(f"w_mod{i}", [D, 6 * D])
        b_mod = kb.din(f"b_mod{i}", [6 * D])
        n1g = kb.din(f"n1g{i}", [D])
        n2g = kb.din(f"n2g{i}", [D])
        w_in = kb.din(f"w_in{i}", [D, NIN])
        w_perm = kb.din(f"w_perm{i}", [D, NROPE])
        w_out, wq, uT, v = lw[i]["w_out"], lw[i]["wq"], lw[i]["uT"], lw[i]["v"]
        keysT = kb.din(f"keysT{i}", [128, 16, 128])
        modrow, MRO = scratch(f"modrow{i}", [2, 6, D], F32)
        with kb.scope(f"L{i}a_"):
            if i == 0 and USE_CONV:
                convert(0)
            emit_pre_f(kb, even, x_src, XS, csT, w_mod, b_mod, n1g, w_in, w_perm, cosT, sinT, fmT, FMT, tmv, TMV, modrow, MRO)
        if even:
            nbias = kb.din(f"nbias{j}", [5, 8, 128, NA_NBMAX * 128])
            lam = kb.din(f"lam{j}", [4, 64])
            subg = kb.din(f"subg{j}", [128])
            lami = kb.din(f"lam_init{j}", [128, 2])
            for k in range(4):
                kb.dma("sp", dk_send[k][0], fmT[1536:2048, k * 1024:(k + 1) * 1024], reads=[FMT], writes=[dk_send[k][1]])
            for k in range(8):
                kb.dma("sp", dv_send[k][0], tmv[k * 512:(k + 1) * 512, 520:1036], reads=[TMV], writes=[dv_send[k][1]])
            kb.dma("sp", nbk_send[:, 0:256], fmT[512:1024, TQ - 256:TQ], reads=[FMT], writes=[NBKS])
            kb.dma("sp", nbk_send[:, 256:512], fmT[512:1024, 0:256], reads=[FMT], writes=[NBKS])
            kb.dma("sp", nbv_send[0:256, :], tmv[TQ - 256:TQ, 0:520], reads=[TMV], writes=[NBVS])
            kb.dma("sp", nbv_send[256:512, :], tmv[0:256, 0:520], reads=[TMV], writes=[NBVS])
            for k in range(4):
                kb.cc("AllGather", RG, dk_send[k][0], dk_recv[k][0], reads=[dk_send[k][1]], writes=[dk_recv[k][1]])
            for k in range(8):
                kb.cc("AllGather", RG, dv_send[k][0], dv_recv[k][0], reads=[dv_send[k][1]], writes=[dv_recv[k][1]])
            kb.cc("AllGather", RG, nbk_send, nbk_recv, reads=[NBKS], writes=[NBKR])
            kb.cc("AllGather", RG, nbv_send, nbv_recv, reads=[NBVS], writes=[NBVR])
            with kb.scope(f"L{i}b_"):
                emit_attn_even_f(kb, fmT, FMT, tmv, TMV, dk_recv, None, dv_recv, None, nbk_recv, NBKR, nbv_recv, NBVR,
                                 nbias, lam, subg, lami, ao, AO)
        else:
            sbias = kb.din(f"sbias{j}", [3, 128, 768])
            sink = kb.din(f"sink{j}", [16])
            kb.dma("sp", sbk_send[:, 0:128], fmT[1024:1280, TQ - 128:TQ], reads=[FMT], writes=[SBKS])
            kb.dma("sp", sbk_send[:, 128:256], fmT[1024:1280, 0:128], reads=[FMT], writes=[SBKS])
            kb.dma("sp", sbv_send[0:128, :], tmv[TQ - 128:TQ, 0:260], reads=[TMV], writes=[SBVS])
            kb.dma("sp", sbv_send[128:256, :], tmv[0:128, 0:260], reads=[TMV], writes=[SBVS])
            kb.cc("AllGather", RG, sbk_send, sbk_recv, reads=[SBKS], writes=[SBKR])
            kb.cc("AllGather", RG, sbv_send, sbv_recv, reads=[SBVS], writes=[SBVR])
            with kb.scope(f"L{i}b_"):
                emit_attn_odd_f(kb, fmT, FMT, tmv, TMV, sbk_recv, SBKR, sbv_recv, SBVR, sbias, sink, ao, AO)
        x_dst, XD = xbufs[i % 2]
        last = (i == nlayers - 1)
        with kb.scope(f"L{i}c_"):
            if not last and USE_CONV:
                convert(i + 1)
            emit_post(kb, NT, x_src, ao, AO, modrow, w_out, n2g, wq, keysT, uT, v, x_dst, XD,
                      final_g=fg if last else None, xn_out=xn_out if last else None, XN=XN if last else None,
                      tile_sets=[0] * NTL + [1] * NTC, conv=lw[i]["conv"] if USE_CONV else None)
        x_src, XS = x_dst, XD
    return kb.finish()


def _na_bias_f(rpb, qr):
    H = rpb.shape[0]
    R0 = qr * 64
    out = np.full((5, H, 128, NA_NBMAX, 128), NEG, np.float32)
    kk = np.arange(128)
    qq = np.arange(128)
    for slot, qt in enumerate((0, 1, 10, NTL - 2, NTL - 1)):
        r0 = R0 + 2 * qt
        if slot == 2:
            r0 = 100
        o0, onb, cands = _na_blocklist(qt)
        blocks = [((r0 - 4 + 2 * b) if slot == 2 else (R0 + o0 // 64 + 2 * b), None) for b in range(onb)]
        for r in range(4):
            for (which, cb) in cands:
                if which == "tail":
                    blocks.append((R0 - 4 + 2 * cb, r == qr - 1))
                else:
                    blocks.append((R0 + 64 + 2 * cb, r == qr + 1))
        qrow = r0 + qq // 64
        qc = qq % 64
        rs = np.clip(qrow - 4, 0, 256 - 8)
        cs = np.clip(qc - 8, 0, 64 - 16)
        for bi, (krow0, ok) in enumerate(blocks):
            if ok is False:
                continue
            kr = krow0 + kk // 64
            kc = kk % 64
            valid = ((kr[:, None] >= rs[None, :]) & (kr[:, None] < rs[None, :] + 8) &
                     (kc[:, None] >= cs[None, :]) & (kc[:, None] < cs[None, :] + 16) &
                     (kr[:, None] >= 0) & (kr[:, None] < 256))
            dr = np.clip(kr[:, None] - qrow[None, :] + 7, 0, 14)
            dc = np.clip(kc[:, None] - qc[None, :] + 15, 0, 30)
            vals = rpb[:, dr, dc]
            out[slot, :, :, bi, :] = np.where(valid[None], vals, NEG)
    return out.reshape(5, H, 128, NA_NBMAX * 128)


def _swa_bias_f(qr):
    out = np.full((3, 128, 6, 128), NEG, np.float32)
    k = np.arange(128)[:, None]
    q = np.arange(128)[None, :]
    band = lambda off: np.where(np.abs(off * 128 + k - q) <= 128, 0.0, NEG).astype(np.float32)
    out[0, :, 0] = band(0)
    out[0, :, 1] = band(1)
    for r in range(4):
        if r == qr - 1:
            out[0, :, 2 + r] = band(-1)
    out[1, :, 0] = band(-1)
    out[1, :, 1] = band(0)
    out[1, :, 2] = band(1)
    out[2, :, 0] = band(-1)
    out[2, :, 1] = band(0)
    for r in range(4):
        if r == qr + 1:
            out[2, :, 2 + r] = band(1)
    return out.reshape(3, 128, 768)


_FUSED = {}


def kernel(x, c, ctx, c_ctx, w_mod, b_mod, norm1_g, norm2_g, w_in_even, w_out_even, na_rpb, diff_lambda,
           diff_subln_g, w_in_odd, w_out_odd, swa_sink, peer_wq, peer_keys, peer_u, peer_v, final_g, _nlayers=4):
    f32 = np.float32
    x = np.asarray(x, f32)
    ctx = np.asarray(ctx, f32)
    if _nlayers not in _FUSED:
        _FUSED[_nlayers] = build_fused(_nlayers)
    nc = _FUSED[_nlayers]
    shared = {"c_ident": np.eye(128, dtype=f32), "fg": np.asarray(final_g, f32)}
    for i in range(_nlayers):
        even = (i % 2 == 0)
        j = i // 2
        w_in = np.asarray(w_in_even[j] if even else w_in_odd[j], f32)
        rc = w_in[:, 1536:2560] if even else w_in[:, 0:1280]
        shared[f"w_mod{i}"] = np.asarray(w_mod[i], f32)
        shared[f"b_mod{i}"] = np.asarray(b_mod[i], f32)
        shared[f"n1g{i}"] = np.asarray(norm1_g[i], f32)
        shared[f"n2g{i}"] = np.asarray(norm2_g[i], f32)
        shared[f"w_in{i}"] = w_in
        shared[f"w_perm{i}"] = np.ascontiguousarray(rc.reshape(D, -1, 64)[:, :, _PERM64].reshape(D, -1))
        shared[f"w_out{i}"] = np.asarray(w_out_even[j] if even else w_out_odd[j], f32)
        shared[f"wq{i}"] = np.asarray(peer_wq[i], f32)
        shared[f"keysT{i}"] = np.ascontiguousarray(np.asarray(peer_keys[i], f32).reshape(16, 128, 128).transpose(2, 0, 1))
        shared[f"uT{i}"] = np.ascontiguousarray(np.asarray(peer_u[i], f32).T)
        shared[f"v{i}"] = np.asarray(peer_v[i], f32)
        if even:
            shared[f"lam{j}"] = np.asarray(diff_lambda[j], f32)
            shared[f"subg{j}"] = np.asarray(diff_subln_g[j], f32)
            li = 0.8 - 0.6 * math.exp(-0.3 * i)
            shared[f"lam_init{j}"] = np.tile(np.array([[li, 1.0 - li]], f32), (128, 1))
        else:
            shared[f"sink{j}"] = np.asarray(swa_sink[j], f32)
    ims = []
    for core in range(NCORES):
        b, qr = divmod(core, 4)
        im = dict(shared)
        im["x"] = np.concatenate([x[b, qr * TQ:(qr + 1) * TQ], ctx[b]], axis=0)
        cs = np.stack([np.asarray(c, f32)[b], np.asarray(c_ctx, f32)], 0)
        im["csT"] = np.ascontiguousarray(cs.reshape(2, 8, 128).transpose(2, 0, 1).reshape(128, 16))
        im["cosT"], im["sinT"] = _rope_tables_T(qr * TQ)
        for i in range(_nlayers):
            j = i // 2
            if i % 2 == 0:
                im[f"nbias{j}"] = _na_bias_f(np.asarray(na_rpb[j], f32), qr)
            else:
                im[f"sbias{j}"] = _swa_bias_f(qr)
        ims.append(im)
    res = run_bass_kernel_spmd(nc, ims, core_ids=list(range(NCORES))).results
    out = np.zeros((2, SEQ, D), f32)
    for core in range(NCORES):
        b, qr = divmod(core, 4)
        out[b, qr * TQ:(qr + 1) * TQ] = np.asarray(res[core]["xn_out"])[:TQ]
    return out
```

```python
from contextlib import ExitStack
import math
import numpy as np
import ml_dtypes
import concourse.bass as bass
import concourse.mybir as mybir
from concourse.bass_utils import run_bass_kernel_spmd

F32 = mybir.dt.float32
BF16 = mybir.dt.bfloat16
AF = mybir.ActivationFunctionType
ALU = mybir.AluOpType
AX = mybir.AxisListType
NPBF = ml_dtypes.bfloat16

ENGS = ("pe", "act", "dve", "pool", "sp")

D = 1024
NCORES = 8
SEQ = 16384
CTX = 256
TQ = SEQ // 4
NTL = TQ // 128
NTC = CTX // 128
NT = NTL + NTC
TALL = TQ + CTX
EPS = 1e-6
NEG = -1.0e30
PEER_MARGIN = 1e-4
import os
USE_CONV = os.environ.get('K_CONV', '1') == '1'


class Buf:
    __slots__ = ("name", "writers", "readers", "excl")

    def __init__(self, name="", excl=False):
        self.name = name
        self.writers = {}
        self.readers = {}
        self.excl = excl


class Op:
    __slots__ = ("id", "eng", "fn", "dma", "cc", "deps", "seq", "waits", "signal", "sigidx", "slot", "target")


class Prog:
    RING = {"sp": 14, "act": 4, "pool": 8}

    def __init__(self, nc):
        self.nc = nc
        self.ops = []
        self.seq = {e: 0 for e in ENGS}
        self.dmas = {q: [] for q in self.RING}
        self.ccs = []

    def add(self, eng, fn, reads=(), writes=(), dma=False, cc=False):
        dma = dma or cc
        op = Op()
        op.cc = cc
        op.id = len(self.ops)
        op.eng = eng
        op.fn = fn
        op.dma = dma
        op.signal = False
        op.sigidx = None
        op.slot = None
        op.target = None
        deps = set()
        if cc:
            op.slot = ("cc", len(self.ccs))
            op.target = 1
            self.ccs.append(op.id)
        elif dma:
            ring = self.RING[eng]
            lst = self.dmas[eng]
            n = len(lst)
            op.slot = (eng, n % ring)
            op.target = 16 * (n // ring + 1)
            if n >= ring:
                deps.add(lst[n - ring])
            lst.append(op.id)
        pkey = ("dma", op.slot) if dma else eng
        rd = [b for b in reads if not b.excl]
        wr = list(writes) + [b for b in reads if b.excl]
        for b in rd:
            for k, oid in b.writers.items():
                if k == eng and eng == "pe":
                    continue
                deps.add(oid)
        for b in wr:
            isread = b not in writes
            for k, oid in b.writers.items():
                if k == eng and not dma:
                    if isread and eng != "pe":
                        deps.add(oid)
                    continue
                if dma and isinstance(k, tuple):
                    continue
                deps.add(oid)
            for k, oid in b.readers.items():
                if k == eng and not dma:
                    continue
                deps.add(oid)
        for b in wr:
            if dma:
                b.writers = {k: v for k, v in b.writers.items() if isinstance(k, tuple)}
                b.writers[pkey] = op.id
            else:
                b.writers = {pkey: op.id}
            b.readers = {}
        for b in rd:
            if b in wr:
                continue
            b.readers[pkey] = op.id
        op.seq = self.seq[eng]
        self.seq[eng] += 1
        op.deps = deps
        self.ops.append(op)
        return op.id

    def barrier(self):
        start = getattr(self, "bar_start", 0)
        last = {}
        for op in self.ops[start:]:
            if op.dma:
                last[("dma", op.id)] = op.id
            else:
                last[op.eng] = op.id
        deps = set(last.values())
        for e in ENGS:
            oid = self.add(e, lambda eng: None)
            self.ops[oid].deps |= {d for d in deps if self.ops[d].dma or self.ops[d].eng != e}
        self.bar_start = len(self.ops)

    def emit(self):
        nc = self.nc
        ops = self.ops
        seen = {e: {} for e in ENGS}
        seen_dma = {e: {} for e in ENGS}
        for op in ops:
            best = {}
            waits = []
            for d in op.deps:
                p = ops[d]
                if p.dma:
                    if seen_dma[op.eng].get(p.slot, 0) >= p.target:
                        continue
                    seen_dma[op.eng][p.slot] = p.target
                    waits.append(d)
                else:
                    if p.eng not in best or ops[best[p.eng]].seq < p.seq:
                        best[p.eng] = d
            for e, d in best.items():
                p = ops[d]
                if seen[op.eng].get(e, -1) >= p.seq:
                    continue
                seen[op.eng][e] = p.seq
                p.signal = True
                waits.append(d)
            op.waits = waits
        cnt = {e: 0 for e in ENGS}
        for op in ops:
            if op.signal and not op.dma:
                cnt[op.eng] += 1
                op.sigidx = cnt[op.eng]
        with ExitStack() as es:
            sems = {e: es.enter_context(nc.semaphore("s_" + e)) for e in ENGS}
            rings = {}
            for q, n in self.RING.items():
                for i in range(n):
                    rings[(q, i)] = es.enter_context(nc.semaphore(f"r_{q}{i}"))
            for i in range(len(self.ccs)):
                rings[("cc", i)] = es.enter_context(nc.semaphore(f"cc{i}"))
            block = es.enter_context(nc.Block())
            per_eng = {e: [o for o in ops if o.eng == e] for e in ENGS}

            def run(engname):
                def body(eng):
                    for op in per_eng[engname]:
                        for d in op.waits:
                            p = ops[d]
                            if p.dma:
                                eng.wait_ge(rings[p.slot], p.target)
                            else:
                                eng.wait_ge(sems[p.eng], p.sigidx)
                        ins = op.fn(eng)
                        if ins is None:
                            continue
                        if op.cc:
                            ins.then_inc(rings[op.slot], 1)
                        elif op.dma:
                            ins.then_inc(rings[op.slot], 16)
                        elif op.signal:
                            ins.then_inc(sems[op.eng], 1)
                return body

            block.tensor(run("pe"))
            block.scalar(run("act"))
            block.vector(run("dve"))
            block.gpsimd(run("pool"))
            block.sync(run("sp"))


class KB:
    def __init__(self):
        self.nc = bass.Bass("TRN2", target_bir_lowering=False)
        self.P = Prog(self.nc)
        self.es = ExitStack()
        self.banks = []
        self.outs = []

    def din(self, name, shape, dt=F32):
        return self.nc.dram_tensor(name, list(shape), dt, kind="ExternalInput").ap()

    def dout(self, name, shape, dt=F32):
        b = Buf(name)
        self.outs.append(b)
        return self.nc.dram_tensor(name, list(shape), dt, kind="ExternalOutput").ap(), b

    def dscratch(self, name, shape, dt):
        return self.nc.dram_tensor(name, list(shape), dt, kind="Internal").ap(), Buf(name)

    pfx = ""

    def sb(self, name, shape, dt=F32):
        return self.es.enter_context(self.nc.sbuf_tensor(self.pfx + name, list(shape), dt)), Buf(name)

    def scope(self, pfx):
        kb = self

        class _S:
            def __enter__(s_):
                s_.saved = (kb.es, kb.pfx)
                kb.es = ExitStack()
                kb.pfx = pfx
                kb._ident = None

            def __exit__(s_, *a):
                kb.es.close()
                kb.es, kb.pfx = s_.saved
                kb.P.barrier()
                return False
        return _S()

    _ident = None
    _ident_d = None

    def ident(self):
        if self._ident is None:
            if self._ident_d is None:
                self._ident_d = self.din("c_ident", [128, 128], F32)
            f, IDF = self.sb("ident_f", [128, 128], F32)
            b, ID = self.sb("ident", [128, 128], BF16)
            self.dma("sp", f[:], self._ident_d, writes=[IDF])
            self.op("act", lambda e: e.copy(out=b[:], in_=f[:]), reads=[IDF], writes=[ID])
            self._ident = (f, IDF, b, ID)
        return self._ident

    def cc(self, kind, rg, src, dst, reads=(), writes=()):
        return self.P.add("pool", lambda e: e.collective_compute(kind, ALU.bypass, replica_groups=rg, ins=[src.opt()], outs=[dst.opt()]),
                          reads, writes, cc=True)

    def psum_banks(self):
        for i in range(8):
            t = self.es.enter_context(self.nc.psum_tensor(f"bank{i}", [128, 512], F32))
            self.banks.append((t, Buf(f"bank{i}", excl=True)))

    def op(self, eng, fn, reads=(), writes=()):
        return self.P.add(eng, fn, reads, writes)

    def dma(self, q, out, in_, reads=(), writes=()):
        return self.P.add(q, lambda e: e.dma_start(out=out, in_=in_), reads, writes, dma=True)

    def finish(self):
        self.P.add("sp", lambda e: None, reads=self.outs)
        self.P.emit()
        self.es.close()
        return self.nc


def rstd_ops(kb, sm, SM):
    kb.op("dve", lambda e: e.tensor_scalar(out=sm[:, 2:3], in0=sm[:, 0:1], scalar1=1.0 / D, scalar2=EPS,
                                           op0=ALU.mult, op1=ALU.add), reads=[SM], writes=[SM])
    kb.op("act", lambda e: e.activation(out=sm[:, 3:4], in_=sm[:, 2:3], func=AF.Sqrt), reads=[SM], writes=[SM])
    kb.op("dve", lambda e: e.reciprocal(out=sm[:, 1:2], in_=sm[:, 3:4]), reads=[SM], writes=[SM])


def emit_post(kb, ntiles, x_in, ao_dram, AO, modrow, w_out, norm2g, peer_wq, peer_keysT, peer_uT, peer_v,
              x_out, XOUT, final_g=None, xn_out=None, XN=None, tile_sets=None, conv=None):
    nc, P = kb.nc, kb.P
    B = kb.banks
    if tile_sets is None:
        tile_sets = [0] * ntiles
    ident_f, IDF, ident, ID = kb.ident()

    mod, MOD = kb.sb("modrows", [128, 4, D], F32)
    n2g, N2G = kb.sb("n2g_sb", [128, D], F32)
    kb.dma("sp", n2g[:], norm2g.unsqueeze(0).to_broadcast([128, D]), writes=[N2G])
    fg = None
    if final_g is not None:
        fg, FG = kb.sb("fg_sb", [128, D], F32)
        kb.dma("sp", fg[:], final_g.unsqueeze(0).to_broadcast([128, D]), writes=[FG])

    def load_mod(s):
        for j, src in enumerate((2, 4, 3, 5)):
            kb.dma("sp", mod[:, j, :], modrow[s, src:src + 1, :].to_broadcast([128, D]), writes=[MOD])
        kb.op("dve", lambda e: e.scalar_tensor_tensor(out=mod[:, 1, :], in0=mod[:, 1, :], scalar=1.0, in1=n2g[:],
                                                      op0=ALU.add, op1=ALU.mult), reads=[MOD, N2G], writes=[MOD])

    keys_b, KBF = kb.sb("keys_b", [128, 16, 128], BF16)

    NWR = 3
    wr = [kb.sb(f"wr{i}", [128, 8, 512], BF16) for i in range(NWR)]
    NVR = 2
    vr = [kb.sb(f"vr{i}", [128, 4, D], BF16) for i in range(NVR)]
    wr_i = [0]
    vr_i = [0]

    def load_w(src_cols, pre=None):
        t, b = wr[wr_i[0] % NWR]
        wr_i[0] += 1
        if pre is not None:
            kb.dma("sp", t[:].rearrange("p k n -> p (k n)"), pre[0], reads=[pre[1]], writes=[b])
        else:
            kb.dma("pool", t[:], src_cols.rearrange("(k p) n -> p k n", p=128), writes=[b])
        return t, b

    def load_v(src_rows, pre=None):
        t, b = vr[vr_i[0] % NVR]
        vr_i[0] += 1
        if pre is not None:
            kb.dma("sp", t[:].rearrange("p c d -> p (c d)"), pre[0], reads=[pre[1]], writes=[b])
        else:
            kb.dma("pool", t[:], src_rows.rearrange("(c p) d -> p c d", p=128), writes=[b])
        return t, b

    def pre_of(name, idx):
        return None if conv is None else (conv[name][0][idx], conv[name][1])

    xt, XT = kb.sb("xt", [128, 2, D], F32)
    tmp, TMP = kb.sb("tmp", [128, D], F32)
    ao, AOB = kb.sb("ao_sb", [128, D], BF16)
    aoT, AOT = kb.sb("aoT", [128, 8, 128], BF16)
    h2, H2 = kb.sb("h2", [128, D], BF16)
    h2T, H2T = kb.sb("h2T", [128, 8, 256], BF16)
    qT, QT = kb.sb("qT", [128, 16, 256], BF16)
    s_sb, SSB = kb.sb("s_sb", [128, 16, 128], F32)
    work, WORK = kb.sb("work", [128, 2048], F32)
    kb.dma("sp", work[:], peer_keysT.rearrange("p a n -> p (a n)"), writes=[WORK])
    kb.op("act", lambda e: e.copy(out=keys_b[:].rearrange("p a n -> p (a n)"), in_=work[:]), reads=[WORK], writes=[KBF])
    cand, CAND = kb.sb("cand", [128, 8, 16, 16], F32)
    top, TOP = kb.sb("top", [128, 16, 16], F32)
    ctop, CTOP = kb.sb("ctop", [128, 8, 16], F32)
    sm, SM = kb.sb("sm", [128, 64], F32)
    e16, E16 = kb.sb("e16", [128, 8, 16], F32)
    av, AV = kb.sb("a_vec", [128, 2, 8, 128], F32)
    bv, BV = kb.sb("b_vec", [128, 2, 8, 128], F32)
    diag, DG = kb.sb("diag", [128, 2, 8, 128], BF16)
    pps = [(kb.sb(f"pp{i}", [128, 2, 8, 2, 128], F32)[0], (Buf(f"pp{i}a"), Buf(f"pp{i}b"))) for i in range(2)]
    wps = [kb.sb(f"wp{i}", [128, 2, 8, 2, 128], BF16) for i in range(2)]
    gl = [kb.sb(f"gl{i}", [128, 4, 256], BF16) for i in range(2)]
    at = [kb.sb(f"at{i}", [128, 4, 256], BF16) for i in range(2)]
    xn, XNB = work, WORK

    def bf(bank):
        return bank.bitcast(BF16)

    cur_set = [None]
    npairs = (ntiles + 1) // 2
    for pr in range(npairs):
        tiles = [t for t in (2 * pr, 2 * pr + 1) if t < ntiles]
        nj = len(tiles)
        NTOK = 128 * nj
        if tile_sets[tiles[0]] != cur_set[0]:
            cur_set[0] = tile_sets[tiles[0]]
            load_mod(cur_set[0])
        wo = [load_w(w_out[:, hf * 512:(hf + 1) * 512], pre_of('wout', hf)) for hf in range(2)]
        for j, tt in enumerate(tiles):
            rows = slice(tt * 128, (tt + 1) * 128)
            kb.dma("sp", xt[:, j, :], x_in[rows, :], writes=[XT])
            kb.dma("sp", ao[:], ao_dram[rows, :], reads=[AO], writes=[AOB])
            tb, TB = B[0]
            def tr1(e, tb=tb):
                for k in range(8):
                    ins = e.transpose(bf(tb)[:, k * 128:(k + 1) * 128], ao[:, k * 128:(k + 1) * 128], ident[:])
                return ins
            kb.op("pe", tr1, reads=[AOB, ID], writes=[TB])
            kb.op("act", lambda e, tb=tb: e.copy(out=aoT[:].rearrange("p k t -> p (k t)"), in_=bf(tb)[:, 0:1024]),
                  reads=[TB], writes=[AOT])
            for hf in range(2):
                yb, YB = B[1 + hf]
                wt, WB = wo[hf]
                def mmy(e, yb=yb, wt=wt):
                    for k in range(8):
                        ins = e.matmul(yb[:], lhsT=aoT[:, k, :], rhs=wt[:, k, :], start=(k == 0), stop=(k == 7))
                    return ins
                kb.op("pe", mmy, reads=[AOT, WB], writes=[YB])
                kb.op("dve", lambda e, yb=yb, hf=hf: e.tensor_tensor(out=tmp[:, hf * 512:(hf + 1) * 512], in0=yb[:],
                                                                   in1=mod[:, 0, hf * 512:(hf + 1) * 512], op=ALU.mult),
                      reads=[YB, MOD], writes=[TMP])
            kb.op("pool", lambda e, j=j: e.tensor_tensor(out=xt[:, j, :], in0=xt[:, j, :], in1=tmp[:], op=ALU.add),
                  reads=[XT, TMP], writes=[XT])
            kb.op("act", lambda e, j=j: e.activation(out=tmp[:], in_=xt[:, j, :], func=AF.Square, accum_out=sm[:, 0:1]),
                  reads=[XT], writes=[TMP, SM])
            rstd_ops(kb, sm, SM)
            kb.op("dve", lambda e, j=j: e.scalar_tensor_tensor(out=tmp[:], in0=xt[:, j, :], scalar=sm[:, 1:2], in1=mod[:, 1, :],
                                                               op0=ALU.mult, op1=ALU.mult), reads=[XT, SM, MOD], writes=[TMP])
            kb.op("pool", lambda e: e.tensor_tensor(out=h2[:], in0=tmp[:], in1=mod[:, 2, :], op=ALU.add),
                  reads=[TMP, MOD], writes=[H2])
            tb, TB = B[3]
            def tr2(e, tb=tb):
                for k in range(8):
                    ins = e.transpose(bf(tb)[:, k * 128:(k + 1) * 128], h2[:, k * 128:(k + 1) * 128], ident[:])
                return ins
            kb.op("pe", tr2, reads=[H2, ID], writes=[TB])
            kb.op("act", lambda e, tb=tb, j=j: e.copy(out=h2T[:, :, j * 128:(j + 1) * 128],
                                                      in_=bf(tb)[:, 0:1024].rearrange("p (k t) -> p k t", k=8)),
                  reads=[TB], writes=[H2T])
        for g in range(4):
            wt, WB = load_w(peer_wq[:, g * 512:(g + 1) * 512], pre_of('wq', g))
            for hh in range(2):
                qb, QB = B[4 + (2 * g + hh) % 2]
                def mmq(e, qb=qb, wt=wt, hh=hh, NTOK=NTOK):
                    for i in range(2):
                        for k in range(8):
                            c0 = (hh * 2 + i) * 128
                            ins = e.matmul(qb[:, i * 256:i * 256 + NTOK], lhsT=wt[:, k, c0:c0 + 128], rhs=h2T[:, k, 0:NTOK],
                                           start=(k == 0), stop=(k == 7))
                    return ins
                kb.op("pe", mmq, reads=[WB, H2T], writes=[QB])
                hp0 = g * 4 + hh * 2
                kb.op("act", lambda e, qb=qb, hp0=hp0, NTOK=NTOK: e.copy(out=qT[:, hp0:hp0 + 2, 0:NTOK],
                                                               in_=qb[:].rearrange("p (i t) -> p i t", i=2)[:, :, 0:NTOK]),
                      reads=[QB], writes=[QT])
        for j, tt in enumerate(tiles):
            for g in range(4):
                sbk, SB_ = B[6 + g % 2]
                def mms(e, sbk=sbk, g=g, j=j):
                    for i in range(4):
                        hp = g * 4 + i
                        ins = e.matmul(sbk[:, i * 128:(i + 1) * 128], lhsT=qT[:, hp, j * 128:(j + 1) * 128], rhs=keys_b[:, hp, :],
                                       start=True, stop=True)
                    return ins
                kb.op("pe", mms, reads=[QT, KBF], writes=[SB_])
                kb.op("act", lambda e, sbk=sbk, g=g: e.copy(out=s_sb[:, g * 4:(g + 1) * 4, :].rearrange("p a n -> p (a n)"), in_=sbk[:]),
                      reads=[SB_], writes=[SSB])
            for hp in range(16):
                kb.op("dve", lambda e, hp=hp: e.max(out=top[:, hp, 0:8], in_=s_sb[:, hp, :]), reads=[SSB], writes=[TOP])
                kb.op("dve", lambda e, hp=hp: e.match_replace(out=work[:, hp * 128:(hp + 1) * 128], in_to_replace=top[:, hp, 0:8],
                                                              in_values=s_sb[:, hp, :], imm_value=NEG), reads=[SSB, TOP], writes=[WORK])
                kb.op("dve", lambda e, hp=hp: e.max(out=top[:, hp, 8:16], in_=work[:, hp * 128:(hp + 1) * 128]), reads=[WORK], writes=[TOP])
            def fcand(e):
                t4 = top[:].rearrange("p (h q) k -> p h q k", q=2)
                i0 = t4[:, :, 0, :].unsqueeze(3).to_broadcast([128, 8, 16, 16])
                i1 = t4[:, :, 1, :].unsqueeze(2).to_broadcast([128, 8, 16, 16])
                return e.tensor_tensor(out=cand[:], in0=i0, in1=i1, op=ALU.add)
            kb.op("pool", fcand, reads=[TOP], writes=[CAND])
            for h in range(8):
                cv = cand[:, h, :, :].rearrange("p a b -> p (a b)")
                kb.op("dve", lambda e, h=h, cv=cv: e.max(out=ctop[:, h, 0:8], in_=cv), reads=[CAND], writes=[CTOP])
                kb.op("dve", lambda e, h=h, cv=cv: e.match_replace(out=work[:, h * 256:(h + 1) * 256], in_to_replace=ctop[:, h, 0:8],
                                                                   in_values=cv, imm_value=NEG), reads=[CAND, CTOP], writes=[WORK])
                kb.op("dve", lambda e, h=h: e.max(out=ctop[:, h, 8:16], in_=work[:, h * 256:(h + 1) * 256]), reads=[WORK], writes=[CTOP])
            kb.op("dve", lambda e: e.tensor_scalar(out=sm[:, 8:16], in0=ctop[:, :, 15], scalar1=-1.0, scalar2=PEER_MARGIN,
                                                   op0=ALU.mult, op1=ALU.add), reads=[CTOP], writes=[SM])
            kb.op("dve", lambda e: e.tensor_tensor(out=e16[:], in0=ctop[:], in1=sm[:, 8:16].unsqueeze(2).to_broadcast([128, 8, 16]),
                                                   op=ALU.add), reads=[CTOP, SM], writes=[E16])
            kb.op("act", lambda e: e.activation(out=e16[:], in_=e16[:], func=AF.Exp), reads=[E16], writes=[E16])
            kb.op("dve", lambda e: e.tensor_reduce(out=sm[:, 16:24], in_=e16[:], axis=AX.X, op=ALU.add), reads=[E16], writes=[SM])
            kb.op("dve", lambda e: e.reciprocal(out=sm[:, 24:32], in_=sm[:, 16:24]), reads=[SM], writes=[SM])
            s4 = s_sb[:].rearrange("p (h q) n -> p h q n", q=2)
            kb.op("dve", lambda e, j=j, s4=s4: e.tensor_tensor(out=av[:, j, :, :], in0=s4[:, :, 0, :],
                                                               in1=sm[:, 8:16].unsqueeze(2).to_broadcast([128, 8, 128]), op=ALU.add),
                  reads=[SSB, SM], writes=[AV])
            kb.op("act", lambda e, j=j: e.activation(out=av[:, j, :, :], in_=av[:, j, :, :], func=AF.Exp), reads=[AV], writes=[AV])
            kb.op("act", lambda e, j=j, s4=s4: e.activation(out=bv[:, j, :, :], in_=s4[:, :, 1, :], func=AF.Exp), reads=[SSB], writes=[BV])
            for h in range(8):
                kb.op("pool", lambda e, j=j, h=h: e.tensor_scalar(out=diag[:, j, h, :], in0=ident_f[:], scalar1=sm[:, 24 + h:25 + h],
                                                                  scalar2=None, op0=ALU.mult), reads=[IDF, SM], writes=[DG])
        for cg in range(32):
            ut, UB = load_w(peer_uT[:, cg * 512:(cg + 1) * 512], pre_of('uT', cg))
            vt, VB = load_v(peer_v[cg * 512:(cg + 1) * 512, :], pre_of('v', cg))
            gt, GB = gl[cg % 2]
            att, ATB = at[cg % 2]
            for half in range(2):
                c0 = cg * 4 + half * 2
                wp, WPB = wps[(cg * 2 + half) % 2]
                pp, PP = pps[(cg * 2 + half) % 2]
                def fpp(e, c0=c0, pp=pp):
                    i0 = av[:, 0:1, :, c0:c0 + 2].unsqueeze(4).to_broadcast([128, 1, 8, 2, 128])
                    i1 = bv[:, 0:1, :, :].unsqueeze(3).to_broadcast([128, 1, 8, 2, 128])
                    return e.tensor_tensor(out=pp[:, 0:1], in0=i0, in1=i1, op=ALU.mult)
                kb.op("pool", fpp, reads=[AV, BV], writes=[PP[0]])
                if nj > 1:
                    def fpa(e, c0=c0, pp=pp):
                        for h in range(8):
                            for i in range(2):
                                ins = e.activation(out=pp[:, 1, h, i, :], in_=bv[:, 1, h, :], func=AF.Copy,
                                                   scale=av[:, 1, h, c0 + i:c0 + i + 1])
                        return ins
                    kb.op("act", fpa, reads=[AV, BV], writes=[PP[1]])
                kb.op("dve", lambda e, wp=wp, nj=nj, pp=pp: e.scalar_tensor_tensor(out=wp[:, 0:nj], in0=pp[:, 0:nj], scalar=1.0, in1=pp[:, 0:nj],
                                                                     op0=ALU.is_ge, op1=ALU.mult), reads=[PP[0], PP[1]], writes=[WPB])
                pb, PB = B[0 + half]
                wb, WTB = B[2 + half]
                def mmpre(e, pb=pb, half=half, ut=ut, NTOK=NTOK):
                    for i in range(2):
                        cl = half * 2 + i
                        for k in range(8):
                            ins = e.matmul(pb[:, i * 256:i * 256 + NTOK], lhsT=ut[:, k, cl * 128:(cl + 1) * 128], rhs=h2T[:, k, 0:NTOK],
                                           start=(k == 0), stop=(k == 7))
                    return ins
                kb.op("pe", mmpre, reads=[UB, H2T], writes=[PB])
                def mmwt(e, wb=wb, wp=wp, nj=nj):
                    for i in range(2):
                        for j in range(nj):
                            for h in range(8):
                                ins = e.matmul(wb[:, i * 256 + j * 128:i * 256 + (j + 1) * 128], lhsT=wp[:, j, h, i, :],
                                               rhs=diag[:, j, h, :], start=(h == 0), stop=(h == 7))
                    return ins
                kb.op("pe", mmwt, reads=[WPB, DG], writes=[WTB])
                kb.op("act", lambda e, pb=pb, gt=gt, half=half, NTOK=NTOK: e.activation(
                    out=gt[:, half * 2:half * 2 + 2, 0:NTOK], in_=pb[:].rearrange("p (i t) -> p i t", i=2)[:, :, 0:NTOK], func=AF.Gelu),
                    reads=[PB], writes=[GB])
            for half in range(2):
                wb, WTB = B[2 + half]
                kb.op("dve", lambda e, wb=wb, gt=gt, att=att, half=half, NTOK=NTOK: e.tensor_tensor(
                    out=att[:, half * 2:half * 2 + 2, 0:NTOK], in0=wb[:].rearrange("p (i t) -> p i t", i=2)[:, :, 0:NTOK],
                    in1=gt[:, half * 2:half * 2 + 2, 0:NTOK], op=ALU.mult), reads=[WTB, GB], writes=[ATB])
            for j in range(nj):
                for hf in range(2):
                    ob, OB = B[4 + 2 * j + hf]
                    def mmo(e, ob=ob, att=att, vt=vt, j=j, hf=hf, cg=cg):
                        for cl in range(4):
                            ins = e.matmul(ob[:], lhsT=att[:, cl, j * 128:(j + 1) * 128], rhs=vt[:, cl, hf * 512:(hf + 1) * 512],
                                           start=(cg == 0 and cl == 0), stop=(cg == 31 and cl == 3))
                        return ins
                    kb.op("pe", mmo, reads=[ATB, VB], writes=[OB])
        for j, tt in enumerate(tiles):
            rows = slice(tt * 128, (tt + 1) * 128)
            for hf in range(2):
                ob, OB = B[4 + 2 * j + hf]
                kb.op("dve", lambda e, ob=ob, hf=hf: e.tensor_tensor(out=tmp[:, hf * 512:(hf + 1) * 512], in0=ob[:],
                                                                   in1=mod[:, 3, hf * 512:(hf + 1) * 512], op=ALU.mult),
                      reads=[OB, MOD], writes=[TMP])
            kb.op("pool", lambda e, j=j: e.tensor_tensor(out=xt[:, j, :], in0=xt[:, j, :], in1=tmp[:], op=ALU.add),
                  reads=[XT, TMP], writes=[XT])
            kb.dma("sp", x_out[rows, :], xt[:, j, :], reads=[XT], writes=[XOUT])
            if final_g is not None:
                kb.op("act", lambda e, j=j: e.activation(out=tmp[:], in_=xt[:, j, :], func=AF.Square, accum_out=sm[:, 0:1]),
                      reads=[XT], writes=[TMP, SM])
                rstd_ops(kb, sm, SM)
                kb.op("dve", lambda e, j=j: e.scalar_tensor_tensor(out=xn[:, 0:D], in0=xt[:, j, :], scalar=sm[:, 1:2], in1=fg[:],
                                                                   op0=ALU.mult, op1=ALU.mult), reads=[XT, SM, FG], writes=[XNB])
                kb.dma("sp", xn_out[rows, :], xn[:, 0:D], reads=[XNB], writes=[XN])


def build_pre(even):
    kb = KB()
    kb.psum_banks()
    B = kb.banks
    if even:
        NIN = 3072
        fm_blocks = [(c * 128, None) for c in range(8)] + [(1536 + c * 128, c) for c in range(8)]
        tm_groups = [(1024, 512), (2560, 512)]
    else:
        NIN = 1536
        fm_blocks = [(c * 128, c) for c in range(10)]
        tm_groups = [(1280, 256)]
    NROPE = 128 * sum(1 for _, r in fm_blocks if r is not None)
    NFM = len(fm_blocks)
    NTM = sum(n for _, n in tm_groups)
    x_in = kb.din("x", [TALL, D])
    csT = kb.din("csT", [128, 16])
    w_mod = kb.din("w_mod", [D, 6 * D])
    b_mod = kb.din("b_mod", [6 * D])
    n1g_in = kb.din("n1g", [D])
    w_in = kb.din("w_in", [D, NIN])
    w_perm = kb.din("w_perm", [D, NROPE])
    cosT = kb.din("cosT", [128, TALL])
    sinT = kb.din("sinT", [128, TALL])
    ident_d = kb.din("c_ident", [128, 128])
    fm_out, FMO = kb.dout("fmT", [NFM * 128, TALL], BF16)
    tm_out, TMO = kb.dout("tm", [TALL, NTM], BF16)
    modrow, MRO = kb.dout("modrow", [2, 6, D])

    ident_f, IDF = kb.sb("ident_f", [128, 128], F32)
    ident, ID = kb.sb("ident", [128, 128], BF16)
    kb.dma("sp", ident_f[:], ident_d, writes=[IDF])
    kb.op("act", lambda e: e.copy(out=ident[:], in_=ident_f[:]), reads=[IDF], writes=[ID])
    win, WIN = kb.sb("win", [128, 8, NIN], BF16)
    wpm, WPM = kb.sb("wpm", [128, 8, NROPE], BF16)
    for c0 in range(0, NIN, 512):
        kb.dma("pool", win[:, :, c0:c0 + 512], w_in[:, c0:c0 + 512].rearrange("(k p) n -> p k n", p=128), writes=[WIN])
    for c0 in range(0, NROPE, 512):
        n = min(512, NROPE - c0)
        kb.dma("pool", wpm[:, :, c0:c0 + n], w_perm[:, c0:c0 + n].rearrange("(k p) n -> p k n", p=128), writes=[WPM])
    cs_f, CSF = kb.sb("cs_f", [128, 16], F32)
    cs_b, CSB = kb.sb("cs_b", [128, 16], BF16)
    csbc, CSBC = kb.sb("csbc", [128, 16, 128], BF16)
    kb.dma("sp", cs_f[:], csT, writes=[CSF])
    kb.op("act", lambda e: e.activation(out=cs_b[:], in_=cs_f[:], func=AF.Silu), reads=[CSF], writes=[CSB])
    kb.op("dve", lambda e: e.tensor_copy(out=csbc[:], in_=cs_b[:].unsqueeze(2).to_broadcast([128, 16, 128])),
          reads=[CSB], writes=[CSBC])
    n1g, N1G = kb.sb("n1g_sb", [128, D], F32)
    kb.dma("sp", n1g[:], n1g_in.unsqueeze(0).to_broadcast([128, D]), writes=[N1G])
    g1s, G1S = kb.sb("g1s", [128, 2, 2, D], F32)
    wmr = [kb.sb(f"wmr{i}", [128, 8, 512], BF16) for i in range(2)]
    bmr = [kb.sb(f"bmr{i}", [128, 512], F32) for i in range(2)]
    mtmp = [kb.sb(f"mtmp{i}", [128, 512], F32) for i in range(2)]
    for cgp in range(12):
        wt, WB = wmr[cgp % 2]
        bt, BB = bmr[cgp % 2]
        cols = slice(cgp * 512, (cgp + 1) * 512)
        kb.dma("pool", wt[:], w_mod[:, cols].rearrange("(k p) n -> p k n", p=128), writes=[WB])
        kb.dma("sp", bt[:], b_mod[cols].unsqueeze(0).to_broadcast([128, 512]), writes=[BB])
        chunk = cgp // 2
        half = cgp % 2
        for s in range(2):
            mb, MB = B[6 + s]
            def mmm(e, mb=mb, wt=wt, s=s):
                for k in range(8):
                    ins = e.matmul(mb[:], lhsT=csbc[:, s * 8 + k, :], rhs=wt[:, k, :], start=(k == 0), stop=(k == 7))
                return ins
            kb.op("pe", mmm, reads=[CSBC, WB], writes=[MB])
            if chunk < 2:
                dst = g1s[:, s, chunk, half * 512:(half + 1) * 512]
                kb.op("dve", lambda e, mb=mb, bt=bt, dst=dst: e.tensor_tensor(out=dst, in0=mb[:], in1=bt[:], op=ALU.add),
                      reads=[MB, BB], writes=[G1S])
                kb.dma("sp", modrow[s, chunk:chunk + 1, half * 512:(half + 1) * 512], dst[0:1, :], reads=[G1S], writes=[MRO])
            else:
                mt, MT = mtmp[s]
                kb.op("dve", lambda e, mb=mb, bt=bt, mt=mt: e.tensor_tensor(out=mt[:], in0=mb[:], in1=bt[:], op=ALU.add),
                      reads=[MB, BB], writes=[MT])
                kb.dma("sp", modrow[s, chunk:chunk + 1, half * 512:(half + 1) * 512], mt[0:1, :], reads=[MT], writes=[MRO])
    for s in range(2):
        kb.op("dve", lambda e, s=s: e.scalar_tensor_tensor(out=g1s[:, s, 1, :], in0=g1s[:, s, 1, :], scalar=1.0, in1=n1g[:],
                                                           op0=ALU.add, op1=ALU.mult), reads=[G1S, N1G], writes=[G1S])
    xt = [kb.sb(f"xt{i}", [128, D], F32) for i in range(2)]
    tmp, TMP = kb.sb("tmp", [128, D], F32)
    hx, HX = kb.sb("hx", [128, D], BF16)
    hxT, HXT = kb.sb("hxT", [128, 8, 512], BF16)
    sm, SM = kb.sb("sm", [128, 8], F32)
    cst = [kb.sb(f"cst{i}", [128, 512], F32) for i in range(2)]
    snt = [kb.sb(f"snt{i}", [128, 512], F32) for i in range(2)]
    r1, R1 = kb.sb("r1", [128, 512], F32)
    r2, R2 = kb.sb("r2", [128, 512], F32)
    fmo = [kb.sb(f"fmo{i}", [128, 512], BF16) for i in range(2)]
    tmo = [kb.sb(f"tmo{i}", [128, 512], BF16) for i in range(2)]
    groups = [(g * 4, 4, 0) for g in range(NTL // 4)] + [(NTL, NTC, 1)]
    xi = 0
    fi = 0
    ti = 0
    for gi, (t0, ntl, s) in enumerate(groups):
        NTOK = ntl * 128
        tok0 = t0 * 128
        ct, CT = cst[gi % 2]
        st, ST = snt[gi % 2]
        kb.dma("sp", ct[:, 0:NTOK], cosT[:, tok0:tok0 + NTOK], writes=[CT])
        kb.dma("sp", st[:, 0:NTOK], sinT[:, tok0:tok0 + NTOK], writes=[ST])
        for j in range(ntl):
            x_t, XB = xt[xi % 2]
            xi += 1
            rows = slice(tok0 + j * 128, tok0 + (j + 1) * 128)
            kb.dma("sp", x_t[:], x_in[rows, :], writes=[XB])
            kb.op("act", lambda e, x_t=x_t: e.activation(out=tmp[:], in_=x_t[:], func=AF.Square, accum_out=sm[:, 0:1]),
                  reads=[XB], writes=[TMP, SM])
            rstd_ops(kb, sm, SM)
            kb.op("dve", lambda e, x_t=x_t, s=s: e.scalar_tensor_tensor(out=tmp[:], in0=x_t[:], scalar=sm[:, 1:2], in1=g1s[:, s, 1, :],
                                                                         op0=ALU.mult, op1=ALU.mult), reads=[XB, SM, G1S], writes=[TMP])
            kb.op("pool", lambda e, s=s: e.tensor_tensor(out=hx[:], in0=tmp[:], in1=g1s[:, s, 0, :], op=ALU.add),
                  reads=[TMP, G1S], writes=[HX])
            tb, TB = B[0]
            def tr(e, tb=tb):
                for k in range(8):
                    ins = e.transpose(tb.bitcast(BF16)[:, k * 128:(k + 1) * 128], hx[:, k * 128:(k + 1) * 128], ident[:])
                return ins
            kb.op("pe", tr, reads=[HX, ID], writes=[TB])
            kb.op("act", lambda e, tb=tb, j=j: e.copy(out=hxT[:, :, j * 128:(j + 1) * 128],
                                                      in_=tb.bitcast(BF16)[:, 0:1024].rearrange("p (k t) -> p k t", k=8)),
                  reads=[TB], writes=[HXT])
        for bi, (c0, ridx) in enumerate(fm_blocks):
            pa, PA = B[1 + bi % 2]
            def mma(e, pa=pa, c0=c0, NTOK=NTOK):
                for k in range(8):
                    ins = e.matmul(pa[:, 0:NTOK], lhsT=win[:, k, c0:c0 + 128], rhs=hxT[:, k, 0:NTOK], start=(k == 0), stop=(k == 7))
                return ins
            kb.op("pe", mma, reads=[WIN, HXT], writes=[PA])
            fo, FO = fmo[fi % 2]
            fi += 1
            if ridx is None:
                kb.op("act", lambda e, pa=pa, fo=fo, NTOK=NTOK: e.copy(out=fo[:, 0:NTOK], in_=pa[:, 0:NTOK]), reads=[PA], writes=[FO])
            else:
                pb, PB = B[3 + bi % 2]
                def mmb(e, pb=pb, ridx=ridx, NTOK=NTOK):
                    for k in range(8):
                        ins = e.matmul(pb[:, 0:NTOK], lhsT=wpm[:, k, ridx * 128:(ridx + 1) * 128], rhs=hxT[:, k, 0:NTOK],
                                       start=(k == 0), stop=(k == 7))
                    return ins
                kb.op("pe", mmb, reads=[WPM, HXT], writes=[PB])
                kb.op("dve", lambda e, pa=pa, ct=ct, NTOK=NTOK: e.tensor_tensor(out=r1[:, 0:NTOK], in0=pa[:, 0:NTOK], in1=ct[:, 0:NTOK], op=ALU.mult),
                      reads=[PA, CT], writes=[R1])
                kb.op("dve", lambda e, pb=pb, st=st, NTOK=NTOK: e.tensor_tensor(out=r2[:, 0:NTOK], in0=pb[:, 0:NTOK], in1=st[:, 0:NTOK], op=ALU.mult),
                      reads=[PB, ST], writes=[R2])
                kb.op("pool", lambda e, fo=fo, NTOK=NTOK: e.tensor_tensor(out=fo[:, 0:NTOK], in0=r1[:, 0:NTOK], in1=r2[:, 0:NTOK], op=ALU.add),
                      reads=[R1, R2], writes=[FO])
            for c0_ in range(0, NTOK, 256):
                kb.dma("sp", fm_out[bi * 128:(bi + 1) * 128, tok0 + c0_:tok0 + c0_ + 256], fo[:, c0_:c0_ + 256], reads=[FO], writes=[FMO])
        for j in range(ntl):
            oc = 0
            for (c0, ncol) in tm_groups:
                pt, PT = B[5 + ti % 2]
                to, TO = tmo[ti % 2]
                ti += 1
                def mmt(e, pt=pt, c0=c0, ncol=ncol, j=j):
                    for k in range(8):
                        ins = e.matmul(pt[:, 0:ncol], lhsT=hxT[:, k, j * 128:(j + 1) * 128], rhs=win[:, k, c0:c0 + ncol],
                                       start=(k == 0), stop=(k == 7))
                    return ins
                kb.op("pe", mmt, reads=[HXT, WIN], writes=[PT])
                kb.op("act", lambda e, pt=pt, to=to, ncol=ncol: e.copy(out=to[:, 0:ncol], in_=pt[:, 0:ncol]), reads=[PT], writes=[TO])
                rows = slice(tok0 + j * 128, tok0 + (j + 1) * 128)
                kb.dma("sp", tm_out[rows, oc:oc + ncol], to[:, 0:ncol], reads=[TO], writes=[TMO])
                oc += ncol
    return kb.finish()


def emit_attn_even(kb, ao, AO):
    B = kb.banks
    naqT = kb.din("naqT", [512, TALL], BF16)
    nakT = kb.din("nakT_h", [512, 74 * 64], BF16)
    nav = kb.din("nav_h", [74 * 64, 520], BF16)
    nakTc = kb.din("nakT_c", [512, CTX], BF16)
    navc = kb.din("nav_c", [CTX, 520], BF16)
    dqT = kb.din("dqT", [512, TALL], BF16)
    dkT = kb.din("dkT_all", [512, CTX + SEQ], BF16)
    dv = kb.din("dv_all", [CTX + SEQ, 516], BF16)
    nbias = kb.din("nbias", [5, 8, 128, 640])
    lam_in = kb.din("lam", [4, 64])
    subg_in = kb.din("subg", [128])
    lami_in = kb.din("lam_init", [128, 2])
    sc = 64 ** -0.5
    lm, LM = kb.sb("lm", [128, 4, 64], F32)
    lms, LMS = kb.sb("lms", [128, 16], F32)
    subg, SUBG = kb.sb("subg_sb", [128, 128], F32)
    kb.dma("sp", lm[:].rearrange("p a b -> p (a b)"), lam_in.rearrange("a b -> (a b)").unsqueeze(0).to_broadcast([128, 256]), writes=[LM])
    kb.dma("sp", subg[:], subg_in.unsqueeze(0).to_broadcast([128, 128]), writes=[SUBG])
    kb.dma("sp", lms[:, 8:10], lami_in, writes=[LMS])
    kb.op("dve", lambda e: e.tensor_tensor(out=lm[:, 0, :], in0=lm[:, 0, :], in1=lm[:, 1, :], op=ALU.mult), reads=[LM], writes=[LM])
    kb.op("dve", lambda e: e.tensor_tensor(out=lm[:, 2, :], in0=lm[:, 2, :], in1=lm[:, 3, :], op=ALU.mult), reads=[LM], writes=[LM])
    kb.op("dve", lambda e: e.tensor_reduce(out=lms[:, 0:1], in_=lm[:, 0, :], axis=AX.X, op=ALU.add), reads=[LM], writes=[LMS])
    kb.op("dve", lambda e: e.tensor_reduce(out=lms[:, 1:2], in_=lm[:, 2, :], axis=AX.X, op=ALU.add), reads=[LM], writes=[LMS])
    kb.op("act", lambda e: e.activation(out=lms[:, 2:4], in_=lms[:, 0:2], func=AF.Exp), reads=[LMS], writes=[LMS])
    kb.op("dve", lambda e: e.tensor_tensor(out=lms[:, 4:5], in0=lms[:, 2:3], in1=lms[:, 3:4], op=ALU.subtract), reads=[LMS], writes=[LMS])
    kb.op("dve", lambda e: e.tensor_tensor(out=lms[:, 5:6], in0=lms[:, 4:5], in1=lms[:, 8:9], op=ALU.add), reads=[LMS], writes=[LMS])
    kb.op("dve", lambda e: e.tensor_scalar(out=lms[:, 6:7], in0=lms[:, 5:6], scalar1=-1.0, scalar2=None, op0=ALU.mult), reads=[LMS], writes=[LMS])
    kb.op("dve", lambda e: e.tensor_scalar(out=subg[:], in0=subg[:], scalar1=lms[:, 9:10], scalar2=None, op0=ALU.mult), reads=[SUBG, LMS], writes=[SUBG])

    kcT, KCT = kb.sb("na_kcT", [128, 4, CTX], BF16)
    vca, VCA = kb.sb("na_vca", [128, 2, 8, 65], BF16)
    kb.dma("sp", kcT[:], nakTc.rearrange("(a p) n -> p a n", p=128), writes=[KCT])
    kb.dma("sp", vca[:].rearrange("p b h c -> p b (h c)"), navc.rearrange("(b p) c -> p b c", p=128), writes=[VCA])
    kts = [kb.sb(f"na_kt{i}", [128, 4, 640], BF16) for i in range(2)]
    vts = [kb.sb(f"na_vt{i}", [128, 5, 8, 65], BF16) for i in range(2)]
    qts = [kb.sb(f"na_qt{i}", [128, 4, 128], BF16) for i in range(2)]
    bts = [kb.sb(f"na_bt{i}", [128, 640], F32) for i in range(2)]
    sts = [kb.sb(f"na_st{i}", [128, 640], F32) for i in range(2)]
    pts = [kb.sb(f"na_pt{i}", [128, 896], BF16) for i in range(2)]
    aot = [kb.sb(f"na_ao{i}", [128, 512], BF16) for i in range(2)]
    rc, RC = kb.sb("na_rc", [128, 8], F32)
    hi = 0
    for qt in range(NT):
        isctx = qt >= NTL
        q_t, QB = qts[qt % 2]
        kb.dma("sp", q_t[:], naqT[:, qt * 128:(qt + 1) * 128].rearrange("(a p) n -> p a n", p=128), writes=[QB])
        if not isctx:
            k_t, KB_ = kts[qt % 2]
            v_t, VB = vts[qt % 2]
            kb.dma("sp", k_t[:], nakT[:, qt * 128:qt * 128 + 640].rearrange("(a p) n -> p a n", p=128), writes=[KB_])
            kb.dma("sp", v_t[:].rearrange("p b h c -> p b (h c)"), nav[qt * 128:qt * 128 + 640, :].rearrange("(b p) c -> p b c", p=128), writes=[VB])
            slot = 0 if qt == 0 else 1 if qt == 1 else 3 if qt == NTL - 2 else 4 if qt == NTL - 1 else 2
        a_t, AB = aot[qt % 2]
        for h in range(8):
            a, off = h // 2, (h % 2) * 64
            pa, PA = B[(2 * hi) % 4]
            pb, PB = B[(2 * hi + 1) % 4]
            st_, STB = sts[hi % 2]
            pt_, PTB = pts[hi % 2]
            acc, ACC = B[4 + (h // 4)]
            hi += 1
            if not isctx:
                b_t, BB = bts[hi % 2]
                kb.dma("sp", b_t[:], nbias[slot, h], writes=[BB])
                def mms(e, pa=pa, pb=pb, k_t=k_t, q_t=q_t, a=a, off=off):
                    for blk in range(4):
                        e.matmul(pa[:, blk * 128:(blk + 1) * 128], lhsT=k_t[off:off + 64, a, blk * 128:(blk + 1) * 128],
                                 rhs=q_t[off:off + 64, a, :], start=True, stop=True)
                    e.matmul(pb[:, 0:128], lhsT=k_t[off:off + 64, a, 512:640], rhs=q_t[off:off + 64, a, :], start=True, stop=True)
                    for cb in range(2):
                        ins = e.matmul(pb[:, 128 + cb * 128:256 + cb * 128], lhsT=kcT[off:off + 64, a, cb * 128:(cb + 1) * 128],
                                       rhs=q_t[off:off + 64, a, :], start=True, stop=True)
                    return ins
                kb.op("pe", mms, reads=[KB_, QB, KCT], writes=[PA, PB])
                kb.op("dve", lambda e, pa=pa, st_=st_, b_t=b_t: e.scalar_tensor_tensor(out=st_[:, 0:512], in0=pa[:], scalar=sc, in1=b_t[:, 0:512],
                                                                                    op0=ALU.mult, op1=ALU.add), reads=[PA, BB], writes=[STB])
                kb.op("dve", lambda e, pb=pb, st_=st_, b_t=b_t: e.scalar_tensor_tensor(out=st_[:, 512:640], in0=pb[:, 0:128], scalar=sc,
                                                                                    in1=b_t[:, 512:640], op0=ALU.mult, op1=ALU.add),
                      reads=[PB, BB], writes=[STB])
                kb.op("act", lambda e, st_=st_, pt_=pt_: e.activation(out=pt_[:, 0:640], in_=st_[:], func=AF.Exp), reads=[STB], writes=[PTB])
                kb.op("act", lambda e, pb=pb, pt_=pt_: e.activation(out=pt_[:, 640:896], in_=pb[:, 128:384], func=AF.Exp, scale=sc),
                      reads=[PB], writes=[PTB])
                def mmv(e, acc=acc, pt_=pt_, v_t=v_t, h=h):
                    o = acc[:, (h % 4) * 65:(h % 4) * 65 + 65]
                    for blk in range(5):
                        e.matmul(o, lhsT=pt_[:, blk * 128:(blk + 1) * 128], rhs=v_t[:, blk, h, :], start=(blk == 0), stop=False)
                    for cb in range(2):
                        ins = e.matmul(o, lhsT=pt_[:, 640 + cb * 128:768 + cb * 128], rhs=vca[:, cb, h, :], start=False, stop=(cb == 1))
                    return ins
                kb.op("pe", mmv, reads=[PTB, VB, VCA], writes=[ACC])
            else:
                def mms(e, pb=pb, q_t=q_t, a=a, off=off):
                    for cb in range(2):
                        ins = e.matmul(pb[:, 128 + cb * 128:256 + cb * 128], lhsT=kcT[off:off + 64, a, cb * 128:(cb + 1) * 128],
                                       rhs=q_t[off:off + 64, a, :], start=True, stop=True)
                    return ins
                kb.op("pe", mms, reads=[QB, KCT], writes=[PB])
                kb.op("act", lambda e, pb=pb, pt_=pt_: e.activation(out=pt_[:, 640:896], in_=pb[:, 128:384], func=AF.Exp, scale=sc),
                      reads=[PB], writes=[PTB])
                def mmv(e, acc=acc, pt_=pt_, h=h):
                    o = acc[:, (h % 4) * 65:(h % 4) * 65 + 65]
                    for cb in range(2):
                        ins = e.matmul(o, lhsT=pt_[:, 640 + cb * 128:768 + cb * 128], rhs=vca[:, cb, h, :], start=(cb == 0), stop=(cb == 1))
                    return ins
                kb.op("pe", mmv, reads=[PTB, VCA], writes=[ACC])
            if h % 4 == 3:
                g4 = h // 4
                av = acc[:, 0:260].rearrange("p (h c) -> p h c", c=65)
                kb.op("dve", lambda e, av=av, g4=g4: e.reciprocal(out=rc[:, g4 * 4:g4 * 4 + 4], in_=av[:, :, 64]), reads=[ACC], writes=[RC])
                kb.op("dve", lambda e, av=av, g4=g4, a_t=a_t: e.tensor_tensor(
                    out=a_t[:, g4 * 256:(g4 + 1) * 256].rearrange("p (h d) -> p h d", d=64), in0=av[:, :, 0:64],
                    in1=rc[:, g4 * 4:g4 * 4 + 4].unsqueeze(2).to_broadcast([128, 4, 64]), op=ALU.mult), reads=[ACC, RC], writes=[AB])
        kb.dma("sp", ao[qt * 128:(qt + 1) * 128, 0:512], a_t[:], reads=[AB], writes=[AO])

    NBLK = (CTX + SEQ) // 128
    dk, DK = kb.sb("d_k", [128, CTX + SEQ], BF16)
    dva, DVA = kb.sb("d_va", [128, NBLK, 129], BF16)
    dq, DQ = kb.sb("d_q", [128, TALL], BF16)
    dpt = [kb.sb(f"d_pt{i}", [128, 512], BF16) for i in range(3)]
    o0, O0 = kb.sb("d_o0", [128, 128], F32)
    o1, O1 = kb.sb("d_o1", [128, 128], F32)
    osq, OSQ = kb.sb("d_osq", [128, 128], F32)
    dsm, DSM = kb.sb("d_sm", [128, 8], F32)
    dob = [kb.sb(f"d_ob{i}", [128, 128], BF16) for i in range(2)]
    si = 0
    oi = 0
    for h in range(4):
        kb.dma("sp", dk[:], dkT[h * 128:(h + 1) * 128, :], writes=[DK])
        for c0 in range(0, NBLK, 26):
            kb.dma("sp", dva[:, c0:c0 + 26, :], dv[c0 * 128:(c0 + 26) * 128, h * 129:(h + 1) * 129].rearrange("(b p) d -> p b d", p=128),
                   writes=[DVA])
        kb.dma("sp", dq[:], dqT[h * 128:(h + 1) * 128, :], writes=[DQ])
        qgroups = [(g * 256, 256, NBLK) for g in range(TQ // 256)] + [(TQ, CTX, CTX // 128)]
        for (q0, nq, nblk) in qgroups:
            nqs = nq // 128
            for blk in range(nblk):
                for m in range(2):
                    ps, PS = B[si % 3]
                    pt_, PTB = dpt[si % 3]
                    si += 1
                    kb.op("pe", lambda e, ps=ps, m=m, blk=blk, q0=q0, nq=nq: e.matmul(
                        ps[:, 0:nq], lhsT=dk[m * 64:(m + 1) * 64, blk * 128:(blk + 1) * 128], rhs=dq[m * 64:(m + 1) * 64, q0:q0 + nq],
                        start=True, stop=True), reads=[DK, DQ], writes=[PS])
                    kb.op("act", lambda e, ps=ps, pt_=pt_, nq=nq: e.activation(out=pt_[:, 0:nq], in_=ps[:, 0:nq], func=AF.Exp, scale=sc),
                          reads=[PS], writes=[PTB])
                    def mmv(e, pt_=pt_, m=m, blk=blk, nqs=nqs, nblk=nblk):
                        for qs in range(nqs):
                            a = qs * 2 + m
                            acc = B[3 + a][0]
                            ins = e.matmul(acc[:, 0:129], lhsT=pt_[:, qs * 128:(qs + 1) * 128], rhs=dva[:, blk, :],
                                           start=(blk == 0), stop=(blk == nblk - 1))
                        return ins
                    kb.op("pe", mmv, reads=[PTB, DVA], writes=[B[3 + qs * 2 + m][1] for qs in range(nqs)])
            for qs in range(nqs):
                accs = []
                for m in range(2):
                    a = qs * 2 + m
                    accs.append((B[3 + a][0][:, 0:129], B[3 + a][1]))
                (a0, A0), (a1, A1) = accs
                kb.op("dve", lambda e, a0=a0: e.reciprocal(out=dsm[:, 0:1], in_=a0[:, 128:129]), reads=[A0], writes=[DSM])
                kb.op("dve", lambda e, a1=a1: e.reciprocal(out=dsm[:, 1:2], in_=a1[:, 128:129]), reads=[A1], writes=[DSM])
                kb.op("dve", lambda e: e.tensor_tensor(out=dsm[:, 1:2], in0=dsm[:, 1:2], in1=lms[:, 6:7], op=ALU.mult), reads=[DSM, LMS], writes=[DSM])
                kb.op("dve", lambda e, a0=a0: e.tensor_scalar(out=o0[:], in0=a0[:, 0:128], scalar1=dsm[:, 0:1], scalar2=None, op0=ALU.mult),
                      reads=[A0, DSM], writes=[O0])
                kb.op("dve", lambda e, a1=a1: e.scalar_tensor_tensor(out=o1[:], in0=a1[:, 0:128], scalar=dsm[:, 1:2], in1=o0[:],
                                                                   op0=ALU.mult, op1=ALU.add), reads=[A1, DSM, O0], writes=[O1])
                kb.op("act", lambda e: e.activation(out=osq[:], in_=o1[:], func=AF.Square, accum_out=dsm[:, 2:3]), reads=[O1], writes=[OSQ, DSM])
                kb.op("dve", lambda e: e.tensor_scalar(out=dsm[:, 3:4], in0=dsm[:, 2:3], scalar1=1.0 / 128, scalar2=EPS, op0=ALU.mult, op1=ALU.add),
                      reads=[DSM], writes=[DSM])
                kb.op("act", lambda e: e.activation(out=dsm[:, 4:5], in_=dsm[:, 3:4], func=AF.Sqrt), reads=[DSM], writes=[DSM])
                kb.op("dve", lambda e: e.reciprocal(out=dsm[:, 5:6], in_=dsm[:, 4:5]), reads=[DSM], writes=[DSM])
                ob, OB = dob[oi % 2]
                oi += 1
                kb.op("dve", lambda e, ob=ob: e.scalar_tensor_tensor(out=ob[:], in0=o1[:], scalar=dsm[:, 5:6], in1=subg[:], op0=ALU.mult, op1=ALU.mult),
                      reads=[O1, DSM, SUBG], writes=[OB])
                r0 = q0 + qs * 128
                kb.dma("sp", ao[r0:r0 + 128, 512 + h * 128:512 + (h + 1) * 128], ob[:], reads=[OB], writes=[AO])


def emit_attn_odd(kb, ao, AO):
    B = kb.banks
    HALO = TQ + 256
    qin = kb.din("swa_q", [64, 16, TALL], BF16)
    kin = kb.din("swa_kT_h", [64, 4, HALO], BF16)
    vin = kb.din("swa_v_h", [HALO, 260], BF16)
    kcin = kb.din("swa_kT_c", [64, 4, CTX], BF16)
    vcin = kb.din("swa_v_c", [CTX, 260], BF16)
    sbias = kb.din("sbias", [3, 128, 384])
    sink_in = kb.din("sink", [16])
    sc = 64 ** -0.5
    snk, SNK = kb.sb("snk", [128, 16], F32)
    kb.dma("sp", snk[:], sink_in.unsqueeze(0).to_broadcast([128, 16]), writes=[SNK])
    kb.op("act", lambda e: e.activation(out=snk[:], in_=snk[:], func=AF.Exp), reads=[SNK], writes=[SNK])
    kT, KT = kb.sb("s_kT", [64, 4, HALO], BF16)
    kb.dma("sp", kT[:], kin, writes=[KT])
    kcT, KCT = kb.sb("s_kcT", [64, 4, CTX], BF16)
    kb.dma("sp", kcT[:], kcin, writes=[KCT])
    va, VA = kb.sb("s_va", [128, HALO // 128, 4, 65], BF16)
    kb.dma("sp", va[:].rearrange("p b h c -> p b (h c)"), vin.rearrange("(b p) c -> p b c", p=128), writes=[VA])
    vca, VCA = kb.sb("s_vca", [128, 2, 4, 65], BF16)
    kb.dma("sp", vca[:].rearrange("p b h c -> p b (h c)"), vcin.rearrange("(b p) c -> p b c", p=128), writes=[VCA])
    sb_, SBB = kb.sb("s_bias", [128, 3, 384], F32)
    kb.dma("sp", sb_[:], sbias.rearrange("s p n -> p s n"), writes=[SBB])
    qts = [kb.sb(f"s_q{i}", [64, 16, 128], BF16) for i in range(2)]
    sts = [kb.sb(f"s_st{i}", [128, 512], F32) for i in range(2)]
    pts = [kb.sb(f"s_pt{i}", [128, 5, 512], BF16) for i in range(2)]
    aot = [kb.sb(f"s_ao{i}", [128, D], BF16) for i in range(2)]
    den, DEN = kb.sb("s_den", [128, 8], F32)
    si = 0
    gi = 0
    for qt in range(NT):
        isctx = qt >= NTL
        q_t, QB = qts[qt % 2]
        kb.dma("sp", q_t[:], qin[:, :, qt * 128:(qt + 1) * 128], writes=[QB])
        a_t, AB = aot[qt % 2]
        slot = 0 if qt == 0 else 2 if qt == NTL - 1 else 1
        for n in range(4):
            pt_, PTB = pts[gi % 2]
            acc, ACC = B[4 + gi % 2]
            gi += 1
            blks = ([] if isctx else [0, 1, 2]) + [3, 4]
            for blk in blks:
                ps, PS = B[si % 3]
                st_, STB = sts[si % 2]
                si += 1
                if blk < 3:
                    kb.op("pe", lambda e, ps=ps, blk=blk, n=n, q_t=q_t, qt=qt: e.matmul(
                        ps[:], lhsT=kT[:, n, (qt + blk) * 128:(qt + blk + 1) * 128], rhs=q_t[:, n * 4:(n + 1) * 4, :], start=True, stop=True),
                        reads=[KT, QB], writes=[PS])
                    kb.op("dve", lambda e, ps=ps, st_=st_, blk=blk, slot=slot: e.scalar_tensor_tensor(
                        out=st_[:].rearrange("p (g q) -> p g q", g=4), in0=ps[:].rearrange("p (g q) -> p g q", g=4), scalar=sc,
                        in1=sb_[:, slot, blk * 128:(blk + 1) * 128].unsqueeze(1).to_broadcast([128, 4, 128]), op0=ALU.mult, op1=ALU.add),
                        reads=[PS, SBB], writes=[STB])
                    kb.op("act", lambda e, st_=st_, pt_=pt_, blk=blk: e.activation(out=pt_[:, blk, :], in_=st_[:], func=AF.Exp), reads=[STB], writes=[PTB])
                else:
                    cb = blk - 3
                    kb.op("pe", lambda e, ps=ps, cb=cb, n=n, q_t=q_t: e.matmul(
                        ps[:], lhsT=kcT[:, n, cb * 128:(cb + 1) * 128], rhs=q_t[:, n * 4:(n + 1) * 4, :], start=True, stop=True),
                        reads=[KCT, QB], writes=[PS])
                    kb.op("act", lambda e, ps=ps, pt_=pt_, blk=blk: e.activation(out=pt_[:, blk, :], in_=ps[:], func=AF.Exp, scale=sc),
                          reads=[PS], writes=[PTB])
            def mmv(e, acc=acc, pt_=pt_, n=n, qt=qt, blks=blks):
                for g in range(4):
                    o = acc[:, g * 65:(g + 1) * 65]
                    for i, blk in enumerate(blks):
                        rhs = va[:, qt + blk, n, :] if blk < 3 else vca[:, blk - 3, n, :]
                        ins = e.matmul(o, lhsT=pt_[:, blk, g * 128:(g + 1) * 128], rhs=rhs, start=(i == 0), stop=(i == len(blks) - 1))
                return ins
            kb.op("pe", mmv, reads=[PTB, VA, VCA], writes=[ACC])
            av = acc[:, 0:260].rearrange("p (g c) -> p g c", c=65)
            kb.op("dve", lambda e, av=av, n=n: e.tensor_tensor(out=den[:, 0:4], in0=av[:, :, 64], in1=snk[:, n * 4:(n + 1) * 4], op=ALU.add),
                  reads=[ACC, SNK], writes=[DEN])
            kb.op("dve", lambda e: e.reciprocal(out=den[:, 4:8], in_=den[:, 0:4]), reads=[DEN], writes=[DEN])
            kb.op("dve", lambda e, av=av, n=n, a_t=a_t: e.tensor_tensor(
                out=a_t[:, n * 256:(n + 1) * 256].rearrange("p (g d) -> p g d", d=64), in0=av[:, :, 0:64],
                in1=den[:, 4:8].unsqueeze(2).to_broadcast([128, 4, 64]), op=ALU.mult), reads=[ACC, DEN], writes=[AB])
        kb.dma("sp", ao[qt * 128:(qt + 1) * 128, :], a_t[:], reads=[AB], writes=[AO])


def build_post(even):
    kb = KB()
    kb.psum_banks()
    ao, AO = kb.dscratch("ao_scr", [TALL, D], BF16)
    with ExitStack() as es:
        saved = kb.es
        kb.es = es
        if even:
            emit_attn_even(kb, ao, AO)
        else:
            emit_attn_odd(kb, ao, AO)
        kb.es = saved
        kb.P.barrier()
    x_in = kb.din("x", [TALL, D])
    modrow = kb.din("modrow", [2, 6, D])
    w_out = kb.din("w_out", [D, D])
    n2g = kb.din("n2g", [D])
    fg = kb.din("fg", [D])
    wq = kb.din("wq", [D, 2048])
    keysT = kb.din("keysT", [128, 16, 128])
    uT = kb.din("uT", [D, 16384])
    v = kb.din("v", [16384, D])
    x_out, XOUT = kb.dout("x_out", [TALL, D])
    xn_out, XN = kb.dout("xn_out", [TALL, D])
    emit_post(kb, NT, x_in, ao, AO, modrow, w_out, n2g, wq, keysT, uT, v, x_out, XOUT, final_g=fg, xn_out=xn_out, XN=XN,
              tile_sets=[0] * NTL + [1] * NTC)
    return kb.finish()


GRID_W = 64
_PERM64 = np.concatenate([np.arange(16, 32), np.arange(0, 16), np.arange(48, 64), np.arange(32, 48)])
_SGN64 = np.concatenate([-np.ones(16), np.ones(16), -np.ones(16), np.ones(16)]).astype(np.float32)
_PROGS = {}


def _prog(name):
    if name not in _PROGS:
        kind, par = name.split("_")
        _PROGS[name] = build_pre(par == "even") if kind == "pre" else build_post(par == "even")
    return _PROGS[name]


def _rope_tables_T(tok0):
    t = np.arange(tok0, tok0 + TQ)
    row = (t // GRID_W).astype(np.float32)
    col = (t % GRID_W).astype(np.float32)
    half = 32
    inv = (10000.0 ** (-np.arange(0, half, 2, dtype=np.float32) / half)).astype(np.float32)
    ar = row[:, None] * inv
    ac = col[:, None] * inv
    ang = np.concatenate([ar, ar, ac, ac], axis=-1)
    cos = np.cos(ang).astype(np.float32)
    sin = np.sin(ang).astype(np.float32) * _SGN64[None, :]
    cosT = np.ones((128, TALL), np.float32)
    sinT = np.zeros((128, TALL), np.float32)
    cosT[:, :TQ] = np.tile(cos.T, (2, 1))
    sinT[:, :TQ] = np.tile(sin.T, (2, 1))
    return cosT, sinT


def _na_bias(rpb, R0):
    H = rpb.shape[0]
    out = np.full((5, H, 128, 5, 128), NEG, np.float32)
    kk = np.arange(128)
    qq = np.arange(128)
    for slot, r0 in enumerate((R0, R0 + 2, R0 + 32, R0 + 60, R0 + 62)):
        if slot == 2:
            r0 = 100 if R0 not in (0,) else 100
        qr = r0 + qq // 64
        qc = qq % 64
        rs = np.clip(qr - 4, 0, 256 - 8)
        cs = np.clip(qc - 8, 0, 64 - 16)
        for blk in range(5):
            kr = r0 - 4 + 2 * blk + kk // 64
            kc = kk % 64
            valid = ((kr[:, None] >= rs[None, :]) & (kr[:, None] < rs[None, :] + 8) &
                     (kc[:, None] >= cs[None, :]) & (kc[:, None] < cs[None, :] + 16) &
                     (kr[:, None] >= 0) & (kr[:, None] < 256))
            dr = np.clip(kr[:, None] - qr[None, :] + 7, 0, 14)
            dc = np.clip(kc[:, None] - qc[None, :] + 15, 0, 30)
            vals = rpb[:, dr, dc]
            out[slot, :, :, blk, :] = np.where(valid[None], vals, NEG)
    return out.reshape(5, H, 128, 640)


def _swa_bias(qr):
    out = np.full((3, 128, 3, 128), NEG, np.float32)
    k = np.arange(128)[:, None]
    q = np.arange(128)[None, :]
    for slot, gb in enumerate((qr * 32, qr * 32 + 5, qr * 32 + 31)):
        for blk in range(3):
            kb_ = gb - 1 + blk
            if kb_ < 0 or kb_ >= SEQ // 128:
                continue
            diff = (blk - 1) * 128 + k - q
            out[slot, :, blk, :] = np.where(np.abs(diff) <= 128, 0.0, NEG)
    return out.reshape(3, 128, 384)


def _ones_col(v, nh, dv):
    T = v.shape[0]
    o = np.ones((T, nh, dv + 1), v.dtype)
    o[:, :, :dv] = v.reshape(T, nh, dv)
    return o.reshape(T, nh * (dv + 1))


def _halo(arr, axis, lo, hi):
    n = arr.shape[axis]
    shape = list(arr.shape)
    shape[axis] = hi - lo
    out = np.zeros(shape, arr.dtype)
    s0, s1 = max(lo, 0), min(hi, n)
    src = [slice(None)] * arr.ndim
    dst = [slice(None)] * arr.ndim
    src[axis] = slice(s0, s1)
    dst[axis] = slice(s0 - lo, s1 - lo)
    out[tuple(dst)] = arr[tuple(src)]
    return out


def _run(name, in_maps):
    res = run_bass_kernel_spmd(_prog(name), in_maps, core_ids=list(range(NCORES)))
    return res.results


def kernel(x, c, ctx, c_ctx, w_mod, b_mod, norm1_g, norm2_g, w_in_even, w_out_even, na_rpb, diff_lambda,
           diff_subln_g, w_in_odd, w_out_odd, swa_sink, peer_wq, peer_keys, peer_u, peer_v, final_g, _nlayers=4, _dbg=None):
    f32 = np.float32
    x = np.asarray(x, f32)
    ctx = np.asarray(ctx, f32)
    ident = np.eye(128, dtype=f32)
    xs = []
    for core in range(NCORES):
        b, qr = divmod(core, 4)
        xs.append(np.concatenate([x[b, qr * TQ:(qr + 1) * TQ], ctx[b]], axis=0))
    csT = []
    for core in range(NCORES):
        b = core // 4
        cs = np.stack([np.asarray(c, f32)[b], np.asarray(c_ctx, f32)], 0)
        csT.append(np.ascontiguousarray(cs.reshape(2, 8, 128).transpose(2, 0, 1).reshape(128, 16)))
    ropes = [_rope_tables_T((core % 4) * TQ) for core in range(NCORES)]
    xn = None
    for i in range(_nlayers):
        even = (i % 2 == 0)
        j = i // 2
        w_in = np.asarray(w_in_even[j] if even else w_in_odd[j], f32)
        if even:
            rc = w_in[:, 1536:2560]
        else:
            rc = w_in[:, 0:1280]
        w_perm = np.ascontiguousarray(rc.reshape(D, -1, 64)[:, :, _PERM64].reshape(D, -1))
        ims = []
        for core in range(NCORES):
            ims.append({"x": xs[core], "csT": csT[core], "w_mod": np.asarray(w_mod[i], f32), "b_mod": np.asarray(b_mod[i], f32),
                        "n1g": np.asarray(norm1_g[i], f32), "w_in": w_in, "w_perm": w_perm,
                        "cosT": ropes[core][0], "sinT": ropes[core][1], "c_ident": ident})
        pre = _run("pre_even" if even else "pre_odd", ims)
        fm = [np.asarray(r["fmT"]) for r in pre]
        tm = [np.asarray(r["tm"]) for r in pre]
        if _dbg is not None:
            _dbg[f"pre{i}"] = (fm, tm, [np.asarray(r["modrow"]) for r in pre])
        keysT = np.ascontiguousarray(np.asarray(peer_keys[i], f32).reshape(16, 128, 128).transpose(2, 0, 1))
        common = {"w_out": np.asarray(w_out_even[j] if even else w_out_odd[j], f32), "n2g": np.asarray(norm2_g[i], f32),
                  "fg": np.asarray(final_g, f32), "wq": np.asarray(peer_wq[i], f32), "keysT": keysT,
                  "uT": np.ascontiguousarray(np.asarray(peer_u[i], f32).T), "v": np.asarray(peer_v[i], f32), "c_ident": ident}
        ims = []
        for core in range(NCORES):
            b, qr = divmod(core, 4)
            grp = [b * 4 + k for k in range(4)]
            im = dict(common)
            im["x"] = xs[core]
            im["modrow"] = np.asarray(pre[core]["modrow"])
            if even:
                kT_lat = np.concatenate([fm[g][512:1024, :TQ] for g in grp], axis=1)
                v_lat = np.concatenate([tm[g][:TQ, 0:512] for g in grp], axis=0)
                lo = (qr * 64 - 4) * 64
                im["naqT"] = np.ascontiguousarray(fm[core][0:512])
                im["nakT_h"] = _halo(kT_lat, 1, lo, lo + 74 * 64)
                im["nav_h"] = _ones_col(_halo(v_lat, 0, lo, lo + 74 * 64), 8, 64)
                im["nakT_c"] = np.ascontiguousarray(fm[core][512:1024, TQ:])
                im["nav_c"] = _ones_col(tm[core][TQ:, 0:512], 8, 64)
                im["dqT"] = np.ascontiguousarray(fm[core][1024:1536])
                im["dkT_all"] = np.concatenate([fm[core][1536:2048, TQ:]] + [fm[g][1536:2048, :TQ] for g in grp], axis=1)
                im["dv_all"] = _ones_col(np.concatenate([tm[core][TQ:, 512:1024]] + [tm[g][:TQ, 512:1024] for g in grp], axis=0), 4, 128)
                im["nbias"] = _na_bias(np.asarray(na_rpb[j], f32), qr * 64)
                im["lam"] = np.asarray(diff_lambda[j], f32)
                im["subg"] = np.asarray(diff_subln_g[j], f32)
                li = 0.8 - 0.6 * math.exp(-0.3 * i)
                im["lam_init"] = np.tile(np.array([[li, 1.0 - li]], f32), (128, 1))
            else:
                kT_lat = np.concatenate([fm[g][1024:1280, :TQ] for g in grp], axis=1)
                v_lat = np.concatenate([tm[g][:TQ, :] for g in grp], axis=0)
                lo = qr * TQ - 128
                im["swa_q"] = np.ascontiguousarray(fm[core][0:1024].reshape(16, 64, TALL).transpose(1, 0, 2))
                im["swa_kT_h"] = np.ascontiguousarray(_halo(kT_lat, 1, lo, lo + TQ + 256).reshape(4, 64, TQ + 256).transpose(1, 0, 2))
                im["swa_v_h"] = _ones_col(_halo(v_lat, 0, lo, lo + TQ + 256), 4, 64)
                im["swa_kT_c"] = np.ascontiguousarray(fm[core][1024:1280, TQ:].reshape(4, 64, CTX).transpose(1, 0, 2))
                im["swa_v_c"] = _ones_col(tm[core][TQ:, :], 4, 64)
                im["sbias"] = _swa_bias(qr)
                im["sink"] = np.asarray(swa_sink[j], f32)
            ims.append(im)
        post = _run("post_even" if even else "post_odd", ims)
        xs = [np.asarray(r["x_out"]) for r in post]
        xn = [np.asarray(r["xn_out"]) for r in post]
        if _dbg is not None:
            _dbg[f"x{i}"] = xs
    out = np.zeros((2, SEQ, D), f32)
    for core in range(NCORES):
        b, qr = divmod(core, 4)
        out[b, qr * TQ:(qr + 1) * TQ] = xn[core][:TQ]
    return out


RG = [[0, 1, 2, 3], [4, 5, 6, 7]]


def emit_pre_f(kb, even, x_src, XS, csT, w_mod, b_mod, n1g_in, w_in, w_perm, cosT, sinT, fm_out, FMO, tm_out, TMO, modrow, MRO):
    B = kb.banks
    if even:
        NIN = 3072
        fm_blocks = [(c * 128, None) for c in range(8)] + [(1536 + c * 128, c) for c in range(8)]
        tm_groups = [(1024, 512, 8, 64, 0), (2560, 512, 4, 128, 520)]
    else:
        NIN = 1536
        fm_blocks = [(c * 128, c) for c in range(10)]
        tm_groups = [(1280, 256, 4, 64, 0)]
    NROPE = 128 * sum(1 for _, r in fm_blocks if r is not None)
    ident_f, IDF, ident, ID = kb.ident()
    win, WIN = kb.sb("win", [128, 8, NIN], BF16)
    wpm, WPM = kb.sb("wpm", [128, 8, NROPE], BF16)
    for c0 in range(0, NIN, 512):
        kb.dma("pool", win[:, :, c0:c0 + 512], w_in[:, c0:c0 + 512].rearrange("(k p) n -> p k n", p=128), writes=[WIN])
    for c0 in range(0, NROPE, 512):
        n = min(512, NROPE - c0)
        kb.dma("pool", wpm[:, :, c0:c0 + n], w_perm[:, c0:c0 + n].rearrange("(k p) n -> p k n", p=128), writes=[WPM])
    cs_f, CSF = kb.sb("cs_f", [128, 16], F32)
    cs_b, CSB = kb.sb("cs_b", [128, 16], BF16)
    csbc, CSBC = kb.sb("csbc", [128, 16, 128], BF16)
    kb.dma("sp", cs_f[:], csT, writes=[CSF])
    kb.op("act", lambda e: e.activation(out=cs_b[:], in_=cs_f[:], func=AF.Silu), reads=[CSF], writes=[CSB])
    kb.op("dve", lambda e: e.tensor_copy(out=csbc[:], in_=cs_b[:].unsqueeze(2).to_broadcast([128, 16, 128])),
          reads=[CSB], writes=[CSBC])
    n1g, N1G = kb.sb("n1g_sb", [128, D], F32)
    kb.dma("sp", n1g[:], n1g_in.unsqueeze(0).to_broadcast([128, D]), writes=[N1G])
    g1s, G1S = kb.sb("g1s", [128, 2, 2, D], F32)
    wmr = [kb.sb(f"wmr{i}", [128, 8, 512], BF16) for i in range(2)]
    bmr = [kb.sb(f"bmr{i}", [128, 512], F32) for i in range(2)]
    mtmp = [kb.sb(f"mtmp{i}", [128, 512], F32) for i in range(2)]
    for cgp in range(12):
        wt, WB = wmr[cgp % 2]
        bt, BB = bmr[cgp % 2]
        cols = slice(cgp * 512, (cgp + 1) * 512)
        kb.dma("pool", wt[:], w_mod[:, cols].rearrange("(k p) n -> p k n", p=128), writes=[WB])
        kb.dma("sp", bt[:], b_mod[cols].unsqueeze(0).to_broadcast([128, 512]), writes=[BB])
        chunk = cgp // 2
        half = cgp % 2
        for s in range(2):
            mb, MB = B[6 + s]
            def mmm(e, mb=mb, wt=wt, s=s):
                for k in range(8):
                    ins = e.matmul(mb[:], lhsT=csbc[:, s * 8 + k, :], rhs=wt[:, k, :], start=(k == 0), stop=(k == 7))
                return ins
            kb.op("pe", mmm, reads=[CSBC, WB], writes=[MB])
            if chunk < 2:
                dst = g1s[:, s, chunk, half * 512:(half + 1) * 512]
                kb.op("dve", lambda e, mb=mb, bt=bt, dst=dst: e.tensor_tensor(out=dst, in0=mb[:], in1=bt[:], op=ALU.add),
                      reads=[MB, BB], writes=[G1S])
                kb.dma("sp", modrow[s, chunk:chunk + 1, half * 512:(half + 1) * 512], dst[0:1, :], reads=[G1S], writes=[MRO])
            else:
                mt, MT = mtmp[s]
                kb.op("dve", lambda e, mb=mb, bt=bt, mt=mt: e.tensor_tensor(out=mt[:], in0=mb[:], in1=bt[:], op=ALU.add),
                      reads=[MB, BB], writes=[MT])
                kb.dma("sp", modrow[s, chunk:chunk + 1, half * 512:(half + 1) * 512], mt[0:1, :], reads=[MT], writes=[MRO])
    for s in range(2):
        kb.op("dve", lambda e, s=s: e.scalar_tensor_tensor(out=g1s[:, s, 1, :], in0=g1s[:, s, 1, :], scalar=1.0, in1=n1g[:],
                                                           op0=ALU.add, op1=ALU.mult), reads=[G1S, N1G], writes=[G1S])
    xt = [kb.sb(f"xt{i}", [128, D], F32) for i in range(2)]
    tmp, TMP = kb.sb("tmp", [128, D], F32)
    hx, HX = kb.sb("hx", [128, D], BF16)
    hxT, HXT = kb.sb("hxT", [128, 8, 512], BF16)
    sm, SM = kb.sb("sm", [128, 8], F32)
    cst = [kb.sb(f"cst{i}", [128, 512], F32) for i in range(2)]
    snt = [kb.sb(f"snt{i}", [128, 512], F32) for i in range(2)]
    r1, R1 = kb.sb("r1", [128, 512], F32)
    r2, R2 = kb.sb("r2", [128, 512], F32)
    fmo = [kb.sb(f"fmo{i}", [128, 512], BF16) for i in range(2)]
    tmo = [kb.sb(f"tmo{i}", [128, 520], BF16) for i in range(2)]
    for to, TO in tmo:
        kb.op("pool", lambda e, to=to: e.memset(to[:], 1.0), writes=[TO])
    groups = [(g * 4, 4, 0) for g in range(NTL // 4)] + [(NTL, NTC, 1)]
    xi = fi = ti = 0
    for gi, (t0, ntl, s) in enumerate(groups):
        NTOK = ntl * 128
        tok0 = t0 * 128
        ct, CT = cst[gi % 2]
        st, ST = snt[gi % 2]
        kb.dma("sp", ct[:, 0:NTOK], cosT[:, tok0:tok0 + NTOK], writes=[CT])
        kb.dma("sp", st[:, 0:NTOK], sinT[:, tok0:tok0 + NTOK], writes=[ST])
        for j in range(ntl):
            x_t, XB = xt[xi % 2]
            xi += 1
            rows = slice(tok0 + j * 128, tok0 + (j + 1) * 128)
            kb.dma("sp", x_t[:], x_src[rows, :], reads=[XS], writes=[XB])
            kb.op("act", lambda e, x_t=x_t: e.activation(out=tmp[:], in_=x_t[:], func=AF.Square, accum_out=sm[:, 0:1]),
                  reads=[XB], writes=[TMP, SM])
            rstd_ops(kb, sm, SM)
            kb.op("dve", lambda e, x_t=x_t, s=s: e.scalar_tensor_tensor(out=tmp[:], in0=x_t[:], scalar=sm[:, 1:2], in1=g1s[:, s, 1, :],
                                                                         op0=ALU.mult, op1=ALU.mult), reads=[XB, SM, G1S], writes=[TMP])
            kb.op("pool", lambda e, s=s: e.tensor_tensor(out=hx[:], in0=tmp[:], in1=g1s[:, s, 0, :], op=ALU.add),
                  reads=[TMP, G1S], writes=[HX])
            tb, TB = B[0]
            def tr(e, tb=tb):
                for k in range(8):
                    ins = e.transpose(tb.bitcast(BF16)[:, k * 128:(k + 1) * 128], hx[:, k * 128:(k + 1) * 128], ident[:])
                return ins
            kb.op("pe", tr, reads=[HX, ID], writes=[TB])
            kb.op("act", lambda e, tb=tb, j=j: e.copy(out=hxT[:, :, j * 128:(j + 1) * 128],
                                                      in_=tb.bitcast(BF16)[:, 0:1024].rearrange("p (k t) -> p k t", k=8)),
                  reads=[TB], writes=[HXT])
        for bi, (c0, ridx) in enumerate(fm_blocks):
            pa, PA = B[1 + bi % 2]
            def mma(e, pa=pa, c0=c0, NTOK=NTOK):
                for k in range(8):
                    ins = e.matmul(pa[:, 0:NTOK], lhsT=win[:, k, c0:c0 + 128], rhs=hxT[:, k, 0:NTOK], start=(k == 0), stop=(k == 7))
                return ins
            kb.op("pe", mma, reads=[WIN, HXT], writes=[PA])
            fo, FO = fmo[fi % 2]
            fi += 1
            if ridx is None:
                kb.op("act", lambda e, pa=pa, fo=fo, NTOK=NTOK: e.copy(out=fo[:, 0:NTOK], in_=pa[:, 0:NTOK]), reads=[PA], writes=[FO])
            else:
                pb, PB = B[3 + bi % 2]
                def mmb(e, pb=pb, ridx=ridx, NTOK=NTOK):
                    for k in range(8):
                        ins = e.matmul(pb[:, 0:NTOK], lhsT=wpm[:, k, ridx * 128:(ridx + 1) * 128], rhs=hxT[:, k, 0:NTOK],
                                       start=(k == 0), stop=(k == 7))
                    return ins
                kb.op("pe", mmb, reads=[WPM, HXT], writes=[PB])
                kb.op("dve", lambda e, pa=pa, ct=ct, NTOK=NTOK: e.tensor_tensor(out=r1[:, 0:NTOK], in0=pa[:, 0:NTOK], in1=ct[:, 0:NTOK], op=ALU.mult),
                      reads=[PA, CT], writes=[R1])
                kb.op("dve", lambda e, pb=pb, st=st, NTOK=NTOK: e.tensor_tensor(out=r2[:, 0:NTOK], in0=pb[:, 0:NTOK], in1=st[:, 0:NTOK], op=ALU.mult),
                      reads=[PB, ST], writes=[R2])
                kb.op("pool", lambda e, fo=fo, NTOK=NTOK: e.tensor_tensor(out=fo[:, 0:NTOK], in0=r1[:, 0:NTOK], in1=r2[:, 0:NTOK], op=ALU.add),
                      reads=[R1, R2], writes=[FO])
            kb.dma("sp", fm_out[bi * 128:(bi + 1) * 128, tok0:tok0 + NTOK], fo[:, 0:NTOK], reads=[FO], writes=[FMO])
        for j in range(ntl):
            for (c0, ncol, nh, dv, oc) in tm_groups:
                pt, PT = B[5 + ti % 2]
                to, TO = tmo[ti % 2]
                ti += 1
                def mmt(e, pt=pt, c0=c0, ncol=ncol, j=j):
                    for k in range(8):
                        ins = e.matmul(pt[:, 0:ncol], lhsT=hxT[:, k, j * 128:(j + 1) * 128], rhs=win[:, k, c0:c0 + ncol],
                                       start=(k == 0), stop=(k == 7))
                    return ins
                kb.op("pe", mmt, reads=[HXT, WIN], writes=[PT])
                wdt = nh * (dv + 1)
                kb.op("act", lambda e, pt=pt, to=to, ncol=ncol, nh=nh, dv=dv, wdt=wdt: e.copy(
                    out=to[:, 0:wdt].rearrange("p (h c) -> p h c", c=dv + 1)[:, :, 0:dv],
                    in_=pt[:, 0:ncol].rearrange("p (h c) -> p h c", c=dv)), reads=[PT], writes=[TO])
                rows = slice(tok0 + j * 128, tok0 + (j + 1) * 128)
                kb.dma("sp", tm_out[rows, oc:oc + wdt], to[:, 0:wdt], reads=[TO], writes=[TMO])


NA_NBMAX = 12


def _na_blocklist(qt):
    if qt == 0:
        return 0, 4, [("tail", 0), ("tail", 1)]
    if qt == 1:
        return 0, 4, [("tail", 1)]
    if qt == NTL - 2:
        return TQ - 512, 4, [("head", 0)]
    if qt == NTL - 1:
        return TQ - 512, 4, [("head", 0), ("head", 1)]
    return qt * 128 - 256, 5, []


def emit_attn_even_f(kb, fmT, FMT, tmv, TMV, dk_recv, DKR, dv_recv, DVR, nbk_recv, NBKR, nbv_recv, NBVR,
                     nbias, lam_in, subg_in, lami_in, ao, AO):
    B = kb.banks
    sc = 64 ** -0.5
    lm, LM = kb.sb("lm", [128, 4, 64], F32)
    lms, LMS = kb.sb("lms", [128, 16], F32)
    subg, SUBG = kb.sb("subg_sb", [128, 128], F32)
    kb.dma("sp", lm[:].rearrange("p a b -> p (a b)"), lam_in.rearrange("a b -> (a b)").unsqueeze(0).to_broadcast([128, 256]), writes=[LM])
    kb.dma("sp", subg[:], subg_in.unsqueeze(0).to_broadcast([128, 128]), writes=[SUBG])
    kb.dma("sp", lms[:, 8:10], lami_in, writes=[LMS])
    kb.op("dve", lambda e: e.tensor_tensor(out=lm[:, 0, :], in0=lm[:, 0, :], in1=lm[:, 1, :], op=ALU.mult), reads=[LM], writes=[LM])
    kb.op("dve", lambda e: e.tensor_tensor(out=lm[:, 2, :], in0=lm[:, 2, :], in1=lm[:, 3, :], op=ALU.mult), reads=[LM], writes=[LM])
    kb.op("dve", lambda e: e.tensor_reduce(out=lms[:, 0:1], in_=lm[:, 0, :], axis=AX.X, op=ALU.add), reads=[LM], writes=[LMS])
    kb.op("dve", lambda e: e.tensor_reduce(out=lms[:, 1:2], in_=lm[:, 2, :], axis=AX.X, op=ALU.add), reads=[LM], writes=[LMS])
    kb.op("act", lambda e: e.activation(out=lms[:, 2:4], in_=lms[:, 0:2], func=AF.Exp), reads=[LMS], writes=[LMS])
    kb.op("dve", lambda e: e.tensor_tensor(out=lms[:, 4:5], in0=lms[:, 2:3], in1=lms[:, 3:4], op=ALU.subtract), reads=[LMS], writes=[LMS])
    kb.op("dve", lambda e: e.tensor_tensor(out=lms[:, 5:6], in0=lms[:, 4:5], in1=lms[:, 8:9], op=ALU.add), reads=[LMS], writes=[LMS])
    kb.op("dve", lambda e: e.tensor_scalar(out=lms[:, 6:7], in0=lms[:, 5:6], scalar1=-1.0, scalar2=None, op0=ALU.mult), reads=[LMS], writes=[LMS])
    kb.op("dve", lambda e: e.tensor_scalar(out=subg[:], in0=subg[:], scalar1=lms[:, 9:10], scalar2=None, op0=ALU.mult), reads=[SUBG, LMS], writes=[SUBG])

    naqT = fmT[0:512, :]
    nakT = fmT[512:1024, :]
    kcT, KCT = kb.sb("na_kcT", [128, 4, CTX], BF16)
    vca, VCA = kb.sb("na_vca", [128, 2, 8, 65], BF16)
    kb.dma("sp", kcT[:], nakT[:, TQ:TALL].rearrange("(a p) n -> p a n", p=128), reads=[FMT], writes=[KCT])
    kb.dma("sp", vca[:].rearrange("p b h c -> p b (h c)"), tmv[TQ:TALL, 0:520].rearrange("(b p) c -> p b c", p=128), reads=[TMV], writes=[VCA])
    kts = [kb.sb(f"na_kt{i}", [128, 4, NA_NBMAX * 128], BF16) for i in range(2)]
    vts = [kb.sb(f"na_vt{i}", [128, NA_NBMAX, 8, 65], BF16) for i in range(2)]
    qts = [kb.sb(f"na_qt{i}", [128, 4, 128], BF16) for i in range(2)]
    bts = [kb.sb(f"na_bt{i}", [128, NA_NBMAX * 128], F32) for i in range(2)]
    sts = [kb.sb(f"na_st{i}", [128, NA_NBMAX * 128], F32) for i in range(2)]
    pts = [kb.sb(f"na_pt{i}", [128, (NA_NBMAX + 2) * 128], BF16) for i in range(2)]
    aot = [kb.sb(f"na_ao{i}", [128, 512], BF16) for i in range(2)]
    rc, RC = kb.sb("na_rc", [128, 8], F32)
    hi = 0
    bk = 0
    for qt in range(NT):
        isctx = qt >= NTL
        q_t, QB = qts[qt % 2]
        kb.dma("sp", q_t[:], naqT[:, qt * 128:(qt + 1) * 128].rearrange("(a p) n -> p a n", p=128), reads=[FMT], writes=[QB])
        nb = 0
        if not isctx:
            k_t, KB_ = kts[qt % 2]
            v_t, VB = vts[qt % 2]
            o0, onb, cands = _na_blocklist(qt)
            kb.dma("sp", k_t[:, :, 0:onb * 128], nakT[:, o0:o0 + onb * 128].rearrange("(a p) n -> p a n", p=128), reads=[FMT], writes=[KB_])
            kb.dma("sp", v_t[:, 0:onb].rearrange("p b h c -> p b (h c)"), tmv[o0:o0 + onb * 128, 0:520].rearrange("(b p) c -> p b c", p=128),
                   reads=[TMV], writes=[VB])
            nb = onb
            ncb = len(cands)
            if ncb:
                which = cands[0][0]
                cb0 = cands[0][1]
                col0 = (0 if which == "tail" else 256) + cb0 * 128
                for r in range(4):
                    kb.dma("sp", k_t[:, :, nb * 128:(nb + ncb) * 128],
                           nbk_recv[r * 512:(r + 1) * 512, col0:col0 + ncb * 128].rearrange("(a p) n -> p a n", p=128), reads=[NBKR], writes=[KB_])
                    kb.dma("sp", v_t[:, nb:nb + ncb].rearrange("p b h c -> p b (h c)"),
                           nbv_recv[r * 512 + col0:r * 512 + col0 + ncb * 128, :].rearrange("(b p) c -> p b c", p=128), reads=[NBVR], writes=[VB])
                    nb += ncb
            slot = 0 if qt == 0 else 1 if qt == 1 else 3 if qt == NTL - 2 else 4 if qt == NTL - 1 else 2
        a_t, AB = aot[qt % 2]
        for h in range(8):
            a, off = h // 2, (h % 2) * 64
            st_, STB = sts[hi % 2]
            pt_, PTB = pts[hi % 2]
            acc, ACC = B[4 + (h // 4)]
            hi += 1
            if not isctx:
                b_t, BB = bts[hi % 2]
                kb.dma("sp", b_t[:, 0:nb * 128], nbias[slot, h, :, 0:nb * 128], writes=[BB])
                for c0 in range(0, nb, 4):
                    cn = min(4, nb - c0)
                    ps, PS = B[bk % 4]
                    bk += 1
                    def mms(e, ps=ps, k_t=k_t, q_t=q_t, a=a, off=off, c0=c0, cn=cn):
                        for i in range(cn):
                            ins = e.matmul(ps[:, i * 128:(i + 1) * 128], lhsT=k_t[off:off + 64, a, (c0 + i) * 128:(c0 + i + 1) * 128],
                                           rhs=q_t[off:off + 64, a, :], start=True, stop=True)
                        return ins
                    kb.op("pe", mms, reads=[KB_, QB], writes=[PS])
                    kb.op("dve", lambda e, ps=ps, st_=st_, b_t=b_t, c0=c0, cn=cn: e.scalar_tensor_tensor(
                        out=st_[:, c0 * 128:(c0 + cn) * 128], in0=ps[:, 0:cn * 128], scalar=sc, in1=b_t[:, c0 * 128:(c0 + cn) * 128],
                        op0=ALU.mult, op1=ALU.add), reads=[PS, BB], writes=[STB])
                kb.op("act", lambda e, st_=st_, pt_=pt_, nb=nb: e.activation(out=pt_[:, 0:nb * 128], in_=st_[:, 0:nb * 128], func=AF.Exp),
                      reads=[STB], writes=[PTB])
            ps, PS = B[bk % 4]
            bk += 1
            def mmc(e, ps=ps, q_t=q_t, a=a, off=off):
                for cb in range(2):
                    ins = e.matmul(ps[:, cb * 128:(cb + 1) * 128], lhsT=kcT[off:off + 64, a, cb * 128:(cb + 1) * 128],
                                   rhs=q_t[off:off + 64, a, :], start=True, stop=True)
                return ins
            kb.op("pe", mmc, reads=[QB, KCT], writes=[PS])
            kb.op("act", lambda e, ps=ps, pt_=pt_, nb=nb: e.activation(out=pt_[:, nb * 128:(nb + 2) * 128], in_=ps[:, 0:256], func=AF.Exp, scale=sc),
                  reads=[PS], writes=[PTB])
            if not isctx:
                def mmv(e, acc=acc, pt_=pt_, v_t=v_t, h=h, nb=nb):
                    o = acc[:, (h % 4) * 65:(h % 4) * 65 + 65]
                    for blk in range(nb):
                        e.matmul(o, lhsT=pt_[:, blk * 128:(blk + 1) * 128], rhs=v_t[:, blk, h, :], start=(blk == 0), stop=False)
                    for cb in range(2):
                        ins = e.matmul(o, lhsT=pt_[:, (nb + cb) * 128:(nb + cb + 1) * 128], rhs=vca[:, cb, h, :], start=False, stop=(cb == 1))
                    return ins
                kb.op("pe", mmv, reads=[PTB, VB, VCA], writes=[ACC])
            else:
                def mmv(e, acc=acc, pt_=pt_, h=h):
                    o = acc[:, (h % 4) * 65:(h % 4) * 65 + 65]
                    for cb in range(2):
                        ins = e.matmul(o, lhsT=pt_[:, cb * 128:(cb + 1) * 128], rhs=vca[:, cb, h, :], start=(cb == 0), stop=(cb == 1))
                    return ins
                kb.op("pe", mmv, reads=[PTB, VCA], writes=[ACC])
            if h % 4 == 3:
                g4 = h // 4
                av = acc[:, 0:260].rearrange("p (h c) -> p h c", c=65)
                kb.op("dve", lambda e, av=av, g4=g4: e.reciprocal(out=rc[:, g4 * 4:g4 * 4 + 4], in_=av[:, :, 64]), reads=[ACC], writes=[RC])
                kb.op("dve", lambda e, av=av, g4=g4, a_t=a_t: e.tensor_tensor(
                    out=a_t[:, g4 * 256:(g4 + 1) * 256].rearrange("p (h d) -> p h d", d=64), in0=av[:, :, 0:64],
                    in1=rc[:, g4 * 4:g4 * 4 + 4].unsqueeze(2).to_broadcast([128, 4, 64]), op=ALU.mult), reads=[ACC, RC], writes=[AB])
        kb.dma("sp", ao[qt * 128:(qt + 1) * 128, 0:512], a_t[:], reads=[AB], writes=[AO])

    NBLK = (CTX + SEQ) // 128
    dk, DK = kb.sb("d_k", [128, CTX + SEQ], BF16)
    dva, DVA = kb.sb("d_va", [128, NBLK, 129], BF16)
    dq, DQ = kb.sb("d_q", [128, TALL], BF16)
    dpt = [kb.sb(f"d_pt{i}", [128, 512], BF16) for i in range(4)]
    o0_, O0 = kb.sb("d_o0", [128, 128], F32)
    o1, O1 = kb.sb("d_o1", [128, 128], F32)
    osq, OSQ = kb.sb("d_osq", [128, 128], F32)
    dsm, DSM = kb.sb("d_sm", [128, 8], F32)
    dob = [kb.sb(f"d_ob{i}", [128, 128], BF16) for i in range(2)]
    si = 0
    oi = 0
    for h in range(4):
        kb.dma("sp", dk[:, 0:CTX], fmT[1536 + h * 128:1536 + (h + 1) * 128, TQ:TALL], reads=[FMT], writes=[DK])
        for r in range(4):
            for k in range(4):
                kb.dma("sp", dk[:, CTX + r * TQ + k * 1024:CTX + r * TQ + (k + 1) * 1024],
                       dk_recv[k][0][r * 512 + h * 128:r * 512 + (h + 1) * 128, :], reads=[dk_recv[k][1]], writes=[DK])
        kb.dma("sp", dva[:, 0:2, :], tmv[TQ:TALL, 520 + h * 129:520 + (h + 1) * 129].rearrange("(b p) d -> p b d", p=128), reads=[TMV], writes=[DVA])
        for r in range(4):
            for k in range(8):
                b0 = 2 + (r * TQ + k * 512) // 128
                kb.dma("sp", dva[:, b0:b0 + 4, :], dv_recv[k][0][r * 512:(r + 1) * 512, h * 129:(h + 1) * 129].rearrange("(b p) d -> p b d", p=128),
                       reads=[dv_recv[k][1]], writes=[DVA])
        kb.dma("sp", dq[:], fmT[1024 + h * 128:1024 + (h + 1) * 128, :], reads=[FMT], writes=[DQ])
        qgroups = [(g * 256, 256, NBLK) for g in range(TQ // 256)] + [(TQ, CTX, CTX // 128)]
        for (q0, nq, nblk) in qgroups:
            nqs = nq // 128
            steps = [(bp, m) for bp in range(nblk // 2) for m in range(2)]
            nbp = nblk // 2
            LOOK = 3
            bufs = {}
            for idx in range(len(steps) + LOOK):
                if idx < len(steps):
                    bp, m = steps[idx]
                    ps, PS = B[(0, 1, 2, 7)[si % 4]]
                    pt_, PTB = dpt[si % 4]
                    si += 1
                    bufs[idx] = (pt_, PTB)
                    def mms2(e, ps=ps, m=m, bp=bp, q0=q0, nq=nq):
                        for b2 in range(2):
                            blk = 2 * bp + b2
                            ins = e.matmul(ps[:, b2 * nq:(b2 + 1) * nq], lhsT=dk[m * 64:(m + 1) * 64, blk * 128:(blk + 1) * 128],
                                           rhs=dq[m * 64:(m + 1) * 64, q0:q0 + nq], start=True, stop=True)
                        return ins
                    kb.op("pe", mms2, reads=[DK, DQ], writes=[PS])
                    kb.op("act", lambda e, ps=ps, pt_=pt_, nq=nq: e.activation(out=pt_[:, 0:2 * nq], in_=ps[:, 0:2 * nq], func=AF.Exp, scale=sc),
                          reads=[PS], writes=[PTB])
                if idx - LOOK >= 0:
                    bp, m = steps[idx - LOOK]
                    pt_, PTB = bufs.pop(idx - LOOK)
                    def mmv(e, pt_=pt_, m=m, bp=bp, nqs=nqs, nbp=nbp, nq=nq):
                        for b2 in range(2):
                            for qs in range(nqs):
                                acc = B[3 + qs * 2 + m][0]
                                ins = e.matmul(acc[:, 0:129], lhsT=pt_[:, b2 * nq + qs * 128:b2 * nq + (qs + 1) * 128], rhs=dva[:, 2 * bp + b2, :],
                                               start=(bp == 0 and b2 == 0), stop=(bp == nbp - 1 and b2 == 1))
                        return ins
                    kb.op("pe", mmv, reads=[PTB, DVA], writes=[B[3 + qs * 2 + m][1] for qs in range(nqs)])
            for qs in range(nqs):
                (a0, A0), (a1, A1) = [(B[3 + qs * 2 + m][0][:, 0:129], B[3 + qs * 2 + m][1]) for m in range(2)]
                kb.op("dve", lambda e, a0=a0: e.reciprocal(out=dsm[:, 0:1], in_=a0[:, 128:129]), reads=[A0], writes=[DSM])
                kb.op("dve", lambda e, a1=a1: e.reciprocal(out=dsm[:, 1:2], in_=a1[:, 128:129]), reads=[A1], writes=[DSM])
                kb.op("dve", lambda e: e.tensor_tensor(out=dsm[:, 1:2], in0=dsm[:, 1:2], in1=lms[:, 6:7], op=ALU.mult), reads=[DSM, LMS], writes=[DSM])
                kb.op("dve", lambda e, a0=a0: e.tensor_scalar(out=o0_[:], in0=a0[:, 0:128], scalar1=dsm[:, 0:1], scalar2=None, op0=ALU.mult),
                      reads=[A0, DSM], writes=[O0])
                kb.op("dve", lambda e, a1=a1: e.scalar_tensor_tensor(out=o1[:], in0=a1[:, 0:128], scalar=dsm[:, 1:2], in1=o0_[:],
                                                                   op0=ALU.mult, op1=ALU.add), reads=[A1, DSM, O0], writes=[O1])
                kb.op("act", lambda e: e.activation(out=osq[:], in_=o1[:], func=AF.Square, accum_out=dsm[:, 2:3]), reads=[O1], writes=[OSQ, DSM])
                kb.op("dve", lambda e: e.tensor_scalar(out=dsm[:, 3:4], in0=dsm[:, 2:3], scalar1=1.0 / 128, scalar2=EPS, op0=ALU.mult, op1=ALU.add),
                      reads=[DSM], writes=[DSM])
                kb.op("act", lambda e: e.activation(out=dsm[:, 4:5], in_=dsm[:, 3:4], func=AF.Sqrt), reads=[DSM], writes=[DSM])
                kb.op("dve", lambda e: e.reciprocal(out=dsm[:, 5:6], in_=dsm[:, 4:5]), reads=[DSM], writes=[DSM])
                ob, OB = dob[oi % 2]
                oi += 1
                kb.op("dve", lambda e, ob=ob: e.scalar_tensor_tensor(out=ob[:], in0=o1[:], scalar=dsm[:, 5:6], in1=subg[:], op0=ALU.mult, op1=ALU.mult),
                      reads=[O1, DSM, SUBG], writes=[OB])
                r0 = q0 + qs * 128
                kb.dma("sp", ao[r0:r0 + 128, 512 + h * 128:512 + (h + 1) * 128], ob[:], reads=[OB], writes=[AO])


def emit_attn_odd_f(kb, fmT, FMT, tmv, TMV, sbk_recv, SBKR, sbv_recv, SBVR, sbias, sink_in, ao, AO):
    B = kb.banks
    sc = 64 ** -0.5
    snk, SNK = kb.sb("snk", [128, 16], F32)
    kb.dma("sp", snk[:], sink_in.unsqueeze(0).to_broadcast([128, 16]), writes=[SNK])
    kb.op("act", lambda e: e.activation(out=snk[:], in_=snk[:], func=AF.Exp), reads=[SNK], writes=[SNK])
    kT, KT = kb.sb("s_kT", [64, 4, TQ], BF16)
    kb.dma("sp", kT[:], fmT[1024:1280, 0:TQ].rearrange("(n d) t -> d n t", d=64), reads=[FMT], writes=[KT])
    kcT, KCT = kb.sb("s_kcT", [64, 4, CTX], BF16)
    kb.dma("sp", kcT[:], fmT[1024:1280, TQ:TALL].rearrange("(n d) t -> d n t", d=64), reads=[FMT], writes=[KCT])
    ck, CK = kb.sb("s_ck", [64, 4, 4, 256], BF16)
    for r in range(4):
        kb.dma("sp", ck[:, r], sbk_recv[r * 256:(r + 1) * 256, :].rearrange("(n d) t -> d n t", d=64), reads=[SBKR], writes=[CK])
    va, VA = kb.sb("s_va", [128, NTL, 4, 65], BF16)
    kb.dma("sp", va[:].rearrange("p b h c -> p b (h c)"), tmv[0:TQ, 0:260].rearrange("(b p) c -> p b c", p=128), reads=[TMV], writes=[VA])
    vca, VCA = kb.sb("s_vca", [128, 2, 4, 65], BF16)
    kb.dma("sp", vca[:].rearrange("p b h c -> p b (h c)"), tmv[TQ:TALL, 0:260].rearrange("(b p) c -> p b c", p=128), reads=[TMV], writes=[VCA])
    cv, CV = kb.sb("s_cv", [128, 8, 4, 65], BF16)
    kb.dma("sp", cv[:].rearrange("p b h c -> p b (h c)"), sbv_recv.rearrange("(b p) c -> p b c", p=128), reads=[SBVR], writes=[CV])
    sb_, SBB = kb.sb("s_bias", [128, 3, 768], F32)
    kb.dma("sp", sb_[:], sbias.rearrange("s p n -> p s n"), writes=[SBB])
    qts = [kb.sb(f"s_q{i}", [64, 16, 128], BF16) for i in range(2)]
    sts = [kb.sb(f"s_st{i}", [128, 512], F32) for i in range(2)]
    pts = [kb.sb(f"s_pt{i}", [128, 8, 512], BF16) for i in range(2)]
    aot = [kb.sb(f"s_ao{i}", [128, D], BF16) for i in range(2)]
    den, DEN = kb.sb("s_den", [128, 8], F32)
    si = 0
    gi = 0
    for qt in range(NT):
        isctx = qt >= NTL
        q_t, QB = qts[qt % 2]
        kb.dma("sp", q_t[:], fmT[0:1024, qt * 128:(qt + 1) * 128].rearrange("(h d) t -> d h t", d=64), reads=[FMT], writes=[QB])
        a_t, AB = aot[qt % 2]
        if isctx:
            nbl = []
            slot = 1
        elif qt == 0:
            nbl = [("own", 0), ("own", 1)] + [("cand", r, 0) for r in range(4)]
            slot = 0
        elif qt == NTL - 1:
            nbl = [("own", NTL - 2), ("own", NTL - 1)] + [("cand", r, 1) for r in range(4)]
            slot = 2
        else:
            nbl = [("own", qt - 1), ("own", qt), ("own", qt + 1)]
            slot = 1
        nnb = len(nbl)
        for n in range(4):
            pt_, PTB = pts[gi % 2]
            acc, ACC = B[4 + gi % 2]
            gi += 1
            for bi, bl in enumerate(nbl + [("ctx", 0), ("ctx", 1)]):
                ps, PS = B[si % 3]
                st_, STB = sts[si % 2]
                si += 1
                if bl[0] == "own":
                    lhsT = kT[:, n, bl[1] * 128:(bl[1] + 1) * 128]
                    rd = [KT, QB]
                elif bl[0] == "cand":
                    lhsT = ck[:, bl[1], n, bl[2] * 128:(bl[2] + 1) * 128]
                    rd = [CK, QB]
                else:
                    lhsT = kcT[:, n, bl[1] * 128:(bl[1] + 1) * 128]
                    rd = [KCT, QB]
                kb.op("pe", lambda e, ps=ps, lhsT=lhsT, n=n, q_t=q_t: e.matmul(ps[:], lhsT=lhsT, rhs=q_t[:, n * 4:(n + 1) * 4, :], start=True, stop=True),
                      reads=rd, writes=[PS])
                if bl[0] != "ctx":
                    kb.op("dve", lambda e, ps=ps, st_=st_, bi=bi, slot=slot: e.scalar_tensor_tensor(
                        out=st_[:].rearrange("p (g q) -> p g q", g=4), in0=ps[:].rearrange("p (g q) -> p g q", g=4), scalar=sc,
                        in1=sb_[:, slot, bi * 128:(bi + 1) * 128].unsqueeze(1).to_broadcast([128, 4, 128]), op0=ALU.mult, op1=ALU.add),
                        reads=[PS, SBB], writes=[STB])
                    kb.op("act", lambda e, st_=st_, pt_=pt_, bi=bi: e.activation(out=pt_[:, bi, :], in_=st_[:], func=AF.Exp), reads=[STB], writes=[PTB])
                else:
                    kb.op("act", lambda e, ps=ps, pt_=pt_, bi=bi: e.activation(out=pt_[:, bi, :], in_=ps[:], func=AF.Exp, scale=sc),
                          reads=[PS], writes=[PTB])
            allb = nbl + [("ctx", 0), ("ctx", 1)]
            def mmv(e, acc=acc, pt_=pt_, n=n, allb=allb):
                for g in range(4):
                    o = acc[:, g * 65:(g + 1) * 65]
                    for i, bl in enumerate(allb):
                        if bl[0] == "own":
                            rhs = va[:, bl[1], n, :]
                        elif bl[0] == "cand":
                            rhs = cv[:, bl[1] * 2 + bl[2], n, :]
                        else:
                            rhs = vca[:, bl[1], n, :]
                        ins = e.matmul(o, lhsT=pt_[:, i, g * 128:(g + 1) * 128], rhs=rhs, start=(i == 0), stop=(i == len(allb) - 1))
                return ins
            kb.op("pe", mmv, reads=[PTB, VA, VCA, CV], writes=[ACC])
            av = acc[:, 0:260].rearrange("p (g c) -> p g c", c=65)
            kb.op("dve", lambda e, av=av, n=n: e.tensor_tensor(out=den[:, 0:4], in0=av[:, :, 64], in1=snk[:, n * 4:(n + 1) * 4], op=ALU.add),
                  reads=[ACC, SNK], writes=[DEN])
            kb.op("dve", lambda e: e.reciprocal(out=den[:, 4:8], in_=den[:, 0:4]), reads=[DEN], writes=[DEN])
            kb.op("dve", lambda e, av=av, n=n, a_t=a_t: e.tensor_tensor(
                out=a_t[:, n * 256:(n + 1) * 256].rearrange("p (g d) -> p g d", d=64), in0=av[:, :, 0:64],
                in1=den[:, 4:8].unsqueeze(2).to_broadcast([128, 4, 64]), op=ALU.mult), reads=[ACC, DEN], writes=[AB])
        kb.dma("sp", ao[qt * 128:(qt + 1) * 128, :], a_t[:], reads=[AB], writes=[AO])


def build_fused(nlayers=4):
    kb = KB()
    kb.psum_banks()
    nc = kb.nc

    def scratch(name, shape, dt=BF16):
        return nc.dram_tensor(name, list(shape), dt).ap(), Buf(name)

    x_ext = kb.din("x", [TALL, D])
    csT = kb.din("csT", [128, 16])
    cosT = kb.din("cosT", [128, TALL])
    sinT = kb.din("sinT", [128, TALL])
    fg = kb.din("fg", [D])
    xn_out, XN = kb.dout("xn_out", [TALL, D])
    xbufs = [scratch(f"xs{i}", [TALL, D], F32) for i in range(2)]
    ao, AO = scratch("ao_scr", [TALL, D])
    fmT, FMT = scratch("fmT", [2048, TALL])
    tmv, TMV = scratch("tmv", [TALL, 1036])
    dk_send = [scratch(f"dk_send{k}", [512, 1024]) for k in range(4)]
    dk_recv = [scratch(f"dk_recv{k}", [2048, 1024]) for k in range(4)]
    dv_send = [scratch(f"dv_send{k}", [512, 516]) for k in range(8)]
    dv_recv = [scratch(f"dv_recv{k}", [2048, 516]) for k in range(8)]
    nbk_send, NBKS = scratch("nbk_send", [512, 512])
    nbk_recv, NBKR = scratch("nbk_recv", [2048, 512])
    nbv_send, NBVS = scratch("nbv_send", [512, 520])
    nbv_recv, NBVR = scratch("nbv_recv", [2048, 520])
    sbk_send, SBKS = scratch("sbk_send", [256, 256])
    sbk_recv, SBKR = scratch("sbk_recv", [1024, 256])
    sbv_send, SBVS = scratch("sbv_send", [256, 260])
    sbv_recv, SBVR = scratch("sbv_recv", [1024, 260])
    x_src, XS = x_ext, Buf("x_ext")
    lw = []
    for i in range(nlayers):
        d = {"w_out": kb.din(f"w_out{i}", [D, D]), "wq": kb.din(f"wq{i}", [D, 2048]),
             "uT": kb.din(f"uT{i}", [D, 16384]), "v": kb.din(f"v{i}", [16384, D])}
        d["conv"] = {"wout": scratch(f"woutb{i}", [2, 128, 4096]), "wq": scratch(f"wqb{i}", [4, 128, 4096]),
                     "uT": scratch(f"uTb{i}", [32, 128, 4096]), "v": scratch(f"vb{i}", [32, 128, 4096])}
        lw.append(d)

    def convert(i):
        d = lw[i]
        c = d["conv"]
        for hf in range(2):
            kb.dma("pool", c["wout"][0][hf].rearrange("p (k n) -> p k n", k=8),
                   d["w_out"][:, hf * 512:(hf + 1) * 512].rearrange("(k p) n -> p k n", p=128), writes=[c["wout"][1]])
        for g in range(4):
            kb.dma("pool", c["wq"][0][g].rearrange("p (k n) -> p k n", k=8),
                   d["wq"][:, g * 512:(g + 1) * 512].rearrange("(k p) n -> p k n", p=128), writes=[c["wq"][1]])
        for cg in range(32):
            kb.dma("pool", c["uT"][0][cg].rearrange("p (k n) -> p k n", k=8),
                   d["uT"][:, cg * 512:(cg + 1) * 512].rearrange("(k p) n -> p k n", p=128), writes=[c["uT"][1]])
            kb.dma("pool", c["v"][0][cg].rearrange("p (c d) -> p c d", c=4),
                   d["v"][cg * 512:(cg + 1) * 512, :].rearrange("(c p) d -> p c d", p=128), writes=[c["v"][1]])

    for i in range(nlayers):
        even = (i % 2 == 0)
        j = i // 2
        NIN = 3072 if even else 1536
        NROPE = 1024 if even else 1280
        w_mod = kb.din(f"w_mod{i}", [D, 6 * D])
        b_mod = kb.din(f"b_mod{i}", [6 * D])
        n1g = kb.din(f"n1g{i}", [D])
        n2g = kb.din(f"n2g{i}", [D])
        w_in = kb.din(f"w_in{i}", [D, NIN])
        w_perm = kb.din(f"w_perm{i}", [D, NROPE])
        w_out, wq, uT, v = lw[i]["w_out"], lw[i]["wq"], lw[i]["uT"], lw[i]["v"]
        keysT = kb.din(f"keysT{i}", [128, 16, 128])
        modrow, MRO = scratch(f"modrow{i}", [2, 6, D], F32)
        with kb.scope(f"L{i}a_"):
            if i == 0 and USE_CONV:
                convert(0)
            emit_pre_f(kb, even, x_src, XS, csT, w_mod, b_mod, n1g, w_in, w_perm, cosT, sinT, fmT, FMT, tmv, TMV, modrow, MRO)
        if even:
            nbias = kb.din(f"nbias{j}", [5, 8, 128, NA_NBMAX * 128])
            lam = kb.din(f"lam{j}", [4, 64])
            subg = kb.din(f"subg{j}", [128])
            lami = kb.din(f"lam_init{j}", [128, 2])
            for k in range(4):
                kb.dma("sp", dk_send[k][0], fmT[1536:2048, k * 1024:(k + 1) * 1024], reads=[FMT], writes=[dk_send[k][1]])
            for k in range(8):
                kb.dma("sp", dv_send[k][0], tmv[k * 512:(k + 1) * 512, 520:1036], reads=[TMV], writes=[dv_send[k][1]])
            kb.dma("sp", nbk_send[:, 0:256], fmT[512:1024, TQ - 256:TQ], reads=[FMT], writes=[NBKS])
            kb.dma("sp", nbk_send[:, 256:512], fmT[512:1024, 0:256], reads=[FMT], writes=[NBKS])
            kb.dma("sp", nbv_send[0:256, :], tmv[TQ - 256:TQ, 0:520], reads=[TMV], writes=[NBVS])
            kb.dma("sp", nbv_send[256:512, :], tmv[0:256, 0:520], reads=[TMV], writes=[NBVS])
            for k in range(4):
                kb.cc("AllGather", RG, dk_send[k][0], dk_recv[k][0], reads=[dk_send[k][1]], writes=[dk_recv[k][1]])
            for k in range(8):
                kb.cc("AllGather", RG, dv_send[k][0], dv_recv[k][0], reads=[dv_send[k][1]], writes=[dv_recv[k][1]])
            kb.cc("AllGather", RG, nbk_send, nbk_recv, reads=[NBKS], writes=[NBKR])
            kb.cc("AllGather", RG, nbv_send, nbv_recv, reads=[NBVS], writes=[NBVR])
            with kb.scope(f"L{i}b_"):
                emit_attn_even_f(kb, fmT, FMT, tmv, TMV, dk_recv, None, dv_recv, None, nbk_recv, NBKR, nbv_recv, NBVR,
                                 nbias, lam, subg, lami, ao, AO)
        else:
            sbias = kb.din(f"sbias{j}", [3, 128, 768])
            sink = kb.din(f"sink{j}", [16])
            kb.dma("sp", sbk_send[:, 0:128], fmT[1024:1280, TQ - 128:TQ], reads=[FMT], writes=[SBKS])
            kb.dma("sp", sbk_send[:, 128:256], fmT[1024:1280, 0:128], reads=[FMT], writes=[SBKS])
            kb.dma("sp", sbv_send[0:128, :], tmv[TQ - 128:TQ, 0:260], reads=[TMV], writes=[SBVS])
            kb.dma("sp", sbv_send[128:256, :], tmv[0:128, 0:260], reads=[TMV], writes=[SBVS])
            kb.cc("AllGather", RG, sbk_send, sbk_recv, reads=[SBKS], writes=[SBKR])
            kb.cc("AllGather", RG, sbv_send, sbv_recv, reads=[SBVS], writes=[SBVR])
            with kb.scope(f"L{i}b_"):
                emit_attn_odd_f(kb, fmT, FMT, tmv, TMV, sbk_recv, SBKR, sbv_recv, SBVR, sbias, sink, ao, AO)
        x_dst, XD = xbufs[i % 2]
        last = (i == nlayers - 1)
        with kb.scope(f"L{i}c_"):
            if not last and USE_CONV:
                convert(i + 1)
            emit_post(kb, NT, x_src, ao, AO, modrow, w_out, n2g, wq, keysT, uT, v, x_dst, XD,
                      final_g=fg if last else None, xn_out=xn_out if last else None, XN=XN if last else None,
                      tile_sets=[0] * NTL + [1] * NTC, conv=lw[i]["conv"] if USE_CONV else None)
        x_src, XS = x_dst, XD
    return kb.finish()


def _na_bias_f(rpb, qr):
    H = rpb.shape[0]
    R0 = qr * 64
    out = np.full((5, H, 128, NA_NBMAX, 128), NEG, np.float32)
    kk = np.arange(128)
    qq = np.arange(128)
    for slot, qt in enumerate((0, 1, 10, NTL - 2, NTL - 1)):
        r0 = R0 + 2 * qt
        if slot == 2:
            r0 = 100
        o0, onb, cands = _na_blocklist(qt)
        blocks = [((r0 - 4 + 2 * b) if slot == 2 else (R0 + o0 // 64 + 2 * b), None) for b in range(onb)]
        for r in range(4):
            for (which, cb) in cands:
                if which == "tail":
                    blocks.append((R0 - 4 + 2 * cb, r == qr - 1))
                else:
                    blocks.append((R0 + 64 + 2 * cb, r == qr + 1))
        qrow = r0 + qq // 64
        qc = qq % 64
        rs = np.clip(qrow - 4, 0, 256 - 8)
        cs = np.clip(qc - 8, 0, 64 - 16)
        for bi, (krow0, ok) in enumerate(blocks):
            if ok is False:
                continue
            kr = krow0 + kk // 64
            kc = kk % 64
            valid = ((kr[:, None] >= rs[None, :]) & (kr[:, None] < rs[None, :] + 8) &
                     (kc[:, None] >= cs[None, :]) & (kc[:, None] < cs[None, :] + 16) &
                     (kr[:, None] >= 0) & (kr[:, None] < 256))
            dr = np.clip(kr[:, None] - qrow[None, :] + 7, 0, 14)
            dc = np.clip(kc[:, None] - qc[None, :] + 15, 0, 30)
            vals = rpb[:, dr, dc]
            out[slot, :, :, bi, :] = np.where(valid[None], vals, NEG)
    return out.reshape(5, H, 128, NA_NBMAX * 128)


def _swa_bias_f(qr):
    out = np.full((3, 128, 6, 128), NEG, np.float32)
    k = np.arange(128)[:, None]
    q = np.arange(128)[None, :]
    band = lambda off: np.where(np.abs(off * 128 + k - q) <= 128, 0.0, NEG).astype(np.float32)
    out[0, :, 0] = band(0)
    out[0, :, 1] = band(1)
    for r in range(4):
        if r == qr - 1:
            out[0, :, 2 + r] = band(-1)
    out[1, :, 0] = band(-1)
    out[1, :, 1] = band(0)
    out[1, :, 2] = band(1)
    out[2, :, 0] = band(-1)
    out[2, :, 1] = band(0)
    for r in range(4):
        if r == qr + 1:
            out[2, :, 2 + r] = band(1)
    return out.reshape(3, 128, 768)


_FUSED = {}


def kernel(x, c, ctx, c_ctx, w_mod, b_mod, norm1_g, norm2_g, w_in_even, w_out_even, na_rpb, diff_lambda,
           diff_subln_g, w_in_odd, w_out_odd, swa_sink, peer_wq, peer_keys, peer_u, peer_v, final_g, _nlayers=4):
    f32 = np.float32
    x = np.asarray(x, f32)
    ctx = np.asarray(ctx, f32)
    if _nlayers not in _FUSED:
        _FUSED[_nlayers] = build_fused(_nlayers)
    nc = _FUSED[_nlayers]
    shared = {"c_ident": np.eye(128, dtype=f32), "fg": np.asarray(final_g, f32)}
    for i in range(_nlayers):
        even = (i % 2 == 0)
        j = i // 2
        w_in = np.asarray(w_in_even[j] if even else w_in_odd[j], f32)
        rc = w_in[:, 1536:2560] if even else w_in[:, 0:1280]
        shared[f"w_mod{i}"] = np.asarray(w_mod[i], f32)
        shared[f"b_mod{i}"] = np.asarray(b_mod[i], f32)
        shared[f"n1g{i}"] = np.asarray(norm1_g[i], f32)
        shared[f"n2g{i}"] = np.asarray(norm2_g[i], f32)
        shared[f"w_in{i}"] = w_in
        shared[f"w_perm{i}"] = np.ascontiguousarray(rc.reshape(D, -1, 64)[:, :, _PERM64].reshape(D, -1))
        shared[f"w_out{i}"] = np.asarray(w_out_even[j] if even else w_out_odd[j], f32)
        shared[f"wq{i}"] = np.asarray(peer_wq[i], f32)
        shared[f"keysT{i}"] = np.ascontiguousarray(np.asarray(peer_keys[i], f32).reshape(16, 128, 128).transpose(2, 0, 1))
        shared[f"uT{i}"] = np.ascontiguousarray(np.asarray(peer_u[i], f32).T)
        shared[f"v{i}"] = np.asarray(peer_v[i], f32)
        if even:
            shared[f"lam{j}"] = np.asarray(diff_lambda[j], f32)
            shared[f"subg{j}"] = np.asarray(diff_subln_g[j], f32)
            li = 0.8 - 0.6 * math.exp(-0.3 * i)
            shared[f"lam_init{j}"] = np.tile(np.array([[li, 1.0 - li]], f32), (128, 1))
        else:
            shared[f"sink{j}"] = np.asarray(swa_sink[j], f32)
    ims = []
    for core in range(NCORES):
        b, qr = divmod(core, 4)
        im = dict(shared)
        im["x"] = np.concatenate([x[b, qr * TQ:(qr + 1) * TQ], ctx[b]], axis=0)
        cs = np.stack([np.asarray(c, f32)[b], np.asarray(c_ctx, f32)], 0)
        im["csT"] = np.ascontiguousarray(cs.reshape(2, 8, 128).transpose(2, 0, 1).reshape(128, 16))
        im["cosT"], im["sinT"] = _rope_tables_T(qr * TQ)
        for i in range(_nlayers):
            j = i // 2
            if i % 2 == 0:
                im[f"nbias{j}"] = _na_bias_f(np.asarray(na_rpb[j], f32), qr)
            else:
                im[f"sbias{j}"] = _swa_bias_f(qr)
        ims.append(im)
    res = run_bass_kernel_spmd(nc, ims, core_ids=list(range(NCORES))).results
    out = np.zeros((2, SEQ, D), f32)
    for core in range(NCORES):
        b, qr = divmod(core, 4)
        out[b, qr * TQ:(qr + 1) * TQ] = np.asarray(res[core]["xn_out"])[:TQ]
    return out
```

```python
from contextlib import ExitStack
import math
import numpy as np
import ml_dtypes
import concourse.bass as bass
import concourse.mybir as mybir
from concourse.bass_utils import run_bass_kernel_spmd

F32 = mybir.dt.float32
BF16 = mybir.dt.bfloat16
AF = mybir.ActivationFunctionType
ALU = mybir.AluOpType
AX = mybir.AxisListType
NPBF = ml_dtypes.bfloat16

ENGS = ("pe", "act", "dve", "pool", "sp")

D = 1024
NCORES = 8
SEQ = 16384
CTX = 256
TQ = SEQ // 4
NTL = TQ // 128
NTC = CTX // 128
NT = NTL + NTC
TALL = TQ + CTX
EPS = 1e-6
NEG = -1.0e30
PEER_MARGIN = 1e-4
import os
USE_CONV = os.environ.get('K_CONV', '1') == '1'


class Buf:
    __slots__ = ("name", "writers", "readers", "excl")

    def __init__(self, name="", excl=False):
        self.name = name
        self.writers = {}
        self.readers = {}
        self.excl = excl


class Op:
    __slots__ = ("id", "eng", "fn", "dma", "cc", "deps", "seq", "waits", "signal", "sigidx", "slot", "target")


class Prog:
    RING = {"sp": 14, "act": 4, "pool": 8}

    def __init__(self, nc):
        self.nc = nc
        self.ops = []
        self.seq = {e: 0 for e in ENGS}
        self.dmas = {q: [] for q in self.RING}
        self.ccs = []

    def add(self, eng, fn, reads=(), writes=(), dma=False, cc=False):
        dma = dma or cc
        op = Op()
        op.cc = cc
        op.id = len(self.ops)
        op.eng = eng
        op.fn = fn
        op.dma = dma
        op.signal = False
        op.sigidx = None
        op.slot = None
        op.target = None
        deps = set()
        if cc:
            op.slot = ("cc", len(self.ccs))
            op.target = 1
            self.ccs.append(op.id)
        elif dma:
            ring = self.RING[eng]
            lst = self.dmas[eng]
            n = len(lst)
            op.slot = (eng, n % ring)
            op.target = 16 * (n // ring + 1)
            if n >= ring:
                deps.add(lst[n - ring])
            lst.append(op.id)
        pkey = ("dma", op.slot) if dma else eng
        rd = [b for b in reads if not b.excl]
        wr = list(writes) + [b for b in reads if b.excl]
        for b in rd:
            for k, oid in b.writers.items():
                if k == eng and eng == "pe":
                    continue
                deps.add(oid)
        for b in wr:
            isread = b not in writes
            for k, oid in b.writers.items():
                if k == eng and not dma:
                    if isread and eng != "pe":
                        deps.add(oid)
                    continue
                if dma and isinstance(k, tuple):
                    continue
                deps.add(oid)
            for k, oid in b.readers.items():
                if k == eng and not dma:
                    continue
                deps.add(oid)
        for b in wr:
            if dma:
                b.writers = {k: v for k, v in b.writers.items() if isinstance(k, tuple)}
                b.writers[pkey] = op.id
            else:
                b.writers = {pkey: op.id}
            b.readers = {}
        for b in rd:
            if b in wr:
                continue
            b.readers[pkey] = op.id
        op.seq = self.seq[eng]
        self.seq[eng] += 1
        op.deps = deps
        self.ops.append(op)
        return op.id

    def barrier(self):
        start = getattr(self, "bar_start", 0)
        last = {}
        for op in self.ops[start:]:
            if op.dma:
                last[("dma", op.id)] = op.id
            else:
                last[op.eng] = op.id
        deps = set(last.values())
        for e in ENGS:
            oid = self.add(e, lambda eng: None)
            self.ops[oid].deps |= {d for d in deps if self.ops[d].dma or self.ops[d].eng != e}
        self.bar_start = len(self.ops)

    def emit(self):
        nc = self.nc
        ops = self.ops
        seen = {e: {} for e in ENGS}
        seen_dma = {e: {} for e in ENGS}
        for op in ops:
            best = {}
            waits = []
            for d in op.deps:
                p = ops[d]
                if p.dma:
                    if seen_dma[op.eng].get(p.slot, 0) >= p.target:
                        continue
                    seen_dma[op.eng][p.slot] = p.target
                    waits.append(d)
                else:
                    if p.eng not in best or ops[best[p.eng]].seq < p.seq:
                        best[p.eng] = d
            for e, d in best.items():
                p = ops[d]
                if seen[op.eng].get(e, -1) >= p.seq:
                    continue
                seen[op.eng][e] = p.seq
                p.signal = True
                waits.append(d)
            op.waits = waits
        cnt = {e: 0 for e in ENGS}
        for op in ops:
            if op.signal and not op.dma:
                cnt[op.eng] += 1
                op.sigidx = cnt[op.eng]
        with ExitStack() as es:
            sems = {e: es.enter_context(nc.semaphore("s_" + e)) for e in ENGS}
            rings = {}
            for q, n in self.RING.items():
                for i in range(n):
                    rings[(q, i)] = es.enter_context(nc.semaphore(f"r_{q}{i}"))
            for i in range(len(self.ccs)):
                rings[("cc", i)] = es.enter_context(nc.semaphore(f"cc{i}"))
            block = es.enter_context(nc.Block())
            per_eng = {e: [o for o in ops if o.eng == e] for e in ENGS}

            def run(engname):
                def body(eng):
                    for op in per_eng[engname]:
                        for d in op.waits:
                            p = ops[d]
                            if p.dma:
                                eng.wait_ge(rings[p.slot], p.target)
                            else:
                                eng.wait_ge(sems[p.eng], p.sigidx)
                        ins = op.fn(eng)
                        if ins is None:
                            continue
                        if op.cc:
                            ins.then_inc(rings[op.slot], 1)
                        elif op.dma:
                            ins.then_inc(rings[op.slot], 16)
                        elif op.signal:
                            ins.then_inc(sems[op.eng], 1)
                return body

            block.tensor(run("pe"))
            block.scalar(run("act"))
            block.vector(run("dve"))
            block.gpsimd(run("pool"))
            block.sync(run("sp"))


class KB:
    def __init__(self):
        self.nc = bass.Bass("TRN2", target_bir_lowering=False)
        self.P = Prog(self.nc)
        self.es = ExitStack()
        self.banks = []
        self.outs = []

    def din(self, name, shape, dt=F32):
        return self.nc.dram_tensor(name, list(shape), dt, kind="ExternalInput").ap()

    def dout(self, name, shape, dt=F32):
        b = Buf(name)
        self.outs.append(b)
        return self.nc.dram_tensor(name, list(shape), dt, kind="ExternalOutput").ap(), b

    def dscratch(self, name, shape, dt):
        return self.nc.dram_tensor(name, list(shape), dt, kind="Internal").ap(), Buf(name)

    pfx = ""

    def sb(self, name, shape, dt=F32):
        return self.es.enter_context(self.nc.sbuf_tensor(self.pfx + name, list(shape), dt)), Buf(name)

    def scope(self, pfx):
        kb = self

        class _S:
            def __enter__(s_):
                s_.saved = (kb.es, kb.pfx)
                kb.es = ExitStack()
                kb.pfx = pfx
                kb._ident = None

            def __exit__(s_, *a):
                kb.es.close()
                kb.es, kb.pfx = s_.saved
                kb.P.barrier()
                return False
        return _S()

    _ident = None
    _ident_d = None

    def ident(self):
        if self._ident is None:
            if self._ident_d is None:
                self._ident_d = self.din("c_ident", [128, 128], F32)
            f, IDF = self.sb("ident_f", [128, 128], F32)
            b, ID = self.sb("ident", [128, 128], BF16)
            self.dma("sp", f[:], self._ident_d, writes=[IDF])
            self.op("act", lambda e: e.copy(out=b[:], in_=f[:]), reads=[IDF], writes=[ID])
            self._ident = (f, IDF, b, ID)
        return self._ident

    def cc(self, kind, rg, src, dst, reads=(), writes=()):
        return self.P.add("pool", lambda e: e.collective_compute(kind, ALU.bypass, replica_groups=rg, ins=[src.opt()], outs=[dst.opt()]),
                          reads, writes, cc=True)

    def psum_banks(self):
        for i in range(8):
            t = self.es.enter_context(self.nc.psum_tensor(f"bank{i}", [128, 512], F32))
            self.banks.append((t, Buf(f"bank{i}", excl=True)))

    def op(self, eng, fn, reads=(), writes=()):
        return self.P.add(eng, fn, reads, writes)

    def dma(self, q, out, in_, reads=(), writes=()):
        return self.P.add(q, lambda e: e.dma_start(out=out, in_=in_), reads, writes, dma=True)

    def finish(self):
        self.P.add("sp", lambda e: None, reads=self.outs)
        self.P.emit()
        self.es.close()
        return self.nc


def rstd_ops(kb, sm, SM):
    kb.op("dve", lambda e: e.tensor_scalar(out=sm[:, 2:3], in0=sm[:, 0:1], scalar1=1.0 / D, scalar2=EPS,
                                           op0=ALU.mult, op1=ALU.add), reads=[SM], writes=[SM])
    kb.op("act", lambda e: e.activation(out=sm[:, 3:4], in_=sm[:, 2:3], func=AF.Sqrt), reads=[SM], writes=[SM])
    kb.op("dve", lambda e: e.reciprocal(out=sm[:, 1:2], in_=sm[:, 3:4]), reads=[SM], writes=[SM])


def emit_post(kb, ntiles, x_in, ao_dram, AO, modrow, w_out, norm2g, peer_wq, peer_keysT, peer_uT, peer_v,
              x_out, XOUT, final_g=None, xn_out=None, XN=None, tile_sets=None, conv=None):
    nc, P = kb.nc, kb.P
    B = kb.banks
    if tile_sets is None:
        tile_sets = [0] * ntiles
    ident_f, IDF, ident, ID = kb.ident()

    mod, MOD = kb.sb("modrows", [128, 4, D], F32)
    n2g, N2G = kb.sb("n2g_sb", [128, D], F32)
    kb.dma("sp", n2g[:], norm2g.unsqueeze(0).to_broadcast([128, D]), writes=[N2G])
    fg = None
    if final_g is not None:
        fg, FG = kb.sb("fg_sb", [128, D], F32)
        kb.dma("sp", fg[:], final_g.unsqueeze(0).to_broadcast([128, D]), writes=[FG])

    def load_mod(s):
        for j, src in enumerate((2, 4, 3, 5)):
            kb.dma("sp", mod[:, j, :], modrow[s, src:src + 1, :].to_broadcast([128, D]), writes=[MOD])
        kb.op("dve", lambda e: e.scalar_tensor_tensor(out=mod[:, 1, :], in0=mod[:, 1, :], scalar=1.0, in1=n2g[:],
                                                      op0=ALU.add, op1=ALU.mult), reads=[MOD, N2G], writes=[MOD])

    keys_b, KBF = kb.sb("keys_b", [128, 16, 128], BF16)

    NWR = 3
    wr = [kb.sb(f"wr{i}", [128, 8, 512], BF16) for i in range(NWR)]
    NVR = 2
    vr = [kb.sb(f"vr{i}", [128, 4, D], BF16) for i in range(NVR)]
    wr_i = [0]
    vr_i = [0]

    def load_w(src_cols, pre=None):
        t, b = wr[wr_i[0] % NWR]
        wr_i[0] += 1
        if pre is not None:
            kb.dma("sp", t[:].rearrange("p k n -> p (k n)"), pre[0], reads=[pre[1]], writes=[b])
        else:
            kb.dma("pool", t[:], src_cols.rearrange("(k p) n -> p k n", p=128), writes=[b])
        return t, b

    def load_v(src_rows, pre=None):
        t, b = vr[vr_i[0] % NVR]
        vr_i[0] += 1
        if pre is not None:
            kb.dma("sp", t[:].rearrange("p c d -> p (c d)"), pre[0], reads=[pre[1]], writes=[b])
        else:
            kb.dma("pool", t[:], src_rows.rearrange("(c p) d -> p c d", p=128), writes=[b])
        return t, b

    def pre_of(name, idx):
        return None if conv is None else (conv[name][0][idx], conv[name][1])

    xt, XT = kb.sb("xt", [128, 2, D], F32)
    tmp, TMP = kb.sb("tmp", [128, D], F32)
    ao, AOB = kb.sb("ao_sb", [128, D], BF16)
    aoT, AOT = kb.sb("aoT", [128, 8, 128], BF16)
    h2, H2 = kb.sb("h2", [128, D], BF16)
    h2T, H2T = kb.sb("h2T", [128, 8, 256], BF16)
    qT, QT = kb.sb("qT", [128, 16, 256], BF16)
    s_sb, SSB = kb.sb("s_sb", [128, 16, 128], F32)
    work, WORK = kb.sb("work", [128, 2048], F32)
    kb.dma("sp", work[:], peer_keysT.rearrange("p a n -> p (a n)"), writes=[WORK])
    kb.op("act", lambda e: e.copy(out=keys_b[:].rearrange("p a n -> p (a n)"), in_=work[:]), reads=[WORK], writes=[KBF])
    cand, CAND = kb.sb("cand", [128, 8, 16, 16], F32)
    top, TOP = kb.sb("top", [128, 16, 16], F32)
    ctop, CTOP = kb.sb("ctop", [128, 8, 16], F32)
    sm, SM = kb.sb("sm", [128, 64], F32)
    e16, E16 = kb.sb("e16", [128, 8, 16], F32)
    av, AV = kb.sb("a_vec", [128, 2, 8, 128], F32)
    bv, BV = kb.sb("b_vec", [128, 2, 8, 128], F32)
    diag, DG = kb.sb("diag", [128, 2, 8, 128], BF16)
    pps = [kb.sb(f"pp{i}", [128, 2, 8, 2, 128], F32) for i in range(2)]
    wps = [kb.sb(f"wp{i}", [128, 2, 8, 2, 128], BF16) for i in range(2)]
    gl = [kb.sb(f"gl{i}", [128, 4, 256], BF16) for i in range(2)]
    at = [kb.sb(f"at{i}", [128, 4, 256], BF16) for i in range(2)]
    xn, XNB = work, WORK

    def bf(bank):
        return bank.bitcast(BF16)

    cur_set = [None]
    npairs = (ntiles + 1) // 2
    for pr in range(npairs):
        tiles = [t for t in (2 * pr, 2 * pr + 1) if t < ntiles]
        nj = len(tiles)
        NTOK = 128 * nj
        if tile_sets[tiles[0]] != cur_set[0]:
            cur_set[0] = tile_sets[tiles[0]]
            load_mod(cur_set[0])
        wo = [load_w(w_out[:, hf * 512:(hf + 1) * 512], pre_of('wout', hf)) for hf in range(2)]
        for j, tt in enumerate(tiles):
            rows = slice(tt * 128, (tt + 1) * 128)
            kb.dma("sp", xt[:, j, :], x_in[rows, :], writes=[XT])
            kb.dma("sp", ao[:], ao_dram[rows, :], reads=[AO], writes=[AOB])
            tb, TB = B[0]
            def tr1(e, tb=tb):
                for k in range(8):
                    ins = e.transpose(bf(tb)[:, k * 128:(k + 1) * 128], ao[:, k * 128:(k + 1) * 128], ident[:])
                return ins
            kb.op("pe", tr1, reads=[AOB, ID], writes=[TB])
            kb.op("act", lambda e, tb=tb: e.copy(out=aoT[:].rearrange("p k t -> p (k t)"), in_=bf(tb)[:, 0:1024]),
                  reads=[TB], writes=[AOT])
            for hf in range(2):
                yb, YB = B[1 + hf]
                wt, WB = wo[hf]
                def mmy(e, yb=yb, wt=wt):
                    for k in range(8):
                        ins = e.matmul(yb[:], lhsT=aoT[:, k, :], rhs=wt[:, k, :], start=(k == 0), stop=(k == 7))
                    return ins
                kb.op("pe", mmy, reads=[AOT, WB], writes=[YB])
                kb.op("dve", lambda e, yb=yb, hf=hf: e.tensor_tensor(out=tmp[:, hf * 512:(hf + 1) * 512], in0=yb[:],
                                                                   in1=mod[:, 0, hf * 512:(hf + 1) * 512], op=ALU.mult),
                      reads=[YB, MOD], writes=[TMP])
            kb.op("pool", lambda e, j=j: e.tensor_tensor(out=xt[:, j, :], in0=xt[:, j, :], in1=tmp[:], op=ALU.add),
                  reads=[XT, TMP], writes=[XT])
            kb.op("act", lambda e, j=j: e.activation(out=tmp[:], in_=xt[:, j, :], func=AF.Square, accum_out=sm[:, 0:1]),
                  reads=[XT], writes=[TMP, SM])
            rstd_ops(kb, sm, SM)
            kb.op("dve", lambda e, j=j: e.scalar_tensor_tensor(out=tmp[:], in0=xt[:, j, :], scalar=sm[:, 1:2], in1=mod[:, 1, :],
                                                               op0=ALU.mult, op1=ALU.mult), reads=[XT, SM, MOD], writes=[TMP])
            kb.op("pool", lambda e: e.tensor_tensor(out=h2[:], in0=tmp[:], in1=mod[:, 2, :], op=ALU.add),
                  reads=[TMP, MOD], writes=[H2])
            tb, TB = B[3]
            def tr2(e, tb=tb):
                for k in range(8):
                    ins = e.transpose(bf(tb)[:, k * 128:(k + 1) * 128], h2[:, k * 128:(k + 1) * 128], ident[:])
                return ins
            kb.op("pe", tr2, reads=[H2, ID], writes=[TB])
            kb.op("act", lambda e, tb=tb, j=j: e.copy(out=h2T[:, :, j * 128:(j + 1) * 128],
                                                      in_=bf(tb)[:, 0:1024].rearrange("p (k t) -> p k t", k=8)),
                  reads=[TB], writes=[H2T])
        for g in range(4):
            wt, WB = load_w(peer_wq[:, g * 512:(g + 1) * 512], pre_of('wq', g))
            for hh in range(2):
                qb, QB = B[4 + (2 * g + hh) % 2]
                def mmq(e, qb=qb, wt=wt, hh=hh, NTOK=NTOK):
                    for i in range(2):
                        for k in range(8):
                            c0 = (hh * 2 + i) * 128
                            ins = e.matmul(qb[:, i * 256:i * 256 + NTOK], lhsT=wt[:, k, c0:c0 + 128], rhs=h2T[:, k, 0:NTOK],
                                           start=(k == 0), stop=(k == 7))
                    return ins
                kb.op("pe", mmq, reads=[WB, H2T], writes=[QB])
                hp0 = g * 4 + hh * 2
                kb.op("act", lambda e, qb=qb, hp0=hp0, NTOK=NTOK: e.copy(out=qT[:, hp0:hp0 + 2, 0:NTOK],
                                                               in_=qb[:].rearrange("p (i t) -> p i t", i=2)[:, :, 0:NTOK]),
                      reads=[QB], writes=[QT])
        for j, tt in enumerate(tiles):
            for g in range(4):
                sbk, SB_ = B[6 + g % 2]
                def mms(e, sbk=sbk, g=g, j=j):
                    for i in range(4):
                        hp = g * 4 + i
                        ins = e.matmul(sbk[:, i * 128:(i + 1) * 128], lhsT=qT[:, hp, j * 128:(j + 1) * 128], rhs=keys_b[:, hp, :],
                                       start=True, stop=True)
                    return ins
                kb.op("pe", mms, reads=[QT, KBF], writes=[SB_])
                kb.op("act", lambda e, sbk=sbk, g=g: e.copy(out=s_sb[:, g * 4:(g + 1) * 4, :].rearrange("p a n -> p (a n)"), in_=sbk[:]),
                      reads=[SB_], writes=[SSB])
            for hp in range(16):
                kb.op("dve", lambda e, hp=hp: e.max(out=top[:, hp, 0:8], in_=s_sb[:, hp, :]), reads=[SSB], writes=[TOP])
            for hp in range(16):
                kb.op("dve", lambda e, hp=hp: e.match_replace(out=work[:, hp * 128:(hp + 1) * 128], in_to_replace=top[:, hp, 0:8],
                                                              in_values=s_sb[:, hp, :], imm_value=NEG), reads=[SSB, TOP], writes=[WORK])
            for hp in range(16):
                kb.op("dve", lambda e, hp=hp: e.max(out=top[:, hp, 8:16], in_=work[:, hp * 128:(hp + 1) * 128]), reads=[WORK], writes=[TOP])
            def fcand(e):
                t4 = top[:].rearrange("p (h q) k -> p h q k", q=2)
                i0 = t4[:, :, 0, :].unsqueeze(3).to_broadcast([128, 8, 16, 16])
                i1 = t4[:, :, 1, :].unsqueeze(2).to_broadcast([128, 8, 16, 16])
                return e.tensor_tensor(out=cand[:], in0=i0, in1=i1, op=ALU.add)
            kb.op("pool", fcand, reads=[TOP], writes=[CAND])
            cvs = [cand[:, h, :, :].rearrange("p a b -> p (a b)") for h in range(8)]
            for h in range(8):
                kb.op("dve", lambda e, h=h, cv=cvs[h]: e.max(out=ctop[:, h, 0:8], in_=cv), reads=[CAND], writes=[CTOP])
            for h in range(8):
                kb.op("dve", lambda e, h=h, cv=cvs[h]: e.match_replace(out=work[:, h * 256:(h + 1) * 256], in_to_replace=ctop[:, h, 0:8],
                                                                       in_values=cv, imm_value=NEG), reads=[CAND, CTOP], writes=[WORK])
            for h in range(8):
                kb.op("dve", lambda e, h=h: e.max(out=ctop[:, h, 8:16], in_=work[:, h * 256:(h + 1) * 256]), reads=[WORK], writes=[CTOP])
            kb.op("dve", lambda e: e.tensor_scalar(out=sm[:, 8:16], in0=ctop[:, :, 15], scalar1=-1.0, scalar2=PEER_MARGIN,
                                                   op0=ALU.mult, op1=ALU.add), reads=[CTOP], writes=[SM])
            kb.op("dve", lambda e: e.tensor_tensor(out=e16[:], in0=ctop[:], in1=sm[:, 8:16].unsqueeze(2).to_broadcast([128, 8, 16]),
                                                   op=ALU.add), reads=[CTOP, SM], writes=[E16])
            kb.op("act", lambda e: e.activation(out=e16[:], in_=e16[:], func=AF.Exp), reads=[E16], writes=[E16])
            kb.op("dve", lambda e: e.tensor_reduce(out=sm[:, 16:24], in_=e16[:], axis=AX.X, op=ALU.add), reads=[E16], writes=[SM])
            kb.op("dve", lambda e: e.reciprocal(out=sm[:, 24:32], in_=sm[:, 16:24]), reads=[SM], writes=[SM])
            s4 = s_sb[:].rearrange("p (h q) n -> p h q n", q=2)
            kb.op("dve", lambda e, j=j, s4=s4: e.tensor_tensor(out=av[:, j, :, :], in0=s4[:, :, 0, :],
                                                               in1=sm[:, 8:16].unsqueeze(2).to_broadcast([128, 8, 128]), op=ALU.add),
                  reads=[SSB, SM], writes=[AV])
            kb.op("act", lambda e, j=j: e.activation(out=av[:, j, :, :], in_=av[:, j, :, :], func=AF.Exp), reads=[AV], writes=[AV])
            kb.op("act", lambda e, j=j, s4=s4: e.activation(out=bv[:, j, :, :], in_=s4[:, :, 1, :], func=AF.Exp), reads=[SSB], writes=[BV])
            for h in range(8):
                kb.op("pool", lambda e, j=j, h=h: e.tensor_scalar(out=diag[:, j, h, :], in0=ident_f[:], scalar1=sm[:, 24 + h:25 + h],
                                                                  scalar2=None, op0=ALU.mult), reads=[IDF, SM], writes=[DG])
        for cg in range(32):
            ut, UB = load_w(peer_uT[:, cg * 512:(cg + 1) * 512], pre_of('uT', cg))
            vt, VB = load_v(peer_v[cg * 512:(cg + 1) * 512, :], pre_of('v', cg))
            gt, GB = gl[cg % 2]
            att, ATB = at[cg % 2]
            for half in range(2):
                c0 = cg * 4 + half * 2
                wp, WPB = wps[(cg * 2 + half) % 2]
                pp, PP = pps[(cg * 2 + half) % 2]
                def fpp(e, c0=c0, nj=nj, pp=pp):
                    i0 = av[:, 0:nj, :, c0:c0 + 2].unsqueeze(4).to_broadcast([128, nj, 8, 2, 128])
                    i1 = bv[:, 0:nj, :, :].unsqueeze(3).to_broadcast([128, nj, 8, 2, 128])
                    return e.tensor_tensor(out=pp[:, 0:nj], in0=i0, in1=i1, op=ALU.mult)
                kb.op("pool", fpp, reads=[AV, BV], writes=[PP])
                kb.op("dve", lambda e, wp=wp, nj=nj, pp=pp: e.scalar_tensor_tensor(out=wp[:, 0:nj], in0=pp[:, 0:nj], scalar=1.0, in1=pp[:, 0:nj],
                                                                     op0=ALU.is_ge, op1=ALU.mult), reads=[PP], writes=[WPB])
                pb, PB = B[0 + half]
                wb, WTB = B[2 + half]
                def mmpre(e, pb=pb, half=half, ut=ut, NTOK=NTOK):
                    for i in range(2):
                        cl = half * 2 + i
                        for k in range(8):
                            ins = e.matmul(pb[:, i * 256:i * 256 + NTOK], lhsT=ut[:, k, cl * 128:(cl + 1) * 128], rhs=h2T[:, k, 0:NTOK],
                                           start=(k == 0), stop=(k == 7))
                    return ins
                kb.op("pe", mmpre, reads=[UB, H2T], writes=[PB])
                def mmwt(e, wb=wb, wp=wp, nj=nj):
                    for i in range(2):
                        for j in range(nj):
                            for h in range(8):
                                ins = e.matmul(wb[:, i * 256 + j * 128:i * 256 + (j + 1) * 128], lhsT=wp[:, j, h, i, :],
                                               rhs=diag[:, j, h, :], start=(h == 0), stop=(h == 7))
                    return ins
                kb.op("pe", mmwt, reads=[WPB, DG], writes=[WTB])
                kb.op("act", lambda e, pb=pb, gt=gt, half=half, NTOK=NTOK: e.activation(
                    out=gt[:, half * 2:half * 2 + 2, 0:NTOK], in_=pb[:].rearrange("p (i t) -> p i t", i=2)[:, :, 0:NTOK], func=AF.Gelu),
                    reads=[PB], writes=[GB])
            for half in range(2):
                wb, WTB = B[2 + half]
                kb.op("dve", lambda e, wb=wb, gt=gt, att=att, half=half, NTOK=NTOK: e.tensor_tensor(
                    out=att[:, half * 2:half * 2 + 2, 0:NTOK], in0=wb[:].rearrange("p (i t) -> p i t", i=2)[:, :, 0:NTOK],
                    in1=gt[:, half * 2:half * 2 + 2, 0:NTOK], op=ALU.mult), reads=[WTB, GB], writes=[ATB])
            for j in range(nj):
                for hf in range(2):
                    ob, OB = B[4 + 2 * j + hf]
                    def mmo(e, ob=ob, att=att, vt=vt, j=j, hf=hf, cg=cg):
                        for cl in range(4):
                            ins = e.matmul(ob[:], lhsT=att[:, cl, j * 128:(j + 1) * 128], rhs=vt[:, cl, hf * 512:(hf + 1) * 512],
                                           start=(cg == 0 and cl == 0), stop=(cg == 31 and cl == 3))
                        return ins
                    kb.op("pe", mmo, reads=[ATB, VB], writes=[OB])
        for j, tt in enumerate(tiles):
            rows = slice(tt * 128, (tt + 1) * 128)
            for hf in range(2):
                ob, OB = B[4 + 2 * j + hf]
                kb.op("dve", lambda e, ob=ob, hf=hf: e.tensor_tensor(out=tmp[:, hf * 512:(hf + 1) * 512], in0=ob[:],
                                                                   in1=mod[:, 3, hf * 512:(hf + 1) * 512], op=ALU.mult),
                      reads=[OB, MOD], writes=[TMP])
            kb.op("pool", lambda e, j=j: e.tensor_tensor(out=xt[:, j, :], in0=xt[:, j, :], in1=tmp[:], op=ALU.add),
                  reads=[XT, TMP], writes=[XT])
            kb.dma("sp", x_out[rows, :], xt[:, j, :], reads=[XT], writes=[XOUT])
            if final_g is not None:
                kb.op("act", lambda e, j=j: e.activation(out=tmp[:], in_=xt[:, j, :], func=AF.Square, accum_out=sm[:, 0:1]),
                      reads=[XT], writes=[TMP, SM])
                rstd_ops(kb, sm, SM)
                kb.op("dve", lambda e, j=j: e.scalar_tensor_tensor(out=xn[:, 0:D], in0=xt[:, j, :], scalar=sm[:, 1:2], in1=fg[:],
                                                                   op0=ALU.mult, op1=ALU.mult), reads=[XT, SM, FG], writes=[XNB])
                kb.dma("sp", xn_out[rows, :], xn[:, 0:D], reads=[XNB], writes=[XN])


def build_pre(even):
    kb = KB()
    kb.psum_banks()
    B = kb.banks
    if even:
        NIN = 3072
        fm_blocks = [(c * 128, None) for c in range(8)] + [(1536 + c * 128, c) for c in range(8)]
        tm_groups = [(1024, 512), (2560, 512)]
    else:
        NIN = 1536
        fm_blocks = [(c * 128, c) for c in range(10)]
        tm_groups = [(1280, 256)]
    NROPE = 128 * sum(1 for _, r in fm_blocks if r is not None)
    NFM = len(fm_blocks)
    NTM = sum(n for _, n in tm_groups)
    x_in = kb.din("x", [TALL, D])
    csT = kb.din("csT", [128, 16])
    w_mod = kb.din("w_mod", [D, 6 * D])
    b_mod = kb.din("b_mod", [6 * D])
    n1g_in = kb.din("n1g", [D])
    w_in = kb.din("w_in", [D, NIN])
    w_perm = kb.din("w_perm", [D, NROPE])
    cosT = kb.din("cosT", [128, TALL])
    sinT = kb.din("sinT", [128, TALL])
    ident_d = kb.din("c_ident", [128, 128])
    fm_out, FMO = kb.dout("fmT", [NFM * 128, TALL], BF16)
    tm_out, TMO = kb.dout("tm", [TALL, NTM], BF16)
    modrow, MRO = kb.dout("modrow", [2, 6, D])

    ident_f, IDF = kb.sb("ident_f", [128, 128], F32)
    ident, ID = kb.sb("ident", [128, 128], BF16)
    kb.dma("sp", ident_f[:], ident_d, writes=[IDF])
    kb.op("act", lambda e: e.copy(out=ident[:], in_=ident_f[:]), reads=[IDF], writes=[ID])
    win, WIN = kb.sb("win", [128, 8, NIN], BF16)
    wpm, WPM = kb.sb("wpm", [128, 8, NROPE], BF16)
    for c0 in range(0, NIN, 512):
        kb.dma("pool", win[:, :, c0:c0 + 512], w_in[:, c0:c0 + 512].rearrange("(k p) n -> p k n", p=128), writes=[WIN])
    for c0 in range(0, NROPE, 512):
        n = min(512, NROPE - c0)
        kb.dma("pool", wpm[:, :, c0:c0 + n], w_perm[:, c0:c0 + n].rearrange("(k p) n -> p k n", p=128), writes=[WPM])
    cs_f, CSF = kb.sb("cs_f", [128, 16], F32)
    cs_b, CSB = kb.sb("cs_b", [128, 16], BF16)
    csbc, CSBC = kb.sb("csbc", [128, 16, 128], BF16)
    kb.dma("sp", cs_f[:], csT, writes=[CSF])
    kb.op("act", lambda e: e.activation(out=cs_b[:], in_=cs_f[:], func=AF.Silu), reads=[CSF], writes=[CSB])
    kb.op("dve", lambda e: e.tensor_copy(out=csbc[:], in_=cs_b[:].unsqueeze(2).to_broadcast([128, 16, 128])),
          reads=[CSB], writes=[CSBC])
    n1g, N1G = kb.sb("n1g_sb", [128, D], F32)
    kb.dma("sp", n1g[:], n1g_in.unsqueeze(0).to_broadcast([128, D]), writes=[N1G])
    g1s, G1S = kb.sb("g1s", [128, 2, 2, D], F32)
    wmr = [kb.sb(f"wmr{i}", [128, 8, 512], BF16) for i in range(2)]
    bmr = [kb.sb(f"bmr{i}", [128, 512], F32) for i in range(2)]
    mtmp = [kb.sb(f"mtmp{i}", [128, 512], F32) for i in range(2)]
    for cgp in range(12):
        wt, WB = wmr[cgp % 2]
        bt, BB = bmr[cgp % 2]
        cols = slice(cgp * 512, (cgp + 1) * 512)
        kb.dma("pool", wt[:], w_mod[:, cols].rearrange("(k p) n -> p k n", p=128), writes=[WB])
        kb.dma("sp", bt[:], b_mod[cols].unsqueeze(0).to_broadcast([128, 512]), writes=[BB])
        chunk = cgp // 2
        half = cgp % 2
        for s in range(2):
            mb, MB = B[6 + s]
            def mmm(e, mb=mb, wt=wt, s=s):
                for k in range(8):
                    ins = e.matmul(mb[:], lhsT=csbc[:, s * 8 + k, :], rhs=wt[:, k, :], start=(k == 0), stop=(k == 7))
                return ins
            kb.op("pe", mmm, reads=[CSBC, WB], writes=[MB])
            if chunk < 2:
                dst = g1s[:, s, chunk, half * 512:(half + 1) * 512]
                kb.op("dve", lambda e, mb=mb, bt=bt, dst=dst: e.tensor_tensor(out=dst, in0=mb[:], in1=bt[:], op=ALU.add),
                      reads=[MB, BB], writes=[G1S])
                kb.dma("sp", modrow[s, chunk:chunk + 1, half * 512:(half + 1) * 512], dst[0:1, :], reads=[G1S], writes=[MRO])
            else:
                mt, MT = mtmp[s]
                kb.op("dve", lambda e, mb=mb, bt=bt, mt=mt: e.tensor_tensor(out=mt[:], in0=mb[:], in1=bt[:], op=ALU.add),
                      reads=[MB, BB], writes=[MT])
                kb.dma("sp", modrow[s, chunk:chunk + 1, half * 512:(half + 1) * 512], mt[0:1, :], reads=[MT], writes=[MRO])
    for s in range(2):
        kb.op("dve", lambda e, s=s: e.scalar_tensor_tensor(out=g1s[:, s, 1, :], in0=g1s[:, s, 1, :], scalar=1.0, in1=n1g[:],
                                                           op0=ALU.add, op1=ALU.mult), reads=[G1S, N1G], writes=[G1S])
    xt = [kb.sb(f"xt{i}", [128, D], F32) for i in range(2)]
    tmp, TMP = kb.sb("tmp", [128, D], F32)
    hx, HX = kb.sb("hx", [128, D], BF16)
    hxT, HXT = kb.sb("hxT", [128, 8, 512], BF16)
    sm, SM = kb.sb("sm", [128, 8], F32)
    cst = [kb.sb(f"cst{i}", [128, 512], F32) for i in range(2)]
    snt = [kb.sb(f"snt{i}", [128, 512], F32) for i in range(2)]
    r1, R1 = kb.sb("r1", [128, 512], F32)
    r2, R2 = kb.sb("r2", [128, 512], F32)
    fmo = [kb.sb(f"fmo{i}", [128, 512], BF16) for i in range(2)]
    tmo = [kb.sb(f"tmo{i}", [128, 512], BF16) for i in range(2)]
    groups = [(g * 4, 4, 0) for g in range(NTL // 4)] + [(NTL, NTC, 1)]
    xi = 0
    fi = 0
    ti = 0
    for gi, (t0, ntl, s) in enumerate(groups):
        NTOK = ntl * 128
        tok0 = t0 * 128
        ct, CT = cst[gi % 2]
        st, ST = snt[gi % 2]
        kb.dma("sp", ct[:, 0:NTOK], cosT[:, tok0:tok0 + NTOK], writes=[CT])
        kb.dma("sp", st[:, 0:NTOK], sinT[:, tok0:tok0 + NTOK], writes=[ST])
        for j in range(ntl):
            x_t, XB = xt[xi % 2]
            xi += 1
            rows = slice(tok0 + j * 128, tok0 + (j + 1) * 128)
            kb.dma("sp", x_t[:], x_in[rows, :], writes=[XB])
            kb.op("act", lambda e, x_t=x_t: e.activation(out=tmp[:], in_=x_t[:], func=AF.Square, accum_out=sm[:, 0:1]),
                  reads=[XB], writes=[TMP, SM])
            rstd_ops(kb, sm, SM)
            kb.op("dve", lambda e, x_t=x_t, s=s: e.scalar_tensor_tensor(out=tmp[:], in0=x_t[:], scalar=sm[:, 1:2], in1=g1s[:, s, 1, :],
                                                                         op0=ALU.mult, op1=ALU.mult), reads=[XB, SM, G1S], writes=[TMP])
            kb.op("pool", lambda e, s=s: e.tensor_tensor(out=hx[:], in0=tmp[:], in1=g1s[:, s, 0, :], op=ALU.add),
                  reads=[TMP, G1S], writes=[HX])
            tb, TB = B[0]
            def tr(e, tb=tb):
                for k in range(8):
                    ins = e.transpose(tb.bitcast(BF16)[:, k * 128:(k + 1) * 128], hx[:, k * 128:(k + 1) * 128], ident[:])
                return ins
            kb.op("pe", tr, reads=[HX, ID], writes=[TB])
            kb.op("act", lambda e, tb=tb, j=j: e.copy(out=hxT[:, :, j * 128:(j + 1) * 128],
                                                      in_=tb.bitcast(BF16)[:, 0:1024].rearrange("p (k t) -> p k t", k=8)),
                  reads=[TB], writes=[HXT])
        for bi, (c0, ridx) in enumerate(fm_blocks):
            pa, PA = B[1 + bi % 2]
            def mma(e, pa=pa, c0=c0, NTOK=NTOK):
                for k in range(8):
                    ins = e.matmul(pa[:, 0:NTOK], lhsT=win[:, k, c0:c0 + 128], rhs=hxT[:, k, 0:NTOK], start=(k == 0), stop=(k == 7))
                return ins
            kb.op("pe", mma, reads=[WIN, HXT], writes=[PA])
            fo, FO = fmo[fi % 2]
            fi += 1
            if ridx is None:
                kb.op("act", lambda e, pa=pa, fo=fo, NTOK=NTOK: e.copy(out=fo[:, 0:NTOK], in_=pa[:, 0:NTOK]), reads=[PA], writes=[FO])
            else:
                pb, PB = B[3 + bi % 2]
                def mmb(e, pb=pb, ridx=ridx, NTOK=NTOK):
                    for k in range(8):
                        ins = e.matmul(pb[:, 0:NTOK], lhsT=wpm[:, k, ridx * 128:(ridx + 1) * 128], rhs=hxT[:, k, 0:NTOK],
                                       start=(k == 0), stop=(k == 7))
                    return ins
                kb.op("pe", mmb, reads=[WPM, HXT], writes=[PB])
                kb.op("dve", lambda e, pa=pa, ct=ct, NTOK=NTOK: e.tensor_tensor(out=r1[:, 0:NTOK], in0=pa[:, 0:NTOK], in1=ct[:, 0:NTOK], op=ALU.mult),
                      reads=[PA, CT], writes=[R1])
                kb.op("dve", lambda e, pb=pb, st=st, NTOK=NTOK: e.tensor_tensor(out=r2[:, 0:NTOK], in0=pb[:, 0:NTOK], in1=st[:, 0:NTOK], op=ALU.mult),
                      reads=[PB, ST], writes=[R2])
                kb.op("pool", lambda e, fo=fo, NTOK=NTOK: e.tensor_tensor(out=fo[:, 0:NTOK], in0=r1[:, 0:NTOK], in1=r2[:, 0:NTOK], op=ALU.add),
                      reads=[R1, R2], writes=[FO])
            for c0_ in range(0, NTOK, 256):
                kb.dma("sp", fm_out[bi * 128:(bi + 1) * 128, tok0 + c0_:tok0 + c0_ + 256], fo[:, c0_:c0_ + 256], reads=[FO], writes=[FMO])
        for j in range(ntl):
            oc = 0
            for (c0, ncol) in tm_groups:
                pt, PT = B[5 + ti % 2]
                to, TO = tmo[ti % 2]
                ti += 1
                def mmt(e, pt=pt, c0=c0, ncol=ncol, j=j):
                    for k in range(8):
                        ins = e.matmul(pt[:, 0:ncol], lhsT=hxT[:, k, j * 128:(j + 1) * 128], rhs=win[:, k, c0:c0 + ncol],
                                       start=(k == 0), stop=(k == 7))
                    return ins
                kb.op("pe", mmt, reads=[HXT, WIN], writes=[PT])
                kb.op("act", lambda e, pt=pt, to=to, ncol=ncol: e.copy(out=to[:, 0:ncol], in_=pt[:, 0:ncol]), reads=[PT], writes=[TO])
                rows = slice(tok0 + j * 128, tok0 + (j + 1) * 128)
                kb.dma("sp", tm_out[rows, oc:oc + ncol], to[:, 0:ncol], reads=[TO], writes=[TMO])
                oc += ncol
    return kb.finish()


def emit_attn_even(kb, ao, AO):
    B = kb.banks
    naqT = kb.din("naqT", [512, TALL], BF16)
    nakT = kb.din("nakT_h", [512, 74 * 64], BF16)
    nav = kb.din("nav_h", [74 * 64, 520], BF16)
    nakTc = kb.din("nakT_c", [512, CTX], BF16)
    navc = kb.din("nav_c", [CTX, 520], BF16)
    dqT = kb.din("dqT", [512, TALL], BF16)
    dkT = kb.din("dkT_all", [512, CTX + SEQ], BF16)
    dv = kb.din("dv_all", [CTX + SEQ, 516], BF16)
    nbias = kb.din("nbias", [5, 8, 128, 640])
    lam_in = kb.din("lam", [4, 64])
    subg_in = kb.din("subg", [128])
    lami_in = kb.din("lam_init", [128, 2])
    sc = 64 ** -0.5
    lm, LM = kb.sb("lm", [128, 4, 64], F32)
    lms, LMS = kb.sb("lms", [128, 16], F32)
    subg, SUBG = kb.sb("subg_sb", [128, 128], F32)
    kb.dma("sp", lm[:].rearrange("p a b -> p (a b)"), lam_in.rearrange("a b -> (a b)").unsqueeze(0).to_broadcast([128, 256]), writes=[LM])
    kb.dma("sp", subg[:], subg_in.unsqueeze(0).to_broadcast([128, 128]), writes=[SUBG])
    kb.dma("sp", lms[:, 8:10], lami_in, writes=[LMS])
    kb.op("dve", lambda e: e.tensor_tensor(out=lm[:, 0, :], in0=lm[:, 0, :], in1=lm[:, 1, :], op=ALU.mult), reads=[LM], writes=[LM])
    kb.op("dve", lambda e: e.tensor_tensor(out=lm[:, 2, :], in0=lm[:, 2, :], in1=lm[:, 3, :], op=ALU.mult), reads=[LM], writes=[LM])
    kb.op("dve", lambda e: e.tensor_reduce(out=lms[:, 0:1], in_=lm[:, 0, :], axis=AX.X, op=ALU.add), reads=[LM], writes=[LMS])
    kb.op("dve", lambda e: e.tensor_reduce(out=lms[:, 1:2], in_=lm[:, 2, :], axis=AX.X, op=ALU.add), reads=[LM], writes=[LMS])
    kb.op("act", lambda e: e.activation(out=lms[:, 2:4], in_=lms[:, 0:2], func=AF.Exp), reads=[LMS], writes=[LMS])
    kb.op("dve", lambda e: e.tensor_tensor(out=lms[:, 4:5], in0=lms[:, 2:3], in1=lms[:, 3:4], op=ALU.subtract), reads=[LMS], writes=[LMS])
    kb.op("dve", lambda e: e.tensor_tensor(out=lms[:, 5:6], in0=lms[:, 4:5], in1=lms[:, 8:9], op=ALU.add), reads=[LMS], writes=[LMS])
    kb.op("dve", lambda e: e.tensor_scalar(out=lms[:, 6:7], in0=lms[:, 5:6], scalar1=-1.0, scalar2=None, op0=ALU.mult), reads=[LMS], writes=[LMS])
    kb.op("dve", lambda e: e.tensor_scalar(out=subg[:], in0=subg[:], scalar1=lms[:, 9:10], scalar2=None, op0=ALU.mult), reads=[SUBG, LMS], writes=[SUBG])

    kcT, KCT = kb.sb("na_kcT", [128, 4, CTX], BF16)
    vca, VCA = kb.sb("na_vca", [128, 2, 8, 65], BF16)
    kb.dma("sp", kcT[:], nakTc.rearrange("(a p) n -> p a n", p=128), writes=[KCT])
    kb.dma("sp", vca[:].rearrange("p b h c -> p b (h c)"), navc.rearrange("(b p) c -> p b c", p=128), writes=[VCA])
    kts = [kb.sb(f"na_kt{i}", [128, 4, 640], BF16) for i in range(2)]
    vts = [kb.sb(f"na_vt{i}", [128, 5, 8, 65], BF16) for i in range(2)]
    qts = [kb.sb(f"na_qt{i}", [128, 4, 128], BF16) for i in range(2)]
    bts = [kb.sb(f"na_bt{i}", [128, 640], F32) for i in range(2)]
    sts = [kb.sb(f"na_st{i}", [128, 640], F32) for i in range(2)]
    pts = [kb.sb(f"na_pt{i}", [128, 896], BF16) for i in range(2)]
    aot = [kb.sb(f"na_ao{i}", [128, 512], BF16) for i in range(2)]
    rc, RC = kb.sb("na_rc", [128, 8], F32)
    hi = 0
    for qt in range(NT):
        isctx = qt >= NTL
        q_t, QB = qts[qt % 2]
        kb.dma("sp", q_t[:], naqT[:, qt * 128:(qt + 1) * 128].rearrange("(a p) n -> p a n", p=128), writes=[QB])
        if not isctx:
            k_t, KB_ = kts[qt % 2]
            v_t, VB = vts[qt % 2]
            kb.dma("sp", k_t[:], nakT[:, qt * 128:qt * 128 + 640].rearrange("(a p) n -> p a n", p=128), writes=[KB_])
            kb.dma("sp", v_t[:].rearrange("p b h c -> p b (h c)"), nav[qt * 128:qt * 128 + 640, :].rearrange("(b p) c -> p b c", p=128), writes=[VB])
            slot = 0 if qt == 0 else 1 if qt == 1 else 3 if qt == NTL - 2 else 4 if qt == NTL - 1 else 2
        a_t, AB = aot[qt % 2]
        for h in range(8):
            a, off = h // 2, (h % 2) * 64
            pa, PA = B[(2 * hi) % 4]
            pb, PB = B[(2 * hi + 1) % 4]
            st_, STB = sts[hi % 2]
            pt_, PTB = pts[hi % 2]
            acc, ACC = B[4 + (h // 4)]
            hi += 1
            if not isctx:
                b_t, BB = bts[hi % 2]
                kb.dma("sp", b_t[:], nbias[slot, h], writes=[BB])
                def mms(e, pa=pa, pb=pb, k_t=k_t, q_t=q_t, a=a, off=off):
                    for blk in range(4):
                        e.matmul(pa[:, blk * 128:(blk + 1) * 128], lhsT=k_t[off:off + 64, a, blk * 128:(blk + 1) * 128],
                                 rhs=q_t[off:off + 64, a, :], start=True, stop=True)
                    e.matmul(pb[:, 0:128], lhsT=k_t[off:off + 64, a, 512:640], rhs=q_t[off:off + 64, a, :], start=True, stop=True)
                    for cb in range(2):
                        ins = e.matmul(pb[:, 128 + cb * 128:256 + cb * 128], lhsT=kcT[off:off + 64, a, cb * 128:(cb + 1) * 128],
                                       rhs=q_t[off:off + 64, a, :], start=True, stop=True)
                    return ins
                kb.op("pe", mms, reads=[KB_, QB, KCT], writes=[PA, PB])
                kb.op("dve", lambda e, pa=pa, st_=st_, b_t=b_t: e.scalar_tensor_tensor(out=st_[:, 0:512], in0=pa[:], scalar=sc, in1=b_t[:, 0:512],
                                                                                    op0=ALU.mult, op1=ALU.add), reads=[PA, BB], writes=[STB])
                kb.op("dve", lambda e, pb=pb, st_=st_, b_t=b_t: e.scalar_tensor_tensor(out=st_[:, 512:640], in0=pb[:, 0:128], scalar=sc,
                                                                                    in1=b_t[:, 512:640], op0=ALU.mult, op1=ALU.add),
                      reads=[PB, BB], writes=[STB])
                kb.op("act", lambda e, st_=st_, pt_=pt_: e.activation(out=pt_[:, 0:640], in_=st_[:], func=AF.Exp), reads=[STB], writes=[PTB])
                kb.op("act", lambda e, pb=pb, pt_=pt_: e.activation(out=pt_[:, 640:896], in_=pb[:, 128:384], func=AF.Exp, scale=sc),
                      reads=[PB], writes=[PTB])
                def mmv(e, acc=acc, pt_=pt_, v_t=v_t, h=h):
                    o = acc[:, (h % 4) * 65:(h % 4) * 65 + 65]
                    for blk in range(5):
                        e.matmul(o, lhsT=pt_[:, blk * 128:(blk + 1) * 128], rhs=v_t[:, blk, h, :], start=(blk == 0), stop=False)
                    for cb in range(2):
                        ins = e.matmul(o, lhsT=pt_[:, 640 + cb * 128:768 + cb * 128], rhs=vca[:, cb, h, :], start=False, stop=(cb == 1))
                    return ins
                kb.op("pe", mmv, reads=[PTB, VB, VCA], writes=[ACC])
            else:
                def mms(e, pb=pb, q_t=q_t, a=a, off=off):
                    for cb in range(2):
                        ins = e.matmul(pb[:, 128 + cb * 128:256 + cb * 128], lhsT=kcT[off:off + 64, a, cb * 128:(cb + 1) * 128],
                                       rhs=q_t[off:off + 64, a, :], start=True, stop=True)
                    return ins
                kb.op("pe", mms, reads=[QB, KCT], writes=[PB])
                kb.op("act", lambda e, pb=pb, pt_=pt_: e.activation(out=pt_[:, 640:896], in_=pb[:, 128:384], func=AF.Exp, scale=sc),
                      reads=[PB], writes=[PTB])
                def mmv(e, acc=acc, pt_=pt_, h=h):
                    o = acc[:, (h % 4) * 65:(h % 4) * 65 + 65]
                    for cb in range(2):
                        ins = e.matmul(o, lhsT=pt_[:, 640 + cb * 128:768 + cb * 128], rhs=vca[:, cb, h, :], start=(cb == 0), stop=(cb == 1))
                    return ins
                kb.op("pe", mmv, reads=[PTB, VCA], writes=[ACC])
            if h % 4 == 3:
                g4 = h // 4
                av = acc[:, 0:260].rearrange("p (h c) -> p h c", c=65)
                kb.op("dve", lambda e, av=av, g4=g4: e.reciprocal(out=rc[:, g4 * 4:g4 * 4 + 4], in_=av[:, :, 64]), reads=[ACC], writes=[RC])
                kb.op("dve", lambda e, av=av, g4=g4, a_t=a_t: e.tensor_tensor(
                    out=a_t[:, g4 * 256:(g4 + 1) * 256].rearrange("p (h d) -> p h d", d=64), in0=av[:, :, 0:64],
                    in1=rc[:, g4 * 4:g4 * 4 + 4].unsqueeze(2).to_broadcast([128, 4, 64]), op=ALU.mult), reads=[ACC, RC], writes=[AB])
        kb.dma("sp", ao[qt * 128:(qt + 1) * 128, 0:512], a_t[:], reads=[AB], writes=[AO])

    NBLK = (CTX + SEQ) // 128
    dk, DK = kb.sb("d_k", [128, CTX + SEQ], BF16)
    dva, DVA = kb.sb("d_va", [128, NBLK, 129], BF16)
    dq, DQ = kb.sb("d_q", [128, TALL], BF16)
    dpt = [kb.sb(f"d_pt{i}", [128, 512], BF16) for i in range(3)]
    o0, O0 = kb.sb("d_o0", [128, 128], F32)
    o1, O1 = kb.sb("d_o1", [128, 128], F32)
    osq, OSQ = kb.sb("d_osq", [128, 128], F32)
    dsm, DSM = kb.sb("d_sm", [128, 8], F32)
    dob = [kb.sb(f"d_ob{i}", [128, 128], BF16) for i in range(2)]
    si = 0
    oi = 0
    for h in range(4):
        kb.dma("sp", dk[:], dkT[h * 128:(h + 1) * 128, :], writes=[DK])
        for c0 in range(0, NBLK, 26):
            kb.dma("sp", dva[:, c0:c0 + 26, :], dv[c0 * 128:(c0 + 26) * 128, h * 129:(h + 1) * 129].rearrange("(b p) d -> p b d", p=128),
                   writes=[DVA])
        kb.dma("sp", dq[:], dqT[h * 128:(h + 1) * 128, :], writes=[DQ])
        qgroups = [(g * 256, 256, NBLK) for g in range(TQ // 256)] + [(TQ, CTX, CTX // 128)]
        for (q0, nq, nblk) in qgroups:
            nqs = nq // 128
            for blk in range(nblk):
                for m in range(2):
                    ps, PS = B[si % 3]
                    pt_, PTB = dpt[si % 3]
                    si += 1
                    kb.op("pe", lambda e, ps=ps, m=m, blk=blk, q0=q0, nq=nq: e.matmul(
                        ps[:, 0:nq], lhsT=dk[m * 64:(m + 1) * 64, blk * 128:(blk + 1) * 128], rhs=dq[m * 64:(m + 1) * 64, q0:q0 + nq],
                        start=True, stop=True), reads=[DK, DQ], writes=[PS])
                    kb.op("act", lambda e, ps=ps, pt_=pt_, nq=nq: e.activation(out=pt_[:, 0:nq], in_=ps[:, 0:nq], func=AF.Exp, scale=sc),
                          reads=[PS], writes=[PTB])
                    def mmv(e, pt_=pt_, m=m, blk=blk, nqs=nqs, nblk=nblk):
                        for qs in range(nqs):
                            a = qs * 2 + m
                            acc = B[3 + a][0]
                            ins = e.matmul(acc[:, 0:129], lhsT=pt_[:, qs * 128:(qs + 1) * 128], rhs=dva[:, blk, :],
                                           start=(blk == 0), stop=(blk == nblk - 1))
                        return ins
                    kb.op("pe", mmv, reads=[PTB, DVA], writes=[B[3 + qs * 2 + m][1] for qs in range(nqs)])
            for qs in range(nqs):
                accs = []
                for m in range(2):
                    a = qs * 2 + m
                    accs.append((B[3 + a][0][:, 0:129], B[3 + a][1]))
                (a0, A0), (a1, A1) = accs
                kb.op("dve", lambda e, a0=a0: e.reciprocal(out=dsm[:, 0:1], in_=a0[:, 128:129]), reads=[A0], writes=[DSM])
                kb.op("dve", lambda e, a1=a1: e.reciprocal(out=dsm[:, 1:2], in_=a1[:, 128:129]), reads=[A1], writes=[DSM])
                kb.op("dve", lambda e: e.tensor_tensor(out=dsm[:, 1:2], in0=dsm[:, 1:2], in1=lms[:, 6:7], op=ALU.mult), reads=[DSM, LMS], writes=[DSM])
                kb.op("dve", lambda e, a0=a0: e.tensor_scalar(out=o0[:], in0=a0[:, 0:128], scalar1=dsm[:, 0:1], scalar2=None, op0=ALU.mult),
                      reads=[A0, DSM], writes=[O0])
                kb.op("dve", lambda e, a1=a1: e.scalar_tensor_tensor(out=o1[:], in0=a1[:, 0:128], scalar=dsm[:, 1:2], in1=o0[:],
                                                                   op0=ALU.mult, op1=ALU.add), reads=[A1, DSM, O0], writes=[O1])
                kb.op("act", lambda e: e.activation(out=osq[:], in_=o1[:], func=AF.Square, accum_out=dsm[:, 2:3]), reads=[O1], writes=[OSQ, DSM])
                kb.op("dve", lambda e: e.tensor_scalar(out=dsm[:, 3:4], in0=dsm[:, 2:3], scalar1=1.0 / 128, scalar2=EPS, op0=ALU.mult, op1=ALU.add),
                      reads=[DSM], writes=[DSM])
                kb.op("act", lambda e: e.activation(out=dsm[:, 4:5], in_=dsm[:, 3:4], func=AF.Sqrt), reads=[DSM], writes=[DSM])
                kb.op("dve", lambda e: e.reciprocal(out=dsm[:, 5:6], in_=dsm[:, 4:5]), reads=[DSM], writes=[DSM])
                ob, OB = dob[oi % 2]
                oi += 1
                kb.op("dve", lambda e, ob=ob: e.scalar_tensor_tensor(out=ob[:], in0=o1[:], scalar=dsm[:, 5:6], in1=subg[:], op0=ALU.mult, op1=ALU.mult),
                      reads=[O1, DSM, SUBG], writes=[OB])
                r0 = q0 + qs * 128
                kb.dma("sp", ao[r0:r0 + 128, 512 + h * 128:512 + (h + 1) * 128], ob[:], reads=[OB], writes=[AO])


def emit_attn_odd(kb, ao, AO):
    B = kb.banks
    HALO = TQ + 256
    qin = kb.din("swa_q", [64, 16, TALL], BF16)
    kin = kb.din("swa_kT_h", [64, 4, HALO], BF16)
    vin = kb.din("swa_v_h", [HALO, 260], BF16)
    kcin = kb.din("swa_kT_c", [64, 4, CTX], BF16)
    vcin = kb.din("swa_v_c", [CTX, 260], BF16)
    sbias = kb.din("sbias", [3, 128, 384])
    sink_in = kb.din("sink", [16])
    sc = 64 ** -0.5
    snk, SNK = kb.sb("snk", [128, 16], F32)
    kb.dma("sp", snk[:], sink_in.unsqueeze(0).to_broadcast([128, 16]), writes=[SNK])
    kb.op("act", lambda e: e.activation(out=snk[:], in_=snk[:], func=AF.Exp), reads=[SNK], writes=[SNK])
    kT, KT = kb.sb("s_kT", [64, 4, HALO], BF16)
    kb.dma("sp", kT[:], kin, writes=[KT])
    kcT, KCT = kb.sb("s_kcT", [64, 4, CTX], BF16)
    kb.dma("sp", kcT[:], kcin, writes=[KCT])
    va, VA = kb.sb("s_va", [128, HALO // 128, 4, 65], BF16)
    kb.dma("sp", va[:].rearrange("p b h c -> p b (h c)"), vin.rearrange("(b p) c -> p b c", p=128), writes=[VA])
    vca, VCA = kb.sb("s_vca", [128, 2, 4, 65], BF16)
    kb.dma("sp", vca[:].rearrange("p b h c -> p b (h c)"), vcin.rearrange("(b p) c -> p b c", p=128), writes=[VCA])
    sb_, SBB = kb.sb("s_bias", [128, 3, 384], F32)
    kb.dma("sp", sb_[:], sbias.rearrange("s p n -> p s n"), writes=[SBB])
    qts = [kb.sb(f"s_q{i}", [64, 16, 128], BF16) for i in range(2)]
    sts = [kb.sb(f"s_st{i}", [128, 512], F32) for i in range(2)]
    pts = [kb.sb(f"s_pt{i}", [128, 5, 512], BF16) for i in range(2)]
    aot = [kb.sb(f"s_ao{i}", [128, D], BF16) for i in range(2)]
    den, DEN = kb.sb("s_den", [128, 8], F32)
    si = 0
    gi = 0
    for qt in range(NT):
        isctx = qt >= NTL
        q_t, QB = qts[qt % 2]
        kb.dma("sp", q_t[:], qin[:, :, qt * 128:(qt + 1) * 128], writes=[QB])
        a_t, AB = aot[qt % 2]
        slot = 0 if qt == 0 else 2 if qt == NTL - 1 else 1
        for n in range(4):
            pt_, PTB = pts[gi % 2]
            acc, ACC = B[4 + gi % 2]
            gi += 1
            blks = ([] if isctx else [0, 1, 2]) + [3, 4]
            for blk in blks:
                ps, PS = B[si % 3]
                st_, STB = sts[si % 2]
                si += 1
                if blk < 3:
                    kb.op("pe", lambda e, ps=ps, blk=blk, n=n, q_t=q_t, qt=qt: e.matmul(
                        ps[:], lhsT=kT[:, n, (qt + blk) * 128:(qt + blk + 1) * 128], rhs=q_t[:, n * 4:(n + 1) * 4, :], start=True, stop=True),
                        reads=[KT, QB], writes=[PS])
                    kb.op("dve", lambda e, ps=ps, st_=st_, blk=blk, slot=slot: e.scalar_tensor_tensor(
                        out=st_[:].rearrange("p (g q) -> p g q", g=4), in0=ps[:].rearrange("p (g q) -> p g q", g=4), scalar=sc,
                        in1=sb_[:, slot, blk * 128:(blk + 1) * 128].unsqueeze(1).to_broadcast([128, 4, 128]), op0=ALU.mult, op1=ALU.add),
                        reads=[PS, SBB], writes=[STB])
                    kb.op("act", lambda e, st_=st_, pt_=pt_, blk=blk: e.activation(out=pt_[:, blk, :], in_=st_[:], func=AF.Exp), reads=[STB], writes=[PTB])
                else:
                    cb = blk - 3
                    kb.op("pe", lambda e, ps=ps, cb=cb, n=n, q_t=q_t: e.matmul(
                        ps[:], lhsT=kcT[:, n, cb * 128:(cb + 1) * 128], rhs=q_t[:, n * 4:(n + 1) * 4, :], start=True, stop=True),
                        reads=[KCT, QB], writes=[PS])
                    kb.op("act", lambda e, ps=ps, pt_=pt_, blk=blk: e.activation(out=pt_[:, blk, :], in_=ps[:], func=AF.Exp, scale=sc),
                          reads=[PS], writes=[PTB])
            def mmv(e, acc=acc, pt_=pt_, n=n, qt=qt, blks=blks):
                for g in range(4):
                    o = acc[:, g * 65:(g + 1) * 65]
                    for i, blk in enumerate(blks):
                        rhs = va[:, qt + blk, n, :] if blk < 3 else vca[:, blk - 3, n, :]
                        ins = e.matmul(o, lhsT=pt_[:, blk, g * 128:(g + 1) * 128], rhs=rhs, start=(i == 0), stop=(i == len(blks) - 1))
                return ins
            kb.op("pe", mmv, reads=[PTB, VA, VCA], writes=[ACC])
            av = acc[:, 0:260].rearrange("p (g c) -> p g c", c=65)
            kb.op("dve", lambda e, av=av, n=n: e.tensor_tensor(out=den[:, 0:4], in0=av[:, :, 64], in1=snk[:, n * 4:(n + 1) * 4], op=ALU.add),
                  reads=[ACC, SNK], writes=[DEN])
            kb.op("dve", lambda e: e.reciprocal(out=den[:, 4:8], in_=den[:, 0:4]), reads=[DEN], writes=[DEN])
            kb.op("dve", lambda e, av=av, n=n, a_t=a_t: e.tensor_tensor(
                out=a_t[:, n * 256:(n + 1) * 256].rearrange("p (g d) -> p g d", d=64), in0=av[:, :, 0:64],
                in1=den[:, 4:8].unsqueeze(2).to_broadcast([128, 4, 64]), op=ALU.mult), reads=[ACC, DEN], writes=[AB])
        kb.dma("sp", ao[qt * 128:(qt + 1) * 128, :], a_t[:], reads=[AB], writes=[AO])


def build_post(even):
    kb = KB()
    kb.psum_banks()
    ao, AO = kb.dscratch("ao_scr", [TALL, D], BF16)
    with ExitStack() as es:
        saved = kb.es
        kb.es = es
        if even:
            emit_attn_even(kb, ao, AO)
        else:
            emit_attn_odd(kb, ao, AO)
        kb.es = saved
        kb.P.barrier()
    x_in = kb.din("x", [TALL, D])
    modrow = kb.din("modrow", [2, 6, D])
    w_out = kb.din("w_out", [D, D])
    n2g = kb.din("n2g", [D])
    fg = kb.din("fg", [D])
    wq = kb.din("wq", [D, 2048])
    keysT = kb.din("keysT", [128, 16, 128])
    uT = kb.din("uT", [D, 16384])
    v = kb.din("v", [16384, D])
    x_out, XOUT = kb.dout("x_out", [TALL, D])
    xn_out, XN = kb.dout("xn_out", [TALL, D])
    emit_post(kb, NT, x_in, ao, AO, modrow, w_out, n2g, wq, keysT, uT, v, x_out, XOUT, final_g=fg, xn_out=xn_out, XN=XN,
              tile_sets=[0] * NTL + [1] * NTC)
    return kb.finish()


GRID_W = 64
_PERM64 = np.concatenate([np.arange(16, 32), np.arange(0, 16), np.arange(48, 64), np.arange(32, 48)])
_SGN64 = np.concatenate([-np.ones(16), np.ones(16), -np.ones(16), np.ones(16)]).astype(np.float32)
_PROGS = {}


def _prog(name):
    if name not in _PROGS:
        kind, par = name.split("_")
        _PROGS[name] = build_pre(par == "even") if kind == "pre" else build_post(par == "even")
    return _PROGS[name]


def _rope_tables_T(tok0):
    t = np.arange(tok0, tok0 + TQ)
    row = (t // GRID_W).astype(np.float32)
    col = (t % GRID_W).astype(np.float32)
    half = 32
    inv = (10000.0 ** (-np.arange(0, half, 2, dtype=np.float32) / half)).astype(np.float32)
    ar = row[:, None] * inv
    ac = col[:, None] * inv
    ang = np.concatenate([ar, ar, ac, ac], axis=-1)
    cos = np.cos(ang).astype(np.float32)
    sin = np.sin(ang).astype(np.float32) * _SGN64[None, :]
    cosT = np.ones((128, TALL), np.float32)
    sinT = np.zeros((128, TALL), np.float32)
    cosT[:, :TQ] = np.tile(cos.T, (2, 1))
    sinT[:, :TQ] = np.tile(sin.T, (2, 1))
    return cosT, sinT


def _na_bias(rpb, R0):
    H = rpb.shape[0]
    out = np.full((5, H, 128, 5, 128), NEG, np.float32)
    kk = np.arange(128)
    qq = np.arange(128)
    for slot, r0 in enumerate((R0, R0 + 2, R0 + 32, R0 + 60, R0 + 62)):
        if slot == 2:
            r0 = 100 if R0 not in (0,) else 100
        qr = r0 + qq // 64
        qc = qq % 64
        rs = np.clip(qr - 4, 0, 256 - 8)
        cs = np.clip(qc - 8, 0, 64 - 16)
        for blk in range(5):
            kr = r0 - 4 + 2 * blk + kk // 64
            kc = kk % 64
            valid = ((kr[:, None] >= rs[None, :]) & (kr[:, None] < rs[None, :] + 8) &
                     (kc[:, None] >= cs[None, :]) & (kc[:, None] < cs[None, :] + 16) &
                     (kr[:, None] >= 0) & (kr[:, None] < 256))
            dr = np.clip(kr[:, None] - qr[None, :] + 7, 0, 14)
            dc = np.clip(kc[:, None] - qc[None, :] + 15, 0, 30)
            vals = rpb[:, dr, dc]
            out[slot, :, :, blk, :] = np.where(valid[None], vals, NEG)
    return out.reshape(5, H, 128, 640)


def _swa_bias(qr):
    out = np.full((3, 128, 3, 128), NEG, np.float32)
    k = np.arange(128)[:, None]
    q = np.arange(128)[None, :]
    for slot, gb in enumerate((qr * 32, qr * 32 + 5, qr * 32 + 31)):
        for blk in range(3):
            kb_ = gb - 1 + blk
            if kb_ < 0 or kb_ >= SEQ // 128:
                continue
            diff = (blk - 1) * 128 + k - q
            out[slot, :, blk, :] = np.where(np.abs(diff) <= 128, 0.0, NEG)
    return out.reshape(3, 128, 384)


def _ones_col(v, nh, dv):
    T = v.shape[0]
    o = np.ones((T, nh, dv + 1), v.dtype)
    o[:, :, :dv] = v.reshape(T, nh, dv)
    return o.reshape(T, nh * (dv + 1))


def _halo(arr, axis, lo, hi):
    n = arr.shape[axis]
    shape = list(arr.shape)
    shape[axis] = hi - lo
    out = np.zeros(shape, arr.dtype)
    s0, s1 = max(lo, 0), min(hi, n)
    src = [slice(None)] * arr.ndim
    dst = [slice(None)] * arr.ndim
    src[axis] = slice(s0, s1)
    dst[axis] = slice(s0 - lo, s1 - lo)
    out[tuple(dst)] = arr[tuple(src)]
    return out


def _run(name, in_maps):
    res = run_bass_kernel_spmd(_prog(name), in_maps, core_ids=list(range(NCORES)))
    return res.results


def kernel(x, c, ctx, c_ctx, w_mod, b_mod, norm1_g, norm2_g, w_in_even, w_out_even, na_rpb, diff_lambda,
           diff_subln_g, w_in_odd, w_out_odd, swa_sink, peer_wq, peer_keys, peer_u, peer_v, final_g, _nlayers=4, _dbg=None):
    f32 = np.float32
    x = np.asarray(x, f32)
    ctx = np.asarray(ctx, f32)
    ident = np.eye(128, dtype=f32)
    xs = []
    for core in range(NCORES):
        b, qr = divmod(core, 4)
        xs.append(np.concatenate([x[b, qr * TQ:(qr + 1) * TQ], ctx[b]], axis=0))
    csT = []
    for core in range(NCORES):
        b = core // 4
        cs = np.stack([np.asarray(c, f32)[b], np.asarray(c_ctx, f32)], 0)
        csT.append(np.ascontiguousarray(cs.reshape(2, 8, 128).transpose(2, 0, 1).reshape(128, 16)))
    ropes = [_rope_tables_T((core % 4) * TQ) for core in range(NCORES)]
    xn = None
    for i in range(_nlayers):
        even = (i % 2 == 0)
        j = i // 2
        w_in = np.asarray(w_in_even[j] if even else w_in_odd[j], f32)
        if even:
            rc = w_in[:, 1536:2560]
        else:
            rc = w_in[:, 0:1280]
        w_perm = np.ascontiguousarray(rc.reshape(D, -1, 64)[:, :, _PERM64].reshape(D, -1))
        ims = []
        for core in range(NCORES):
            ims.append({"x": xs[core], "csT": csT[core], "w_mod": np.asarray(w_mod[i], f32), "b_mod": np.asarray(b_mod[i], f32),
                        "n1g": np.asarray(norm1_g[i], f32), "w_in": w_in, "w_perm": w_perm,
                        "cosT": ropes[core][0], "sinT": ropes[core][1], "c_ident": ident})
        pre = _run("pre_even" if even else "pre_odd", ims)
        fm = [np.asarray(r["fmT"]) for r in pre]
        tm = [np.asarray(r["tm"]) for r in pre]
        if _dbg is not None:
            _dbg[f"pre{i}"] = (fm, tm, [np.asarray(r["modrow"]) for r in pre])
        keysT = np.ascontiguousarray(np.asarray(peer_keys[i], f32).reshape(16, 128, 128).transpose(2, 0, 1))
        common = {"w_out": np.asarray(w_out_even[j] if even else w_out_odd[j], f32), "n2g": np.asarray(norm2_g[i], f32),
                  "fg": np.asarray(final_g, f32), "wq": np.asarray(peer_wq[i], f32), "keysT": keysT,
                  "uT": np.ascontiguousarray(np.asarray(peer_u[i], f32).T), "v": np.asarray(peer_v[i], f32), "c_ident": ident}
        ims = []
        for core in range(NCORES):
            b, qr = divmod(core, 4)
            grp = [b * 4 + k for k in range(4)]
            im = dict(common)
            im["x"] = xs[core]
            im["modrow"] = np.asarray(pre[core]["modrow"])
            if even:
                kT_lat = np.concatenate([fm[g][512:1024, :TQ] for g in grp], axis=1)
                v_lat = np.concatenate([tm[g][:TQ, 0:512] for g in grp], axis=0)
                lo = (qr * 64 - 4) * 64
                im["naqT"] = np.ascontiguousarray(fm[core][0:512])
                im["nakT_h"] = _halo(kT_lat, 1, lo, lo + 74 * 64)
                im["nav_h"] = _ones_col(_halo(v_lat, 0, lo, lo + 74 * 64), 8, 64)
                im["nakT_c"] = np.ascontiguousarray(fm[core][512:1024, TQ:])
                im["nav_c"] = _ones_col(tm[core][TQ:, 0:512], 8, 64)
                im["dqT"] = np.ascontiguousarray(fm[core][1024:1536])
                im["dkT_all"] = np.concatenate([fm[core][1536:2048, TQ:]] + [fm[g][1536:2048, :TQ] for g in grp], axis=1)
                im["dv_all"] = _ones_col(np.concatenate([tm[core][TQ:, 512:1024]] + [tm[g][:TQ, 512:1024] for g in grp], axis=0), 4, 128)
                im["nbias"] = _na_bias(np.asarray(na_rpb[j], f32), qr * 64)
                im["lam"] = np.asarray(diff_lambda[j], f32)
                im["subg"] = np.asarray(diff_subln_g[j], f32)
                li = 0.8 - 0.6 * math.exp(-0.3 * i)
                im["lam_init"] = np.tile(np.array([[li, 1.0 - li]], f32), (128, 1))
            else:
                kT_lat = np.concatenate([fm[g][1024:1280, :TQ] for g in grp], axis=1)
                v_lat = np.concatenate([tm[g][:TQ, :] for g in grp], axis=0)
                lo = qr * TQ - 128
                im["swa_q"] = np.ascontiguousarray(fm[core][0:1024].reshape(16, 64, TALL).transpose(1, 0, 2))
                im["swa_kT_h"] = np.ascontiguousarray(_halo(kT_lat, 1, lo, lo + TQ + 256).reshape(4, 64, TQ + 256).transpose(1, 0, 2))
                im["swa_v_h"] = _ones_col(_halo(v_lat, 0, lo, lo + TQ + 256), 4, 64)
                im["swa_kT_c"] = np.ascontiguousarray(fm[core][1024:1280, TQ:].reshape(4, 64, CTX).transpose(1, 0, 2))
                im["swa_v_c"] = _ones_col(tm[core][TQ:, :], 4, 64)
                im["sbias"] = _swa_bias(qr)
                im["sink"] = np.asarray(swa_sink[j], f32)
            ims.append(im)
        post = _run("post_even" if even else "post_odd", ims)
        xs = [np.asarray(r["x_out"]) for r in post]
        xn = [np.asarray(r["xn_out"]) for r in post]
        if _dbg is not None:
            _dbg[f"x{i}"] = xs
    out = np.zeros((2, SEQ, D), f32)
    for core in range(NCORES):
        b, qr = divmod(core, 4)
        out[b, qr * TQ:(qr + 1) * TQ] = xn[core][:TQ]
    return out


RG = [[0, 1, 2, 3], [4, 5, 6, 7]]


def emit_pre_f(kb, even, x_src, XS, csT, w_mod, b_mod, n1g_in, w_in, w_perm, cosT, sinT, fm_out, FMO, tm_out, TMO, modrow, MRO):
    B = kb.banks
    if even:
        NIN = 3072
        fm_blocks = [(c * 128, None) for c in range(8)] + [(1536 + c * 128, c) for c in range(8)]
        tm_groups = [(1024, 512, 8, 64, 0), (2560, 512, 4, 128, 520)]
    else:
        NIN = 1536
        fm_blocks = [(c * 128, c) for c in range(10)]
        tm_groups = [(1280, 256, 4, 64, 0)]
    NROPE = 128 * sum(1 for _, r in fm_blocks if r is not None)
    ident_f, IDF, ident, ID = kb.ident()
    win, WIN = kb.sb("win", [128, 8, NIN], BF16)
    wpm, WPM = kb.sb("wpm", [128, 8, NROPE], BF16)
    for c0 in range(0, NIN, 512):
        kb.dma("pool", win[:, :, c0:c0 + 512], w_in[:, c0:c0 + 512].rearrange("(k p) n -> p k n", p=128), writes=[WIN])
    for c0 in range(0, NROPE, 512):
        n = min(512, NROPE - c0)
        kb.dma("pool", wpm[:, :, c0:c0 + n], w_perm[:, c0:c0 + n].rearrange("(k p) n -> p k n", p=128), writes=[WPM])
    cs_f, CSF = kb.sb("cs_f", [128, 16], F32)
    cs_b, CSB = kb.sb("cs_b", [128, 16], BF16)
    csbc, CSBC = kb.sb("csbc", [128, 16, 128], BF16)
    kb.dma("sp", cs_f[:], csT, writes=[CSF])
    kb.op("act", lambda e: e.activation(out=cs_b[:], in_=cs_f[:], func=AF.Silu), reads=[CSF], writes=[CSB])
    kb.op("dve", lambda e: e.tensor_copy(out=csbc[:], in_=cs_b[:].unsqueeze(2).to_broadcast([128, 16, 128])),
          reads=[CSB], writes=[CSBC])
    n1g, N1G = kb.sb("n1g_sb", [128, D], F32)
    kb.dma("sp", n1g[:], n1g_in.unsqueeze(0).to_broadcast([128, D]), writes=[N1G])
    g1s, G1S = kb.sb("g1s", [128, 2, 2, D], F32)
    wmr = [kb.sb(f"wmr{i}", [128, 8, 512], BF16) for i in range(2)]
    bmr = [kb.sb(f"bmr{i}", [128, 512], F32) for i in range(2)]
    mtmp = [kb.sb(f"mtmp{i}", [128, 512], F32) for i in range(2)]
    for cgp in range(12):
        wt, WB = wmr[cgp % 2]
        bt, BB = bmr[cgp % 2]
        cols = slice(cgp * 512, (cgp + 1) * 512)
        kb.dma("pool", wt[:], w_mod[:, cols].rearrange("(k p) n -> p k n", p=128), writes=[WB])
        kb.dma("sp", bt[:], b_mod[cols].unsqueeze(0).to_broadcast([128, 512]), writes=[BB])
        chunk = cgp // 2
        half = cgp % 2
        for s in range(2):
            mb, MB = B[6 + s]
            def mmm(e, mb=mb, wt=wt, s=s):
                for k in range(8):
                    ins = e.matmul(mb[:], lhsT=csbc[:, s * 8 + k, :], rhs=wt[:, k, :], start=(k == 0), stop=(k == 7))
                return ins
            kb.op("pe", mmm, reads=[CSBC, WB], writes=[MB])
            if chunk < 2:
                dst = g1s[:, s, chunk, half * 512:(half + 1) * 512]
                kb.op("dve", lambda e, mb=mb, bt=bt, dst=dst: e.tensor_tensor(out=dst, in0=mb[:], in1=bt[:], op=ALU.add),
                      reads=[MB, BB], writes=[G1S])
                kb.dma("sp", modrow[s, chunk:chunk + 1, half * 512:(half + 1) * 512], dst[0:1, :], reads=[G1S], writes=[MRO])
            else:
                mt, MT = mtmp[s]
                kb.op("dve", lambda e, mb=mb, bt=bt, mt=mt: e.tensor_tensor(out=mt[:], in0=mb[:], in1=bt[:], op=ALU.add),
                      reads=[MB, BB], writes=[MT])
                kb.dma("sp", modrow[s, chunk:chunk + 1, half * 512:(half + 1) * 512], mt[0:1, :], reads=[MT], writes=[MRO])
    for s in range(2):
        kb.op("dve", lambda e, s=s: e.scalar_tensor_tensor(out=g1s[:, s, 1, :], in0=g1s[:, s, 1, :], scalar=1.0, in1=n1g[:],
                                                           op0=ALU.add, op1=ALU.mult), reads=[G1S, N1G], writes=[G1S])
    xt = [kb.sb(f"xt{i}", [128, D], F32) for i in range(2)]
    tmp, TMP = kb.sb("tmp", [128, D], F32)
    hx, HX = kb.sb("hx", [128, D], BF16)
    hxT, HXT = kb.sb("hxT", [128, 8, 512], BF16)
    sm, SM = kb.sb("sm", [128, 8], F32)
    cst = [kb.sb(f"cst{i}", [128, 512], F32) for i in range(2)]
    snt = [kb.sb(f"snt{i}", [128, 512], F32) for i in range(2)]
    r1, R1 = kb.sb("r1", [128, 512], F32)
    r2, R2 = kb.sb("r2", [128, 512], F32)
    fmo = [kb.sb(f"fmo{i}", [128, 512], BF16) for i in range(2)]
    tmo = [kb.sb(f"tmo{i}", [128, 520], BF16) for i in range(2)]
    for to, TO in tmo:
        kb.op("pool", lambda e, to=to: e.memset(to[:], 1.0), writes=[TO])
    groups = [(g * 4, 4, 0) for g in range(NTL // 4)] + [(NTL, NTC, 1)]
    xi = fi = ti = 0
    for gi, (t0, ntl, s) in enumerate(groups):
        NTOK = ntl * 128
        tok0 = t0 * 128
        ct, CT = cst[gi % 2]
        st, ST = snt[gi % 2]
        kb.dma("sp", ct[:, 0:NTOK], cosT[:, tok0:tok0 + NTOK], writes=[CT])
        kb.dma("sp", st[:, 0:NTOK], sinT[:, tok0:tok0 + NTOK], writes=[ST])
        for j in range(ntl):
            x_t, XB = xt[xi % 2]
            xi += 1
            rows = slice(tok0 + j * 128, tok0 + (j + 1) * 128)
            kb.dma("sp", x_t[:], x_src[rows, :], reads=[XS], writes=[XB])
            kb.op("act", lambda e, x_t=x_t: e.activation(out=tmp[:], in_=x_t[:], func=AF.Square, accum_out=sm[:, 0:1]),
                  reads=[XB], writes=[TMP, SM])
            rstd_ops(kb, sm, SM)
            kb.op("dve", lambda e, x_t=x_t, s=s: e.scalar_tensor_tensor(out=tmp[:], in0=x_t[:], scalar=sm[:, 1:2], in1=g1s[:, s, 1, :],
                                                                         op0=ALU.mult, op1=ALU.mult), reads=[XB, SM, G1S], writes=[TMP])
            kb.op("pool", lambda e, s=s: e.tensor_tensor(out=hx[:], in0=tmp[:], in1=g1s[:, s, 0, :], op=ALU.add),
                  reads=[TMP, G1S], writes=[HX])
            tb, TB = B[0]
            def tr(e, tb=tb):
                for k in range(8):
                    ins = e.transpose(tb.bitcast(BF16)[:, k * 128:(k + 1) * 128], hx[:, k * 128:(k + 1) * 128], ident[:])
                return ins
            kb.op("pe", tr, reads=[HX, ID], writes=[TB])
            kb.op("act", lambda e, tb=tb, j=j: e.copy(out=hxT[:, :, j * 128:(j + 1) * 128],
                                                      in_=tb.bitcast(BF16)[:, 0:1024].rearrange("p (k t) -> p k t", k=8)),
                  reads=[TB], writes=[HXT])
        for bi, (c0, ridx) in enumerate(fm_blocks):
            pa, PA = B[1 + bi % 2]
            def mma(e, pa=pa, c0=c0, NTOK=NTOK):
                for k in range(8):
                    ins = e.matmul(pa[:, 0:NTOK], lhsT=win[:, k, c0:c0 + 128], rhs=hxT[:, k, 0:NTOK], start=(k == 0), stop=(k == 7))
                return ins
            kb.op("pe", mma, reads=[WIN, HXT], writes=[PA])
            fo, FO = fmo[fi % 2]
            fi += 1
            if ridx is None:
                kb.op("act", lambda e, pa=pa, fo=fo, NTOK=NTOK: e.copy(out=fo[:, 0:NTOK], in_=pa[:, 0:NTOK]), reads=[PA], writes=[FO])
            else:
                pb, PB = B[3 + bi % 2]
                def mmb(e, pb=pb, ridx=ridx, NTOK=NTOK):
                    for k in range(8):
                        ins = e.matmul(pb[:, 0:NTOK], lhsT=wpm[:, k, ridx * 128:(ridx + 1) * 128], rhs=hxT[:, k, 0:NTOK],
                                       start=(k == 0), stop=(k == 7))
                    return ins
                kb.op("pe", mmb, reads=[WPM, HXT], writes=[PB])
                kb.op("dve", lambda e, pa=pa, ct=ct, NTOK=NTOK: e.tensor_tensor(out=r1[:, 0:NTOK], in0=pa[:, 0:NTOK], in1=ct[:, 0:NTOK], op=ALU.mult),
                      reads=[PA, CT], writes=[R1])
                kb.op("dve", lambda e, pb=pb, st=st, NTOK=NTOK: e.tensor_tensor(out=r2[:, 0:NTOK], in0=pb[:, 0:NTOK], in1=st[:, 0:NTOK], op=ALU.mult),
                      reads=[PB, ST], writes=[R2])
                kb.op("pool", lambda e, fo=fo, NTOK=NTOK: e.tensor_tensor(out=fo[:, 0:NTOK], in0=r1[:, 0:NTOK], in1=r2[:, 0:NTOK], op=ALU.add),
                      reads=[R1, R2], writes=[FO])
            kb.dma("sp", fm_out[bi * 128:(bi + 1) * 128, tok0:tok0 + NTOK], fo[:, 0:NTOK], reads=[FO], writes=[FMO])
        for j in range(ntl):
            for (c0, ncol, nh, dv, oc) in tm_groups:
                pt, PT = B[5 + ti % 2]
                to, TO = tmo[ti % 2]
                ti += 1
                def mmt(e, pt=pt, c0=c0, ncol=ncol, j=j):
                    for k in range(8):
                        ins = e.matmul(pt[:, 0:ncol], lhsT=hxT[:, k, j * 128:(j + 1) * 128], rhs=win[:, k, c0:c0 + ncol],
                                       start=(k == 0), stop=(k == 7))
                    return ins
                kb.op("pe", mmt, reads=[HXT, WIN], writes=[PT])
                wdt = nh * (dv + 1)
                kb.op("act", lambda e, pt=pt, to=to, ncol=ncol, nh=nh, dv=dv, wdt=wdt: e.copy(
                    out=to[:, 0:wdt].rearrange("p (h c) -> p h c", c=dv + 1)[:, :, 0:dv],
                    in_=pt[:, 0:ncol].rearrange("p (h c) -> p h c", c=dv)), reads=[PT], writes=[TO])
                rows = slice(tok0 + j * 128, tok0 + (j + 1) * 128)
                kb.dma("sp", tm_out[rows, oc:oc + wdt], to[:, 0:wdt], reads=[TO], writes=[TMO])


NA_NBMAX = 12


def _na_blocklist(qt):
    if qt == 0:
        return 0, 4, [("tail", 0), ("tail", 1)]
    if qt == 1:
        return 0, 4, [("tail", 1)]
    if qt == NTL - 2:
        return TQ - 512, 4, [("head", 0)]
    if qt == NTL - 1:
        return TQ - 512, 4, [("head", 0), ("head", 1)]
    return qt * 128 - 256, 5, []


def emit_attn_even_f(kb, fmT, FMT, tmv, TMV, dk_recv, DKR, dv_recv, DVR, nbk_recv, NBKR, nbv_recv, NBVR,
                     nbias, lam_in, subg_in, lami_in, ao, AO):
    B = kb.banks
    sc = 64 ** -0.5
    lm, LM = kb.sb("lm", [128, 4, 64], F32)
    lms, LMS = kb.sb("lms", [128, 16], F32)
    subg, SUBG = kb.sb("subg_sb", [128, 128], F32)
    kb.dma("sp", lm[:].rearrange("p a b -> p (a b)"), lam_in.rearrange("a b -> (a b)").unsqueeze(0).to_broadcast([128, 256]), writes=[LM])
    kb.dma("sp", subg[:], subg_in.unsqueeze(0).to_broadcast([128, 128]), writes=[SUBG])
    kb.dma("sp", lms[:, 8:10], lami_in, writes=[LMS])
    kb.op("dve", lambda e: e.tensor_tensor(out=lm[:, 0, :], in0=lm[:, 0, :], in1=lm[:, 1, :], op=ALU.mult), reads=[LM], writes=[LM])
    kb.op("dve", lambda e: e.tensor_tensor(out=lm[:, 2, :], in0=lm[:, 2, :], in1=lm[:, 3, :], op=ALU.mult), reads=[LM], writes=[LM])
    kb.op("dve", lambda e: e.tensor_reduce(out=lms[:, 0:1], in_=lm[:, 0, :], axis=AX.X, op=ALU.add), reads=[LM], writes=[LMS])
    kb.op("dve", lambda e: e.tensor_reduce(out=lms[:, 1:2], in_=lm[:, 2, :], axis=AX.X, op=ALU.add), reads=[LM], writes=[LMS])
    kb.op("act", lambda e: e.activation(out=lms[:, 2:4], in_=lms[:, 0:2], func=AF.Exp), reads=[LMS], writes=[LMS])
    kb.op("dve", lambda e: e.tensor_tensor(out=lms[:, 4:5], in0=lms[:, 2:3], in1=lms[:, 3:4], op=ALU.subtract), reads=[LMS], writes=[LMS])
    kb.op("dve", lambda e: e.tensor_tensor(out=lms[:, 5:6], in0=lms[:, 4:5], in1=lms[:, 8:9], op=ALU.add), reads=[LMS], writes=[LMS])
    kb.op("dve", lambda e: e.tensor_scalar(out=lms[:, 6:7], in0=lms[:, 5:6], scalar1=-1.0, scalar2=None, op0=ALU.mult), reads=[LMS], writes=[LMS])
    kb.op("dve", lambda e: e.tensor_scalar(out=subg[:], in0=subg[:], scalar1=lms[:, 9:10], scalar2=None, op0=ALU.mult), reads=[SUBG, LMS], writes=[SUBG])

    naqT = fmT[0:512, :]
    nakT = fmT[512:1024, :]
    kcT, KCT = kb.sb("na_kcT", [128, 4, CTX], BF16)
    vca, VCA = kb.sb("na_vca", [128, 2, 8, 65], BF16)
    kb.dma("sp", kcT[:], nakT[:, TQ:TALL].rearrange("(a p) n -> p a n", p=128), reads=[FMT], writes=[KCT])
    kb.dma("sp", vca[:].rearrange("p b h c -> p b (h c)"), tmv[TQ:TALL, 0:520].rearrange("(b p) c -> p b c", p=128), reads=[TMV], writes=[VCA])
    kts = [kb.sb(f"na_kt{i}", [128, 4, NA_NBMAX * 128], BF16) for i in range(2)]
    vts = [kb.sb(f"na_vt{i}", [128, NA_NBMAX, 8, 65], BF16) for i in range(2)]
    qts = [kb.sb(f"na_qt{i}", [128, 4, 128], BF16) for i in range(2)]
    bts = [kb.sb(f"na_bt{i}", [128, NA_NBMAX * 128], F32) for i in range(2)]
    sts = [kb.sb(f"na_st{i}", [128, NA_NBMAX * 128], F32) for i in range(2)]
    pts = [kb.sb(f"na_pt{i}", [128, (NA_NBMAX + 2) * 128], BF16) for i in range(2)]
    aot = [kb.sb(f"na_ao{i}", [128, 512], BF16) for i in range(2)]
    rc, RC = kb.sb("na_rc", [128, 8], F32)
    hi = 0
    bk = 0
    for qt in range(NT):
        isctx = qt >= NTL
        q_t, QB = qts[qt % 2]
        kb.dma("sp", q_t[:], naqT[:, qt * 128:(qt + 1) * 128].rearrange("(a p) n -> p a n", p=128), reads=[FMT], writes=[QB])
        nb = 0
        if not isctx:
            k_t, KB_ = kts[qt % 2]
            v_t, VB = vts[qt % 2]
            o0, onb, cands = _na_blocklist(qt)
            kb.dma("sp", k_t[:, :, 0:onb * 128], nakT[:, o0:o0 + onb * 128].rearrange("(a p) n -> p a n", p=128), reads=[FMT], writes=[KB_])
            kb.dma("sp", v_t[:, 0:onb].rearrange("p b h c -> p b (h c)"), tmv[o0:o0 + onb * 128, 0:520].rearrange("(b p) c -> p b c", p=128),
                   reads=[TMV], writes=[VB])
            nb = onb
            ncb = len(cands)
            if ncb:
                which = cands[0][0]
                cb0 = cands[0][1]
                col0 = (0 if which == "tail" else 256) + cb0 * 128
                for r in range(4):
                    kb.dma("sp", k_t[:, :, nb * 128:(nb + ncb) * 128],
                           nbk_recv[r * 512:(r + 1) * 512, col0:col0 + ncb * 128].rearrange("(a p) n -> p a n", p=128), reads=[NBKR], writes=[KB_])
                    kb.dma("sp", v_t[:, nb:nb + ncb].rearrange("p b h c -> p b (h c)"),
                           nbv_recv[r * 512 + col0:r * 512 + col0 + ncb * 128, :].rearrange("(b p) c -> p b c", p=128), reads=[NBVR], writes=[VB])
                    nb += ncb
            slot = 0 if qt == 0 else 1 if qt == 1 else 3 if qt == NTL - 2 else 4 if qt == NTL - 1 else 2
        a_t, AB = aot[qt % 2]
        for h in range(8):
            a, off = h // 2, (h % 2) * 64
            st_, STB = sts[hi % 2]
            pt_, PTB = pts[hi % 2]
            acc, ACC = B[4 + (h // 4)]
            hi += 1
            if not isctx:
                b_t, BB = bts[hi % 2]
                kb.dma("sp", b_t[:, 0:nb * 128], nbias[slot, h, :, 0:nb * 128], writes=[BB])
                for c0 in range(0, nb, 4):
                    cn = min(4, nb - c0)
                    ps, PS = B[bk % 4]
                    bk += 1
                    def mms(e, ps=ps, k_t=k_t, q_t=q_t, a=a, off=off, c0=c0, cn=cn):
                        for i in range(cn):
                            ins = e.matmul(ps[:, i * 128:(i + 1) * 128], lhsT=k_t[off:off + 64, a, (c0 + i) * 128:(c0 + i + 1) * 128],
                                           rhs=q_t[off:off + 64, a, :], start=True, stop=True)
                        return ins
                    kb.op("pe", mms, reads=[KB_, QB], writes=[PS])
                    kb.op("dve", lambda e, ps=ps, st_=st_, b_t=b_t, c0=c0, cn=cn: e.scalar_tensor_tensor(
                        out=st_[:, c0 * 128:(c0 + cn) * 128], in0=ps[:, 0:cn * 128], scalar=sc, in1=b_t[:, c0 * 128:(c0 + cn) * 128],
                        op0=ALU.mult, op1=ALU.add), reads=[PS, BB], writes=[STB])
                kb.op("act", lambda e, st_=st_, pt_=pt_, nb=nb: e.activation(out=pt_[:, 0:nb * 128], in_=st_[:, 0:nb * 128], func=AF.Exp),
                      reads=[STB], writes=[PTB])
            ps, PS = B[bk % 4]
            bk += 1
            def mmc(e, ps=ps, q_t=q_t, a=a, off=off):
                for cb in range(2):
                    ins = e.matmul(ps[:, cb * 128:(cb + 1) * 128], lhsT=kcT[off:off + 64, a, cb * 128:(cb + 1) * 128],
                                   rhs=q_t[off:off + 64, a, :], start=True, stop=True)
                return ins
            kb.op("pe", mmc, reads=[QB, KCT], writes=[PS])
            kb.op("act", lambda e, ps=ps, pt_=pt_, nb=nb: e.activation(out=pt_[:, nb * 128:(nb + 2) * 128], in_=ps[:, 0:256], func=AF.Exp, scale=sc),
                  reads=[PS], writes=[PTB])
            if not isctx:
                def mmv(e, acc=acc, pt_=pt_, v_t=v_t, h=h, nb=nb):
                    o = acc[:, (h % 4) * 65:(h % 4) * 65 + 65]
                    for blk in range(nb):
                        e.matmul(o, lhsT=pt_[:, blk * 128:(blk + 1) * 128], rhs=v_t[:, blk, h, :], start=(blk == 0), stop=False)
                    for cb in range(2):
                        ins = e.matmul(o, lhsT=pt_[:, (nb + cb) * 128:(nb + cb + 1) * 128], rhs=vca[:, cb, h, :], start=False, stop=(cb == 1))
                    return ins
                kb.op("pe", mmv, reads=[PTB, VB, VCA], writes=[ACC])
            else:
                def mmv(e, acc=acc, pt_=pt_, h=h):
                    o = acc[:, (h % 4) * 65:(h % 4) * 65 + 65]
                    for cb in range(2):
                        ins = e.matmul(o, lhsT=pt_[:, cb * 128:(cb + 1) * 128], rhs=vca[:, cb, h, :], start=(cb == 0), stop=(cb == 1))
                    return ins
                kb.op("pe", mmv, reads=[PTB, VCA], writes=[ACC])
            if h % 4 == 3:
                g4 = h // 4
                av = acc[:, 0:260].rearrange("p (h c) -> p h c", c=65)
                kb.op("dve", lambda e, av=av, g4=g4: e.reciprocal(out=rc[:, g4 * 4:g4 * 4 + 4], in_=av[:, :, 64]), reads=[ACC], writes=[RC])
                kb.op("dve", lambda e, av=av, g4=g4, a_t=a_t: e.tensor_tensor(
                    out=a_t[:, g4 * 256:(g4 + 1) * 256].rearrange("p (h d) -> p h d", d=64), in0=av[:, :, 0:64],
                    in1=rc[:, g4 * 4:g4 * 4 + 4].unsqueeze(2).to_broadcast([128, 4, 64]), op=ALU.mult), reads=[ACC, RC], writes=[AB])
        kb.dma("sp", ao[qt * 128:(qt + 1) * 128, 0:512], a_t[:], reads=[AB], writes=[AO])

    NBLK = (CTX + SEQ) // 128
    dk, DK = kb.sb("d_k", [128, CTX + SEQ], BF16)
    dva, DVA = kb.sb("d_va", [128, NBLK, 129], BF16)
    dq, DQ = kb.sb("d_q", [128, TALL], BF16)
    dpt = [kb.sb(f"d_pt{i}", [128, 512], BF16) for i in range(4)]
    o0_, O0 = kb.sb("d_o0", [128, 128], F32)
    o1, O1 = kb.sb("d_o1", [128, 128], F32)
    osq, OSQ = kb.sb("d_osq", [128, 128], F32)
    dsm, DSM = kb.sb("d_sm", [128, 8], F32)
    dob = [kb.sb(f"d_ob{i}", [128, 128], BF16) for i in range(2)]
    si = 0
    oi = 0
    for h in range(4):
        kb.dma("sp", dk[:, 0:CTX], fmT[1536 + h * 128:1536 + (h + 1) * 128, TQ:TALL], reads=[FMT], writes=[DK])
        for r in range(4):
            for k in range(4):
                kb.dma("sp", dk[:, CTX + r * TQ + k * 1024:CTX + r * TQ + (k + 1) * 1024],
                       dk_recv[k][0][r * 512 + h * 128:r * 512 + (h + 1) * 128, :], reads=[dk_recv[k][1]], writes=[DK])
        kb.dma("sp", dva[:, 0:2, :], tmv[TQ:TALL, 520 + h * 129:520 + (h + 1) * 129].rearrange("(b p) d -> p b d", p=128), reads=[TMV], writes=[DVA])
        for r in range(4):
            for k in range(8):
                b0 = 2 + (r * TQ + k * 512) // 128
                kb.dma("sp", dva[:, b0:b0 + 4, :], dv_recv[k][0][r * 512:(r + 1) * 512, h * 129:(h + 1) * 129].rearrange("(b p) d -> p b d", p=128),
                       reads=[dv_recv[k][1]], writes=[DVA])
        kb.dma("sp", dq[:], fmT[1024 + h * 128:1024 + (h + 1) * 128, :], reads=[FMT], writes=[DQ])
        qgroups = [(g * 256, 256, NBLK) for g in range(TQ // 256)] + [(TQ, CTX, CTX // 128)]
        for (q0, nq, nblk) in qgroups:
            nqs = nq // 128
            steps = [(bp, m) for bp in range(nblk // 2) for m in range(2)]
            nbp = nblk // 2
            LOOK = 3
            bufs = {}
            for idx in range(len(steps) + LOOK):
                if idx < len(steps):
                    bp, m = steps[idx]
                    ps, PS = B[(0, 1, 2, 7)[si % 4]]
                    pt_, PTB = dpt[si % 4]
                    si += 1
                    bufs[idx] = (pt_, PTB)
                    def mms2(e, ps=ps, m=m, bp=bp, q0=q0, nq=nq):
                        for b2 in range(2):
                            blk = 2 * bp + b2
                            ins = e.matmul(ps[:, b2 * nq:(b2 + 1) * nq], lhsT=dk[m * 64:(m + 1) * 64, blk * 128:(blk + 1) * 128],
                                           rhs=dq[m * 64:(m + 1) * 64, q0:q0 + nq], start=True, stop=True)
                        return ins
                    kb.op("pe", mms2, reads=[DK, DQ], writes=[PS])
                    kb.op("act", lambda e, ps=ps, pt_=pt_, nq=nq: e.activation(out=pt_[:, 0:2 * nq], in_=ps[:, 0:2 * nq], func=AF.Exp, scale=sc),
                          reads=[PS], writes=[PTB])
                if idx - LOOK >= 0:
                    bp, m = steps[idx - LOOK]
                    pt_, PTB = bufs.pop(idx - LOOK)
                    def mmv(e, pt_=pt_, m=m, bp=bp, nqs=nqs, nbp=nbp, nq=nq):
                        for b2 in range(2):
                            for qs in range(nqs):
                                acc = B[3 + qs * 2 + m][0]
                                ins = e.matmul(acc[:, 0:129], lhsT=pt_[:, b2 * nq + qs * 128:b2 * nq + (qs + 1) * 128], rhs=dva[:, 2 * bp + b2, :],
                                               start=(bp == 0 and b2 == 0), stop=(bp == nbp - 1 and b2 == 1))
                        return ins
                    kb.op("pe", mmv, reads=[PTB, DVA], writes=[B[3 + qs * 2 + m][1] for qs in range(nqs)])
            for qs in range(nqs):
                (a0, A0), (a1, A1) = [(B[3 + qs * 2 + m][0][:, 0:129], B[3 + qs * 2 + m][1]) for m in range(2)]
                kb.op("dve", lambda e, a0=a0: e.reciprocal(out=dsm[:, 0:1], in_=a0[:, 128:129]), reads=[A0], writes=[DSM])
                kb.op("dve", lambda e, a1=a1: e.reciprocal(out=dsm[:, 1:2], in_=a1[:, 128:129]), reads=[A1], writes=[DSM])
                kb.op("dve", lambda e: e.tensor_tensor(out=dsm[:, 1:2], in0=dsm[:, 1:2], in1=lms[:, 6:7], op=ALU.mult), reads=[DSM, LMS], writes=[DSM])
                kb.op("dve", lambda e, a0=a0: e.tensor_scalar(out=o0_[:], in0=a0[:, 0:128], scalar1=dsm[:, 0:1], scalar2=None, op0=ALU.mult),
                      reads=[A0, DSM], writes=[O0])
                kb.op("dve", lambda e, a1=a1: e.scalar_tensor_tensor(out=o1[:], in0=a1[:, 0:128], scalar=dsm[:, 1:2], in1=o0_[:],
                                                                   op0=ALU.mult, op1=ALU.add), reads=[A1, DSM, O0], writes=[O1])
                kb.op("act", lambda e: e.activation(out=osq[:], in_=o1[:], func=AF.Square, accum_out=dsm[:, 2:3]), reads=[O1], writes=[OSQ, DSM])
                kb.op("dve", lambda e: e.tensor_scalar(out=dsm[:, 3:4], in0=dsm[:, 2:3], scalar1=1.0 / 128, scalar2=EPS, op0=ALU.mult, op1=ALU.add),
                      reads=[DSM], writes=[DSM])
                kb.op("act", lambda e: e.activation(out=dsm[:, 4:5], in_=dsm[:, 3:4], func=AF.Sqrt), reads=[DSM], writes=[DSM])
                kb.op("dve", lambda e: e.reciprocal(out=dsm[:, 5:6], in_=dsm[:, 4:5]), reads=[DSM], writes=[DSM])
                ob, OB = dob[oi % 2]
                oi += 1
                kb.op("dve", lambda e, ob=ob: e.scalar_tensor_tensor(out=ob[:], in0=o1[:], scalar=dsm[:, 5:6], in1=subg[:], op0=ALU.mult, op1=ALU.mult),
                      reads=[O1, DSM, SUBG], writes=[OB])
                r0 = q0 + qs * 128
                kb.dma("sp", ao[r0:r0 + 128, 512 + h * 128:512 + (h + 1) * 128], ob[:], reads=[OB], writes=[AO])


def emit_attn_odd_f(kb, fmT, FMT, tmv, TMV, sbk_recv, SBKR, sbv_recv, SBVR, sbias, sink_in, ao, AO):
    B = kb.banks
    sc = 64 ** -0.5
    snk, SNK = kb.sb("snk", [128, 16], F32)
    kb.dma("sp", snk[:], sink_in.unsqueeze(0).to_broadcast([128, 16]), writes=[SNK])
    kb.op("act", lambda e: e.activation(out=snk[:], in_=snk[:], func=AF.Exp), reads=[SNK], writes=[SNK])
    kT, KT = kb.sb("s_kT", [64, 4, TQ], BF16)
    kb.dma("sp", kT[:], fmT[1024:1280, 0:TQ].rearrange("(n d) t -> d n t", d=64), reads=[FMT], writes=[KT])
    kcT, KCT = kb.sb("s_kcT", [64, 4, CTX], BF16)
    kb.dma("sp", kcT[:], fmT[1024:1280, TQ:TALL].rearrange("(n d) t -> d n t", d=64), reads=[FMT], writes=[KCT])
    ck, CK = kb.sb("s_ck", [64, 4, 4, 256], BF16)
    for r in range(4):
        kb.dma("sp", ck[:, r], sbk_recv[r * 256:(r + 1) * 256, :].rearrange("(n d) t -> d n t", d=64), reads=[SBKR], writes=[CK])
    va, VA = kb.sb("s_va", [128, NTL, 4, 65], BF16)
    kb.dma("sp", va[:].rearrange("p b h c -> p b (h c)"), tmv[0:TQ, 0:260].rearrange("(b p) c -> p b c", p=128), reads=[TMV], writes=[VA])
    vca, VCA = kb.sb("s_vca", [128, 2, 4, 65], BF16)
    kb.dma("sp", vca[:].rearrange("p b h c -> p b (h c)"), tmv[TQ:TALL, 0:260].rearrange("(b p) c -> p b c", p=128), reads=[TMV], writes=[VCA])
    cv, CV = kb.sb("s_cv", [128, 8, 4, 65], BF16)
    kb.dma("sp", cv[:].rearrange("p b h c -> p b (h c)"), sbv_recv.rearrange("(b p) c -> p b c", p=128), reads=[SBVR], writes=[CV])
    sb_, SBB = kb.sb("s_bias", [128, 3, 768], F32)
    kb.dma("sp", sb_[:], sbias.rearrange("s p n -> p s n"), writes=[SBB])
    qts = [kb.sb(f"s_q{i}", [64, 16, 128], BF16) for i in range(2)]
    sts = [kb.sb(f"s_st{i}", [128, 512], F32) for i in range(2)]
    pts = [kb.sb(f"s_pt{i}", [128, 8, 512], BF16) for i in range(2)]
    aot = [kb.sb(f"s_ao{i}", [128, D], BF16) for i in range(2)]
    den, DEN = kb.sb("s_den", [128, 8], F32)
    si = 0
    gi = 0
    for qt in range(NT):
        isctx = qt >= NTL
        q_t, QB = qts[qt % 2]
        kb.dma("sp", q_t[:], fmT[0:1024, qt * 128:(qt + 1) * 128].rearrange("(h d) t -> d h t", d=64), reads=[FMT], writes=[QB])
        a_t, AB = aot[qt % 2]
        if isctx:
            nbl = []
            slot = 1
        elif qt == 0:
            nbl = [("own", 0), ("own", 1)] + [("cand", r, 0) for r in range(4)]
            slot = 0
        elif qt == NTL - 1:
            nbl = [("own", NTL - 2), ("own", NTL - 1)] + [("cand", r, 1) for r in range(4)]
            slot = 2
        else:
            nbl = [("own", qt - 1), ("own", qt), ("own", qt + 1)]
            slot = 1
        nnb = len(nbl)
        for n in range(4):
            pt_, PTB = pts[gi % 2]
            acc, ACC = B[4 + gi % 2]
            gi += 1
            for bi, bl in enumerate(nbl + [("ctx", 0), ("ctx", 1)]):
                ps, PS = B[si % 3]
                st_, STB = sts[si % 2]
                si += 1
                if bl[0] == "own":
                    lhsT = kT[:, n, bl[1] * 128:(bl[1] + 1) * 128]
                    rd = [KT, QB]
                elif bl[0] == "cand":
                    lhsT = ck[:, bl[1], n, bl[2] * 128:(bl[2] + 1) * 128]
                    rd = [CK, QB]
                else:
                    lhsT = kcT[:, n, bl[1] * 128:(bl[1] + 1) * 128]
                    rd = [KCT, QB]
                kb.op("pe", lambda e, ps=ps, lhsT=lhsT, n=n, q_t=q_t: e.matmul(ps[:], lhsT=lhsT, rhs=q_t[:, n * 4:(n + 1) * 4, :], start=True, stop=True),
                      reads=rd, writes=[PS])
                if bl[0] != "ctx":
                    kb.op("dve", lambda e, ps=ps, st_=st_, bi=bi, slot=slot: e.scalar_tensor_tensor(
                        out=st_[:].rearrange("p (g q) -> p g q", g=4), in0=ps[:].rearrange("p (g q) -> p g q", g=4), scalar=sc,
                        in1=sb_[:, slot, bi * 128:(bi + 1) * 128].unsqueeze(1).to_broadcast([128, 4, 128]), op0=ALU.mult, op1=ALU.add),
                        reads=[PS, SBB], writes=[STB])
                    kb.op("act", lambda e, st_=st_, pt_=pt_, bi=bi: e.activation(out=pt_[:, bi, :], in_=st_[:], func=AF.Exp), reads=[STB], writes=[PTB])
                else:
                    kb.op("act", lambda e, ps=ps, pt_=pt_, bi=bi: e.activation(out=pt_[:, bi, :], in_=ps[:], func=AF.Exp, scale=sc),
                          reads=[PS], writes=[PTB])
            allb = nbl + [("ctx", 0), ("ctx", 1)]
            def mmv(e, acc=acc, pt_=pt_, n=n, allb=allb):
                for g in range(4):
                    o = acc[:, g * 65:(g + 1) * 65]
                    for i, bl in enumerate(allb):
                        if bl[0] == "own":
                            rhs = va[:, bl[1], n, :]
                        elif bl[0] == "cand":
                            rhs = cv[:, bl[1] * 2 + bl[2], n, :]
                        else:
                            rhs = vca[:, bl[1], n, :]
                        ins = e.matmul(o, lhsT=pt_[:, i, g * 128:(g + 1) * 128], rhs=rhs, start=(i == 0), stop=(i == len(allb) - 1))
                return ins
            kb.op("pe", mmv, reads=[PTB, VA, VCA, CV], writes=[ACC])
            av = acc[:, 0:260].rearrange("p (g c) -> p g c", c=65)
            kb.op("dve", lambda e, av=av, n=n: e.tensor_tensor(out=den[:, 0:4], in0=av[:, :, 64], in1=snk[:, n * 4:(n + 1) * 4], op=ALU.add),
                  reads=[ACC, SNK], writes=[DEN])
            kb.op("dve", lambda e: e.reciprocal(out=den[:, 4:8], in_=den[:, 0:4]), reads=[DEN], writes=[DEN])
            kb.op("dve", lambda e, av=av, n=n, a_t=a_t: e.tensor_tensor(
                out=a_t[:, n * 256:(n + 1) * 256].rearrange("p (g d) -> p g d", d=64), in0=av[:, :, 0:64],
                in1=den[:, 4:8].unsqueeze(2).to_broadcast([128, 4, 64]), op=ALU.mult), reads=[ACC, DEN], writes=[AB])
        kb.dma("sp", ao[qt * 128:(qt + 1) * 128, :], a_t[:], reads=[AB], writes=[AO])


def build_fused(nlayers=4):
    kb = KB()
    kb.psum_banks()
    nc = kb.nc

    def scratch(name, shape, dt=BF16):
        return nc.dram_tensor(name, list(shape), dt).ap(), Buf(name)

    x_ext = kb.din("x", [TALL, D])
    csT = kb.din("csT", [128, 16])
    cosT = kb.din("cosT", [128, TALL])
    sinT = kb.din("sinT", [128, TALL])
    fg = kb.din("fg", [D])
    xn_out, XN = kb.dout("xn_out", [TALL, D])
    xbufs = [scratch(f"xs{i}", [TALL, D], F32) for i in range(2)]
    ao, AO = scratch("ao_scr", [TALL, D])
    fmT, FMT = scratch("fmT", [2048, TALL])
    tmv, TMV = scratch("tmv", [TALL, 1036])
    dk_send = [scratch(f"dk_send{k}", [512, 1024]) for k in range(4)]
    dk_recv = [scratch(f"dk_recv{k}", [2048, 1024]) for k in range(4)]
    dv_send = [scratch(f"dv_send{k}", [512, 516]) for k in range(8)]
    dv_recv = [scratch(f"dv_recv{k}", [2048, 516]) for k in range(8)]
    nbk_send, NBKS = scratch("nbk_send", [512, 512])
    nbk_recv, NBKR = scratch("nbk_recv", [2048, 512])
    nbv_send, NBVS = scratch("nbv_send", [512, 520])
    nbv_recv, NBVR = scratch("nbv_recv", [2048, 520])
    sbk_send, SBKS = scratch("sbk_send", [256, 256])
    sbk_recv, SBKR = scratch("sbk_recv", [1024, 256])
    sbv_send, SBVS = scratch("sbv_send", [256, 260])
    sbv_recv, SBVR = scratch("sbv_recv", [1024, 260])
    x_src, XS = x_ext, Buf("x_ext")
    lw = []
    for i in range(nlayers):
        d = {"w_out": kb.din(f"w_out{i}", [D, D]), "wq": kb.din(f"wq{i}", [D, 2048]),
             "uT": kb.din(f"uT{i}", [D, 16384]), "v": kb.din(f"v{i}", [16384, D])}
        d["conv"] = {"wout": scratch(f"woutb{i}", [2, 128, 4096]), "wq": scratch(f"wqb{i}", [4, 128, 4096]),
                     "uT": scratch(f"uTb{i}", [32, 128, 4096]), "v": scratch(f"vb{i}", [32, 128, 4096])}
        lw.append(d)

    def convert(i):
        d = lw[i]
        c = d["conv"]
        for hf in range(2):
            kb.dma("pool", c["wout"][0][hf].rearrange("p (k n) -> p k n", k=8),
                   d["w_out"][:, hf * 512:(hf + 1) * 512].rearrange("(k p) n -> p k n", p=128), writes=[c["wout"][1]])
        for g in range(4):
            kb.dma("pool", c["wq"][0][g].rearrange("p (k n) -> p k n", k=8),
                   d["wq"][:, g * 512:(g + 1) * 512].rearrange("(k p) n -> p k n", p=128), writes=[c["wq"][1]])
        for cg in range(32):
            kb.dma("pool", c["uT"][0][cg].rearrange("p (k n) -> p k n", k=8),
                   d["uT"][:, cg * 512:(cg + 1) * 512].rearrange("(k p) n -> p k n", p=128), writes=[c["uT"][1]])
            kb.dma("pool", c["v"][0][cg].rearrange("p (c d) -> p c d", c=4),
                   d["v"][cg * 512:(cg + 1) * 512, :].rearrange("(c p) d -> p c d", p=128), writes=[c["v"][1]])

    for i in range(nlayers):
        even = (i % 2 == 0)
        j = i // 2
        NIN = 3072 if even else 1536
        NROPE = 1024 if even else 1280
        w_mod = kb.din(f"w_mod{i}", [D, 6 * D])
        b_mod = kb.din(f"b_mod{i}", [6 * D])
        n1g = kb.din(f"n1g{i}", [D])
        n2g = kb.din(f"n2g{i}", [D])
        w_in = kb.din(f"w_in{i}", [D, NIN])
        w_perm = kb.din(f"w_perm{i}", [D, NROPE])
        w_out, wq, uT, v = lw[i]["w_out"], lw[i]["wq"], lw[i]["uT"], lw[i]["v"]
        keysT = kb.din(f"keysT{i}", [128, 16, 128])
        modrow, MRO = scratch(f"modrow{i}", [2, 6, D], F32)
        with kb.scope(f"L{i}a_"):
            if i == 0 and USE_CONV:
                convert(0)
            emit_pre_f(kb, even, x_src, XS, csT, w_mod, b_mod, n1g, w_in, w_perm, cosT, sinT, fmT, FMT, tmv, TMV, modrow, MRO)
        if even:
            nbias = kb.din(f"nbias{j}", [5, 8, 128, NA_NBMAX * 128])
            lam = kb.din(f"lam{j}", [4, 64])
            subg = kb.din(f"subg{j}", [128])
            lami = kb.din(f"lam_init{j}", [128, 2])
            for k in range(4):
                kb.dma("sp", dk_send[k][0], fmT[1536:2048, k * 1024:(k + 1) * 1024], reads=[FMT], writes=[dk_send[k][1]])
            for k in range(8):
                kb.dma("sp", dv_send[k][0], tmv[k * 512:(k + 1) * 512, 520:1036], reads=[TMV], writes=[dv_send[k][1]])
            kb.dma("sp", nbk_send[:, 0:256], fmT[512:1024, TQ - 256:TQ], reads=[FMT], writes=[NBKS])
            kb.dma("sp", nbk_send[:, 256:512], fmT[512:1024, 0:256], reads=[FMT], writes=[NBKS])
            kb.dma("sp", nbv_send[0:256, :], tmv[TQ - 256:TQ, 0:520], reads=[TMV], writes=[NBVS])
            kb.dma("sp", nbv_send[256:512, :], tmv[0:256, 0:520], reads=[TMV], writes=[NBVS])
            for k in range(4):
                kb.cc("AllGather", RG, dk_send[k][0], dk_recv[k][0], reads=[dk_send[k][1]], writes=[dk_recv[k][1]])
            for k in range(8):
                kb.cc("AllGather", RG, dv_send[k][0], dv_recv[k][0], reads=[dv_send[k][1]], writes=[dv_recv[k][1]])
            kb.cc("AllGather", RG, nbk_send, nbk_recv, reads=[NBKS], writes=[NBKR])
            kb.cc("AllGather", RG, nbv_send, nbv_recv, reads=[NBVS], writes=[NBVR])
            with kb.scope(f"L{i}b_"):
                emit_attn_even_f(kb, fmT, FMT, tmv, TMV, dk_recv, None, dv_recv, None, nbk_recv, NBKR, nbv_recv, NBVR,
                                 nbias, lam, subg, lami, ao, AO)
        else:
            sbias = kb.din(f"sbias{j}", [3, 128, 768])
            sink = kb.din(f"sink{j}", [16])
            kb.dma("sp", sbk_send[:, 0:128], fmT[1024:1280, TQ - 128:TQ], reads=[FMT], writes=[SBKS])
            kb.dma("sp", sbk_send[:, 128:256], fmT[1024:1280, 0:128], reads=[FMT], writes=[SBKS])
            kb.dma("sp", sbv_send[0:128, :], tmv[TQ - 128:TQ, 0:260], reads=[TMV], writes=[SBVS])
            kb.dma("sp", sbv_send[128:256, :], tmv[0:128, 0:260], reads=[TMV], writes=[SBVS])
            kb.cc("AllGather", RG, sbk_send, sbk_recv, reads=[SBKS], writes=[SBKR])
            kb.cc("AllGather", RG, sbv_send, sbv_recv, reads=[SBVS], writes=[SBVR])
            with kb.scope(f"L{i}b_"):
                emit_attn_odd_f(kb, fmT, FMT, tmv, TMV, sbk_recv, SBKR, sbv_recv, SBVR, sbias, sink, ao, AO)
        x_dst, XD = xbufs[i % 2]
        last = (i == nlayers - 1)
        with kb.scope(f"L{i}c_"):
            if not last and USE_CONV:
                convert(i + 1)
            emit_post(kb, NT, x_src, ao, AO, modrow, w_out, n2g, wq, keysT, uT, v, x_dst, XD,
                      final_g=fg if last else None, xn_out=xn_out if last else None, XN=XN if last else None,
                      tile_sets=[0] * NTL + [1] * NTC, conv=lw[i]["conv"] if USE_CONV else None)
        x_src, XS = x_dst, XD
    return kb.finish()


def _na_bias_f(rpb, qr):
    H = rpb.shape[0]
    R0 = qr * 64
    out = np.full((5, H, 128, NA_NBMAX, 128), NEG, np.float32)
    kk = np.arange(128)
    qq = np.arange(128)
    for slot, qt in enumerate((0, 1, 10, NTL - 2, NTL - 1)):
        r0 = R0 + 2 * qt
        if slot == 2:
            r0 = 100
        o0, onb, cands = _na_blocklist(qt)
        blocks = [((r0 - 4 + 2 * b) if slot == 2 else (R0 + o0 // 64 + 2 * b), None) for b in range(onb)]
        for r in range(4):
            for (which, cb) in cands:
                if which == "tail":
                    blocks.append((R0 - 4 + 2 * cb, r == qr - 1))
                else:
                    blocks.append((R0 + 64 + 2 * cb, r == qr + 1))
        qrow = r0 + qq // 64
        qc = qq % 64
        rs = np.clip(qrow - 4, 0, 256 - 8)
        cs = np.clip(qc - 8, 0, 64 - 16)
        for bi, (krow0, ok) in enumerate(blocks):
            if ok is False:
                continue
            kr = krow0 + kk // 64
            kc = kk % 64
            valid = ((kr[:, None] >= rs[None, :]) & (kr[:, None] < rs[None, :] + 8) &
                     (kc[:, None] >= cs[None, :]) & (kc[:, None] < cs[None, :] + 16) &
                     (kr[:, None] >= 0) & (kr[:, None] < 256))
            dr = np.clip(kr[:, None] - qrow[None, :] + 7, 0, 14)
            dc = np.clip(kc[:, None] - qc[None, :] + 15, 0, 30)
            vals = rpb[:, dr, dc]
            out[slot, :, :, bi, :] = np.where(valid[None], vals, NEG)
    return out.reshape(5, H, 128, NA_NBMAX * 128)


def _swa_bias_f(qr):
    out = np.full((3, 128, 6, 128), NEG, np.float32)
    k = np.arange(128)[:, None]
    q = np.arange(128)[None, :]
    band = lambda off: np.where(np.abs(off * 128 + k - q) <= 128, 0.0, NEG).astype(np.float32)
    out[0, :, 0] = band(0)
    out[0, :, 1] = band(1)
    for r in range(4):
        if r == qr - 1:
            out[0, :, 2 + r] = band(-1)
    out[1, :, 0] = band(-1)
    out[1, :, 1] = band(0)
    out[1, :, 2] = band(1)
    out[2, :, 0] = band(-1)
    out[2, :, 1] = band(0)
    for r in range(4):
        if r == qr + 1:
            out[2, :, 2 + r] = band(1)
    return out.reshape(3, 128, 768)


_FUSED = {}


def kernel(x, c, ctx, c_ctx, w_mod, b_mod, norm1_g, norm2_g, w_in_even, w_out_even, na_rpb, diff_lambda,
           diff_subln_g, w_in_odd, w_out_odd, swa_sink, peer_wq, peer_keys, peer_u, peer_v, final_g, _nlayers=4):
    f32 = np.float32
    x = np.asarray(x, f32)
    ctx = np.asarray(ctx, f32)
    if _nlayers not in _FUSED:
        _FUSED[_nlayers] = build_fused(_nlayers)
    nc = _FUSED[_nlayers]
    shared = {"c_ident": np.eye(128, dtype=f32), "fg": np.asarray(final_g, f32)}
    for i in range(_nlayers):
        even = (i % 2 == 0)
        j = i // 2
        w_in = np.asarray(w_in_even[j] if even else w_in_odd[j], f32)
        rc = w_in[:, 1536:2560] if even else w_in[:, 0:1280]
        shared[f"w_mod{i}"] = np.asarray(w_mod[i], f32)
        shared[f"b_mod{i}"] = np.asarray(b_mod[i], f32)
        shared[f"n1g{i}"] = np.asarray(norm1_g[i], f32)
        shared[f"n2g{i}"] = np.asarray(norm2_g[i], f32)
        shared[f"w_in{i}"] = w_in
        shared[f"w_perm{i}"] = np.ascontiguousarray(rc.reshape(D, -1, 64)[:, :, _PERM64].reshape(D, -1))
        shared[f"w_out{i}"] = np.asarray(w_out_even[j] if even else w_out_odd[j], f32)
        shared[f"wq{i}"] = np.asarray(peer_wq[i], f32)
        shared[f"keysT{i}"] = np.ascontiguousarray(np.asarray(peer_keys[i], f32).reshape(16, 128, 128).transpose(2, 0, 1))
        shared[f"uT{i}"] = np.ascontiguousarray(np.asarray(peer_u[i], f32).T)
        shared[f"v{i}"] = np.asarray(peer_v[i], f32)
        if even:
            shared[f"lam{j}"] = np.asarray(diff_lambda[j], f32)
            shared[f"subg{j}"] = np.asarray(diff_subln_g[j], f32)
            li = 0.8 - 0.6 * math.exp(-0.3 * i)
            shared[f"lam_init{j}"] = np.tile(np.array([[li, 1.0 - li]], f32), (128, 1))
        else:
            shared[f"sink{j}"] = np.asarray(swa_sink[j], f32)
    ims = []
    for core in range(NCORES):
        b, qr = divmod(core, 4)
        im = dict(shared)
        im["x"] = np.concatenate([x[b, qr * TQ:(qr + 1) * TQ], ctx[b]], axis=0)
        cs = np.stack([np.asarray(c, f32)[b], np.asarray(c_ctx, f32)], 0)
        im["csT"] = np.ascontiguousarray(cs.reshape(2, 8, 128).transpose(2, 0, 1).reshape(128, 16))
        im["cosT"], im["sinT"] = _rope_tables_T(qr * TQ)
        for i in range(_nlayers):
            j = i // 2
            if i % 2 == 0:
                im[f"nbias{j}"] = _na_bias_f(np.asarray(na_rpb[j], f32), qr)
            else:
                im[f"sbias{j}"] = _swa_bias_f(qr)
        ims.append(im)
    res = run_bass_kernel_spmd(nc, ims, core_ids=list(range(NCORES))).results
    out = np.zeros((2, SEQ, D), f32)
    for core in range(NCORES):
        b, qr = divmod(core, 4)
        out[b, qr * TQ:(qr + 1) * TQ] = np.asarray(res[core]["xn_out"])[:TQ]
    return out
```
